# Optimizing a Trainium2 kernel written in Bass

```python
import math
import jax, jax.numpy as jnp
from jax import lax
import numpy as np

D_MODEL = 1024
BATCH = 8
SEQ = 4096
DEPTH = 1

D_MIX = D_MODEL
D_MLSTM = D_MIX // 2
D_DIFF = D_MIX - D_MLSTM
M_HEADS = 4
M_HEAD_DIM = D_MLSTM // M_HEADS
CHUNK = 64
CONV_K = 5
N_GATE_COLS = 4 * M_HEADS
A_HEADS = 4
A_HEAD_DIM = D_DIFF // (2 * A_HEADS)
ROPE_THETA = 500000.0
ROT_DIM = A_HEAD_DIM // 4
Q_BLOCK = 128
N_EXPERTS = 16
CAP_FACTOR = 2
D_FF_E = 2 * D_MODEL
EPS = 1e-6
D_IN = 4 * D_MLSTM + N_GATE_COLS + 3 * D_DIFF

kernel_name = 'hybrid_mlstm_diffattn_ec_moe_block'


def rmsnorm(x, g):
    x32 = x.astype(jnp.float32)
    y = x32 * lax.rsqrt(jnp.mean(x32 * x32, axis=-1, keepdims=True) + EPS)
    return (y * g.astype(jnp.float32)).astype(x.dtype)


def modulate(h, shift, scale):
    return h * (1 + scale[:, None, :]) + shift[:, None, :]


def split_cols(u, widths):
    outs = []
    off = 0
    for w in widths:
        outs.append(u[..., off:off + w])
        off += w
    return outs


def conv_centered(u, w):
    S = u.shape[1]
    pad = CONV_K // 2
    up = jnp.pad(u, ((0, 0), (pad, pad), (0, 0)))
    out = up[:, 0:S] * w[0]
    for j in range(1, CONV_K):
        out = out + up[:, j:j + S] * w[j]
    return out


def rope_partial(t, pos):
    inv_freq = ROPE_THETA ** (-jnp.arange(0, ROT_DIM, 2, dtype=jnp.float32) / ROT_DIM)
    ang = pos.astype(jnp.float32)[:, :, None] * inv_freq
    cos = jnp.cos(ang)[:, :, None, None, :].astype(t.dtype)
    sin = jnp.sin(ang)[:, :, None, None, :].astype(t.dtype)
    half = ROT_DIM // 2
    t1 = t[..., :half]
    t2 = t[..., half:ROT_DIM]
    rot = jnp.concatenate([t1 * cos - t2 * sin, t2 * cos + t1 * sin], axis=-1)
    return jnp.concatenate([rot, t[..., ROT_DIM:]], axis=-1)


def mlstm_chunkwise(q, k, v, i_pre, f_pre):
    B, H, S, d = q.shape
    NC = S // CHUNK
    q = q.reshape(B, H, NC, CHUNK, d)
    k = k.reshape(B, H, NC, CHUNK, d)
    v = v.reshape(B, H, NC, CHUNK, d)
    ig = i_pre.reshape(B, H, NC, CHUNK)
    a = jnp.cumsum(jax.nn.log_sigmoid(f_pre).reshape(B, H, NC, CHUNK), axis=-1)
    g = a[..., -1]
    w_log = g[..., None] - a + ig
    m_loc = jnp.max(w_log, axis=-1)
    w = jnp.exp(w_log - m_loc[..., None])
    C_loc = jnp.einsum('bhcl,bhclk,bhclv->bhckv', w, k, v)
    n_loc = jnp.einsum('bhcl,bhclk->bhck', w, k)

    def step(carry, inp):
        C, n, m = carry
        Cl, nl, ml, gc = inp
        m_new = jnp.maximum(gc + m, ml)
        s_prev = jnp.exp(gc + m - m_new)
        s_loc = jnp.exp(ml - m_new)
        C_new = s_prev[..., None, None] * C + s_loc[..., None, None] * Cl
        n_new = s_prev[..., None] * n + s_loc[..., None] * nl
        return (C_new, n_new, m_new), (C, n, m)

    init = (jnp.zeros((B, H, d, d), jnp.float32), jnp.zeros((B, H, d), jnp.float32),
            jnp.zeros((B, H), jnp.float32))
    xs = (jnp.moveaxis(C_loc, 2, 0), jnp.moveaxis(n_loc, 2, 0),
          jnp.moveaxis(m_loc, 2, 0), jnp.moveaxis(g, 2, 0))
    _, (C_prev, n_prev, m_prev) = lax.scan(step, init, xs)
    C_prev = jnp.moveaxis(C_prev, 0, 2)
    n_prev = jnp.moveaxis(n_prev, 0, 2)
    m_prev = jnp.moveaxis(m_prev, 0, 2)
    D = a[..., :, None] - a[..., None, :] + ig[..., None, :]
    tril = jnp.tril(jnp.ones((CHUNK, CHUNK), dtype=bool))
    D = jnp.where(tril, D, -jnp.inf)
    inter_log = a + m_prev[..., None]
    m_j = jnp.maximum(inter_log, jnp.max(D, axis=-1))
    inter_w = jnp.exp(inter_log - m_j)
    s = jnp.einsum('bhcjd,bhcsd->bhcjs', q, k) * jnp.exp(D - m_j[..., None])
    num = (jnp.einsum('bhcjs,bhcsv->bhcjv', s, v)
           + inter_w[..., None] * jnp.einsum('bhcjk,bhckv->bhcjv', q, C_prev))
    den = jnp.sum(s, axis=-1) + inter_w * jnp.einsum('bhcjk,bhck->bhcj', q, n_prev)
    den = jnp.maximum(jnp.abs(den), jnp.exp(-m_j))
    return (num / den[..., None]).reshape(B, H, S, d)


def hybrid_layer(x, c, positions, layer_idx, norm1_g, norm2_g, w_ada, b_ada, w_in,
                 mlstm_conv_w, mlstm_gate_b, mlstm_norm_g, diff_qk_g, diff_lambda,
                 diff_subln_g, w_out, w_router, w_gate_e, w_up_e, w_down_e):
    B, S, _ = x.shape
    dt = x.dtype
    mod = jax.nn.silu(c) @ w_ada + b_ada
    shift1, scale1, gate1, shift2, scale2, gate2 = jnp.split(mod, 6, axis=-1)

    h = modulate(rmsnorm(x, norm1_g), shift1, scale1)
    proj = h @ w_in
    qm, km, vm, om, gm, qa, ka, va = split_cols(
        proj, [D_MLSTM, D_MLSTM, D_MLSTM, D_MLSTM, N_GATE_COLS, D_DIFF, D_DIFF, D_DIFF])

    qk = jax.nn.silu(conv_centered(jnp.concatenate([qm, km], axis=-1), mlstm_conv_w))
    qm, km = qk[..., :D_MLSTM], qk[..., D_MLSTM:]

    def to_heads(t):
        return t.reshape(B, S, M_HEADS, M_HEAD_DIM).transpose(0, 2, 1, 3).astype(jnp.float32)

    q_h = to_heads(qm)
    k_h = to_heads(km) * (M_HEAD_DIM ** -0.5)
    v_h = to_heads(vm)
    gates = (gm + mlstm_gate_b).astype(jnp.float32).reshape(B, S, 4, M_HEADS).transpose(0, 2, 3, 1)
    i_f, f_f, i_b, f_b = gates[:, 0], gates[:, 1], gates[:, 2], gates[:, 3]
    h_fwd = mlstm_chunkwise(q_h, k_h, v_h, i_f, f_f)
    fl = lambda t: jnp.flip(t, axis=2)
    h_bwd = fl(mlstm_chunkwise(fl(q_h), fl(k_h), fl(v_h), fl(i_b), fl(f_b)))
    h_m = (h_fwd + h_bwd).transpose(0, 2, 1, 3)
    h_m = rmsnorm(h_m, mlstm_norm_g.reshape(M_HEADS, M_HEAD_DIM)).astype(dt)
    y_m = jax.nn.sigmoid(om) * h_m.reshape(B, S, D_MLSTM)

    lam_init = 0.8 - 0.6 * math.exp(-0.3 * layer_idx)
    lam_p = diff_lambda.astype(jnp.float32)
    lam = (jnp.exp(jnp.sum(lam_p[0] * lam_p[1])) - jnp.exp(jnp.sum(lam_p[2] * lam_p[3]))
           + lam_init)
    qa = rope_partial(rmsnorm(qa.reshape(B, S, A_HEADS, 2, A_HEAD_DIM), diff_qk_g[0]), positions)
    ka = rope_partial(rmsnorm(ka.reshape(B, S, A_HEADS, 2, A_HEAD_DIM), diff_qk_g[1]), positions)
    q_t = qa.transpose(0, 2, 3, 1, 4)
    k_t = ka.transpose(0, 2, 3, 1, 4)
    v_t = va.reshape(B, S, A_HEADS, 2 * A_HEAD_DIM).transpose(0, 2, 1, 3)
    NB = S // Q_BLOCK
    q_blocks = q_t.reshape(B, A_HEADS, 2, NB, Q_BLOCK, A_HEAD_DIM).transpose(3, 0, 1, 2, 4, 5)
    scale = A_HEAD_DIM ** -0.5

    def attend(q_blk):
        s = jnp.einsum('bhpqd,bhpkd->bhpqk', q_blk, k_t).astype(jnp.float32) * scale
        p = jax.nn.softmax(s, axis=-1)
        a_map = p[:, :, 0] - lam * p[:, :, 1]
        return jnp.einsum('bhqk,bhkv->bhqv', a_map.astype(v_t.dtype), v_t)

    o = lax.map(attend, q_blocks)
    o = o.transpose(1, 0, 3, 2, 4).reshape(B, S, A_HEADS, 2 * A_HEAD_DIM)
    o = rmsnorm(o, diff_subln_g) * (1.0 - lam_init)
    y_a = o.reshape(B, S, D_DIFF).astype(dt)

    mix = jnp.concatenate([y_m, y_a], axis=-1) @ w_out
    x = x + gate1[:, None, :] * mix

    h2 = modulate(rmsnorm(x, norm2_g), shift2, scale2)
    aff = jax.nn.softmax(jnp.einsum('bsd,de->bse', h2, w_router).astype(jnp.float32), axis=-1)
    cap = CAP_FACTOR * S // N_EXPERTS
    vals, idx = lax.top_k(aff.transpose(0, 2, 1), cap)
    xe = jax.vmap(lambda hb, ib: hb[ib])(h2, idx)
    g_e = jnp.einsum('becd,edf->becf', xe, w_gate_e)
    u_e = jnp.einsum('becd,edf->becf', xe, w_up_e)
    out_e = jnp.einsum('becf,efd->becd', jax.nn.silu(g_e) * u_e, w_down_e)
    out_e = out_e * vals[..., None].astype(dt)
    moe = jnp.zeros_like(h2).at[jnp.arange(B)[:, None, None], idx].add(out_e)
    return x + gate2[:, None, :] * moe


def setup_inputs(seed: int = 0) -> dict:
    key = jax.random.key(seed)
    ks = jax.random.split(key, 24)
    D = D_MODEL
    nrm = lambda k, shape, s: jax.random.normal(k, shape, jnp.float32) * s
    x = jax.random.normal(ks[0], (BATCH, SEQ, D), jnp.float32)
    c = jax.random.normal(ks[1], (BATCH, D), jnp.float32)
    offs = jax.random.randint(ks[2], (BATCH, 1), 0, 1024, dtype=jnp.int32)
    positions = offs + jnp.arange(SEQ, dtype=jnp.int32)[None, :]
    norm1_g = 1.0 + nrm(ks[3], (DEPTH, D), 0.02)
    norm2_g = 1.0 + nrm(ks[4], (DEPTH, D), 0.02)
    w_ada = nrm(ks[5], (DEPTH, D, 6 * D), 0.5 * D ** -0.5)
    b_ada = nrm(ks[6], (DEPTH, 6 * D), 0.02)
    w_in = nrm(ks[7], (DEPTH, D, D_IN), D ** -0.5)
    mlstm_conv_w = nrm(ks[8], (DEPTH, CONV_K, 2 * D_MLSTM), CONV_K ** -0.5)
    gk = jax.random.split(ks[9], 4)
    f_init = jnp.linspace(3.0, 6.0, M_HEADS, dtype=jnp.float32)
    mlstm_gate_b = jnp.concatenate([
        nrm(gk[0], (DEPTH, M_HEADS), 0.1),
        f_init + nrm(gk[1], (DEPTH, M_HEADS), 0.1),
        nrm(gk[2], (DEPTH, M_HEADS), 0.1),
        f_init + nrm(gk[3], (DEPTH, M_HEADS), 0.1)], axis=-1)
    mlstm_norm_g = 1.0 + nrm(ks[10], (DEPTH, D_MLSTM), 0.02)
    diff_qk_g = 1.0 + nrm(ks[11], (DEPTH, 2, A_HEAD_DIM), 0.02)
    diff_lambda = nrm(ks[12], (DEPTH, 4, A_HEAD_DIM), 0.1)
    diff_subln_g = 1.0 + nrm(ks[13], (DEPTH, 2 * A_HEAD_DIM), 0.02)
    w_out = nrm(ks[14], (DEPTH, D_MIX, D), D_MIX ** -0.5)
    w_router = nrm(ks[15], (DEPTH, D, N_EXPERTS), D ** -0.5)
    w_gate_e = nrm(ks[16], (DEPTH, N_EXPERTS, D, D_FF_E), D ** -0.5)
    w_up_e = nrm(ks[17], (DEPTH, N_EXPERTS, D, D_FF_E), D ** -0.5)
    w_down_e = nrm(ks[18], (DEPTH, N_EXPERTS, D_FF_E, D), D_FF_E ** -0.5)
    return {'x': x, 'c': c, 'positions': positions, 'norm1_g': norm1_g, 'norm2_g': norm2_g,
            'w_ada': w_ada, 'b_ada': b_ada, 'w_in': w_in, 'mlstm_conv_w': mlstm_conv_w,
            'mlstm_gate_b': mlstm_gate_b, 'mlstm_norm_g': mlstm_norm_g, 'diff_qk_g': diff_qk_g,
            'diff_lambda': diff_lambda, 'diff_subln_g': diff_subln_g, 'w_out': w_out,
            'w_router': w_router, 'w_gate_e': w_gate_e, 'w_up_e': w_up_e, 'w_down_e': w_down_e}


def reference(x, c, positions, norm1_g, norm2_g, w_ada, b_ada, w_in, mlstm_conv_w,
              mlstm_gate_b, mlstm_norm_g, diff_qk_g, diff_lambda, diff_subln_g, w_out,
              w_router, w_gate_e, w_up_e, w_down_e):
    for l in range(DEPTH):
        x = hybrid_layer(x, c, positions, l, norm1_g[l], norm2_g[l], w_ada[l], b_ada[l],
                         w_in[l], mlstm_conv_w[l], mlstm_gate_b[l], mlstm_norm_g[l],
                         diff_qk_g[l], diff_lambda[l], diff_subln_g[l], w_out[l],
                         w_router[l], w_gate_e[l], w_up_e[l], w_down_e[l])
    return x
```

```python
import math
from contextlib import ExitStack

import numpy as np
import concourse.bass as bass
import concourse.mybir as mybir
from concourse.bass_utils import run_bass_kernel_spmd

F32 = mybir.dt.float32
BF16 = mybir.dt.bfloat16
I32 = mybir.dt.int32
AF = mybir.ActivationFunctionType
ALU = mybir.AluOpType
AX = mybir.AxisListType

S = 4096
D = 1024
NT = 32
NG = 8
EPS = 1e-6
D_IN = 3600
NE = 16
CAP = 512
LAM_INIT = 0.8 - 0.6 * math.exp(-0.3 * 0)
NBIS = 27

SAME_ENG_SYNC = True

C_IDENT = 0
C_TL = 128
C_TU = 256
C_TLS = 384
C_TUS = 512
C_INDA = 640
C_INDB = 768
C_IOTA = 896
C_TLO = 1408
C_FREQ = 1409
C_THI = 1417
NCONST = 1449


def make_consts():
    c = np.zeros((128, NCONST), np.float32)
    p = np.arange(128)
    s = p[:, None]
    j = p[None, :]
    same = (s // 64) == (j // 64)
    c[:, C_IDENT:C_IDENT + 128] = (s == j)
    c[:, C_TL:C_TL + 128] = same & (s <= j)
    c[:, C_TU:C_TU + 128] = same & (s >= j)
    c[:, C_TLS:C_TLS + 128] = same & (s < j)
    c[:, C_TUS:C_TUS + 128] = same & (s > j)
    c[:, C_INDA:C_INDA + 128] = (s < 64) & (j >= 0)
    c[:, C_INDB:C_INDB + 128] = (s >= 64) & (j >= 0)
    c[:, C_IOTA:C_IOTA + 512] = np.arange(512)[None, :]
    c[:, C_TLO] = p
    inv_freq = (500000.0 ** (-np.arange(0, 16, 2, dtype=np.float32) / 16)).astype(np.float32)
    c[:, C_FREQ:C_FREQ + 8] = (inv_freq.astype(np.float64) / (2 * np.pi)).astype(np.float32)[None, :]
    c[:, C_THI:C_THI + 32] = np.arange(32)[None, :]
    return c


class Sched:
    def __init__(self, nc, es):
        self.nc = nc
        self.es = es
        self.E = {'pe': nc.tensor, 'act': nc.scalar, 'dve': nc.vector, 'pool': nc.gpsimd, 'sp': nc.sync}
        self.sem = {k: es.enter_context(nc.semaphore('s_' + k)) for k in self.E}
        self.cnt = {k: 0 for k in self.E}
        self.seen = {k: {} for k in self.E}
        self.reg = {}
        self.dsem = {}
        self.dsem_by_sid = {}
        self.ninstr = {k: 0 for k in self.E}

    def _deps(self, reads, writes):
        deps = {}

        def add(tok):
            if tok is None:
                return
            if tok[0].startswith('d_'):
                tok = (tok[0], tok[1], self.dsem_by_sid[tok[0]][1])
            if tok[0] not in deps or deps[tok[0]][2] < tok[2]:
                deps[tok[0]] = tok
        for r in reads:
            st = self.reg.get(r)
            if st:
                add(st[0])
        for w in writes:
            st = self.reg.get(w)
            if st:
                add(st[0])
                for t in st[1].values():
                    add(t)
        return deps

    def _wait(self, eng, deps):
        for sid, (_, h, v) in deps.items():
            if sid == 'e_' + eng and not SAME_ENG_SYNC:
                continue
            if self.seen[eng].get(sid, 0) >= v:
                continue
            self.E[eng].wait_ge(h, v)
            self.seen[eng][sid] = v

    def _commit(self, tok, reads, writes):
        for r in reads:
            st = self.reg.setdefault(r, [None, {}])
            st[1][tok[0]] = tok
        for w in writes:
            self.reg[w] = [tok, {}]

    def op(self, eng, fn, reads=(), writes=()):
        self._wait(eng, self._deps(reads, writes))
        ins = fn(self.E[eng])
        self.cnt[eng] += 1
        ins.then_inc(self.sem[eng], 1)
        self._commit(('e_' + eng, self.sem[eng], self.cnt[eng]), reads, writes)

    def dma(self, q, fn, semkey, reads=(), writes=()):
        self._wait(q, self._deps(reads, writes))
        ins = fn(self.E[q])
        d = self.dsem.get(semkey)
        if d is None:
            d = [self.es.enter_context(self.nc.semaphore('d%d' % len(self.dsem))), 0]
            self.dsem[semkey] = d
            self.dsem_by_sid['d_' + str(semkey)] = d
        d[1] += 16
        ins.then_inc(d[0], 16)
        self._commit(('d_' + str(semkey), d[0], d[1]), reads, writes)

    def barrier(self):
        toks = {}
        for k in self.E:
            if self.cnt[k]:
                toks['e_' + k] = ('e_' + k, self.sem[k], self.cnt[k])
        for key, d in self.dsem.items():
            if d[1]:
                toks['d_' + str(key)] = ('d_' + str(key), d[0], d[1])
        for k in self.E:
            self._wait(k, toks)

    def finish(self):
        toks = {}
        for k in self.E:
            if self.cnt[k]:
                toks['e_' + k] = ('e_' + k, self.sem[k], self.cnt[k])
        for key, d in self.dsem.items():
            if d[1]:
                toks['d_' + str(key)] = ('d_' + str(key), d[0], d[1])
        self._wait('sp', toks)


def build(stage=99, debug=False):
    nc = bass.Bass("TRN2", target_bir_lowering=False)
    es = ExitStack()
    sc = Sched(nc, es)

    def din(name, shape, dt=F32):
        return nc.dram_tensor(name, list(shape), dt, kind="ExternalInput").ap()

    x_d = din("x", [S, D])
    cT_d = din("cT", [128, 8])
    pos_d = din("pos", [128, NT], I32)
    n1g_d = din("norm1_g", [1, D])
    n2g_d = din("norm2_g", [1, D])
    wada_d = din("w_ada", [D, 6 * D])
    bada_d = din("b_ada", [1, 6 * D])
    win_d = din("w_in", [D, D_IN])
    convT_d = din("convT", [128, 40])
    gateb_d = din("gate_b", [1, 16])
    mng_d = din("mnorm_g", [1, 512])
    qkg_d = din("qk_g", [1, 128])
    lam_d = din("lam", [1, 256])
    subg_d = din("subln_g", [1, 128])
    wout_d = din("w_out", [D, D])
    wr_d = din("w_router", [D, NE])
    consts_d = din("consts", [128, NCONST])
    if stage >= 5:
        wg_d = din("w_gate_e", [NE * D, 2 * D])
        wu_d = din("w_up_e", [NE * D, 2 * D])
        wd_d = din("w_down_e", [NE * 2 * D, D])
    out_d = nc.dram_tensor("out", [S, D], F32, kind="ExternalOutput").ap()
    skind = "ExternalOutput" if debug else "Internal"
    yT_d = nc.dram_tensor("yT_d", [D, S], BF16, kind=skind).ap()
    h2_d = nc.dram_tensor("h2_d", [S, D], BF16, kind=skind).ap()
    dbg = {}

    def dbg_out(name, shape, dt=F32):
        if debug:
            dbg[name] = nc.dram_tensor(name, list(shape), dt, kind="ExternalOutput").ap()
            return dbg[name]
        return None

    uniq = [0]

    def sb(name, shape, dt=F32, stack=es):
        uniq[0] += 1
        return stack.enter_context(nc.sbuf_tensor("sb%d_%s" % (uniq[0], name), list(shape), dt))

    ps = es.enter_context(nc.psum_tensor("ps", [128, 8, 512], F32))
    consts = sb("consts", [128, NCONST])
    identb = sb("identb", [128, 128], BF16)
    s1 = ExitStack()
    prm = sb("prm", [128, 16 + 512 + 128 + 256 + 128], F32, s1)
    P_GB, P_MNG, P_QKG, P_LAM, P_SUB = 0, 16, 528, 656, 912
    convT = sb("convT", [128, 40], F32, s1)
    maskF = sb("maskF", [128, 128], BF16, s1)
    maskB = sb("maskB", [128, 128], BF16, s1)
    small = sb("small", [128, 64], F32, s1)
    SM_NB, SM_NLAM = 0, 1
    ident = consts[:, C_IDENT:C_IDENT + 128]

    h1T = sb("h1T", [128, 8, S], BF16, s1)
    gm = sb("gm", [128, NT, 16], F32, s1)
    smod = ExitStack()
    mod = sb("mod", [128, 6 * D], F32, smod)
    modrow_d = nc.dram_tensor("modrow_d", [1, 6 * D], F32, kind="Internal").ap()
    CK = 'consts'
    sc.dma('sp', lambda e: e.dma_start(out=consts[:], in_=consts_d[:, :]), CK, writes=['consts'])
    sc.dma('sp', lambda e: e.dma_start(out=convT[:], in_=convT_d[:, :]), CK, writes=['convT'])
    for (off, src, n) in [(P_GB, gateb_d, 16), (P_MNG, mng_d, 512),
                          (P_QKG, qkg_d, 128), (P_LAM, lam_d, 256), (P_SUB, subg_d, 128)]:
        sc.dma('sp', lambda e, off=off, src=src, n=n: e.dma_start(
            out=prm[:, off:off + n], in_=src[0:1, :].partition_broadcast(128)), CK, writes=['prm'])
    sc.dma('sp', lambda e: e.dma_start(out=mod[:], in_=bada_d[0:1, :].partition_broadcast(128)), CK, writes=['mod'])

    with ExitStack() as p0:
        cT = sb("cT", [128, 8], F32, p0)
        prmA = sb("prmA", [128, 2 * D], F32, p0)
        sc.dma('sp', lambda e: e.dma_start(out=prmA[:, 0:D], in_=n1g_d[0:1, :].partition_broadcast(128)), CK, writes=['prmA'])
        sc.dma('sp', lambda e: e.dma_start(out=prmA[:, D:2 * D], in_=n2g_d[0:1, :].partition_broadcast(128)), CK, writes=['prmA'])
        scT = sb("scT", [128, 8], F32, p0)
        lc = sb("lc", [128, 8, 128], BF16, p0)
        wa = [sb("wa%d" % i, [128, 8, 512], BF16, p0) for i in range(2)]
        sc.dma('sp', lambda e: e.dma_start(out=cT[:], in_=cT_d[:, :]), 'cT', writes=['cT'])
        sc.op('act', lambda e: e.activation(out=scT[:], in_=cT[:], func=AF.Silu), reads=['cT'], writes=['scT'])
        sc.op('dve', lambda e: e.tensor_copy(out=lc[:], in_=scT[:].unsqueeze(2).broadcast_to([128, 8, 128])),
              reads=['scT'], writes=['lc'])
        sc.op('dve', lambda e: e.tensor_copy(out=identb[:], in_=ident), reads=['consts'], writes=['identb'])
        sc.op('dve', lambda e: e.tensor_copy(out=maskF[:], in_=consts[:, C_TL:C_TL + 128]), reads=['consts'], writes=['maskF'])
        sc.op('dve', lambda e: e.tensor_copy(out=maskB[:], in_=consts[:, C_TU:C_TU + 128]), reads=['consts'], writes=['maskB'])
        wada_v = wada_d.rearrange("(kc p) n -> p kc n", p=128)
        for n in range(12):
            w = wa[n % 2]
            wk = ('wa', n % 2)
            sc.dma('pool', lambda e, w=w, n=n: e.dma_start(out=w[:], in_=wada_v[:, :, n * 512:(n + 1) * 512]),
                   wk, writes=[wk])
            bank = ('ps', n % 2)

            def mm(e, w=w, n=n):
                for kc in range(8):
                    r = e.matmul(ps[:, n % 2, :], lhsT=lc[:, kc, :], rhs=w[:, kc, :], start=(kc == 0), stop=(kc == 7))
                return r
            sc.op('pe', mm, reads=['lc', wk], writes=[bank])
            sc.op('dve', lambda e, n=n: e.tensor_tensor(out=mod[:, n * 512:(n + 1) * 512], in0=ps[:, n % 2, :],
                                                        in1=mod[:, n * 512:(n + 1) * 512], op=ALU.add),
                  reads=[bank, 'mod'], writes=['mod'])
        sc.op('dve', lambda e: e.scalar_tensor_tensor(out=mod[:, D:2 * D], in0=mod[:, D:2 * D], scalar=1.0,
                                                      in1=prmA[:, 0:D], op0=ALU.add, op1=ALU.mult),
              reads=['mod', 'prmA'], writes=['mod'])
        sc.op('dve', lambda e: e.scalar_tensor_tensor(out=mod[:, 4 * D:5 * D], in0=mod[:, 4 * D:5 * D], scalar=1.0,
                                                      in1=prmA[:, D:2 * D], op0=ALU.add, op1=ALU.mult),
              reads=['mod', 'prmA'], writes=['mod'])
        if debug:
            o = dbg_out("dbg_mod", [128, 6 * D])
            sc.dma('sp', lambda e: e.dma_start(out=o[:, :], in_=mod[:]), 'dbgmod', reads=['mod'])
        sc.barrier()
    SHIFT1, GS1, GATE1, SHIFT2, GS2, GATE2 = [mod[:, i * D:(i + 1) * D] for i in range(6)]

    if stage <= 0:
        sc.finish()
        return nc, dbg

    with ExitStack() as p1:
        xt = [sb("xt%d" % i, [128, D], F32, p1) for i in range(2)]
        junk = sb("junk1", [128, D], BF16, p1)
        tmp = [sb("tmp1_%d" % i, [128, D], F32, p1) for i in range(2)]
        h1b = [sb("h1b%d" % i, [128, D], BF16, p1) for i in range(2)]
        st1 = sb("st1", [128, NT, 3], F32, p1)
        wgt = sb("wgt", [128, 8, 16], BF16, p1)
        win_v = win_d.rearrange("(kc p) n -> p kc n", p=128)
        sc.dma('pool', lambda e: e.dma_start(out=wgt[:], in_=win_v[:, :, 2048:2064]), 'wgt', writes=['wgt'])
        for i in range(NT):
            b = i % 2
            xk, tk, hk = ('xt', b), ('tmp1', b), ('h1b', b)
            sc.dma('sp', lambda e, i=i, b=b: e.dma_start(out=xt[b][:], in_=x_d[i * 128:(i + 1) * 128, :]), xk, writes=[xk])
            sc.op('act', lambda e, i=i, b=b: e.activation(out=junk[:], in_=xt[b][:], func=AF.Square,
                                                          accum_out=st1[:, i, 0:1]), reads=[xk], writes=['junk1', ('st1', i)])
            sc.op('act', lambda e, i=i: e.activation(out=st1[:, i, 1:2], in_=st1[:, i, 0:1], func=AF.Sqrt,
                                                     scale=1.0 / D, bias=EPS), reads=[('st1', i)], writes=[('st1', i)])
            sc.op('dve', lambda e, i=i: e.reciprocal(out=st1[:, i, 2:3], in_=st1[:, i, 1:2]),
                  reads=[('st1', i)], writes=[('st1', i)])
            sc.op('dve', lambda e, i=i, b=b: e.scalar_tensor_tensor(out=tmp[b][:], in0=xt[b][:], scalar=st1[:, i, 2:3],
                                                                    in1=GS1, op0=ALU.mult, op1=ALU.mult),
                  reads=[xk, ('st1', i), 'mod'], writes=[tk])
            sc.op('pool', lambda e, b=b: e.tensor_tensor(out=h1b[b][:], in0=tmp[b][:], in1=SHIFT1, op=ALU.add),
                  reads=[tk, 'mod'], writes=[hk])
            bank = ('ps', 2 + b)
            pb = ps[:, 2 + b, :].bitcast(BF16)

            def tr(e, b=b, pb=pb):
                for kc in range(8):
                    r = e.transpose(out=pb[:, kc * 128:(kc + 1) * 128], in_=h1b[b][:, kc * 128:(kc + 1) * 128], identity=identb[:])
                return r
            sc.op('pe', tr, reads=[hk, 'identb'], writes=[bank])
            sc.op('act', lambda e, i=i, pb=pb: e.copy(out=h1T[:, :, i * 128:(i + 1) * 128],
                                                      in_=pb.rearrange("p (k t) -> p k t", k=8)),
                  reads=[bank], writes=[('h1T', i)])
            gbank = ('ps', 4 + b)

            def gmm(e, i=i, b=b):
                for kc in range(8):
                    r = e.matmul(ps[:, 4 + b, 0:16], lhsT=h1T[:, kc, i * 128:(i + 1) * 128], rhs=wgt[:, kc, :],
                                 start=(kc == 0), stop=(kc == 7))
                return r
            sc.op('pe', gmm, reads=[('h1T', i), 'wgt'], writes=[gbank])
            sc.op('dve', lambda e, i=i, b=b: e.tensor_tensor(out=gm[:, i, :], in0=ps[:, 4 + b, 0:16],
                                                             in1=prm[:, P_GB:P_GB + 16], op=ALU.add),
                  reads=[gbank, 'prm'], writes=['gm'])
        if debug:
            o = dbg_out("dbg_h1T", [128, 8, S], BF16)
            sc.dma('sp', lambda e: e.dma_start(out=o[:, :, :], in_=h1T[:]), 'dbgh1T', reads=[('h1T', i) for i in range(NT)])
            o3 = dbg_out("dbg_st1", [128, NT, 3])
            sc.dma('sp', lambda e: e.dma_start(out=o3[:, :, :], in_=st1[:]), 'dbgst1', reads=[('st1', i) for i in range(NT)])
            o4 = dbg_out("dbg_tmp", [128, D])
            sc.dma('sp', lambda e: e.dma_start(out=o4[:, :], in_=tmp[1][:]), 'dbgtmp', reads=[('tmp1', 1)])
            o5 = dbg_out("dbg_h1b", [128, D], BF16)
            sc.dma('sp', lambda e: e.dma_start(out=o5[:, :], in_=h1b[1][:]), 'dbgh1b', reads=[('h1b', 1)])
            o2 = dbg_out("dbg_gm", [128, NT, 16])
            sc.dma('sp', lambda e: e.dma_start(out=o2[:, :, :], in_=gm[:]), 'dbggm', reads=['gm'])
        sc.barrier()

    sc.dma('sp', lambda e: e.dma_start(out=modrow_d[0:1, :], in_=mod[0:1, :]), 'modrow', reads=['mod'], writes=['modrow_d'])
    sc.barrier()
    smod.close()

    if stage <= 1:
        sc.finish()
        return nc, dbg

    LNK = -0.5 * math.log(128.0)
    cum = sb("cum", [128, 4, NT, 4], F32, s1)
    egs = sb("egs", [128, 4, NT, 4], F32, s1)
    tw = sb("tw", [128, 6, NT, 4], F32, s1)
    with ExitStack() as pg:
        lsg = sb("lsg", [128, 2, NT, 4], F32, pg)
        tg = sb("tg", [128, 2, NT, 4], F32, pg)
        for d, c0 in ((0, 4), (1, 12)):
            sc.op('act', lambda e, d=d, c0=c0: e.activation(out=tg[:, d, :, :], in_=gm[:, :, c0:c0 + 4], func=AF.Exp, scale=-1.0),
                  reads=['gm'], writes=['tg'])
        sc.op('act', lambda e: e.activation(out=tg[:], in_=tg[:], func=AF.Ln, bias=1.0), reads=['tg'], writes=['tg'])
        sc.op('dve', lambda e: e.tensor_scalar(out=lsg[:], in0=tg[:], scalar1=-1.0, scalar2=None, op0=ALU.mult),
              reads=['tg'], writes=['lsg'])
        lf = lsg[:, 0, :, :].rearrange("p t h -> p (t h)")
        lb = lsg[:, 1, :, :].rearrange("p t h -> p (t h)")
        specs = [(C_TL, lf), (C_TUS, lf), (C_TU, lb), (C_TLS, lb)]

        def cmm(e):
            for k, (co, rhs) in enumerate(specs):
                r = e.matmul(ps[:, 6, k * 128:(k + 1) * 128], lhsT=consts[:, co:co + 128], rhs=rhs, start=True, stop=True)
            return r
        sc.op('pe', cmm, reads=['lsg', 'consts'], writes=[('ps', 6)])
        sc.op('act', lambda e: e.copy(out=cum[:].rearrange("p a t h -> p (a t h)"), in_=ps[:, 6, :]),
              reads=[('ps', 6)], writes=['cum'])
        specs2 = [(C_INDA, lf), (C_INDB, lf), (C_INDA, lb), (C_INDB, lb)]

        def gmm2(e):
            for k, (co, rhs) in enumerate(specs2):
                r = e.matmul(ps[:, 7, k * 128:(k + 1) * 128], lhsT=consts[:, co:co + 128], rhs=rhs, start=True, stop=True)
            return r
        sc.op('pe', gmm2, reads=['lsg', 'consts'], writes=[('ps', 7)])
        sc.op('act', lambda e: e.activation(out=egs[:].rearrange("p a t h -> p (a t h)"), in_=ps[:, 7, :], func=AF.Exp),
              reads=[('ps', 7)], writes=['egs'])
        for d, ic in ((0, 0), (1, 8)):
            ig = gm[:, :, ic:ic + 4]
            a_ = cum[:, 2 * d, :, :]
            r_ = cum[:, 2 * d + 1, :, :]
            sc.op('dve', lambda e, d=d, ig=ig, a_=a_: e.tensor_tensor(out=tw[:, 3 * d, :, :], in0=ig, in1=a_, op=ALU.subtract),
                  reads=['gm', 'cum'], writes=['tw'])
            sc.op('dve', lambda e, d=d, ig=ig, r_=r_: e.tensor_tensor(out=tw[:, 3 * d + 1, :, :], in0=ig, in1=r_, op=ALU.add),
                  reads=['gm', 'cum'], writes=['tw'])
            sc.op('act', lambda e, d=d: e.activation(out=tw[:, 3 * d:3 * d + 2, :, :], in_=tw[:, 3 * d:3 * d + 2, :, :],
                                                     func=AF.Exp, bias=LNK), reads=['tw'], writes=['tw'])
            sc.op('act', lambda e, d=d, a_=a_: e.activation(out=tw[:, 3 * d + 2, :, :], in_=a_, func=AF.Exp),
                  reads=['cum'], writes=['tw'])
        if debug:
            o = dbg_out("dbg_tw", [128, 6, NT, 4])
            sc.dma('sp', lambda e: e.dma_start(out=o[:, :, :, :], in_=tw[:]), 'dbgtw', reads=['tw'])
            o = dbg_out("dbg_egs", [128, 4, NT, 4])
            sc.dma('sp', lambda e: e.dma_start(out=o[:, :, :, :], in_=egs[:]), 'dbgegs', reads=['egs'])
        sc.barrier()

    if stage <= 1.5:
        sc.finish()
        return nc, dbg

    win_v = win_d.rearrange("(kc p) n -> p kc n", p=128)
    yT_v = yT_d.rearrange("(c p) t -> p c t", p=128)

    def emit_y_tile(ph, h_chunk, i, ym, ymk, yst, ystk):
        g4 = i % 4
        bank = ('ps', 5)
        pb = ps[:, 5, :].bitcast(BF16)
        sc.op('pe', lambda e: e.transpose(out=pb[:, g4 * 128:(g4 + 1) * 128], in_=ym, identity=identb[:]),
              reads=[ymk, 'identb'], writes=[bank])
        sc.op('act', lambda e: e.copy(out=yst[:, g4 * 128:(g4 + 1) * 128], in_=pb[:, g4 * 128:(g4 + 1) * 128]),
              reads=[bank], writes=[ystk])
        if g4 == 3:
            t0 = (i - 3) * 128
            sc.dma('sp', lambda e: e.dma_start(out=yT_v[:, h_chunk, t0:t0 + 512], in_=yst[:]), ystk, reads=[ystk], writes=['yT_d'])

    n_mheads = 4 if stage >= 2.5 else 1
    for h in range(n_mheads):
        with ExitStack() as ph:
            wm = sb("wm", [128, 8, 512], BF16, ph)
            qkT = sb("qkT", [128, 2, S], BF16, ph)
            pre = sb("pre", [128, 2, 516], F32, ph)
            acc = sb("acc", [128, 2, 512], F32, ph)
            vext = sb("vext", [128, NT, 129], BF16, ph)
            sgo = sb("sgo", [128, NT, 128], BF16, ph)
            ktok = sb("ktok", [128, NT, 128], BF16, ph)
            Cst = sb("Cst", [128, 2, 65, 129], BF16, ph)
            stt = sb("stt", [128, 2, 2, 129], F32, ph)
            vtmp = [sb("vtmp%d" % k, [128, 129], BF16, ph) for k in range(4)]
            smt = [sb("smt%d" % k, [128, 128], BF16, ph) for k in range(4)]
            pst = sb("pst", [128, 2, 8], F32, ph)
            hf = [sb("hf%d" % k, [128, 128], F32, ph) for k in range(2)]
            hs = [sb("hs%d" % k, [128, 128], F32, ph) for k in range(2)]
            jk = sb("jk", [128, 128], F32, ph)
            ymt = [sb("ymt%d" % k, [128, 128], BF16, ph) for k in range(2)]
            yst = [sb("yst%d" % k, [128, 512], BF16, ph) for k in range(2)]
            for k, c0 in enumerate((h * 128, 512 + h * 128, 1024 + h * 128, 1536 + h * 128)):
                sc.dma('pool', lambda e, k=k, c0=c0: e.dma_start(out=wm[:, :, k * 128:(k + 1) * 128], in_=win_v[:, :, c0:c0 + 128]),
                       'wm', writes=['wm'])
            sc.op('pool', lambda e: e.memset(vext[:, :, 128:129], 1.0), writes=['vext1'])
            sc.op('pool', lambda e: e.memset(Cst[:, :, 0, :], 0.0), writes=[('Cst', 0, 0), ('Cst', 1, 0)])
            sc.op('pool', lambda e: e.memset(stt[:, :, 0, :], 0.0), writes=[('stt', 0, 0), ('stt', 1, 0)])
            sc.op('pool', lambda e: e.memset(pre[:, :, 0:4], 0.0), writes=['pre'])
            for g in range(NG + 1):
                if g < NG:
                    for qk in range(2):
                        bank = ('ps', qk)

                        def mm(e, qk=qk, g=g):
                            for kc in range(8):
                                r = e.matmul(ps[:, qk, :], lhsT=wm[:, kc, qk * 128:(qk + 1) * 128],
                                             rhs=h1T[:, kc, g * 512:(g + 1) * 512], start=(kc == 0), stop=(kc == 7))
                            return r
                        sc.op('pe', mm, reads=['wm'] + [('h1T', 4 * g + k) for k in range(4)], writes=[bank])
                        sc.op('act', lambda e, qk=qk: e.copy(out=pre[:, qk, 4:516], in_=ps[:, qk, :]), reads=[bank], writes=['pre'])
                else:
                    sc.op('pool', lambda e: e.memset(pre[:, :, 4:8], 0.0), writes=['pre'])
                W = 512 if g < NG else 2
                for qk in range(2):
                    cb = (4 * qk + h) * 5
                    sc.op('dve', lambda e, qk=qk, cb=cb, W=W: e.tensor_scalar(out=acc[:, qk, 0:W], in0=pre[:, qk, 0:W], scalar1=convT[:, cb:cb + 1],
                                                                            scalar2=None, op0=ALU.mult), reads=['pre', 'convT'], writes=[('acc', qk)])
                    for j in range(1, 5):
                        sc.op('dve', lambda e, qk=qk, cb=cb, j=j, W=W: e.scalar_tensor_tensor(
                            out=acc[:, qk, 0:W], in0=pre[:, qk, j:j + W], scalar=convT[:, cb + j:cb + j + 1], in1=acc[:, qk, 0:W],
                            op0=ALU.mult, op1=ALU.add), reads=['pre', 'convT', ('acc', qk)], writes=[('acc', qk)])
                    n0 = 2 if g == 0 else 0
                    t0 = 512 * g - 2 + n0
                    sc.op('act', lambda e, qk=qk, n0=n0, t0=t0, W=W: e.activation(out=qkT[:, qk, t0:t0 + W - n0], in_=acc[:, qk, n0:W], func=AF.Silu),
                          reads=[('acc', qk)], writes=[('qkT', qk)])
                if g < NG:
                    sc.op('pool', lambda e: e.tensor_copy(out=pre[:, :, 0:4], in_=pre[:, :, 512:516]), reads=['pre'], writes=['pre'])
                    for k in range(4):
                        i = 4 * g + k
                        bank = ('ps', 2 + (i % 2))

                        def mmvo(e, i=i):
                            for kc in range(8):
                                r = e.matmul(ps[:, 2 + (i % 2), 0:256], lhsT=h1T[:, kc, i * 128:(i + 1) * 128], rhs=wm[:, kc, 256:512],
                                             start=(kc == 0), stop=(kc == 7))
                            return r
                        sc.op('pe', mmvo, reads=['wm', ('h1T', i)], writes=[bank])
                        sc.op('act', lambda e, i=i: e.copy(out=vext[:, i, 0:128], in_=ps[:, 2 + (i % 2), 0:128]), reads=[bank], writes=[('vext', i)])
                        sc.op('act', lambda e, i=i: e.activation(out=sgo[:, i, :], in_=ps[:, 2 + (i % 2), 128:256], func=AF.Sigmoid),
                              reads=[bank], writes=[('sgo', i)])
            for g in range(NG):
                bank = ('ps', 4)
                pb = ps[:, 4, :].bitcast(BF16)

                def trk(e, g=g, pb=pb):
                    for k in range(4):
                        i = 4 * g + k
                        r = e.transpose(out=pb[:, k * 128:(k + 1) * 128], in_=qkT[:, 1, i * 128:(i + 1) * 128], identity=identb[:])
                    return r
                sc.op('pe', trk, reads=[('qkT', 1), 'identb'], writes=[bank])
                sc.op('act', lambda e, g=g, pb=pb: e.copy(out=ktok[:, 4 * g:4 * g + 4, :], in_=pb[:, 0:512].rearrange("p (k d) -> p k d", k=4)),
                      reads=[bank], writes=['ktok'])
            nv = 0
            for step in range(64):
                for d in range(2):
                    c = step if d == 0 else 63 - step
                    i, half = c // 2, c % 2
                    first_of_tile = (half == 0) if d == 0 else (half == 1)
                    vk = ('vhat', d)
                    vb = vtmp[d]
                    if first_of_tile:
                        sc.op('act', lambda e, d=d, i=i, vb=vb: e.activation(out=vb[:], in_=vext[:, i, :], func=AF.Copy,
                                                                             scale=tw[:, 3 * d + 1, i, h:h + 1]),
                              reads=[('vext', i), 'vext1', 'tw'], writes=[vk])
                    slot = nv % 4
                    nv += 1
                    pbank = 4 + slot
                    pc = 0
                    pk = ('ps', pbank)
                    sc.op('pe', lambda e, i=i, half=half, vb=vb, pbank=pbank, pc=pc: e.matmul(
                        ps[:, pbank, pc:pc + 129], lhsT=ktok[half * 64:(half + 1) * 64, i, :], rhs=vb[half * 64:(half + 1) * 64, :],
                        start=True, stop=True), reads=['ktok', vk], writes=[pk])
                    cur, nxt = step % 2, (step + 1) % 2
                    sc.op('dve', lambda e, d=d, i=i, half=half, cur=cur, nxt=nxt, pbank=pbank, pc=pc: e.scalar_tensor_tensor(
                        out=stt[:, d, nxt, :], in0=stt[:, d, cur, :], scalar=egs[:, 2 * d + half, i, h:h + 1], in1=ps[:, pbank, pc:pc + 129],
                        op0=ALU.mult, op1=ALU.add), reads=[('stt', d, cur), 'egs', pk], writes=[('stt', d, nxt)])
                    sc.op('pool', lambda e, d=d, nxt=nxt, step=step: e.tensor_copy(out=Cst[:, d, step + 1, :], in_=stt[:, d, nxt, :]),
                          reads=[('stt', d, nxt)], writes=[('Cst', d, step + 1)])
            for i in range(NT):
                blk = slice(i * 128, (i + 1) * 128)
                sbank = ('ps', 0 + (i % 2))
                sc.op('pe', lambda e, i=i, blk=blk: e.matmul(ps[:, i % 2, 0:128], lhsT=qkT[:, 1, blk], rhs=qkT[:, 0, blk], start=True, stop=True),
                      reads=[('qkT', 0), ('qkT', 1)], writes=[sbank])
                smF, smB = smt[2 * (i % 2)], smt[2 * (i % 2) + 1]
                kF, kB = ('smt', 2 * (i % 2)), ('smt', 2 * (i % 2) + 1)
                sc.op('dve', lambda e, i=i, smF=smF: e.tensor_tensor(out=smF[:], in0=ps[:, i % 2, 0:128], in1=maskF[:], op=ALU.mult),
                      reads=[sbank, 'maskF'], writes=[kF])
                sc.op('dve', lambda e, i=i, smB=smB: e.tensor_tensor(out=smB[:], in0=ps[:, i % 2, 0:128], in1=maskB[:], op=ALU.mult),
                      reads=[sbank, 'maskB'], writes=[kB])
                vF, vB = vtmp[2], vtmp[3]
                sc.op('act', lambda e, i=i, vF=vF: e.activation(out=vF[:], in_=vext[:, i, :], func=AF.Copy, scale=tw[:, 0, i, h:h + 1]),
                      reads=[('vext', i), 'vext1', 'tw'], writes=[('vtil', 0)])
                sc.op('act', lambda e, i=i, vB=vB: e.activation(out=vB[:], in_=vext[:, i, :], func=AF.Copy, scale=tw[:, 3, i, h:h + 1]),
                      reads=[('vext', i), 'vext1', 'tw'], writes=[('vtil', 1)])
                ub = 2 + (i % 2)
                ubank = ('ps', ub)

                def umm(e, i=i, blk=blk, smF=smF, smB=smB, vF=vF, vB=vB, ub=ub):
                    for d, sm, vt in ((0, smF, vF), (1, smB, vB)):
                        o0 = d * 129
                        e.matmul(ps[:, ub, o0:o0 + 129], lhsT=sm[:], rhs=vt[:], start=True, stop=False)
                        for half in range(2):
                            c = 2 * i + half
                            sidx = c if d == 0 else 63 - c
                            t0 = i * 128 + half * 64
                            r = e.matmul(ps[half * 64:(half + 1) * 64, ub, o0:o0 + 129], lhsT=qkT[:, 0, t0:t0 + 64],
                                         rhs=Cst[:, d, sidx, :], start=False, stop=True)
                    return r
                creads = [('Cst', 0, 2 * i), ('Cst', 0, 2 * i + 1), ('Cst', 1, 63 - 2 * i), ('Cst', 1, 62 - 2 * i)]
                sc.op('pe', umm, reads=[kF, kB, ('vtil', 0), ('vtil', 1), ('qkT', 0)] + creads, writes=[ubank])
                P = pst[:, i % 2, :]
                pk = ('pst', i % 2)
                U3 = ps[:, ub, 0:258].rearrange("p (d v) -> p d v", d=2)
                ea2 = tw[:, 2:6:3, i, h:h + 1]
                sc.op('dve', lambda e, P=P, U3=U3, ea2=ea2: e.tensor_tensor(out=P[:, 0:2].unsqueeze(2), in0=U3[:, :, 128:129], in1=ea2, op=ALU.mult),
                      reads=[ubank, 'tw'], writes=[pk])
                sc.op('dve', lambda e, P=P: e.tensor_scalar(out=P[:, 4:6], in0=P[:, 0:2], scalar1=-1.0, scalar2=None, op0=ALU.mult), reads=[pk], writes=[pk])
                sc.op('dve', lambda e, P=P: e.scalar_tensor_tensor(out=P[:, 2:4], in0=P[:, 4:6], scalar=1.0, in1=P[:, 0:2], op0=ALU.max, op1=ALU.max),
                      reads=[pk], writes=[pk])
                sc.op('dve', lambda e, P=P: e.reciprocal(out=P[:, 4:6], in_=P[:, 2:4]), reads=[pk], writes=[pk])
                sc.op('dve', lambda e, P=P, ea2=ea2: e.tensor_tensor(out=P[:, 6:8].unsqueeze(2), in0=P[:, 4:6].unsqueeze(2), in1=ea2, op=ALU.mult),
                      reads=[pk, 'tw'], writes=[pk])
                hfb, hsb = hf[i % 2], hs[i % 2]
                sc.op('dve', lambda e, P=P, hfb=hfb, ub=ub: e.tensor_scalar(out=hfb[:], in0=ps[:, ub, 0:128], scalar1=P[:, 6:7], scalar2=None, op0=ALU.mult),
                      reads=[ubank, pk], writes=[('hf', i % 2)])
                sc.op('dve', lambda e, P=P, hfb=hfb, hsb=hsb, ub=ub: e.scalar_tensor_tensor(out=hsb[:], in0=ps[:, ub, 129:257], scalar=P[:, 7:8], in1=hfb[:],
                                                                                        op0=ALU.mult, op1=ALU.add),
                      reads=[ubank, pk, ('hf', i % 2)], writes=[('hs', i % 2)])
                P2 = pst[:, i % 2, :]
                sc.op('act', lambda e, hsb=hsb, P2=P2: e.activation(out=jk[:], in_=hsb[:], func=AF.Square, accum_out=P2[:, 0:1]),
                      reads=[('hs', i % 2), pk], writes=['jk', pk])
                sc.op('act', lambda e, P2=P2: e.activation(out=P2[:, 1:2], in_=P2[:, 0:1], func=AF.Sqrt, scale=1.0 / 128, bias=EPS), reads=[pk], writes=[pk])
                sc.op('dve', lambda e, P2=P2: e.reciprocal(out=P2[:, 2:3], in_=P2[:, 1:2]), reads=[pk], writes=[pk])
                sc.op('dve', lambda e, hsb=hsb, P2=P2: e.scalar_tensor_tensor(out=hsb[:], in0=hsb[:], scalar=P2[:, 2:3],
                                                                            in1=prm[:, P_MNG + h * 128:P_MNG + (h + 1) * 128], op0=ALU.mult, op1=ALU.mult),
                      reads=[('hs', i % 2), pk, 'prm'], writes=[('hs', i % 2)])
                ym = ymt[i % 2]
                ymk = ('ymt', i % 2)
                sc.op('pool', lambda e, hsb=hsb, ym=ym, i=i: e.tensor_tensor(out=ym[:], in0=hsb[:], in1=sgo[:, i, :], op=ALU.mult),
                      reads=[('hs', i % 2), ('sgo', i)], writes=[ymk])
                emit_y_tile(ph, h, i, ym[:], ymk, yst[(i // 4) % 2], ('yst', (i // 4) % 2))
            if debug and h == 0:
                o = dbg_out("dbg_qkT", [128, 2, S], BF16)
                sc.dma('sp', lambda e: e.dma_start(out=o[:, :, :], in_=qkT[:]), 'dbgqkT', reads=[('qkT', 0), ('qkT', 1)])
                o = dbg_out("dbg_Cst", [128, 2, 65, 129], BF16)
                sc.dma('sp', lambda e: e.dma_start(out=o[:, :, :, :], in_=Cst[:]), 'dbgCst',
                       reads=[('Cst', d, k) for d in range(2) for k in range(65)])
            sc.barrier()

    if stage <= 2.5:
        sc.finish()
        return nc, dbg

    cosT = sb("cosT", [128, NT, 8], F32, s1)
    sinT = sb("sinT", [128, NT, 8], F32, s1)
    qkg4 = sb("qkg4", [128, 4, 64], F32, s1)
    subg8 = sb("subg8", [128, 128], F32, s1)
    TWO_PI = 6.28318
    with ExitStack() as pa:
        posi = sb("posi", [128, NT], I32, pa)
        posf = sb("posf", [128, NT], F32, pa)
        ut = sb("ut", [128, NT, 8], F32, pa)
        ui = sb("ui", [128, NT, 8], I32, pa)
        uf = sb("uf", [128, NT, 8], F32, pa)
        jl = sb("jl", [128, 64], F32, pa)
        sc.dma('sp', lambda e: e.dma_start(out=posi[:], in_=pos_d[:, :]), 'posi', writes=['posi'])
        sc.op('dve', lambda e: e.tensor_copy(out=posf[:], in_=posi[:]), reads=['posi'], writes=['posf'])
        sc.op('dve', lambda e: e.tensor_tensor(out=ut[:], in0=posf[:].unsqueeze(2).broadcast_to([128, NT, 8]),
                                               in1=consts[:, C_FREQ:C_FREQ + 8].unsqueeze(1).broadcast_to([128, NT, 8]), op=ALU.mult),
              reads=['posf', 'consts'], writes=['ut'])
        for tab, shift in ((sinT, 0.0), (cosT, 0.25)):
            if shift:
                sc.op('dve', lambda e, shift=shift: e.tensor_scalar(out=ut[:], in0=ut[:], scalar1=shift, scalar2=None, op0=ALU.add),
                      reads=['ut'], writes=['ut'])
            sc.op('dve', lambda e: e.tensor_copy(out=ui[:], in_=ut[:]), reads=['ut'], writes=['ui'])
            sc.op('dve', lambda e: e.tensor_copy(out=uf[:], in_=ui[:]), reads=['ui'], writes=['uf'])
            sc.op('dve', lambda e: e.tensor_tensor(out=uf[:], in0=ut[:], in1=uf[:], op=ALU.subtract), reads=['ut', 'uf'], writes=['uf'])
            sc.op('act', lambda e, tab=tab: e.activation(out=tab[:], in_=uf[:], func=AF.Sin, scale=TWO_PI), reads=['uf'], writes=['rot'])
        sc.op('dve', lambda e: e.tensor_reduce(out=small[:, 2:4], in_=prm[:, P_QKG:P_QKG + 128].rearrange("p (a d) -> p a d", a=2),
                                               axis=AX.X, op=ALU.max, apply_absolute_value=True), reads=['prm'], writes=['small'])
        sc.op('dve', lambda e: e.scalar_tensor_tensor(out=small[:, SM_NB:SM_NB + 1], in0=small[:, 2:3], scalar=-8.0, in1=small[:, 3:4],
                                                      op0=ALU.mult, op1=ALU.mult), reads=['small'], writes=['small'])
        for k in range(2):
            sc.op('dve', lambda e, k=k: e.tensor_tensor(out=jl[:], in0=prm[:, P_LAM + 128 * k:P_LAM + 128 * k + 64],
                                                        in1=prm[:, P_LAM + 128 * k + 64:P_LAM + 128 * k + 128], op=ALU.mult),
                  reads=['prm'], writes=['jl'])
            sc.op('dve', lambda e, k=k: e.tensor_reduce(out=small[:, 4 + k:5 + k], in_=jl[:], axis=AX.X, op=ALU.add), reads=['jl'], writes=['small'])
        sc.op('act', lambda e: e.activation(out=small[:, 6:8], in_=small[:, 4:6], func=AF.Exp), reads=['small'], writes=['small'])
        sc.op('dve', lambda e: e.scalar_tensor_tensor(out=small[:, SM_NLAM:SM_NLAM + 1], in0=small[:, 7:8], scalar=-LAM_INIT, in1=small[:, 6:7],
                                                      op0=ALU.add, op1=ALU.subtract), reads=['small'], writes=['small'])
        for a in range(4):
            o_ = P_QKG + (64 if a >= 2 else 0)
            sc.op('pool', lambda e, a=a, o_=o_: e.tensor_copy(out=qkg4[:, a, :], in_=prm[:, o_:o_ + 64]), reads=['prm'], writes=['qkg4'])
        sc.op('dve', lambda e: e.tensor_scalar(out=subg8[:], in0=prm[:, P_SUB:P_SUB + 128], scalar1=1.0 - LAM_INIT, scalar2=None, op0=ALU.mult),
              reads=['prm'], writes=['subg8'])
        if debug:
            o = dbg_out("dbg_small", [128, 64])
            sc.op('pool', lambda e: e.memset(small[:, 8:64], 0.0), writes=['small'])
            sc.dma('sp', lambda e: e.dma_start(out=o[:, :], in_=small[:]), 'dbgsmall', reads=['small'])
            o = dbg_out("dbg_cos", [128, NT, 8])
            sc.dma('sp', lambda e: e.dma_start(out=o[:, :, :], in_=cosT[:]), 'dbgcos', reads=['rot'])
        sc.barrier()

    n_aheads = 4 if stage >= 3.5 else 1
    for h in range(n_aheads):
        with ExitStack() as ph:
            wa_ = sb("waa", [128, 8, 384], BF16, ph)
            qTa = sb("qTa", [128, 2, S], BF16, ph)
            kTa = sb("kTa", [128, S], BF16, ph)
            vxa = sb("vxa", [128, NT, 129], BF16, ph)
            xq = [sb("xq%d" % k, [128, 4, 4, 64], F32, ph) for k in range(2)]
            sq = sb("sq", [128, 4, 4, 64], F32, ph)
            rp = [sb("rp%d" % k, [128, 4, 4, 4, 8], F32, ph) for k in range(2)]
            st4 = sb("st4", [128, 2, 48], F32, ph)
            xr = [sb("xr%d" % k, [128, 4, 4, 64], BF16, ph) for k in range(2)]
            pt = [sb("pt%d" % k, [128, 2, 512], BF16, ph) for k in range(3)]
            o0 = sb("o0", [128, 4, 128], F32, ph)
            ob = [sb("ob%d" % k, [128, 128], F32, ph) for k in range(2)]
            st5 = sb("st5", [128, 2, 8], F32, ph)
            jk2 = sb("jk2", [128, 128], F32, ph)
            ymt = [sb("ymta%d" % k, [128, 128], BF16, ph) for k in range(2)]
            yst = [sb("ysta%d" % k, [128, 512], BF16, ph) for k in range(2)]
            for k, c0 in enumerate((2064 + h * 128, 2576 + h * 128, 3088 + h * 128)):
                sc.dma('pool', lambda e, k=k, c0=c0: e.dma_start(out=wa_[:, :, k * 128:(k + 1) * 128], in_=win_v[:, :, c0:c0 + 128]),
                       'waa', writes=['waa'])
            sc.op('pool', lambda e: e.memset(vxa[:, :, 128:129], 1.0), writes=['vxa1'])
            sc.op('pool', lambda e: e.memset(qTa[64:128, 0, :], 0.0), writes=['qTa'])
            sc.op('pool', lambda e: e.memset(qTa[0:64, 1, :], 0.0), writes=['qTa'])
            for g in range(NG):
                b = g % 2
                banks = [('ps', k) for k in range(4)]

                def mmp(e, g=g):
                    for t in range(4):
                        i = 4 * g + t
                        for kc in range(8):
                            r = e.matmul(ps[:, t, 0:384], lhsT=h1T[:, kc, i * 128:(i + 1) * 128], rhs=wa_[:, kc, :], start=(kc == 0), stop=(kc == 7))
                    return r
                sc.op('pe', mmp, reads=['waa'] + [('h1T', 4 * g + t) for t in range(4)], writes=banks)
                xk, rk, sk, xrk = ('xq', b), ('rp', b), ('st4', b), ('xr', b)
                X = xq[b]
                X3 = X[:].rearrange("p t a d -> p t (a d)")
                X16 = X[:].rearrange("p t a d -> p (t a) d")
                sc.op('act', lambda e, X3=X3: e.copy(out=X3, in_=ps[:, 0:4, 0:256]), reads=banks, writes=[xk])
                sc.op('act', lambda e, g=g: e.copy(out=vxa[:, 4 * g:4 * g + 4, 0:128], in_=ps[:, 0:4, 256:384]), reads=banks,
                      writes=[('vxa', 4 * g + t) for t in range(4)])
                sc.op('dve', lambda e, X=X: e.tensor_tensor(out=sq[:], in0=X[:], in1=X[:], op=ALU.mult), reads=[xk], writes=['sq'])
                T4 = st4[:, b, :]
                sc.op('dve', lambda e, T4=T4: e.tensor_reduce(out=T4[:, 0:16], in_=sq[:].rearrange("p t a d -> p (t a) d"), axis=AX.X, op=ALU.add),
                      reads=['sq'], writes=[sk])
                sc.op('act', lambda e, T4=T4: e.activation(out=T4[:, 16:32], in_=T4[:, 0:16], func=AF.Sqrt, scale=1.0 / 64, bias=EPS), reads=[sk], writes=[sk])
                sc.op('dve', lambda e, T4=T4: e.reciprocal(out=T4[:, 32:48], in_=T4[:, 16:32]), reads=[sk], writes=[sk])
                sc.op('dve', lambda e, X16=X16, T4=T4: e.tensor_tensor(out=X16, in0=X16, in1=T4[:, 32:48].unsqueeze(2).broadcast_to([128, 16, 64]), op=ALU.mult),
                      reads=[xk, sk], writes=[xk])
                sc.op('dve', lambda e, X=X: e.tensor_tensor(out=X[:], in0=X[:], in1=qkg4[:].unsqueeze(1).broadcast_to([128, 4, 4, 64]), op=ALU.mult),
                      reads=[xk, 'qkg4'], writes=[xk])
                cs = cosT[:, 4 * g:4 * g + 4, :].unsqueeze(2).broadcast_to([128, 4, 4, 8])
                sn = sinT[:, 4 * g:4 * g + 4, :].unsqueeze(2).broadcast_to([128, 4, 4, 8])
                R = rp[b]
                for k, (tt, tr_) in enumerate(((X[:, :, :, 0:8], cs), (X[:, :, :, 8:16], sn), (X[:, :, :, 8:16], cs), (X[:, :, :, 0:8], sn))):
                    sc.op('pool', lambda e, k=k, tt=tt, tr_=tr_, R=R: e.tensor_tensor(out=R[:, k, :, :, :], in0=tt, in1=tr_, op=ALU.mult),
                          reads=[xk, 'rot'], writes=[rk])
                XR = xr[b]
                sc.op('act', lambda e, XR=XR, X=X: e.copy(out=XR[:], in_=X[:]), reads=[xk], writes=[xrk])
                sc.op('dve', lambda e, XR=XR, R=R: e.tensor_tensor(out=XR[:, :, :, 0:8], in0=R[:, 0, :, :, :], in1=R[:, 1, :, :, :], op=ALU.subtract),
                      reads=[rk, xrk], writes=[xrk])
                sc.op('dve', lambda e, XR=XR, R=R: e.tensor_tensor(out=XR[:, :, :, 8:16], in0=R[:, 2, :, :, :], in1=R[:, 3, :, :, :], op=ALU.add),
                      reads=[rk, xrk], writes=[xrk])
                tbanks = [('ps', 4), ('ps', 5)]
                pbq = ps[:, 4, :].bitcast(BF16)
                pbk = ps[:, 5, :].bitcast(BF16)
                XRf = XR[:].rearrange("p t a d -> p t (a d)")

                def trq(e, XRf=XRf, pbq=pbq, pbk=pbk):
                    for t in range(4):
                        e.transpose(out=pbq[:, t * 128:(t + 1) * 128], in_=XRf[:, t, 0:128], identity=identb[:])
                        r = e.transpose(out=pbk[:, t * 128:(t + 1) * 128], in_=XRf[:, t, 128:256], identity=identb[:])
                    return r
                sc.op('pe', trq, reads=[xrk, 'identb'], writes=tbanks)
                sc.op('act', lambda e, pbq=pbq, g=g: e.copy(out=qTa[0:64, 0, g * 512:(g + 1) * 512], in_=pbq[0:64, 0:512]), reads=tbanks, writes=['qTa'])
                sc.op('act', lambda e, pbq=pbq, g=g: e.copy(out=qTa[64:128, 1, g * 512:(g + 1) * 512], in_=pbq[64:128, 0:512]), reads=tbanks, writes=['qTa'])
                sc.op('act', lambda e, pbk=pbk, g=g: e.copy(out=kTa[:, g * 512:(g + 1) * 512], in_=pbk[:, 0:512]), reads=tbanks, writes=['kTa'])
            if debug and h == 0:
                o = dbg_out("dbg_qTa", [128, S], BF16)
                sc.dma('sp', lambda e: e.dma_start(out=o[0:64, :], in_=qTa[0:64, 0, :]), 'dbgqTa', reads=['qTa'])
                sc.dma('sp', lambda e: e.dma_start(out=o[64:128, :], in_=qTa[64:128, 1, :]), 'dbgqTa', reads=['qTa'])
                o = dbg_out("dbg_kTa", [128, S], BF16)
                sc.dma('sp', lambda e: e.dma_start(out=o[:, :], in_=kTa[:]), 'dbgkTa', reads=['kTa'])
            nqb = 8 if stage >= 3.2 else 1
            for qb in range(nqb):
                for p in range(2):
                    pr = slice(64 * p, 64 * p + 64)
                    def st_mm(j):
                        pb0 = 4 + 2 * (j % 2)

                        def f(e, j=j, pb0=pb0):
                            for u in range(2):
                                kt = 2 * j + u
                                r = e.matmul(ps[:, pb0 + u, :], lhsT=kTa[:, kt * 128:(kt + 1) * 128], rhs=qTa[:, p, qb * 512:(qb + 1) * 512],
                                             start=True, stop=True)
                            return r
                        sc.op('pe', f, reads=['qTa', 'kTa'], writes=[('ps', pb0), ('ps', pb0 + 1)])
                    st_mm(0)
                    for j in range(NT // 2):
                        pb0 = 4 + 2 * (j % 2)
                        sbanks = [('ps', pb0), ('ps', pb0 + 1)]
                        if j + 1 < NT // 2:
                            st_mm(j + 1)
                        ptk = ('pt', j % 3)
                        ptb = pt[j % 3]
                        sc.op('act', lambda e, pb0=pb0, ptb=ptb: e.activation(out=ptb[:], in_=ps[:, pb0:pb0 + 2, :], func=AF.Exp, scale=0.125,
                                                                              bias=small[:, SM_NB:SM_NB + 1]),
                              reads=sbanks + ['small'], writes=[ptk])

                        def pv(e, ptb=ptb, j=j):
                            for u in range(2):
                                kt = 2 * j + u
                                for qs in range(4):
                                    r = e.matmul(ps[:, qs, 0:129], lhsT=ptb[:, u, qs * 128:(qs + 1) * 128], rhs=vxa[:, kt, :],
                                                 start=(kt == 0), stop=(kt == NT - 1))
                            return r
                        sc.op('pe', pv, reads=[ptk, ('vxa', 2 * j), ('vxa', 2 * j + 1), 'vxa1'], writes=[('ps', 0), ('ps', 1), ('ps', 2), ('ps', 3)])
                    for qs in range(4):
                        i = qb * 4 + qs
                        T5 = st5[:, qs % 2, :]
                        tk5 = ('st5', qs % 2)
                        abank = ('ps', qs)
                        sc.op('dve', lambda e, T5=T5, qs=qs: e.reciprocal(out=T5[:, 0:1], in_=ps[:, qs, 128:129]), reads=[abank], writes=[tk5])
                        if p == 0:
                            sc.op('dve', lambda e, T5=T5, qs=qs: e.tensor_scalar(out=o0[:, qs, :], in0=ps[:, qs, 0:128], scalar1=T5[:, 0:1], scalar2=None,
                                                                                op0=ALU.mult), reads=[abank, tk5], writes=[('o0', qs)])
                            continue
                        O = ob[qs % 2]
                        okk = ('ob', qs % 2)
                        sc.op('dve', lambda e, T5=T5: e.tensor_tensor(out=T5[:, 1:2], in0=T5[:, 0:1], in1=small[:, SM_NLAM:SM_NLAM + 1], op=ALU.mult),
                              reads=[tk5, 'small'], writes=[tk5])
                        sc.op('dve', lambda e, T5=T5, qs=qs, O=O: e.scalar_tensor_tensor(out=O[:], in0=ps[:, qs, 0:128], scalar=T5[:, 1:2], in1=o0[:, qs, :],
                                                                                       op0=ALU.mult, op1=ALU.add),
                              reads=[abank, tk5, ('o0', qs)], writes=[okk])
                        sc.op('act', lambda e, T5=T5, O=O: e.activation(out=jk2[:], in_=O[:], func=AF.Square, accum_out=T5[:, 2:3]),
                              reads=[okk, tk5], writes=['jk2', tk5])
                        sc.op('act', lambda e, T5=T5: e.activation(out=T5[:, 3:4], in_=T5[:, 2:3], func=AF.Sqrt, scale=1.0 / 128, bias=EPS), reads=[tk5], writes=[tk5])
                        sc.op('dve', lambda e, T5=T5: e.reciprocal(out=T5[:, 4:5], in_=T5[:, 3:4]), reads=[tk5], writes=[tk5])
                        ym = ymt[qs % 2]
                        ymk = ('ymta', qs % 2)
                        sc.op('dve', lambda e, T5=T5, O=O, ym=ym: e.scalar_tensor_tensor(out=ym[:], in0=O[:], scalar=T5[:, 4:5], in1=subg8[:],
                                                                                       op0=ALU.mult, op1=ALU.mult), reads=[okk, tk5, 'subg8'], writes=[ymk])
                        emit_y_tile(ph, 4 + h, i, ym[:], ymk, yst[qb % 2], ('ysta', qb % 2))
            sc.barrier()

    if stage <= 3.5:
        sc.finish()
        return nc, dbg

    s1.close()

    sW = ExitStack()
    aff = sb("aff", [128, NT, NE], F32, sW)
    g2rep = sb("g2rep", [128, D], F32, sW)
    with ExitStack() as pw:
        woutb = sb("woutb", [128, 8, D], BF16, pw)
        wrt = sb("wrt", [128, 8, NE], F32, pw)
        ytl = [sb("ytl%d" % k, [128, 8, 512], BF16, pw) for k in range(2)]
        xt2 = [sb("xt2_%d" % k, [128, D], F32, pw) for k in range(2)]
        tmpw = [sb("tmpw%d" % k, [128, D], F32, pw) for k in range(2)]
        x1t = [sb("x1t%d" % k, [128, D], F32, pw) for k in range(2)]
        h2f = [sb("h2f%d" % k, [128, D], F32, pw) for k in range(2)]
        h2b = [sb("h2b%d" % k, [128, D], BF16, pw) for k in range(2)]
        h2T = [sb("h2T%d" % k, [128, 8, 128], F32, pw) for k in range(2)]
        junkw = sb("junkw", [128, D], BF16, pw)
        stw = sb("stw", [128, NT, 8], F32, pw)
        esm = sb("esm", [128, 2, NE], F32, pw)
        wout_v = wout_d.rearrange("(kc p) n -> p kc n", p=128)
        sc.dma('pool', lambda e: e.dma_start(out=woutb[:, 0:4, :], in_=wout_v[:, 0:4, :]), 'woutb', writes=['woutb'])
        sc.dma('pool', lambda e: e.dma_start(out=woutb[:, 4:8, :], in_=wout_v[:, 4:8, :]), 'woutb', writes=['woutb'])
        sc.dma('sp', lambda e: e.dma_start(out=wrt[:], in_=wr_d.rearrange("(kc p) n -> p kc n", p=128)), 'wrt', writes=['wrt'])
        modW = sb("modW", [128, 3 * D], F32, pw)
        sc.dma('sp', lambda e: e.dma_start(out=modW[:], in_=modrow_d[0:1, 2 * D:5 * D].partition_broadcast(128)), 'modW', reads=['modrow_d'], writes=['mod'])
        sc.dma('sp', lambda e: e.dma_start(out=g2rep[:], in_=modrow_d[0:1, 5 * D:6 * D].partition_broadcast(128)), 'g2rep', reads=['modrow_d'], writes=['g2rep'])
        GATE1, SHIFT2, GS2 = modW[:, 0:D], modW[:, D:2 * D], modW[:, 2 * D:3 * D]
        for i in range(NT):
            b = i % 2
            g, k4 = i // 4, i % 4
            yk = ('ytl', g % 2)
            if k4 == 0:
                sc.dma('sp', lambda e, g=g: e.dma_start(out=ytl[g % 2][:], in_=yT_v[:, :, g * 512:(g + 1) * 512]), yk, writes=[yk])
            xk = ('xt2', b)
            sc.dma('sp', lambda e, i=i, b=b: e.dma_start(out=xt2[b][:], in_=x_d[i * 128:(i + 1) * 128, :]), xk, writes=[xk])
            banks = [('ps', 2 * b), ('ps', 2 * b + 1)]

            def mmw(e, i=i, b=b, g=g, k4=k4):
                for dh in range(2):
                    for c in range(8):
                        r = e.matmul(ps[:, 2 * b + dh, :], lhsT=ytl[g % 2][:, c, k4 * 128:(k4 + 1) * 128], rhs=woutb[:, c, dh * 512:(dh + 1) * 512],
                                     start=(c == 0), stop=(c == 7))
                return r
            sc.op('pe', mmw, reads=[yk, 'woutb'], writes=banks)
            mixv = ps[:, 2 * b:2 * b + 2, :].rearrange("p a n -> p (a n)")
            sc.op('dve', lambda e, b=b, mixv=mixv: e.tensor_tensor(out=tmpw[b][:], in0=mixv, in1=GATE1, op=ALU.mult), reads=banks + ['mod'], writes=[('tmpw', b)])
            sc.op('pool', lambda e, b=b: e.tensor_tensor(out=x1t[b][:], in0=tmpw[b][:], in1=xt2[b][:], op=ALU.add),
                  reads=[('tmpw', b), xk], writes=[('x1t', b)])
            sc.dma('sp', lambda e, i=i, b=b: e.dma_start(out=out_d[i * 128:(i + 1) * 128, :], in_=x1t[b][:]), ('x1s', b), reads=[('x1t', b)], writes=[('outd', i)])
            T = stw[:, i, :]
            tk = ('stw', i)
            sc.op('act', lambda e, b=b, T=T: e.activation(out=junkw[:], in_=x1t[b][:], func=AF.Square, accum_out=T[:, 0:1]), reads=[('x1t', b)], writes=['junkw', tk])
            sc.op('act', lambda e, T=T: e.activation(out=T[:, 1:2], in_=T[:, 0:1], func=AF.Sqrt, scale=1.0 / D, bias=EPS), reads=[tk], writes=[tk])
            sc.op('dve', lambda e, T=T: e.reciprocal(out=T[:, 2:3], in_=T[:, 1:2]), reads=[tk], writes=[tk])
            sc.op('dve', lambda e, b=b, T=T: e.scalar_tensor_tensor(out=tmpw[b][:], in0=x1t[b][:], scalar=T[:, 2:3], in1=GS2, op0=ALU.mult, op1=ALU.mult),
                  reads=[('x1t', b), tk, 'mod'], writes=[('tmpw', b)])
            sc.op('pool', lambda e, b=b: e.tensor_tensor(out=h2f[b][:], in0=tmpw[b][:], in1=SHIFT2, op=ALU.add), reads=[('tmpw', b), 'mod'], writes=[('h2f', b)])
            sc.op('act', lambda e, b=b: e.copy(out=h2b[b][:], in_=h2f[b][:]), reads=[('h2f', b)], writes=[('h2b', b)])
            sc.dma('sp', lambda e, i=i, b=b: e.dma_start(out=h2_d[i * 128:(i + 1) * 128, :], in_=h2b[b][:]), ('h2s', b), reads=[('h2b', b)], writes=[('h2d', i)])
            tb = [('ps', 4), ('ps', 5)]

            def trh(e, b=b):
                for kc in range(8):
                    r = e.transpose(out=ps[:, 4 + kc // 4, (kc % 4) * 128:(kc % 4 + 1) * 128], in_=h2f[b][:, kc * 128:(kc + 1) * 128], identity=ident)
                return r
            sc.op('pe', trh, reads=[('h2f', b), 'consts'], writes=tb)
            sc.op('act', lambda e, b=b: e.copy(out=h2T[b][:].rearrange("p k t -> p (k t)"), in_=ps[:, 4:6, :].rearrange("p a n -> p (a n)")),
                  reads=tb, writes=[('h2T', b)])
            lb = ('ps', 6 + b)

            def mml(e, b=b):
                for kc in range(8):
                    r = e.matmul(ps[:, 6 + b, 0:NE], lhsT=h2T[b][:, kc, :], rhs=wrt[:, kc, :], start=(kc == 0), stop=(kc == 7))
                return r
            sc.op('pe', mml, reads=[('h2T', b), 'wrt'], writes=[lb])
            sc.op('dve', lambda e, b=b, T=T: e.tensor_reduce(out=T[:, 3:4], in_=ps[:, 6 + b, 0:NE], axis=AX.X, op=ALU.max), reads=[lb], writes=[tk])
            sc.op('dve', lambda e, T=T: e.tensor_scalar(out=T[:, 4:5], in0=T[:, 3:4], scalar1=-1.0, scalar2=None, op0=ALU.mult), reads=[tk], writes=[tk])
            sc.op('act', lambda e, b=b, T=T: e.activation(out=esm[:, b, :], in_=ps[:, 6 + b, 0:NE], func=AF.Exp, bias=T[:, 4:5], accum_out=T[:, 5:6]),
                  reads=[lb, tk], writes=[('esm', b), tk])
            sc.op('dve', lambda e, T=T: e.reciprocal(out=T[:, 6:7], in_=T[:, 5:6]), reads=[tk], writes=[tk])
            sc.op('dve', lambda e, b=b, i=i, T=T: e.tensor_scalar(out=aff[:, i, :], in0=esm[:, b, :], scalar1=T[:, 6:7], scalar2=None, op0=ALU.mult),
                  reads=[('esm', b), tk], writes=['aff'])
        if debug:
            o = dbg_out("dbg_aff", [128, NT, NE])
            sc.dma('sp', lambda e: e.dma_start(out=o[:, :, :], in_=aff[:]), 'dbgaff', reads=['aff'])
        sc.barrier()

    if stage <= 4:
        sc.finish()
        return nc, dbg

    rank_tok = sb("rank_tok", [128, NT, NE], F32, sW)
    R5 = sb("R5", [128, NT, NE, 5], BF16, sW)
    with ExitStack() as pr:
        affT = sb("affT", [NE, S], F32, pr)
        junkR = sb("junkR", [NE, S], F32, pr)
        mkT = sb("mkT", [NE, S], F32, pr)
        csT = sb("csT", [NE, S], F32, pr)
        bs = sb("bs", [NE, 4], F32, pr)
        r1 = sb("r1", [128, NT, NE], F32, pr)
        for rnd in range(2):
            banks = [('ps', k) for k in range(4)]

            def tra(e, rnd=rnd):
                for k in range(16):
                    i = rnd * 16 + k
                    r = e.transpose(out=ps[0:NE, k // 4, (k % 4) * 128:(k % 4 + 1) * 128], in_=aff[:, i, :], identity=ident)
                return r
            sc.op('pe', tra, reads=['aff', 'consts'], writes=banks)
            sc.op('act', lambda e, rnd=rnd: e.copy(out=affT[:, rnd * 2048:(rnd + 1) * 2048], in_=ps[0:NE, 0:4, :].rearrange("p a n -> p (a n)")),
                  reads=banks, writes=['affT'])
        sc.op('dve', lambda e: e.memset(bs[:, 0:1], 0.0), writes=['bs'])
        for n in range(NBIS):
            w = 2.0 ** (-(n + 1))
            sc.op('dve', lambda e, w=w: e.tensor_scalar(out=bs[:, 1:2], in0=bs[:, 0:1], scalar1=w, scalar2=None, op0=ALU.add), reads=['bs'], writes=['bs'])
            sc.op('dve', lambda e: e.tensor_scalar(out=junkR[:], in0=affT[:], scalar1=bs[:, 1:2], scalar2=None, op0=ALU.is_gt, op1=ALU.add,
                                                   accum_out=bs[:, 2:3]), reads=['affT', 'bs'], writes=['junkR', 'bs'])
            sc.op('dve', lambda e: e.tensor_scalar(out=bs[:, 3:4], in0=bs[:, 2:3], scalar1=CAP - 0.5, scalar2=None, op0=ALU.is_gt), reads=['bs'], writes=['bs'])
            sc.op('dve', lambda e, w=w: e.scalar_tensor_tensor(out=bs[:, 0:1], in0=bs[:, 3:4], scalar=w, in1=bs[:, 0:1], op0=ALU.mult, op1=ALU.add),
                  reads=['bs'], writes=['bs'])
        sc.op('dve', lambda e: e.tensor_scalar(out=mkT[:], in0=affT[:], scalar1=bs[:, 0:1], scalar2=None, op0=ALU.is_gt), reads=['affT', 'bs'], writes=['mkT'])
        sc.op('pool', lambda e: e.memset(junkR[:], 1.0), reads=[], writes=['junkR'])
        sc.op('dve', lambda e: e.tensor_tensor_scan(out=csT[:], data0=junkR[:], data1=mkT[:], initial=0.0, op0=ALU.mult, op1=ALU.add),
              reads=['junkR', 'mkT'], writes=['csT'])
        sc.op('dve', lambda e: e.tensor_tensor(out=csT[:], in0=csT[:], in1=mkT[:], op=ALU.mult), reads=['csT', 'mkT'], writes=['csT'])
        sc.op('dve', lambda e: e.tensor_scalar(out=csT[:], in0=csT[:], scalar1=-1.0, scalar2=None, op0=ALU.add), reads=['csT'], writes=['csT'])
        rb = ('ps', 4)

        def trr(e):
            for i in range(NT):
                r = e.transpose(out=ps[:, 4, i * NE:(i + 1) * NE], in_=csT[:, i * 128:(i + 1) * 128], identity=consts[0:NE, C_IDENT:C_IDENT + NE])
            return r
        sc.op('pe', trr, reads=['csT', 'consts'], writes=[rb])
        sc.op('act', lambda e: e.copy(out=rank_tok[:].rearrange("p t e -> p (t e)"), in_=ps[:, 4, :]), reads=[rb], writes=['rank_tok'])
        sc.op('pool', lambda e: e.tensor_copy(out=R5[:, :, :, 0], in_=consts[:, C_TLO:C_TLO + 1].unsqueeze(2).broadcast_to([128, NT, NE])),
              reads=['consts'], writes=['R5'])
        sc.op('pool', lambda e: e.tensor_copy(out=R5[:, :, :, 1], in_=consts[:, C_THI:C_THI + NT].unsqueeze(2).broadcast_to([128, NT, NE])),
              reads=['consts'], writes=['R5'])
        sc.op('dve', lambda e: e.tensor_copy(out=R5[:, :, :, 2], in_=aff[:]), reads=['aff'], writes=['R5'])
        sc.op('dve', lambda e: e.tensor_tensor(out=r1[:], in0=aff[:], in1=R5[:, :, :, 2], op=ALU.subtract), reads=['aff', 'R5'], writes=['r1'])
        sc.op('dve', lambda e: e.tensor_copy(out=R5[:, :, :, 3], in_=r1[:]), reads=['r1'], writes=['R5'])
        sc.op('dve', lambda e: e.tensor_tensor(out=r1[:], in0=r1[:], in1=R5[:, :, :, 3], op=ALU.subtract), reads=['r1', 'R5'], writes=['r1'])
        sc.op('dve', lambda e: e.tensor_copy(out=R5[:, :, :, 4], in_=r1[:]), reads=['r1'], writes=['R5'])
        if debug:
            o = dbg_out("dbg_rank", [128, NT, NE])
            sc.dma('sp', lambda e: e.dma_start(out=o[:, :, :], in_=rank_tok[:]), 'dbgrank', reads=['rank_tok'])
            o = dbg_out("dbg_thr", [NE, 4])
            sc.dma('sp', lambda e: e.dma_start(out=o[:, :], in_=bs[:]), 'dbgthr', reads=['bs'])
        sc.barrier()

    if stage <= 4.5:
        sc.finish()
        return nc, dbg

    n_exp = NE if stage >= 6 else int(round((stage - 5) * 10)) + 1
    with ExitStack() as pe_:
        NSLOT = 6
        ring = [sb("ring%d" % k, [128, 8, D], BF16, pe_) for k in range(NSLOT)]
        Pm = sb("Pm", [128, NT, 512], BF16, pe_)
        xe = sb("xe", [128, 4, D], BF16, pe_)
        xeT2 = [sb("xeT%d" % k, [128, 8, 512], BF16, pe_) for k in range(2)]
        aT = sb("aT", [128, 16, 512], BF16, pe_)
        sgt = [sb("sgt%d" % k, [128, 512], BF16, pe_) for k in range(2)]
        yv = sb("yv", [128, 4, D], F32, pe_)
        idf = sb("idf", [128, 2, 8], F32, pe_)
        idp = sb("idp", [128, 2, 32], F32, pe_)
        idxi = [sb("idxi%d" % k, [128, 4], I32, pe_) for k in range(2)]
        wg_v = wg_d.rearrange("(e kc p) n -> p e kc n", p=128, kc=8)
        wu_v = wu_d.rearrange("(e kc p) n -> p e kc n", p=128, kc=8)
        wd_v = wd_d.rearrange("(e fh fc p) n -> p e fh fc n", p=128, fc=8, fh=2)

        def load_piece(e_, k):
            slot = ring[k]
            key = ('ring', k)
            if k < 4:
                src = (wg_v if k % 2 == 0 else wu_v)[:, e_, :, (k // 2) * D:(k // 2 + 1) * D]
            else:
                src = wd_v[:, e_, k - 4, :, :]
            sc.dma('pool', lambda e, slot=slot, src=src: e.dma_start(out=slot[:], in_=src), key, writes=[key])

        def prep_a(e_):
            for i in range(NT):
                sc.op('dve', lambda e, i=i: e.tensor_scalar(out=Pm[:, i, :], in0=consts[:, C_IOTA:C_IOTA + 512], scalar1=rank_tok[:, i, e_:e_ + 1],
                                                            scalar2=None, op0=ALU.is_equal), reads=['consts', 'rank_tok'], writes=['Pm'])

        def prep_b(e_):
            b = e_ % 2
            ibank = ('ps', 7)

            def imm(e):
                for cc in range(4):
                    for i in range(NT):
                        r = e.matmul(ps[:, 7, cc * 8:cc * 8 + 5], lhsT=Pm[:, i, cc * 128:(cc + 1) * 128], rhs=R5[:, i, e_, :],
                                     start=(i == 0), stop=(i == NT - 1))
                return r
            sc.op('pe', imm, reads=['Pm', 'R5'], writes=[ibank])
            sc.op('act', lambda e, b=b: e.copy(out=idp[:, b, :].rearrange("p (c k) -> p c k", k=8)[:, :, 0:5], in_=ps[:, 7, 0:32].rearrange("p (c k) -> p c k", k=8)[:, :, 0:5]), reads=[ibank], writes=[('idp', b)])
            ibank = ('idp', b)
            I3 = idp[:, b, :].rearrange("p (c k) -> p c k", k=8)
            F_ = idf[:, b, :]
            fk = ('idf', b)
            sc.op('dve', lambda e, F_=F_, I3=I3: e.scalar_tensor_tensor(out=F_[:, 0:4].unsqueeze(2), in0=I3[:, :, 1:2], scalar=128.0, in1=I3[:, :, 0:1],
                                                                      op0=ALU.mult, op1=ALU.add), reads=[ibank], writes=[fk])
            sc.op('dve', lambda e, F_=F_, I3=I3: e.tensor_reduce(out=F_[:, 4:8], in_=I3[:, :, 2:5], axis=AX.X, op=ALU.add), reads=[ibank], writes=[fk])
            ik = ('idxi', b)
            sc.op('dve', lambda e, F_=F_, b=b: e.tensor_copy(out=idxi[b][:], in_=F_[:, 0:4]), reads=[fk], writes=[ik])
            for cc in range(4):
                sc.dma('pool', lambda e, cc=cc, b=b: e.indirect_dma_start(
                    out=xe[:, cc, :], out_offset=None, in_=h2_d[:, :],
                    in_offset=bass.IndirectOffsetOnAxis(ap=idxi[b][:, cc:cc + 1], axis=0)), 'xe', reads=[ik], writes=['xe'])

        def prep_c(e_):
            xeT = xeT2[e_ % 2]
            for kc in range(8):
                tb = 6
                tbank = ('ps', tb)
                pb = ps[:, tb, :].bitcast(BF16)

                def trx(e, kc=kc, pb=pb):
                    for cc in range(4):
                        r = e.transpose(out=pb[:, cc * 128:(cc + 1) * 128], in_=xe[:, cc, kc * 128:(kc + 1) * 128], identity=identb[:])
                    return r
                sc.op('pe', trx, reads=['xe', 'identb'], writes=[tbank])
                sc.op('act', lambda e, kc=kc, pb=pb, xeT=xeT: e.copy(out=xeT[:, kc, :], in_=pb[:, 0:512]), reads=[tbank], writes=[('xeT', e_ % 2)])

        for k in range(NSLOT):
            load_piece(0, k)
        prep_a(0)
        prep_b(0)
        prep_c(0)
        for e_ in range(n_exp):
            b = e_ % 2
            xeT = xeT2[b]
            xk_ = ('xeT', b)
            if e_ + 1 < n_exp:
                prep_a(e_ + 1)
            for fh in range(2):
                gk, uk = ('ring', 2 * fh), ('ring', 2 * fh + 1)
                Wg, Wu = ring[2 * fh], ring[2 * fh + 1]
                for fo in range(8):
                    gb, ubk = fo % 2, 2 + (fo % 2)

                    def mmg(e, Wg=Wg, fo=fo, gb=gb):
                        for kc in range(8):
                            r = e.matmul(ps[:, gb, :], lhsT=Wg[:, kc, fo * 128:(fo + 1) * 128], rhs=xeT[:, kc, :], start=(kc == 0), stop=(kc == 7))
                        return r

                    def mmu(e, Wu=Wu, fo=fo, ubk=ubk):
                        for kc in range(8):
                            r = e.matmul(ps[:, ubk, :], lhsT=Wu[:, kc, fo * 128:(fo + 1) * 128], rhs=xeT[:, kc, :], start=(kc == 0), stop=(kc == 7))
                        return r
                    sc.op('pe', mmg, reads=[gk, xk_], writes=[('ps', gb)])
                    sc.op('pe', mmu, reads=[uk, xk_], writes=[('ps', ubk)])
                    sg = sgt[fo % 2]
                    sk = ('sgt', fo % 2)
                    sc.op('act', lambda e, sg=sg, gb=gb: e.activation(out=sg[:], in_=ps[:, gb, :], func=AF.Silu), reads=[('ps', gb)], writes=[sk])
                    fidx = fh * 8 + fo
                    sc.op('dve', lambda e, sg=sg, ubk=ubk, fidx=fidx: e.tensor_tensor(out=aT[:, fidx, :], in0=ps[:, ubk, :], in1=sg[:], op=ALU.mult),
                          reads=[('ps', ubk), sk], writes=[('aT', fidx)])
                if e_ + 1 < n_exp:
                    load_piece(e_ + 1, 2 * fh)
                    load_piece(e_ + 1, 2 * fh + 1)
                    if fh == 0:
                        prep_b(e_ + 1)
                    else:
                        prep_c(e_ + 1)
            F_ = idf[:, b, :]
            for cc in range(4):
                for dh in range(2):
                    db = 4 + ((cc * 2 + dh) % 2)

                    def mmd(e, cc=cc, dh=dh, db=db):
                        for fc in range(16):
                            r = e.matmul(ps[:, db, :], lhsT=aT[:, fc, cc * 128:(cc + 1) * 128], rhs=ring[4 + fc // 8][:, fc % 8, dh * 512:(dh + 1) * 512],
                                         start=(fc == 0), stop=(fc == 15))
                        return r
                    sc.op('pe', mmd, reads=[('aT', f) for f in range(16)] + [('ring', 4), ('ring', 5)], writes=[('ps', db)])
                    sc.op('dve', lambda e, cc=cc, dh=dh, db=db, F_=F_: e.scalar_tensor_tensor(
                        out=yv[:, cc, dh * 512:(dh + 1) * 512], in0=ps[:, db, :], scalar=F_[:, 4 + cc:5 + cc], in1=g2rep[:, dh * 512:(dh + 1) * 512],
                        op0=ALU.mult, op1=ALU.mult), reads=[('ps', db), ('idf', b), 'g2rep'], writes=[('yv', cc)])
            if e_ + 1 < n_exp:
                load_piece(e_ + 1, 4)
                load_piece(e_ + 1, 5)
            for cc in range(4):
                sc.dma('pool', lambda e, cc=cc, b=b: e.indirect_dma_start(
                    out=out_d[:, :], out_offset=bass.IndirectOffsetOnAxis(ap=idxi[b][:, cc:cc + 1], axis=0),
                    in_=yv[:, cc, :], in_offset=None, compute_op=ALU.add), 'scat', reads=[('yv', cc), ('idxi', b)], writes=['outd'])
        sc.barrier()
    sW.close()
    sc.finish()
    return nc, dbg


def prep_inputs(inputs, b):
    f = lambda a: np.ascontiguousarray(a, dtype=np.float32)
    m = {
        "x": f(inputs["x"][b]),
        "cT": f(inputs["c"][b].reshape(8, 128).T),
        "pos": np.ascontiguousarray(inputs["positions"][b].reshape(NT, 128).T.astype(np.int32)),
        "norm1_g": f(inputs["norm1_g"][0:1]),
        "norm2_g": f(inputs["norm2_g"][0:1]),
        "w_ada": f(inputs["w_ada"][0]),
        "b_ada": f(inputs["b_ada"][0:1]),
        "w_in": f(inputs["w_in"][0]),
        "convT": f(inputs["mlstm_conv_w"][0].T.reshape(8, 128, 5).transpose(1, 0, 2).reshape(128, 40)),
        "gate_b": f(inputs["mlstm_gate_b"][0:1]),
        "mnorm_g": f(inputs["mlstm_norm_g"][0:1]),
        "qk_g": f(inputs["diff_qk_g"][0].reshape(1, 128)),
        "lam": f(inputs["diff_lambda"][0].reshape(1, 256)),
        "subln_g": f(inputs["diff_subln_g"][0:1]),
        "w_out": f(inputs["w_out"][0]),
        "w_router": f(inputs["w_router"][0]),
        "consts": make_consts(),
    }
    return m


def kernel(**inputs):
    nc, _ = build()
    shared = None
    in_maps = []
    for b in range(8):
        m = prep_inputs(inputs, b)
        if shared is None:
            shared = {
                "w_gate_e": np.ascontiguousarray(inputs["w_gate_e"][0].reshape(NE * D, 2 * D), dtype=np.float32),
                "w_up_e": np.ascontiguousarray(inputs["w_up_e"][0].reshape(NE * D, 2 * D), dtype=np.float32),
                "w_down_e": np.ascontiguousarray(inputs["w_down_e"][0].reshape(NE * 2 * D, D), dtype=np.float32),
            }
        m.update(shared)
        in_maps.append(m)
    res = run_bass_kernel_spmd(nc, in_maps, core_ids=list(range(8)))
    return np.stack([np.asarray(r["out"]) for r in res.results], axis=0).astype(np.float32)
```

```python
import math
from contextlib import ExitStack

import numpy as np
import concourse.bass as bass
import concourse.mybir as mybir
from concourse.bass_utils import run_bass_kernel_spmd

F32 = mybir.dt.float32
BF16 = mybir.dt.bfloat16
I32 = mybir.dt.int32
AF = mybir.ActivationFunctionType
ALU = mybir.AluOpType
AX = mybir.AxisListType

S = 4096
D = 1024
NT = 32
NG = 8
EPS = 1e-6
D_IN = 3600
NE = 16
CAP = 512
LAM_INIT = 0.8 - 0.6 * math.exp(-0.3 * 0)
NBIS = 27

SAME_ENG_SYNC = True

C_IDENT = 0
C_TL = 128
C_TU = 256
C_TLS = 384
C_TUS = 512
C_INDA = 640
C_INDB = 768
C_IOTA = 896
C_TLO = 1408
C_FREQ = 1409
C_THI = 1417
NCONST = 1449


def make_consts():
    c = np.zeros((128, NCONST), np.float32)
    p = np.arange(128)
    s = p[:, None]
    j = p[None, :]
    same = (s // 64) == (j // 64)
    c[:, C_IDENT:C_IDENT + 128] = (s == j)
    c[:, C_TL:C_TL + 128] = same & (s <= j)
    c[:, C_TU:C_TU + 128] = same & (s >= j)
    c[:, C_TLS:C_TLS + 128] = same & (s < j)
    c[:, C_TUS:C_TUS + 128] = same & (s > j)
    c[:, C_INDA:C_INDA + 128] = (s < 64) & (j >= 0)
    c[:, C_INDB:C_INDB + 128] = (s >= 64) & (j >= 0)
    c[:, C_IOTA:C_IOTA + 512] = np.arange(512)[None, :]
    c[:, C_TLO] = p
    inv_freq = (500000.0 ** (-np.arange(0, 16, 2, dtype=np.float32) / 16)).astype(np.float32)
    c[:, C_FREQ:C_FREQ + 8] = (inv_freq.astype(np.float64) / (2 * np.pi)).astype(np.float32)[None, :]
    c[:, C_THI:C_THI + 32] = np.arange(32)[None, :]
    return c


class Sched:
    def __init__(self, nc, es):
        self.nc = nc
        self.es = es
        self.E = {'pe': nc.tensor, 'act': nc.scalar, 'dve': nc.vector, 'pool': nc.gpsimd, 'sp': nc.sync}
        self.sem = {k: es.enter_context(nc.semaphore('s_' + k)) for k in self.E}
        self.cnt = {k: 0 for k in self.E}
        self.seen = {k: {} for k in self.E}
        self.reg = {}
        self.dsem = {}
        self.dsem_by_sid = {}
        self.ninstr = {k: 0 for k in self.E}

    def _deps(self, reads, writes):
        deps = {}

        def add(tok):
            if tok is None:
                return
            if tok[0].startswith('d_'):
                tok = (tok[0], tok[1], self.dsem_by_sid[tok[0]][1])
            if tok[0] not in deps or deps[tok[0]][2] < tok[2]:
                deps[tok[0]] = tok
        for r in reads:
            st = self.reg.get(r)
            if st:
                add(st[0])
        for w in writes:
            st = self.reg.get(w)
            if st:
                add(st[0])
                for t in st[1].values():
                    add(t)
        return deps

    def _wait(self, eng, deps):
        for sid, (_, h, v) in deps.items():
            if sid == 'e_' + eng and not SAME_ENG_SYNC:
                continue
            if self.seen[eng].get(sid, 0) >= v:
                continue
            self.E[eng].wait_ge(h, v)
            self.seen[eng][sid] = v

    def _commit(self, tok, reads, writes):
        for r in reads:
            st = self.reg.setdefault(r, [None, {}])
            st[1][tok[0]] = tok
        for w in writes:
            self.reg[w] = [tok, {}]

    def op(self, eng, fn, reads=(), writes=()):
        self._wait(eng, self._deps(reads, writes))
        ins = fn(self.E[eng])
        self.cnt[eng] += 1
        ins.then_inc(self.sem[eng], 1)
        self._commit(('e_' + eng, self.sem[eng], self.cnt[eng]), reads, writes)

    def dma(self, q, fn, semkey, reads=(), writes=()):
        self._wait(q, self._deps(reads, writes))
        ins = fn(self.E[q])
        d = self.dsem.get(semkey)
        if d is None:
            d = [self.es.enter_context(self.nc.semaphore('d%d' % len(self.dsem))), 0]
            self.dsem[semkey] = d
            self.dsem_by_sid['d_' + str(semkey)] = d
        d[1] += 16
        ins.then_inc(d[0], 16)
        self._commit(('d_' + str(semkey), d[0], d[1]), reads, writes)

    def barrier(self):
        toks = {}
        for k in self.E:
            if self.cnt[k]:
                toks['e_' + k] = ('e_' + k, self.sem[k], self.cnt[k])
        for key, d in self.dsem.items():
            if d[1]:
                toks['d_' + str(key)] = ('d_' + str(key), d[0], d[1])
        for k in self.E:
            self._wait(k, toks)

    def finish(self):
        toks = {}
        for k in self.E:
            if self.cnt[k]:
                toks['e_' + k] = ('e_' + k, self.sem[k], self.cnt[k])
        for key, d in self.dsem.items():
            if d[1]:
                toks['d_' + str(key)] = ('d_' + str(key), d[0], d[1])
        self._wait('sp', toks)


def build(stage=99, debug=False):
    nc = bass.Bass("TRN2", target_bir_lowering=False)
    es = ExitStack()
    sc = Sched(nc, es)

    def din(name, shape, dt=F32):
        return nc.dram_tensor(name, list(shape), dt, kind="ExternalInput").ap()

    x_d = din("x", [S, D])
    cT_d = din("cT", [128, 8])
    pos_d = din("pos", [128, NT], I32)
    n1g_d = din("norm1_g", [1, D])
    n2g_d = din("norm2_g", [1, D])
    wada_d = din("w_ada", [D, 6 * D])
    bada_d = din("b_ada", [1, 6 * D])
    win_d = din("w_in", [D, D_IN])
    convT_d = din("convT", [128, 40])
    gateb_d = din("gate_b", [1, 16])
    mng_d = din("mnorm_g", [1, 512])
    qkg_d = din("qk_g", [1, 128])
    lam_d = din("lam", [1, 256])
    subg_d = din("subln_g", [1, 128])
    wout_d = din("w_out", [D, D])
    wr_d = din("w_router", [D, NE])
    consts_d = din("consts", [128, NCONST])
    if stage >= 5:
        wg_d = din("w_gate_e", [NE * D, 2 * D])
        wu_d = din("w_up_e", [NE * D, 2 * D])
        wd_d = din("w_down_e", [NE * 2 * D, D])
    out_d = nc.dram_tensor("out", [S, D], F32, kind="ExternalOutput").ap()
    skind = "ExternalOutput" if debug else "Internal"
    yT_d = nc.dram_tensor("yT_d", [D, S], BF16, kind=skind).ap()
    h2_d = nc.dram_tensor("h2_d", [S, D], BF16, kind=skind).ap()
    dbg = {}

    def dbg_out(name, shape, dt=F32):
        if debug:
            dbg[name] = nc.dram_tensor(name, list(shape), dt, kind="ExternalOutput").ap()
            return dbg[name]
        return None

    uniq = [0]

    def sb(name, shape, dt=F32, stack=es):
        uniq[0] += 1
        return stack.enter_context(nc.sbuf_tensor("sb%d_%s" % (uniq[0], name), list(shape), dt))

    ps = es.enter_context(nc.psum_tensor("ps", [128, 8, 512], F32))
    consts = sb("consts", [128, NCONST])
    identb = sb("identb", [128, 128], BF16)
    s1 = ExitStack()
    prm = sb("prm", [128, 16 + 512 + 128 + 256 + 128], F32, s1)
    P_GB, P_MNG, P_QKG, P_LAM, P_SUB = 0, 16, 528, 656, 912
    convT = sb("convT", [128, 40], F32, s1)
    maskF = sb("maskF", [128, 128], BF16, s1)
    maskB = sb("maskB", [128, 128], BF16, s1)
    small = sb("small", [128, 64], F32, s1)
    SM_NB, SM_NLAM = 0, 1
    ident = consts[:, C_IDENT:C_IDENT + 128]

    h1T = sb("h1T", [128, 8, S], BF16, s1)
    gm = sb("gm", [128, NT, 16], F32, s1)
    smod = ExitStack()
    mod = sb("mod", [128, 6 * D], F32, smod)
    modrow_d = nc.dram_tensor("modrow_d", [1, 6 * D], F32, kind="Internal").ap()
    CK = 'consts'
    sc.dma('sp', lambda e: e.dma_start(out=consts[:], in_=consts_d[:, :]), CK, writes=['consts'])
    sc.dma('sp', lambda e: e.dma_start(out=convT[:], in_=convT_d[:, :]), CK, writes=['convT'])
    for (off, src, n) in [(P_GB, gateb_d, 16), (P_MNG, mng_d, 512),
                          (P_QKG, qkg_d, 128), (P_LAM, lam_d, 256), (P_SUB, subg_d, 128)]:
        sc.dma('sp', lambda e, off=off, src=src, n=n: e.dma_start(
            out=prm[:, off:off + n], in_=src[0:1, :].partition_broadcast(128)), CK, writes=['prm'])
    sc.dma('sp', lambda e: e.dma_start(out=mod[:], in_=bada_d[0:1, :].partition_broadcast(128)), CK, writes=['mod'])

    with ExitStack() as p0:
        cT = sb("cT", [128, 8], F32, p0)
        prmA = sb("prmA", [128, 2 * D], F32, p0)
        sc.dma('sp', lambda e: e.dma_start(out=prmA[:, 0:D], in_=n1g_d[0:1, :].partition_broadcast(128)), CK, writes=['prmA'])
        sc.dma('sp', lambda e: e.dma_start(out=prmA[:, D:2 * D], in_=n2g_d[0:1, :].partition_broadcast(128)), CK, writes=['prmA'])
        scT = sb("scT", [128, 8], F32, p0)
        lc = sb("lc", [128, 8, 128], BF16, p0)
        wa = [sb("wa%d" % i, [128, 8, 512], BF16, p0) for i in range(2)]
        sc.dma('sp', lambda e: e.dma_start(out=cT[:], in_=cT_d[:, :]), 'cT', writes=['cT'])
        sc.op('act', lambda e: e.activation(out=scT[:], in_=cT[:], func=AF.Silu), reads=['cT'], writes=['scT'])
        sc.op('dve', lambda e: e.tensor_copy(out=lc[:], in_=scT[:].unsqueeze(2).broadcast_to([128, 8, 128])),
              reads=['scT'], writes=['lc'])
        sc.op('dve', lambda e: e.tensor_copy(out=identb[:], in_=ident), reads=['consts'], writes=['identb'])
        sc.op('dve', lambda e: e.tensor_copy(out=maskF[:], in_=consts[:, C_TL:C_TL + 128]), reads=['consts'], writes=['maskF'])
        sc.op('dve', lambda e: e.tensor_copy(out=maskB[:], in_=consts[:, C_TU:C_TU + 128]), reads=['consts'], writes=['maskB'])
        wada_v = wada_d.rearrange("(kc p) n -> p kc n", p=128)
        for n in range(12):
            w = wa[n % 2]
            wk = ('wa', n % 2)
            sc.dma('pool', lambda e, w=w, n=n: e.dma_start(out=w[:], in_=wada_v[:, :, n * 512:(n + 1) * 512]),
                   wk, writes=[wk])
            bank = ('ps', n % 2)

            def mm(e, w=w, n=n):
                for kc in range(8):
                    r = e.matmul(ps[:, n % 2, :], lhsT=lc[:, kc, :], rhs=w[:, kc, :], start=(kc == 0), stop=(kc == 7))
                return r
            sc.op('pe', mm, reads=['lc', wk], writes=[bank])
            sc.op('dve', lambda e, n=n: e.tensor_tensor(out=mod[:, n * 512:(n + 1) * 512], in0=ps[:, n % 2, :],
                                                        in1=mod[:, n * 512:(n + 1) * 512], op=ALU.add),
                  reads=[bank, 'mod'], writes=['mod'])
        sc.op('dve', lambda e: e.scalar_tensor_tensor(out=mod[:, D:2 * D], in0=mod[:, D:2 * D], scalar=1.0,
                                                      in1=prmA[:, 0:D], op0=ALU.add, op1=ALU.mult),
              reads=['mod', 'prmA'], writes=['mod'])
        sc.op('dve', lambda e: e.scalar_tensor_tensor(out=mod[:, 4 * D:5 * D], in0=mod[:, 4 * D:5 * D], scalar=1.0,
                                                      in1=prmA[:, D:2 * D], op0=ALU.add, op1=ALU.mult),
              reads=['mod', 'prmA'], writes=['mod'])
        if debug:
            o = dbg_out("dbg_mod", [128, 6 * D])
            sc.dma('sp', lambda e: e.dma_start(out=o[:, :], in_=mod[:]), 'dbgmod', reads=['mod'])
        sc.barrier()
    SHIFT1, GS1, GATE1, SHIFT2, GS2, GATE2 = [mod[:, i * D:(i + 1) * D] for i in range(6)]

    if stage <= 0:
        sc.finish()
        return nc, dbg

    with ExitStack() as p1:
        xt = [sb("xt%d" % i, [128, D], F32, p1) for i in range(2)]
        junk = sb("junk1", [128, D], BF16, p1)
        tmp = [sb("tmp1_%d" % i, [128, D], F32, p1) for i in range(2)]
        h1b = [sb("h1b%d" % i, [128, D], BF16, p1) for i in range(2)]
        st1 = sb("st1", [128, NT, 3], F32, p1)
        wgt = sb("wgt", [128, 8, 16], BF16, p1)
        win_v = win_d.rearrange("(kc p) n -> p kc n", p=128)
        sc.dma('pool', lambda e: e.dma_start(out=wgt[:], in_=win_v[:, :, 2048:2064]), 'wgt', writes=['wgt'])
        for i in range(NT):
            b = i % 2
            xk, tk, hk = ('xt', b), ('tmp1', b), ('h1b', b)
            sc.dma('sp', lambda e, i=i, b=b: e.dma_start(out=xt[b][:], in_=x_d[i * 128:(i + 1) * 128, :]), xk, writes=[xk])
            sc.op('act', lambda e, i=i, b=b: e.activation(out=junk[:], in_=xt[b][:], func=AF.Square,
                                                          accum_out=st1[:, i, 0:1]), reads=[xk], writes=['junk1', ('st1', i)])
            sc.op('act', lambda e, i=i: e.activation(out=st1[:, i, 1:2], in_=st1[:, i, 0:1], func=AF.Sqrt,
                                                     scale=1.0 / D, bias=EPS), reads=[('st1', i)], writes=[('st1', i)])
            sc.op('dve', lambda e, i=i: e.reciprocal(out=st1[:, i, 2:3], in_=st1[:, i, 1:2]),
                  reads=[('st1', i)], writes=[('st1', i)])
            sc.op('dve', lambda e, i=i, b=b: e.scalar_tensor_tensor(out=tmp[b][:], in0=xt[b][:], scalar=st1[:, i, 2:3],
                                                                    in1=GS1, op0=ALU.mult, op1=ALU.mult),
                  reads=[xk, ('st1', i), 'mod'], writes=[tk])
            sc.op('dve', lambda e, b=b: e.tensor_tensor(out=h1b[b][:], in0=tmp[b][:], in1=SHIFT1, op=ALU.add),
                  reads=[tk, 'mod'], writes=[hk])
            bank = ('ps', 2 + b)
            pb = ps[:, 2 + b, :].bitcast(BF16)

            def tr(e, b=b, pb=pb):
                for kc in range(8):
                    r = e.transpose(out=pb[:, kc * 128:(kc + 1) * 128], in_=h1b[b][:, kc * 128:(kc + 1) * 128], identity=identb[:])
                return r
            sc.op('pe', tr, reads=[hk, 'identb'], writes=[bank])
            sc.op('act', lambda e, i=i, pb=pb: e.copy(out=h1T[:, :, i * 128:(i + 1) * 128],
                                                      in_=pb.rearrange("p (k t) -> p k t", k=8)),
                  reads=[bank], writes=[('h1T', i)])
            gbank = ('ps', 4 + b)

            def gmm(e, i=i, b=b):
                for kc in range(8):
                    r = e.matmul(ps[:, 4 + b, 0:16], lhsT=h1T[:, kc, i * 128:(i + 1) * 128], rhs=wgt[:, kc, :],
                                 start=(kc == 0), stop=(kc == 7))
                return r
            sc.op('pe', gmm, reads=[('h1T', i), 'wgt'], writes=[gbank])
            sc.op('dve', lambda e, i=i, b=b: e.tensor_tensor(out=gm[:, i, :], in0=ps[:, 4 + b, 0:16],
                                                             in1=prm[:, P_GB:P_GB + 16], op=ALU.add),
                  reads=[gbank, 'prm'], writes=['gm'])
        if debug:
            o = dbg_out("dbg_h1T", [128, 8, S], BF16)
            sc.dma('sp', lambda e: e.dma_start(out=o[:, :, :], in_=h1T[:]), 'dbgh1T', reads=[('h1T', i) for i in range(NT)])
            o3 = dbg_out("dbg_st1", [128, NT, 3])
            sc.dma('sp', lambda e: e.dma_start(out=o3[:, :, :], in_=st1[:]), 'dbgst1', reads=[('st1', i) for i in range(NT)])
            o4 = dbg_out("dbg_tmp", [128, D])
            sc.dma('sp', lambda e: e.dma_start(out=o4[:, :], in_=tmp[1][:]), 'dbgtmp', reads=[('tmp1', 1)])
            o5 = dbg_out("dbg_h1b", [128, D], BF16)
            sc.dma('sp', lambda e: e.dma_start(out=o5[:, :], in_=h1b[1][:]), 'dbgh1b', reads=[('h1b', 1)])
            o2 = dbg_out("dbg_gm", [128, NT, 16])
            sc.dma('sp', lambda e: e.dma_start(out=o2[:, :, :], in_=gm[:]), 'dbggm', reads=['gm'])
        sc.barrier()

    sc.dma('sp', lambda e: e.dma_start(out=modrow_d[0:1, :], in_=mod[0:1, :]), 'modrow', reads=['mod'], writes=['modrow_d'])
    sc.barrier()
    smod.close()

    if stage <= 1:
        sc.finish()
        return nc, dbg

    LNK = -0.5 * math.log(128.0)
    cum = sb("cum", [128, 4, NT, 4], F32, s1)
    egs = sb("egs", [128, 4, NT, 4], F32, s1)
    tw = sb("tw", [128, 6, NT, 4], F32, s1)
    with ExitStack() as pg:
        lsg = sb("lsg", [128, 2, NT, 4], F32, pg)
        tg = sb("tg", [128, 2, NT, 4], F32, pg)
        for d, c0 in ((0, 4), (1, 12)):
            sc.op('act', lambda e, d=d, c0=c0: e.activation(out=tg[:, d, :, :], in_=gm[:, :, c0:c0 + 4], func=AF.Exp, scale=-1.0),
                  reads=['gm'], writes=['tg'])
        sc.op('act', lambda e: e.activation(out=tg[:], in_=tg[:], func=AF.Ln, bias=1.0), reads=['tg'], writes=['tg'])
        sc.op('dve', lambda e: e.tensor_scalar(out=lsg[:], in0=tg[:], scalar1=-1.0, scalar2=None, op0=ALU.mult),
              reads=['tg'], writes=['lsg'])
        lf = lsg[:, 0, :, :].rearrange("p t h -> p (t h)")
        lb = lsg[:, 1, :, :].rearrange("p t h -> p (t h)")
        specs = [(C_TL, lf), (C_TUS, lf), (C_TU, lb), (C_TLS, lb)]

        def cmm(e):
            for k, (co, rhs) in enumerate(specs):
                r = e.matmul(ps[:, 6, k * 128:(k + 1) * 128], lhsT=consts[:, co:co + 128], rhs=rhs, start=True, stop=True)
            return r
        sc.op('pe', cmm, reads=['lsg', 'consts'], writes=[('ps', 6)])
        sc.op('act', lambda e: e.copy(out=cum[:].rearrange("p a t h -> p (a t h)"), in_=ps[:, 6, :]),
              reads=[('ps', 6)], writes=['cum'])
        specs2 = [(C_INDA, lf), (C_INDB, lf), (C_INDA, lb), (C_INDB, lb)]

        def gmm2(e):
            for k, (co, rhs) in enumerate(specs2):
                r = e.matmul(ps[:, 7, k * 128:(k + 1) * 128], lhsT=consts[:, co:co + 128], rhs=rhs, start=True, stop=True)
            return r
        sc.op('pe', gmm2, reads=['lsg', 'consts'], writes=[('ps', 7)])
        sc.op('act', lambda e: e.activation(out=egs[:].rearrange("p a t h -> p (a t h)"), in_=ps[:, 7, :], func=AF.Exp),
              reads=[('ps', 7)], writes=['egs'])
        for d, ic in ((0, 0), (1, 8)):
            ig = gm[:, :, ic:ic + 4]
            a_ = cum[:, 2 * d, :, :]
            r_ = cum[:, 2 * d + 1, :, :]
            sc.op('dve', lambda e, d=d, ig=ig, a_=a_: e.tensor_tensor(out=tw[:, 3 * d, :, :], in0=ig, in1=a_, op=ALU.subtract),
                  reads=['gm', 'cum'], writes=['tw'])
            sc.op('dve', lambda e, d=d, ig=ig, r_=r_: e.tensor_tensor(out=tw[:, 3 * d + 1, :, :], in0=ig, in1=r_, op=ALU.add),
                  reads=['gm', 'cum'], writes=['tw'])
            sc.op('act', lambda e, d=d: e.activation(out=tw[:, 3 * d:3 * d + 2, :, :], in_=tw[:, 3 * d:3 * d + 2, :, :],
                                                     func=AF.Exp, bias=LNK), reads=['tw'], writes=['tw'])
            sc.op('act', lambda e, d=d, a_=a_: e.activation(out=tw[:, 3 * d + 2, :, :], in_=a_, func=AF.Exp),
                  reads=['cum'], writes=['tw'])
        if debug:
            o = dbg_out("dbg_tw", [128, 6, NT, 4])
            sc.dma('sp', lambda e: e.dma_start(out=o[:, :, :, :], in_=tw[:]), 'dbgtw', reads=['tw'])
            o = dbg_out("dbg_egs", [128, 4, NT, 4])
            sc.dma('sp', lambda e: e.dma_start(out=o[:, :, :, :], in_=egs[:]), 'dbgegs', reads=['egs'])
        sc.barrier()

    if stage <= 1.5:
        sc.finish()
        return nc, dbg

    win_v = win_d.rearrange("(kc p) n -> p kc n", p=128)
    yT_v = yT_d.rearrange("(c p) t -> p c t", p=128)

    def emit_y_tile(ph, h_chunk, i, ym, ymk, yst, ystk):
        g4 = i % 4
        bank = ('ps', 5)
        pb = ps[:, 5, :].bitcast(BF16)
        sc.op('pe', lambda e: e.transpose(out=pb[:, g4 * 128:(g4 + 1) * 128], in_=ym, identity=identb[:]),
              reads=[ymk, 'identb'], writes=[bank])
        sc.op('act', lambda e: e.copy(out=yst[:, g4 * 128:(g4 + 1) * 128], in_=pb[:, g4 * 128:(g4 + 1) * 128]),
              reads=[bank], writes=[ystk])
        if g4 == 3:
            t0 = (i - 3) * 128
            sc.dma('sp', lambda e: e.dma_start(out=yT_v[:, h_chunk, t0:t0 + 512], in_=yst[:]), ystk, reads=[ystk], writes=['yT_d'])

    n_mheads = 4 if stage >= 2.5 else 1
    for h in range(n_mheads):
        with ExitStack() as ph:
            wm = sb("wm", [128, 8, 512], BF16, ph)
            qkT = sb("qkT", [128, 2, S], BF16, ph)
            pre = sb("pre", [128, 2, 516], F32, ph)
            acc = sb("acc", [128, 2, 512], F32, ph)
            vext = sb("vext", [128, NT, 129], BF16, ph)
            sgo = sb("sgo", [128, NT, 128], BF16, ph)
            ktok = sb("ktok", [128, NT, 128], BF16, ph)
            Cst = sb("Cst", [128, 2, 65, 129], BF16, ph)
            stt = sb("stt", [128, 2, 2, 129], F32, ph)
            vtmp = [sb("vtmp%d" % k, [128, 129], BF16, ph) for k in range(2)]
            smg = [sb("smg%d" % k, [128, 2, 4, 128], BF16, ph) for k in range(2)]
            vtg = [sb("vtg%d" % k, [128, 2, 4, 129], BF16, ph) for k in range(2)]
            pst = sb("pst", [128, 2, 48], F32, ph)
            hfg = [sb("hfg%d" % k, [128, 4, 128], F32, ph) for k in range(2)]
            hsg = [sb("hsg%d" % k, [128, 4, 128], F32, ph) for k in range(2)]
            ymg = [sb("ymg%d" % k, [128, 4, 128], BF16, ph) for k in range(2)]
            yst = [sb("yst%d" % k, [128, 512], BF16, ph) for k in range(2)]
            for k, c0 in enumerate((h * 128, 512 + h * 128, 1024 + h * 128, 1536 + h * 128)):
                sc.dma('pool', lambda e, k=k, c0=c0: e.dma_start(out=wm[:, :, k * 128:(k + 1) * 128], in_=win_v[:, :, c0:c0 + 128]),
                       'wm', writes=['wm'])
            sc.op('pool', lambda e: e.memset(vext[:, :, 128:129], 1.0), writes=['vext1'])
            sc.op('pool', lambda e: e.memset(Cst[:, :, 0, :], 0.0), writes=[('Cst', 0, 0), ('Cst', 1, 0)])
            sc.op('pool', lambda e: e.memset(stt[:, :, 0, :], 0.0), writes=[('stt', 0, 0), ('stt', 1, 0)])
            sc.op('pool', lambda e: e.memset(pre[:, :, 0:4], 0.0), writes=['pre'])
            for g in range(NG + 1):
                if g < NG:
                    for qk in range(2):
                        bank = ('ps', qk)

                        def mm(e, qk=qk, g=g):
                            for kc in range(8):
                                r = e.matmul(ps[:, qk, :], lhsT=wm[:, kc, qk * 128:(qk + 1) * 128],
                                             rhs=h1T[:, kc, g * 512:(g + 1) * 512], start=(kc == 0), stop=(kc == 7))
                            return r
                        sc.op('pe', mm, reads=['wm'] + [('h1T', 4 * g + k) for k in range(4)], writes=[bank])
                        sc.op('act', lambda e, qk=qk: e.copy(out=pre[:, qk, 4:516], in_=ps[:, qk, :]), reads=[bank], writes=['pre'])
                else:
                    sc.op('pool', lambda e: e.memset(pre[:, :, 4:8], 0.0), writes=['pre'])
                W = 512 if g < NG else 2
                for qk in range(2):
                    cb = (4 * qk + h) * 5
                    sc.op('dve', lambda e, qk=qk, cb=cb, W=W: e.tensor_scalar(out=acc[:, qk, 0:W], in0=pre[:, qk, 0:W], scalar1=convT[:, cb:cb + 1],
                                                                            scalar2=None, op0=ALU.mult), reads=['pre', 'convT'], writes=[('acc', qk)])
                    for j in range(1, 5):
                        sc.op('dve', lambda e, qk=qk, cb=cb, j=j, W=W: e.scalar_tensor_tensor(
                            out=acc[:, qk, 0:W], in0=pre[:, qk, j:j + W], scalar=convT[:, cb + j:cb + j + 1], in1=acc[:, qk, 0:W],
                            op0=ALU.mult, op1=ALU.add), reads=['pre', 'convT', ('acc', qk)], writes=[('acc', qk)])
                    n0 = 2 if g == 0 else 0
                    t0 = 512 * g - 2 + n0
                    sc.op('act', lambda e, qk=qk, n0=n0, t0=t0, W=W: e.activation(out=qkT[:, qk, t0:t0 + W - n0], in_=acc[:, qk, n0:W], func=AF.Silu),
                          reads=[('acc', qk)], writes=[('qkT', qk)])
                if g < NG:
                    sc.op('pool', lambda e: e.tensor_copy(out=pre[:, :, 0:4], in_=pre[:, :, 512:516]), reads=['pre'], writes=['pre'])
                    for k in range(4):
                        i = 4 * g + k
                        bank = ('ps', 2 + (i % 2))

                        def mmvo(e, i=i):
                            for kc in range(8):
                                r = e.matmul(ps[:, 2 + (i % 2), 0:256], lhsT=h1T[:, kc, i * 128:(i + 1) * 128], rhs=wm[:, kc, 256:512],
                                             start=(kc == 0), stop=(kc == 7))
                            return r
                        sc.op('pe', mmvo, reads=['wm', ('h1T', i)], writes=[bank])
                        sc.op('act', lambda e, i=i: e.copy(out=vext[:, i, 0:128], in_=ps[:, 2 + (i % 2), 0:128]), reads=[bank], writes=[('vext', i)])
                        sc.op('act', lambda e, i=i: e.activation(out=sgo[:, i, :], in_=ps[:, 2 + (i % 2), 128:256], func=AF.Sigmoid),
                              reads=[bank], writes=[('sgo', i)])
            for g in range(NG):
                bank = ('ps', 4)
                pb = ps[:, 4, :].bitcast(BF16)

                def trk(e, g=g, pb=pb):
                    for k in range(4):
                        i = 4 * g + k
                        r = e.transpose(out=pb[:, k * 128:(k + 1) * 128], in_=qkT[:, 1, i * 128:(i + 1) * 128], identity=identb[:])
                    return r
                sc.op('pe', trk, reads=[('qkT', 1), 'identb'], writes=[bank])
                sc.op('act', lambda e, g=g, pb=pb: e.copy(out=ktok[:, 4 * g:4 * g + 4, :], in_=pb[:, 0:512].rearrange("p (k d) -> p k d", k=4)),
                      reads=[bank], writes=['ktok'])
            nv = 0
            for step in range(64):
                for d in range(2):
                    c = step if d == 0 else 63 - step
                    i, half = c // 2, c % 2
                    first_of_tile = (half == 0) if d == 0 else (half == 1)
                    vk = ('vhat', d)
                    vb = vtmp[d]
                    if first_of_tile:
                        sc.op('act', lambda e, d=d, i=i, vb=vb: e.activation(out=vb[:], in_=vext[:, i, :], func=AF.Copy,
                                                                             scale=tw[:, 3 * d + 1, i, h:h + 1]),
                              reads=[('vext', i), 'vext1', 'tw'], writes=[vk])
                    slot = nv % 4
                    nv += 1
                    pbank = 4 + slot
                    pc = 0
                    pk = ('ps', pbank)
                    sc.op('pe', lambda e, i=i, half=half, vb=vb, pbank=pbank, pc=pc: e.matmul(
                        ps[:, pbank, pc:pc + 129], lhsT=ktok[half * 64:(half + 1) * 64, i, :], rhs=vb[half * 64:(half + 1) * 64, :],
                        start=True, stop=True), reads=['ktok', vk], writes=[pk])
                    cur, nxt = step % 2, (step + 1) % 2
                    sc.op('dve', lambda e, d=d, i=i, half=half, cur=cur, nxt=nxt, pbank=pbank, pc=pc: e.scalar_tensor_tensor(
                        out=stt[:, d, nxt, :], in0=stt[:, d, cur, :], scalar=egs[:, 2 * d + half, i, h:h + 1], in1=ps[:, pbank, pc:pc + 129],
                        op0=ALU.mult, op1=ALU.add), reads=[('stt', d, cur), 'egs', pk], writes=[('stt', d, nxt)])
                    sc.op('pool', lambda e, d=d, nxt=nxt, step=step: e.tensor_copy(out=Cst[:, d, step + 1, :], in_=stt[:, d, nxt, :]),
                          reads=[('stt', d, nxt)], writes=[('Cst', d, step + 1)])
            for g in range(NG):
                gb = g % 2
                sbank = ('ps', gb)

                def smm(e, g=g, gb=gb):
                    for t in range(4):
                        blk = slice((4 * g + t) * 128, (4 * g + t + 1) * 128)
                        r = e.matmul(ps[:, gb, t * 128:(t + 1) * 128], lhsT=qkT[:, 1, blk], rhs=qkT[:, 0, blk], start=True, stop=True)
                    return r
                sc.op('pe', smm, reads=[('qkT', 0), ('qkT', 1)], writes=[sbank])
                S4 = ps[:, gb, :].rearrange("p (t j) -> p t j", t=4)
                smF, smB = smg[gb][:, 0, :, :], smg[gb][:, 1, :, :]
                kF, kB = ('smg', gb, 0), ('smg', gb, 1)
                sc.op('dve', lambda e, S4=S4, smF=smF: e.tensor_tensor(out=smF, in0=S4, in1=maskF[:].unsqueeze(1).broadcast_to([128, 4, 128]), op=ALU.mult),
                      reads=[sbank, 'maskF'], writes=[kF])
                sc.op('dve', lambda e, S4=S4, smB=smB: e.tensor_tensor(out=smB, in0=S4, in1=maskB[:].unsqueeze(1).broadcast_to([128, 4, 128]), op=ALU.mult),
                      reads=[sbank, 'maskB'], writes=[kB])
                vtk = ('vtg', gb)
                for d in range(2):
                    sc.op('pool', lambda e, d=d, g=g, gb=gb: e.tensor_tensor(
                        out=vtg[gb][:, d, :, :], in0=vext[:, 4 * g:4 * g + 4, :],
                        in1=tw[:, 3 * d, 4 * g:4 * g + 4, h:h + 1].broadcast_to([128, 4, 129]), op=ALU.mult),
                        reads=[('vext', 4 * g + t) for t in range(4)] + ['vext1', 'tw'], writes=[(vtk, d)])
                ubanks = [('ps', 2 + t) for t in range(4)]

                def umm(e, g=g, gb=gb):
                    for t in range(4):
                        i = 4 * g + t
                        for d in range(2):
                            o0_ = d * 129
                            e.matmul(ps[:, 2 + t, o0_:o0_ + 129], lhsT=smg[gb][:, d, t, :], rhs=vtg[gb][:, d, t, :], start=True, stop=False)
                            for half in range(2):
                                c = 2 * i + half
                                sidx = c if d == 0 else 63 - c
                                t0 = i * 128 + half * 64
                                r = e.matmul(ps[half * 64:(half + 1) * 64, 2 + t, o0_:o0_ + 129], lhsT=qkT[:, 0, t0:t0 + 64],
                                             rhs=Cst[:, d, sidx, :], start=False, stop=True)
                    return r
                creads = []
                for t in range(4):
                    i = 4 * g + t
                    creads += [('Cst', 0, 2 * i), ('Cst', 0, 2 * i + 1), ('Cst', 1, 63 - 2 * i), ('Cst', 1, 62 - 2 * i)]
                sc.op('pe', umm, reads=[kF, kB, (vtk, 0), (vtk, 1), ('qkT', 0)] + creads, writes=ubanks)
                P = pst[:, gb, :]
                pk = ('pst', gb)
                U4 = ps[:, 2:6, 0:258].rearrange("p t (d v) -> p t d v", d=2)
                ea2 = tw[:, 2:6:3, 4 * g:4 * g + 4, h].rearrange("p d t -> p t d")
                Pv = lambda lo: P[:, lo:lo + 8].rearrange("p (t d) -> p t d", d=2)
                sc.op('dve', lambda e: e.tensor_tensor(out=Pv(0), in0=U4[:, :, :, 128], in1=ea2, op=ALU.mult), reads=ubanks + ['tw'], writes=[pk])
                sc.op('dve', lambda e: e.tensor_scalar(out=P[:, 16:24], in0=P[:, 0:8], scalar1=-1.0, scalar2=None, op0=ALU.mult), reads=[pk], writes=[pk])
                sc.op('dve', lambda e: e.scalar_tensor_tensor(out=P[:, 8:16], in0=P[:, 16:24], scalar=1.0, in1=P[:, 0:8], op0=ALU.max, op1=ALU.max),
                      reads=[pk], writes=[pk])
                sc.op('dve', lambda e: e.reciprocal(out=P[:, 16:24], in_=P[:, 8:16]), reads=[pk], writes=[pk])
                sc.op('dve', lambda e: e.tensor_tensor(out=Pv(24), in0=Pv(16), in1=ea2, op=ALU.mult), reads=[pk, 'tw'], writes=[pk])
                RR = Pv(24)
                hfb, hsb = hfg[gb], hsg[gb]
                sc.op('dve', lambda e: e.tensor_tensor(out=hfb[:], in0=U4[:, :, 0, 0:128], in1=RR[:, :, 0:1].broadcast_to([128, 4, 128]), op=ALU.mult),
                      reads=ubanks + [pk], writes=[('hfg', gb)])
                sc.op('dve', lambda e: e.tensor_tensor(out=hsb[:], in0=U4[:, :, 1, 0:128], in1=RR[:, :, 1:2].broadcast_to([128, 4, 128]), op=ALU.mult),
                      reads=ubanks + [pk], writes=[('hsg', gb)])
                sc.op('pool', lambda e: e.tensor_tensor(out=hsb[:], in0=hsb[:], in1=hfb[:], op=ALU.add), reads=[('hsg', gb), ('hfg', gb)], writes=[('hsg', gb)])
                sc.op('pool', lambda e: e.tensor_tensor(out=hfb[:], in0=hsb[:], in1=hsb[:], op=ALU.mult), reads=[('hsg', gb)], writes=[('hfg', gb)])
                sc.op('dve', lambda e: e.tensor_reduce(out=P[:, 32:36], in_=hfb[:], axis=AX.X, op=ALU.add), reads=[('hfg', gb)], writes=[pk])
                sc.op('act', lambda e: e.activation(out=P[:, 36:40], in_=P[:, 32:36], func=AF.Sqrt, scale=1.0 / 128, bias=EPS), reads=[pk], writes=[pk])
                sc.op('dve', lambda e: e.reciprocal(out=P[:, 40:44], in_=P[:, 36:40]), reads=[pk], writes=[pk])
                sc.op('dve', lambda e: e.tensor_tensor(out=hsb[:], in0=hsb[:], in1=P[:, 40:44].unsqueeze(2).broadcast_to([128, 4, 128]), op=ALU.mult),
                      reads=[('hsg', gb), pk], writes=[('hsg', gb)])
                sc.op('pool', lambda e: e.tensor_tensor(out=hsb[:], in0=hsb[:],
                                                        in1=prm[:, P_MNG + h * 128:P_MNG + (h + 1) * 128].unsqueeze(1).broadcast_to([128, 4, 128]), op=ALU.mult),
                      reads=[('hsg', gb), 'prm'], writes=[('hsg', gb)])
                ym = ymg[gb]
                ymk = ('ymg', gb)
                sc.op('dve', lambda e, g=g: e.tensor_tensor(out=ym[:], in0=hsb[:], in1=sgo[:, 4 * g:4 * g + 4, :], op=ALU.mult),
                      reads=[('hsg', gb)] + [('sgo', 4 * g + t) for t in range(4)], writes=[ymk])
                tbank = ('ps', 6)
                pb = ps[:, 6, :].bitcast(BF16)

                def ytr(e, ym=ym, pb=pb):
                    for t in range(4):
                        r = e.transpose(out=pb[:, t * 128:(t + 1) * 128], in_=ym[:, t, :], identity=identb[:])
                    return r
                sc.op('pe', ytr, reads=[ymk, 'identb'], writes=[tbank])
                ys = yst[gb]
                ysk = ('yst', gb)
                sc.op('act', lambda e, ys=ys, pb=pb: e.copy(out=ys[:], in_=pb[:, 0:512]), reads=[tbank], writes=[ysk])
                sc.dma('sp', lambda e, ys=ys, g=g: e.dma_start(out=yT_v[:, h, g * 512:(g + 1) * 512], in_=ys[:]), ysk, reads=[ysk], writes=['yT_d'])
            if debug and h == 0:
                o = dbg_out("dbg_qkT", [128, 2, S], BF16)
                sc.dma('sp', lambda e: e.dma_start(out=o[:, :, :], in_=qkT[:]), 'dbgqkT', reads=[('qkT', 0), ('qkT', 1)])
                o = dbg_out("dbg_Cst", [128, 2, 65, 129], BF16)
                sc.dma('sp', lambda e: e.dma_start(out=o[:, :, :, :], in_=Cst[:]), 'dbgCst',
                       reads=[('Cst', d, k) for d in range(2) for k in range(65)])
            sc.barrier()

    if stage <= 2.5:
        sc.finish()
        return nc, dbg

    cosT = sb("cosT", [128, NT, 8], F32, s1)
    sinT = sb("sinT", [128, NT, 8], F32, s1)
    qkg4 = sb("qkg4", [128, 4, 64], F32, s1)
    subg8 = sb("subg8", [128, 128], F32, s1)
    TWO_PI = 6.28318
    with ExitStack() as pa:
        posi = sb("posi", [128, NT], I32, pa)
        posf = sb("posf", [128, NT], F32, pa)
        ut = sb("ut", [128, NT, 8], F32, pa)
        ui = sb("ui", [128, NT, 8], I32, pa)
        uf = sb("uf", [128, NT, 8], F32, pa)
        jl = sb("jl", [128, 64], F32, pa)
        sc.dma('sp', lambda e: e.dma_start(out=posi[:], in_=pos_d[:, :]), 'posi', writes=['posi'])
        sc.op('dve', lambda e: e.tensor_copy(out=posf[:], in_=posi[:]), reads=['posi'], writes=['posf'])
        sc.op('dve', lambda e: e.tensor_tensor(out=ut[:], in0=posf[:].unsqueeze(2).broadcast_to([128, NT, 8]),
                                               in1=consts[:, C_FREQ:C_FREQ + 8].unsqueeze(1).broadcast_to([128, NT, 8]), op=ALU.mult),
              reads=['posf', 'consts'], writes=['ut'])
        for tab, shift in ((sinT, 0.0), (cosT, 0.25)):
            if shift:
                sc.op('dve', lambda e, shift=shift: e.tensor_scalar(out=ut[:], in0=ut[:], scalar1=shift, scalar2=None, op0=ALU.add),
                      reads=['ut'], writes=['ut'])
            sc.op('dve', lambda e: e.tensor_copy(out=ui[:], in_=ut[:]), reads=['ut'], writes=['ui'])
            sc.op('dve', lambda e: e.tensor_copy(out=uf[:], in_=ui[:]), reads=['ui'], writes=['uf'])
            sc.op('dve', lambda e: e.tensor_tensor(out=uf[:], in0=ut[:], in1=uf[:], op=ALU.subtract), reads=['ut', 'uf'], writes=['uf'])
            sc.op('act', lambda e, tab=tab: e.activation(out=tab[:], in_=uf[:], func=AF.Sin, scale=TWO_PI), reads=['uf'], writes=['rot'])
        sc.op('dve', lambda e: e.tensor_reduce(out=small[:, 2:4], in_=prm[:, P_QKG:P_QKG + 128].rearrange("p (a d) -> p a d", a=2),
                                               axis=AX.X, op=ALU.max, apply_absolute_value=True), reads=['prm'], writes=['small'])
        sc.op('dve', lambda e: e.scalar_tensor_tensor(out=small[:, SM_NB:SM_NB + 1], in0=small[:, 2:3], scalar=-8.0, in1=small[:, 3:4],
                                                      op0=ALU.mult, op1=ALU.mult), reads=['small'], writes=['small'])
        for k in range(2):
            sc.op('dve', lambda e, k=k: e.tensor_tensor(out=jl[:], in0=prm[:, P_LAM + 128 * k:P_LAM + 128 * k + 64],
                                                        in1=prm[:, P_LAM + 128 * k + 64:P_LAM + 128 * k + 128], op=ALU.mult),
                  reads=['prm'], writes=['jl'])
            sc.op('dve', lambda e, k=k: e.tensor_reduce(out=small[:, 4 + k:5 + k], in_=jl[:], axis=AX.X, op=ALU.add), reads=['jl'], writes=['small'])
        sc.op('act', lambda e: e.activation(out=small[:, 6:8], in_=small[:, 4:6], func=AF.Exp), reads=['small'], writes=['small'])
        sc.op('dve', lambda e: e.scalar_tensor_tensor(out=small[:, SM_NLAM:SM_NLAM + 1], in0=small[:, 7:8], scalar=-LAM_INIT, in1=small[:, 6:7],
                                                      op0=ALU.add, op1=ALU.subtract), reads=['small'], writes=['small'])
        for a in range(4):
            o_ = P_QKG + (64 if a >= 2 else 0)
            sc.op('pool', lambda e, a=a, o_=o_: e.tensor_copy(out=qkg4[:, a, :], in_=prm[:, o_:o_ + 64]), reads=['prm'], writes=['qkg4'])
        sc.op('dve', lambda e: e.tensor_scalar(out=subg8[:], in0=prm[:, P_SUB:P_SUB + 128], scalar1=1.0 - LAM_INIT, scalar2=None, op0=ALU.mult),
              reads=['prm'], writes=['subg8'])
        if debug:
            o = dbg_out("dbg_small", [128, 64])
            sc.op('pool', lambda e: e.memset(small[:, 8:64], 0.0), writes=['small'])
            sc.dma('sp', lambda e: e.dma_start(out=o[:, :], in_=small[:]), 'dbgsmall', reads=['small'])
            o = dbg_out("dbg_cos", [128, NT, 8])
            sc.dma('sp', lambda e: e.dma_start(out=o[:, :, :], in_=cosT[:]), 'dbgcos', reads=['rot'])
        sc.barrier()

    n_aheads = 4 if stage >= 3.5 else 1
    for h in range(n_aheads):
        with ExitStack() as ph:
            wa_ = sb("waa", [128, 8, 384], BF16, ph)
            qTa = sb("qTa", [128, 2, S], BF16, ph)
            kTa = sb("kTa", [128, S], BF16, ph)
            vxa = sb("vxa", [128, NT, 129], BF16, ph)
            xq = [sb("xq%d" % k, [128, 4, 4, 64], F32, ph) for k in range(2)]
            sq = sb("sq", [128, 4, 4, 64], F32, ph)
            rp = [sb("rp%d" % k, [128, 4, 4, 4, 8], F32, ph) for k in range(2)]
            st4 = sb("st4", [128, 2, 48], F32, ph)
            xr = [sb("xr%d" % k, [128, 4, 4, 64], BF16, ph) for k in range(2)]
            pt = [sb("pt%d" % k, [128, 2, 512], BF16, ph) for k in range(3)]
            o0 = sb("o0", [128, 4, 128], F32, ph)
            ob = [sb("ob%d" % k, [128, 128], F32, ph) for k in range(2)]
            st5 = sb("st5", [128, 2, 8], F32, ph)
            jk2 = sb("jk2", [128, 128], F32, ph)
            ymt = [sb("ymta%d" % k, [128, 128], BF16, ph) for k in range(2)]
            yst = [sb("ysta%d" % k, [128, 512], BF16, ph) for k in range(2)]
            for k, c0 in enumerate((2064 + h * 128, 2576 + h * 128, 3088 + h * 128)):
                sc.dma('pool', lambda e, k=k, c0=c0: e.dma_start(out=wa_[:, :, k * 128:(k + 1) * 128], in_=win_v[:, :, c0:c0 + 128]),
                       'waa', writes=['waa'])
            sc.op('pool', lambda e: e.memset(vxa[:, :, 128:129], 1.0), writes=['vxa1'])
            sc.op('pool', lambda e: e.memset(qTa[64:128, 0, :], 0.0), writes=['qTa'])
            sc.op('pool', lambda e: e.memset(qTa[0:64, 1, :], 0.0), writes=['qTa'])
            for g in range(NG):
                b = g % 2
                banks = [('ps', k) for k in range(4)]

                def mmp(e, g=g):
                    for t in range(4):
                        i = 4 * g + t
                        for kc in range(8):
                            r = e.matmul(ps[:, t, 0:384], lhsT=h1T[:, kc, i * 128:(i + 1) * 128], rhs=wa_[:, kc, :], start=(kc == 0), stop=(kc == 7))
                    return r
                sc.op('pe', mmp, reads=['waa'] + [('h1T', 4 * g + t) for t in range(4)], writes=banks)
                xk, rk, sk, xrk = ('xq', b), ('rp', b), ('st4', b), ('xr', b)
                X = xq[b]
                X3 = X[:].rearrange("p t a d -> p t (a d)")
                X16 = X[:].rearrange("p t a d -> p (t a) d")
                sc.op('act', lambda e, X3=X3: e.copy(out=X3, in_=ps[:, 0:4, 0:256]), reads=banks, writes=[xk])
                sc.op('act', lambda e, g=g: e.copy(out=vxa[:, 4 * g:4 * g + 4, 0:128], in_=ps[:, 0:4, 256:384]), reads=banks,
                      writes=[('vxa', 4 * g + t) for t in range(4)])
                sc.op('dve', lambda e, X=X: e.tensor_tensor(out=sq[:], in0=X[:], in1=X[:], op=ALU.mult), reads=[xk], writes=['sq'])
                T4 = st4[:, b, :]
                sc.op('dve', lambda e, T4=T4: e.tensor_reduce(out=T4[:, 0:16], in_=sq[:].rearrange("p t a d -> p (t a) d"), axis=AX.X, op=ALU.add),
                      reads=['sq'], writes=[sk])
                sc.op('act', lambda e, T4=T4: e.activation(out=T4[:, 16:32], in_=T4[:, 0:16], func=AF.Sqrt, scale=1.0 / 64, bias=EPS), reads=[sk], writes=[sk])
                sc.op('dve', lambda e, T4=T4: e.reciprocal(out=T4[:, 32:48], in_=T4[:, 16:32]), reads=[sk], writes=[sk])
                sc.op('dve', lambda e, X16=X16, T4=T4: e.tensor_tensor(out=X16, in0=X16, in1=T4[:, 32:48].unsqueeze(2).broadcast_to([128, 16, 64]), op=ALU.mult),
                      reads=[xk, sk], writes=[xk])
                sc.op('dve', lambda e, X=X: e.tensor_tensor(out=X[:], in0=X[:], in1=qkg4[:].unsqueeze(1).broadcast_to([128, 4, 4, 64]), op=ALU.mult),
                      reads=[xk, 'qkg4'], writes=[xk])
                cs = cosT[:, 4 * g:4 * g + 4, :].unsqueeze(2).broadcast_to([128, 4, 4, 8])
                sn = sinT[:, 4 * g:4 * g + 4, :].unsqueeze(2).broadcast_to([128, 4, 4, 8])
                R = rp[b]
                for k, (tt, tr_) in enumerate(((X[:, :, :, 0:8], cs), (X[:, :, :, 8:16], sn), (X[:, :, :, 8:16], cs), (X[:, :, :, 0:8], sn))):
                    sc.op('pool', lambda e, k=k, tt=tt, tr_=tr_, R=R: e.tensor_tensor(out=R[:, k, :, :, :], in0=tt, in1=tr_, op=ALU.mult),
                          reads=[xk, 'rot'], writes=[rk])
                XR = xr[b]
                sc.op('act', lambda e, XR=XR, X=X: e.copy(out=XR[:], in_=X[:]), reads=[xk], writes=[xrk])
                sc.op('dve', lambda e, XR=XR, R=R: e.tensor_tensor(out=XR[:, :, :, 0:8], in0=R[:, 0, :, :, :], in1=R[:, 1, :, :, :], op=ALU.subtract),
                      reads=[rk, xrk], writes=[xrk])
                sc.op('dve', lambda e, XR=XR, R=R: e.tensor_tensor(out=XR[:, :, :, 8:16], in0=R[:, 2, :, :, :], in1=R[:, 3, :, :, :], op=ALU.add),
                      reads=[rk, xrk], writes=[xrk])
                tbanks = [('ps', 4), ('ps', 5)]
                pbq = ps[:, 4, :].bitcast(BF16)
                pbk = ps[:, 5, :].bitcast(BF16)
                XRf = XR[:].rearrange("p t a d -> p t (a d)")

                def trq(e, XRf=XRf, pbq=pbq, pbk=pbk):
                    for t in range(4):
                        e.transpose(out=pbq[:, t * 128:(t + 1) * 128], in_=XRf[:, t, 0:128], identity=identb[:])
                        r = e.transpose(out=pbk[:, t * 128:(t + 1) * 128], in_=XRf[:, t, 128:256], identity=identb[:])
                    return r
                sc.op('pe', trq, reads=[xrk, 'identb'], writes=tbanks)
                sc.op('act', lambda e, pbq=pbq, g=g: e.copy(out=qTa[0:64, 0, g * 512:(g + 1) * 512], in_=pbq[0:64, 0:512]), reads=tbanks, writes=['qTa'])
                sc.op('act', lambda e, pbq=pbq, g=g: e.copy(out=qTa[64:128, 1, g * 512:(g + 1) * 512], in_=pbq[64:128, 0:512]), reads=tbanks, writes=['qTa'])
                sc.op('act', lambda e, pbk=pbk, g=g: e.copy(out=kTa[:, g * 512:(g + 1) * 512], in_=pbk[:, 0:512]), reads=tbanks, writes=['kTa'])
            if debug and h == 0:
                o = dbg_out("dbg_qTa", [128, S], BF16)
                sc.dma('sp', lambda e: e.dma_start(out=o[0:64, :], in_=qTa[0:64, 0, :]), 'dbgqTa', reads=['qTa'])
                sc.dma('sp', lambda e: e.dma_start(out=o[64:128, :], in_=qTa[64:128, 1, :]), 'dbgqTa', reads=['qTa'])
                o = dbg_out("dbg_kTa", [128, S], BF16)
                sc.dma('sp', lambda e: e.dma_start(out=o[:, :], in_=kTa[:]), 'dbgkTa', reads=['kTa'])
            nqb = 8 if stage >= 3.2 else 1
            for qb in range(nqb):
                for p in range(2):
                    pr = slice(64 * p, 64 * p + 64)
                    def st_mm(j):
                        pb0 = 4 + 2 * (j % 2)

                        def f(e, j=j, pb0=pb0):
                            for u in range(2):
                                kt = 2 * j + u
                                r = e.matmul(ps[:, pb0 + u, :], lhsT=kTa[:, kt * 128:(kt + 1) * 128], rhs=qTa[:, p, qb * 512:(qb + 1) * 512],
                                             start=True, stop=True)
                            return r
                        sc.op('pe', f, reads=['qTa', 'kTa'], writes=[('ps', pb0), ('ps', pb0 + 1)])
                    st_mm(0)
                    for j in range(NT // 2):
                        pb0 = 4 + 2 * (j % 2)
                        sbanks = [('ps', pb0), ('ps', pb0 + 1)]
                        if j + 1 < NT // 2:
                            st_mm(j + 1)
                        ptk = ('pt', j % 3)
                        ptb = pt[j % 3]
                        sc.op('act', lambda e, pb0=pb0, ptb=ptb: e.activation(out=ptb[:], in_=ps[:, pb0:pb0 + 2, :], func=AF.Exp, scale=0.125,
                                                                              bias=small[:, SM_NB:SM_NB + 1]),
                              reads=sbanks + ['small'], writes=[ptk])

                        def pv(e, ptb=ptb, j=j):
                            for u in range(2):
                                kt = 2 * j + u
                                for qs in range(4):
                                    r = e.matmul(ps[:, qs, 0:129], lhsT=ptb[:, u, qs * 128:(qs + 1) * 128], rhs=vxa[:, kt, :],
                                                 start=(kt == 0), stop=(kt == NT - 1))
                            return r
                        sc.op('pe', pv, reads=[ptk, ('vxa', 2 * j), ('vxa', 2 * j + 1), 'vxa1'], writes=[('ps', 0), ('ps', 1), ('ps', 2), ('ps', 3)])
                    for qs in range(4):
                        i = qb * 4 + qs
                        T5 = st5[:, qs % 2, :]
                        tk5 = ('st5', qs % 2)
                        abank = ('ps', qs)
                        sc.op('dve', lambda e, T5=T5, qs=qs: e.reciprocal(out=T5[:, 0:1], in_=ps[:, qs, 128:129]), reads=[abank], writes=[tk5])
                        if p == 0:
                            sc.op('dve', lambda e, T5=T5, qs=qs: e.tensor_scalar(out=o0[:, qs, :], in0=ps[:, qs, 0:128], scalar1=T5[:, 0:1], scalar2=None,
                                                                                op0=ALU.mult), reads=[abank, tk5], writes=[('o0', qs)])
                            continue
                        O = ob[qs % 2]
                        okk = ('ob', qs % 2)
                        sc.op('dve', lambda e, T5=T5: e.tensor_tensor(out=T5[:, 1:2], in0=T5[:, 0:1], in1=small[:, SM_NLAM:SM_NLAM + 1], op=ALU.mult),
                              reads=[tk5, 'small'], writes=[tk5])
                        sc.op('dve', lambda e, T5=T5, qs=qs, O=O: e.scalar_tensor_tensor(out=O[:], in0=ps[:, qs, 0:128], scalar=T5[:, 1:2], in1=o0[:, qs, :],
                                                                                       op0=ALU.mult, op1=ALU.add),
                              reads=[abank, tk5, ('o0', qs)], writes=[okk])
                        sc.op('act', lambda e, T5=T5, O=O: e.activation(out=jk2[:], in_=O[:], func=AF.Square, accum_out=T5[:, 2:3]),
                              reads=[okk, tk5], writes=['jk2', tk5])
                        sc.op('act', lambda e, T5=T5: e.activation(out=T5[:, 3:4], in_=T5[:, 2:3], func=AF.Sqrt, scale=1.0 / 128, bias=EPS), reads=[tk5], writes=[tk5])
                        sc.op('dve', lambda e, T5=T5: e.reciprocal(out=T5[:, 4:5], in_=T5[:, 3:4]), reads=[tk5], writes=[tk5])
                        ym = ymt[qs % 2]
                        ymk = ('ymta', qs % 2)
                        sc.op('dve', lambda e, T5=T5, O=O, ym=ym: e.scalar_tensor_tensor(out=ym[:], in0=O[:], scalar=T5[:, 4:5], in1=subg8[:],
                                                                                       op0=ALU.mult, op1=ALU.mult), reads=[okk, tk5, 'subg8'], writes=[ymk])
                        emit_y_tile(ph, 4 + h, i, ym[:], ymk, yst[qb % 2], ('ysta', qb % 2))
            sc.barrier()

    if stage <= 3.5:
        sc.finish()
        return nc, dbg

    s1.close()

    sW = ExitStack()
    aff = sb("aff", [128, NT, NE], F32, sW)
    g2rep = sb("g2rep", [128, D], F32, sW)
    with ExitStack() as pw:
        woutb = sb("woutb", [128, 8, D], BF16, pw)
        wrt = sb("wrt", [128, 8, NE], F32, pw)
        ytl = [sb("ytl%d" % k, [128, 8, 512], BF16, pw) for k in range(2)]
        xt2 = [sb("xt2_%d" % k, [128, D], F32, pw) for k in range(2)]
        tmpw = [sb("tmpw%d" % k, [128, D], F32, pw) for k in range(2)]
        x1t = [sb("x1t%d" % k, [128, D], F32, pw) for k in range(2)]
        h2f = [sb("h2f%d" % k, [128, D], F32, pw) for k in range(2)]
        h2b = [sb("h2b%d" % k, [128, D], BF16, pw) for k in range(2)]
        h2T = [sb("h2T%d" % k, [128, 8, 128], F32, pw) for k in range(2)]
        junkw = sb("junkw", [128, D], BF16, pw)
        stw = sb("stw", [128, NT, 8], F32, pw)
        esm = sb("esm", [128, 2, NE], F32, pw)
        wout_v = wout_d.rearrange("(kc p) n -> p kc n", p=128)
        sc.dma('pool', lambda e: e.dma_start(out=woutb[:, 0:4, :], in_=wout_v[:, 0:4, :]), 'woutb', writes=['woutb'])
        sc.dma('pool', lambda e: e.dma_start(out=woutb[:, 4:8, :], in_=wout_v[:, 4:8, :]), 'woutb', writes=['woutb'])
        sc.dma('sp', lambda e: e.dma_start(out=wrt[:], in_=wr_d.rearrange("(kc p) n -> p kc n", p=128)), 'wrt', writes=['wrt'])
        modW = sb("modW", [128, 3 * D], F32, pw)
        sc.dma('sp', lambda e: e.dma_start(out=modW[:], in_=modrow_d[0:1, 2 * D:5 * D].partition_broadcast(128)), 'modW', reads=['modrow_d'], writes=['mod'])
        sc.dma('sp', lambda e: e.dma_start(out=g2rep[:], in_=modrow_d[0:1, 5 * D:6 * D].partition_broadcast(128)), 'g2rep', reads=['modrow_d'], writes=['g2rep'])
        GATE1, SHIFT2, GS2 = modW[:, 0:D], modW[:, D:2 * D], modW[:, 2 * D:3 * D]
        for i in range(NT):
            b = i % 2
            g, k4 = i // 4, i % 4
            yk = ('ytl', g % 2)
            if k4 == 0:
                sc.dma('sp', lambda e, g=g: e.dma_start(out=ytl[g % 2][:], in_=yT_v[:, :, g * 512:(g + 1) * 512]), yk, writes=[yk])
            xk = ('xt2', b)
            sc.dma('sp', lambda e, i=i, b=b: e.dma_start(out=xt2[b][:], in_=x_d[i * 128:(i + 1) * 128, :]), xk, writes=[xk])
            banks = [('ps', 2 * b), ('ps', 2 * b + 1)]

            def mmw(e, i=i, b=b, g=g, k4=k4):
                for dh in range(2):
                    for c in range(8):
                        r = e.matmul(ps[:, 2 * b + dh, :], lhsT=ytl[g % 2][:, c, k4 * 128:(k4 + 1) * 128], rhs=woutb[:, c, dh * 512:(dh + 1) * 512],
                                     start=(c == 0), stop=(c == 7))
                return r
            sc.op('pe', mmw, reads=[yk, 'woutb'], writes=banks)
            mixv = ps[:, 2 * b:2 * b + 2, :].rearrange("p a n -> p (a n)")
            sc.op('dve', lambda e, b=b, mixv=mixv: e.tensor_tensor(out=tmpw[b][:], in0=mixv, in1=GATE1, op=ALU.mult), reads=banks + ['mod'], writes=[('tmpw', b)])
            sc.op('dve', lambda e, b=b: e.tensor_tensor(out=x1t[b][:], in0=tmpw[b][:], in1=xt2[b][:], op=ALU.add),
                  reads=[('tmpw', b), xk], writes=[('x1t', b)])
            sc.dma('sp', lambda e, i=i, b=b: e.dma_start(out=out_d[i * 128:(i + 1) * 128, :], in_=x1t[b][:]), ('x1s', b), reads=[('x1t', b)], writes=[('outd', i)])
            T = stw[:, i, :]
            tk = ('stw', i)
            sc.op('act', lambda e, b=b, T=T: e.activation(out=junkw[:], in_=x1t[b][:], func=AF.Square, accum_out=T[:, 0:1]), reads=[('x1t', b)], writes=['junkw', tk])
            sc.op('act', lambda e, T=T: e.activation(out=T[:, 1:2], in_=T[:, 0:1], func=AF.Sqrt, scale=1.0 / D, bias=EPS), reads=[tk], writes=[tk])
            sc.op('dve', lambda e, T=T: e.reciprocal(out=T[:, 2:3], in_=T[:, 1:2]), reads=[tk], writes=[tk])
            sc.op('dve', lambda e, b=b, T=T: e.scalar_tensor_tensor(out=tmpw[b][:], in0=x1t[b][:], scalar=T[:, 2:3], in1=GS2, op0=ALU.mult, op1=ALU.mult),
                  reads=[('x1t', b), tk, 'mod'], writes=[('tmpw', b)])
            sc.op('dve', lambda e, b=b: e.tensor_tensor(out=h2f[b][:], in0=tmpw[b][:], in1=SHIFT2, op=ALU.add), reads=[('tmpw', b), 'mod'], writes=[('h2f', b)])
            sc.op('act', lambda e, b=b: e.copy(out=h2b[b][:], in_=h2f[b][:]), reads=[('h2f', b)], writes=[('h2b', b)])
            sc.dma('sp', lambda e, i=i, b=b: e.dma_start(out=h2_d[i * 128:(i + 1) * 128, :], in_=h2b[b][:]), ('h2s', b), reads=[('h2b', b)], writes=[('h2d', i)])
            tb = [('ps', 4), ('ps', 5)]

            def trh(e, b=b):
                for kc in range(8):
                    r = e.transpose(out=ps[:, 4 + kc // 4, (kc % 4) * 128:(kc % 4 + 1) * 128], in_=h2f[b][:, kc * 128:(kc + 1) * 128], identity=ident)
                return r
            sc.op('pe', trh, reads=[('h2f', b), 'consts'], writes=tb)
            sc.op('act', lambda e, b=b: e.copy(out=h2T[b][:].rearrange("p k t -> p (k t)"), in_=ps[:, 4:6, :].rearrange("p a n -> p (a n)")),
                  reads=tb, writes=[('h2T', b)])
            lb = ('ps', 6 + b)

            def mml(e, b=b):
                for kc in range(8):
                    r = e.matmul(ps[:, 6 + b, 0:NE], lhsT=h2T[b][:, kc, :], rhs=wrt[:, kc, :], start=(kc == 0), stop=(kc == 7))
                return r
            sc.op('pe', mml, reads=[('h2T', b), 'wrt'], writes=[lb])
            sc.op('dve', lambda e, b=b, T=T: e.tensor_reduce(out=T[:, 3:4], in_=ps[:, 6 + b, 0:NE], axis=AX.X, op=ALU.max), reads=[lb], writes=[tk])
            sc.op('dve', lambda e, T=T: e.tensor_scalar(out=T[:, 4:5], in0=T[:, 3:4], scalar1=-1.0, scalar2=None, op0=ALU.mult), reads=[tk], writes=[tk])
            sc.op('act', lambda e, b=b, T=T: e.activation(out=esm[:, b, :], in_=ps[:, 6 + b, 0:NE], func=AF.Exp, bias=T[:, 4:5], accum_out=T[:, 5:6]),
                  reads=[lb, tk], writes=[('esm', b), tk])
            sc.op('dve', lambda e, T=T: e.reciprocal(out=T[:, 6:7], in_=T[:, 5:6]), reads=[tk], writes=[tk])
            sc.op('dve', lambda e, b=b, i=i, T=T: e.tensor_scalar(out=aff[:, i, :], in0=esm[:, b, :], scalar1=T[:, 6:7], scalar2=None, op0=ALU.mult),
                  reads=[('esm', b), tk], writes=['aff'])
        if debug:
            o = dbg_out("dbg_aff", [128, NT, NE])
            sc.dma('sp', lambda e: e.dma_start(out=o[:, :, :], in_=aff[:]), 'dbgaff', reads=['aff'])
        sc.barrier()

    if stage <= 4:
        sc.finish()
        return nc, dbg

    rank_tok = sb("rank_tok", [128, NT, NE], F32, sW)
    R5 = sb("R5", [128, NT, NE, 5], BF16, sW)
    with ExitStack() as pr:
        affT = sb("affT", [NE, S], F32, pr)
        junkR = sb("junkR", [NE, S], F32, pr)
        mkT = sb("mkT", [NE, S], F32, pr)
        csT = sb("csT", [NE, S], F32, pr)
        bs = sb("bs", [NE, 4], F32, pr)
        r1 = sb("r1", [128, NT, NE], F32, pr)
        for rnd in range(2):
            banks = [('ps', k) for k in range(4)]

            def tra(e, rnd=rnd):
                for k in range(16):
                    i = rnd * 16 + k
                    r = e.transpose(out=ps[0:NE, k // 4, (k % 4) * 128:(k % 4 + 1) * 128], in_=aff[:, i, :], identity=ident)
                return r
            sc.op('pe', tra, reads=['aff', 'consts'], writes=banks)
            sc.op('act', lambda e, rnd=rnd: e.copy(out=affT[:, rnd * 2048:(rnd + 1) * 2048], in_=ps[0:NE, 0:4, :].rearrange("p a n -> p (a n)")),
                  reads=banks, writes=['affT'])
        sc.op('dve', lambda e: e.memset(bs[:, 0:1], 0.0), writes=['bs'])
        for n in range(NBIS):
            w = 2.0 ** (-(n + 1))
            sc.op('dve', lambda e, w=w: e.tensor_scalar(out=bs[:, 1:2], in0=bs[:, 0:1], scalar1=w, scalar2=None, op0=ALU.add), reads=['bs'], writes=['bs'])
            sc.op('dve', lambda e: e.tensor_scalar(out=junkR[:], in0=affT[:], scalar1=bs[:, 1:2], scalar2=None, op0=ALU.is_gt, op1=ALU.add,
                                                   accum_out=bs[:, 2:3]), reads=['affT', 'bs'], writes=['junkR', 'bs'])
            sc.op('dve', lambda e: e.tensor_scalar(out=bs[:, 3:4], in0=bs[:, 2:3], scalar1=CAP - 0.5, scalar2=None, op0=ALU.is_gt), reads=['bs'], writes=['bs'])
            sc.op('dve', lambda e, w=w: e.scalar_tensor_tensor(out=bs[:, 0:1], in0=bs[:, 3:4], scalar=w, in1=bs[:, 0:1], op0=ALU.mult, op1=ALU.add),
                  reads=['bs'], writes=['bs'])
        sc.op('dve', lambda e: e.tensor_scalar(out=mkT[:], in0=affT[:], scalar1=bs[:, 0:1], scalar2=None, op0=ALU.is_gt), reads=['affT', 'bs'], writes=['mkT'])
        sc.op('pool', lambda e: e.memset(junkR[:], 1.0), reads=[], writes=['junkR'])
        sc.op('dve', lambda e: e.tensor_tensor_scan(out=csT[:], data0=junkR[:], data1=mkT[:], initial=0.0, op0=ALU.mult, op1=ALU.add),
              reads=['junkR', 'mkT'], writes=['csT'])
        sc.op('dve', lambda e: e.tensor_tensor(out=csT[:], in0=csT[:], in1=mkT[:], op=ALU.mult), reads=['csT', 'mkT'], writes=['csT'])
        sc.op('dve', lambda e: e.tensor_scalar(out=csT[:], in0=csT[:], scalar1=-1.0, scalar2=None, op0=ALU.add), reads=['csT'], writes=['csT'])
        rb = ('ps', 4)

        def trr(e):
            for i in range(NT):
                r = e.transpose(out=ps[:, 4, i * NE:(i + 1) * NE], in_=csT[:, i * 128:(i + 1) * 128], identity=consts[0:NE, C_IDENT:C_IDENT + NE])
            return r
        sc.op('pe', trr, reads=['csT', 'consts'], writes=[rb])
        sc.op('act', lambda e: e.copy(out=rank_tok[:].rearrange("p t e -> p (t e)"), in_=ps[:, 4, :]), reads=[rb], writes=['rank_tok'])
        sc.op('pool', lambda e: e.tensor_copy(out=R5[:, :, :, 0], in_=consts[:, C_TLO:C_TLO + 1].unsqueeze(2).broadcast_to([128, NT, NE])),
              reads=['consts'], writes=['R5'])
        sc.op('pool', lambda e: e.tensor_copy(out=R5[:, :, :, 1], in_=consts[:, C_THI:C_THI + NT].unsqueeze(2).broadcast_to([128, NT, NE])),
              reads=['consts'], writes=['R5'])
        sc.op('dve', lambda e: e.tensor_copy(out=R5[:, :, :, 2], in_=aff[:]), reads=['aff'], writes=['R5'])
        sc.op('dve', lambda e: e.tensor_tensor(out=r1[:], in0=aff[:], in1=R5[:, :, :, 2], op=ALU.subtract), reads=['aff', 'R5'], writes=['r1'])
        sc.op('dve', lambda e: e.tensor_copy(out=R5[:, :, :, 3], in_=r1[:]), reads=['r1'], writes=['R5'])
        sc.op('dve', lambda e: e.tensor_tensor(out=r1[:], in0=r1[:], in1=R5[:, :, :, 3], op=ALU.subtract), reads=['r1', 'R5'], writes=['r1'])
        sc.op('dve', lambda e: e.tensor_copy(out=R5[:, :, :, 4], in_=r1[:]), reads=['r1'], writes=['R5'])
        if debug:
            o = dbg_out("dbg_rank", [128, NT, NE])
            sc.dma('sp', lambda e: e.dma_start(out=o[:, :, :], in_=rank_tok[:]), 'dbgrank', reads=['rank_tok'])
            o = dbg_out("dbg_thr", [NE, 4])
            sc.dma('sp', lambda e: e.dma_start(out=o[:, :], in_=bs[:]), 'dbgthr', reads=['bs'])
        sc.barrier()

    if stage <= 4.5:
        sc.finish()
        return nc, dbg

    n_exp = NE if stage >= 6 else int(round((stage - 5) * 10)) + 1
    with ExitStack() as pe_:
        NSLOT = 6
        ring = [sb("ring%d" % k, [128, 8, D], BF16, pe_) for k in range(NSLOT)]
        Pm = sb("Pm", [128, NT, 512], BF16, pe_)
        xe = sb("xe", [128, 4, D], BF16, pe_)
        xeT2 = [sb("xeT%d" % k, [128, 8, 512], BF16, pe_) for k in range(2)]
        aT = sb("aT", [128, 16, 512], BF16, pe_)
        sgt = [sb("sgt%d" % k, [128, 512], BF16, pe_) for k in range(2)]
        yv = sb("yv", [128, 4, D], F32, pe_)
        idf = sb("idf", [128, 2, 8], F32, pe_)
        idp = sb("idp", [128, 2, 32], F32, pe_)
        idxi = [sb("idxi%d" % k, [128, 4], I32, pe_) for k in range(2)]
        wg_v = wg_d.rearrange("(e kc p) n -> p e kc n", p=128, kc=8)
        wu_v = wu_d.rearrange("(e kc p) n -> p e kc n", p=128, kc=8)
        wd_v = wd_d.rearrange("(e fh fc p) n -> p e fh fc n", p=128, fc=8, fh=2)

        def load_piece(e_, k):
            slot = ring[k]
            key = ('ring', k)
            if k < 4:
                src = (wg_v if k % 2 == 0 else wu_v)[:, e_, :, (k // 2) * D:(k // 2 + 1) * D]
            else:
                src = wd_v[:, e_, k - 4, :, :]
            sc.dma('pool', lambda e, slot=slot, src=src: e.dma_start(out=slot[:], in_=src), key, writes=[key])

        def prep_a(e_):
            for i in range(NT):
                sc.op('dve', lambda e, i=i: e.tensor_scalar(out=Pm[:, i, :], in0=consts[:, C_IOTA:C_IOTA + 512], scalar1=rank_tok[:, i, e_:e_ + 1],
                                                            scalar2=None, op0=ALU.is_equal), reads=['consts', 'rank_tok'], writes=['Pm'])

        def prep_b(e_):
            b = e_ % 2
            ibank = ('ps', 7)

            def imm(e):
                for cc in range(4):
                    for i in range(NT):
                        r = e.matmul(ps[:, 7, cc * 8:cc * 8 + 5], lhsT=Pm[:, i, cc * 128:(cc + 1) * 128], rhs=R5[:, i, e_, :],
                                     start=(i == 0), stop=(i == NT - 1))
                return r
            sc.op('pe', imm, reads=['Pm', 'R5'], writes=[ibank])
            sc.op('act', lambda e, b=b: e.copy(out=idp[:, b, :].rearrange("p (c k) -> p c k", k=8)[:, :, 0:5], in_=ps[:, 7, 0:32].rearrange("p (c k) -> p c k", k=8)[:, :, 0:5]), reads=[ibank], writes=[('idp', b)])
            ibank = ('idp', b)
            I3 = idp[:, b, :].rearrange("p (c k) -> p c k", k=8)
            F_ = idf[:, b, :]
            fk = ('idf', b)
            sc.op('dve', lambda e, F_=F_, I3=I3: e.scalar_tensor_tensor(out=F_[:, 0:4].unsqueeze(2), in0=I3[:, :, 1:2], scalar=128.0, in1=I3[:, :, 0:1],
                                                                      op0=ALU.mult, op1=ALU.add), reads=[ibank], writes=[fk])
            sc.op('dve', lambda e, F_=F_, I3=I3: e.tensor_reduce(out=F_[:, 4:8], in_=I3[:, :, 2:5], axis=AX.X, op=ALU.add), reads=[ibank], writes=[fk])
            ik = ('idxi', b)
            sc.op('dve', lambda e, F_=F_, b=b: e.tensor_copy(out=idxi[b][:], in_=F_[:, 0:4]), reads=[fk], writes=[ik])
            for cc in range(4):
                sc.dma('pool', lambda e, cc=cc, b=b: e.indirect_dma_start(
                    out=xe[:, cc, :], out_offset=None, in_=h2_d[:, :],
                    in_offset=bass.IndirectOffsetOnAxis(ap=idxi[b][:, cc:cc + 1], axis=0)), 'xe', reads=[ik], writes=['xe'])

        def prep_c(e_):
            xeT = xeT2[e_ % 2]
            for kc in range(8):
                tb = 6
                tbank = ('ps', tb)
                pb = ps[:, tb, :].bitcast(BF16)

                def trx(e, kc=kc, pb=pb):
                    for cc in range(4):
                        r = e.transpose(out=pb[:, cc * 128:(cc + 1) * 128], in_=xe[:, cc, kc * 128:(kc + 1) * 128], identity=identb[:])
                    return r
                sc.op('pe', trx, reads=['xe', 'identb'], writes=[tbank])
                sc.op('act', lambda e, kc=kc, pb=pb, xeT=xeT: e.copy(out=xeT[:, kc, :], in_=pb[:, 0:512]), reads=[tbank], writes=[('xeT', e_ % 2)])

        for k in range(NSLOT):
            load_piece(0, k)
        prep_a(0)
        prep_b(0)
        prep_c(0)
        for e_ in range(n_exp):
            b = e_ % 2
            xeT = xeT2[b]
            xk_ = ('xeT', b)
            if e_ + 1 < n_exp:
                prep_a(e_ + 1)
            for fh in range(2):
                gk, uk = ('ring', 2 * fh), ('ring', 2 * fh + 1)
                Wg, Wu = ring[2 * fh], ring[2 * fh + 1]
                for fo in range(8):
                    gb, ubk = fo % 2, 2 + (fo % 2)

                    def mmg(e, Wg=Wg, fo=fo, gb=gb):
                        for kc in range(8):
                            r = e.matmul(ps[:, gb, :], lhsT=Wg[:, kc, fo * 128:(fo + 1) * 128], rhs=xeT[:, kc, :], start=(kc == 0), stop=(kc == 7))
                        return r

                    def mmu(e, Wu=Wu, fo=fo, ubk=ubk):
                        for kc in range(8):
                            r = e.matmul(ps[:, ubk, :], lhsT=Wu[:, kc, fo * 128:(fo + 1) * 128], rhs=xeT[:, kc, :], start=(kc == 0), stop=(kc == 7))
                        return r
                    sc.op('pe', mmg, reads=[gk, xk_], writes=[('ps', gb)])
                    sc.op('pe', mmu, reads=[uk, xk_], writes=[('ps', ubk)])
                    sg = sgt[fo % 2]
                    sk = ('sgt', fo % 2)
                    sc.op('act', lambda e, sg=sg, gb=gb: e.activation(out=sg[:], in_=ps[:, gb, :], func=AF.Silu), reads=[('ps', gb)], writes=[sk])
                    fidx = fh * 8 + fo
                    sc.op('dve', lambda e, sg=sg, ubk=ubk, fidx=fidx: e.tensor_tensor(out=aT[:, fidx, :], in0=ps[:, ubk, :], in1=sg[:], op=ALU.mult),
                          reads=[('ps', ubk), sk], writes=[('aT', fidx)])
                if e_ + 1 < n_exp:
                    load_piece(e_ + 1, 2 * fh)
                    load_piece(e_ + 1, 2 * fh + 1)
                    if fh == 0:
                        prep_b(e_ + 1)
                    else:
                        prep_c(e_ + 1)
            F_ = idf[:, b, :]
            for cc in range(4):
                for dh in range(2):
                    db = 4 + ((cc * 2 + dh) % 2)

                    def mmd(e, cc=cc, dh=dh, db=db):
                        for fc in range(16):
                            r = e.matmul(ps[:, db, :], lhsT=aT[:, fc, cc * 128:(cc + 1) * 128], rhs=ring[4 + fc // 8][:, fc % 8, dh * 512:(dh + 1) * 512],
                                         start=(fc == 0), stop=(fc == 15))
                        return r
                    sc.op('pe', mmd, reads=[('aT', f) for f in range(16)] + [('ring', 4), ('ring', 5)], writes=[('ps', db)])
                    sc.op('dve', lambda e, cc=cc, dh=dh, db=db, F_=F_: e.scalar_tensor_tensor(
                        out=yv[:, cc, dh * 512:(dh + 1) * 512], in0=ps[:, db, :], scalar=F_[:, 4 + cc:5 + cc], in1=g2rep[:, dh * 512:(dh + 1) * 512],
                        op0=ALU.mult, op1=ALU.mult), reads=[('ps', db), ('idf', b), 'g2rep'], writes=[('yv', cc)])
            if e_ + 1 < n_exp:
                load_piece(e_ + 1, 4)
                load_piece(e_ + 1, 5)
            for cc in range(4):
                sc.dma('pool', lambda e, cc=cc, b=b: e.indirect_dma_start(
                    out=out_d[:, :], out_offset=bass.IndirectOffsetOnAxis(ap=idxi[b][:, cc:cc + 1], axis=0),
                    in_=yv[:, cc, :], in_offset=None, compute_op=ALU.add), 'scat', reads=[('yv', cc), ('idxi', b)], writes=['outd'])
        sc.barrier()
    sW.close()
    sc.finish()
    return nc, dbg


def prep_inputs(inputs, b):
    f = lambda a: np.ascontiguousarray(a, dtype=np.float32)
    m = {
        "x": f(inputs["x"][b]),
        "cT": f(inputs["c"][b].reshape(8, 128).T),
        "pos": np.ascontiguousarray(inputs["positions"][b].reshape(NT, 128).T.astype(np.int32)),
        "norm1_g": f(inputs["norm1_g"][0:1]),
        "norm2_g": f(inputs["norm2_g"][0:1]),
        "w_ada": f(inputs["w_ada"][0]),
        "b_ada": f(inputs["b_ada"][0:1]),
        "w_in": f(inputs["w_in"][0]),
        "convT": f(inputs["mlstm_conv_w"][0].T.reshape(8, 128, 5).transpose(1, 0, 2).reshape(128, 40)),
        "gate_b": f(inputs["mlstm_gate_b"][0:1]),
        "mnorm_g": f(inputs["mlstm_norm_g"][0:1]),
        "qk_g": f(inputs["diff_qk_g"][0].reshape(1, 128)),
        "lam": f(inputs["diff_lambda"][0].reshape(1, 256)),
        "subln_g": f(inputs["diff_subln_g"][0:1]),
        "w_out": f(inputs["w_out"][0]),
        "w_router": f(inputs["w_router"][0]),
        "consts": make_consts(),
    }
    return m


def kernel(**inputs):
    nc, _ = build()
    shared = None
    in_maps = []
    for b in range(8):
        m = prep_inputs(inputs, b)
        if shared is None:
            shared = {
                "w_gate_e": np.ascontiguousarray(inputs["w_gate_e"][0].reshape(NE * D, 2 * D), dtype=np.float32),
                "w_up_e": np.ascontiguousarray(inputs["w_up_e"][0].reshape(NE * D, 2 * D), dtype=np.float32),
                "w_down_e": np.ascontiguousarray(inputs["w_down_e"][0].reshape(NE * 2 * D, D), dtype=np.float32),
            }
        m.update(shared)
        in_maps.append(m)
    res = run_bass_kernel_spmd(nc, in_maps, core_ids=list(range(8)))
    return np.stack([np.asarray(r["out"]) for r in res.results], axis=0).astype(np.float32)
```

```python
import math
from contextlib import ExitStack

import numpy as np
import concourse.bass as bass
import concourse.mybir as mybir
from concourse.bass_utils import run_bass_kernel_spmd

F32 = mybir.dt.float32
BF16 = mybir.dt.bfloat16
I32 = mybir.dt.int32
AF = mybir.ActivationFunctionType
ALU = mybir.AluOpType
AX = mybir.AxisListType

S = 4096
D = 1024
NT = 32
NG = 8
EPS = 1e-6
D_IN = 3600
NE = 16
CAP = 512
LAM_INIT = 0.8 - 0.6 * math.exp(-0.3 * 0)
NBIS = 27

SAME_ENG_SYNC = True

C_IDENT = 0
C_TL = 128
C_TU = 256
C_TLS = 384
C_TUS = 512
C_INDA = 640
C_INDB = 768
C_IOTA = 896
C_TLO = 1408
C_FREQ = 1409
C_THI = 1417
NCONST = 1449


def make_consts():
    c = np.zeros((128, NCONST), np.float32)
    p = np.arange(128)
    s = p[:, None]
    j = p[None, :]
    same = (s // 64) == (j // 64)
    c[:, C_IDENT:C_IDENT + 128] = (s == j)
    c[:, C_TL:C_TL + 128] = same & (s <= j)
    c[:, C_TU:C_TU + 128] = same & (s >= j)
    c[:, C_TLS:C_TLS + 128] = same & (s < j)
    c[:, C_TUS:C_TUS + 128] = same & (s > j)
    c[:, C_INDA:C_INDA + 128] = (s < 64) & (j >= 0)
    c[:, C_INDB:C_INDB + 128] = (s >= 64) & (j >= 0)
    c[:, C_IOTA:C_IOTA + 512] = np.arange(512)[None, :]
    c[:, C_TLO] = p
    inv_freq = (500000.0 ** (-np.arange(0, 16, 2, dtype=np.float32) / 16)).astype(np.float32)
    c[:, C_FREQ:C_FREQ + 8] = (inv_freq.astype(np.float64) / (2 * np.pi)).astype(np.float32)[None, :]
    c[:, C_THI:C_THI + 32] = np.arange(32)[None, :]
    return c


class Sched:
    def __init__(self, nc, es):
        self.nc = nc
        self.es = es
        self.E = {'pe': nc.tensor, 'act': nc.scalar, 'dve': nc.vector, 'pool': nc.gpsimd, 'sp': nc.sync}
        self.sem = {k: es.enter_context(nc.semaphore('s_' + k)) for k in self.E}
        self.cnt = {k: 0 for k in self.E}
        self.seen = {k: {} for k in self.E}
        self.reg = {}
        self.dsem = {}
        self.dsem_by_sid = {}
        self.ninstr = {k: 0 for k in self.E}

    def _deps(self, reads, writes):
        deps = {}

        def add(tok):
            if tok is None:
                return
            if tok[0].startswith('d_'):
                tok = (tok[0], tok[1], self.dsem_by_sid[tok[0]][1])
            if tok[0] not in deps or deps[tok[0]][2] < tok[2]:
                deps[tok[0]] = tok
        for r in reads:
            st = self.reg.get(r)
            if st:
                add(st[0])
        for w in writes:
            st = self.reg.get(w)
            if st:
                add(st[0])
                for t in st[1].values():
                    add(t)
        return deps

    def _wait(self, eng, deps):
        for sid, (_, h, v) in deps.items():
            if sid == 'e_' + eng and not SAME_ENG_SYNC:
                continue
            if self.seen[eng].get(sid, 0) >= v:
                continue
            self.E[eng].wait_ge(h, v)
            self.seen[eng][sid] = v

    def _commit(self, tok, reads, writes):
        for r in reads:
            st = self.reg.setdefault(r, [None, {}])
            st[1][tok[0]] = tok
        for w in writes:
            self.reg[w] = [tok, {}]

    def op(self, eng, fn, reads=(), writes=()):
        self._wait(eng, self._deps(reads, writes))
        ins = fn(self.E[eng])
        self.cnt[eng] += 1
        ins.then_inc(self.sem[eng], 1)
        self._commit(('e_' + eng, self.sem[eng], self.cnt[eng]), reads, writes)

    def dma(self, q, fn, semkey, reads=(), writes=()):
        self._wait(q, self._deps(reads, writes))
        ins = fn(self.E[q])
        d = self.dsem.get(semkey)
        if d is None:
            d = [self.es.enter_context(self.nc.semaphore('d%d' % len(self.dsem))), 0]
            self.dsem[semkey] = d
            self.dsem_by_sid['d_' + str(semkey)] = d
        d[1] += 16
        ins.then_inc(d[0], 16)
        self._commit(('d_' + str(semkey), d[0], d[1]), reads, writes)

    def barrier(self):
        toks = {}
        for k in self.E:
            if self.cnt[k]:
                toks['e_' + k] = ('e_' + k, self.sem[k], self.cnt[k])
        for key, d in self.dsem.items():
            if d[1]:
                toks['d_' + str(key)] = ('d_' + str(key), d[0], d[1])
        for k in self.E:
            self._wait(k, toks)

    def finish(self):
        toks = {}
        for k in self.E:
            if self.cnt[k]:
                toks['e_' + k] = ('e_' + k, self.sem[k], self.cnt[k])
        for key, d in self.dsem.items():
            if d[1]:
                toks['d_' + str(key)] = ('d_' + str(key), d[0], d[1])
        self._wait('sp', toks)


def build(stage=99, debug=False):
    nc = bass.Bass("TRN2", target_bir_lowering=False)
    es = ExitStack()
    sc = Sched(nc, es)

    def din(name, shape, dt=F32):
        return nc.dram_tensor(name, list(shape), dt, kind="ExternalInput").ap()

    x_d = din("x", [S, D])
    cT_d = din("cT", [128, 8])
    pos_d = din("pos", [128, NT], I32)
    n1g_d = din("norm1_g", [1, D])
    n2g_d = din("norm2_g", [1, D])
    wada_d = din("w_ada", [D, 6 * D])
    bada_d = din("b_ada", [1, 6 * D])
    win_d = din("w_in", [D, D_IN])
    convT_d = din("convT", [128, 40])
    gateb_d = din("gate_b", [1, 16])
    mng_d = din("mnorm_g", [1, 512])
    qkg_d = din("qk_g", [1, 128])
    lam_d = din("lam", [1, 256])
    subg_d = din("subln_g", [1, 128])
    wout_d = din("w_out", [D, D])
    wr_d = din("w_router", [D, NE])
    consts_d = din("consts", [128, NCONST])
    if stage >= 5:
        wg_d = din("w_gate_e", [NE * D, 2 * D])
        wu_d = din("w_up_e", [NE * D, 2 * D])
        wd_d = din("w_down_e", [NE * 2 * D, D])
    out_d = nc.dram_tensor("out", [S, D], F32, kind="ExternalOutput").ap()
    skind = "ExternalOutput" if debug else "Internal"
    yT_d = nc.dram_tensor("yT_d", [D, S], BF16, kind=skind).ap()
    h2_d = nc.dram_tensor("h2_d", [S, D], BF16, kind=skind).ap()
    dbg = {}

    def dbg_out(name, shape, dt=F32):
        if debug:
            dbg[name] = nc.dram_tensor(name, list(shape), dt, kind="ExternalOutput").ap()
            return dbg[name]
        return None

    uniq = [0]

    def sb(name, shape, dt=F32, stack=es):
        uniq[0] += 1
        return stack.enter_context(nc.sbuf_tensor("sb%d_%s" % (uniq[0], name), list(shape), dt))

    ps = es.enter_context(nc.psum_tensor("ps", [128, 8, 512], F32))
    consts = sb("consts", [128, NCONST])
    identb = sb("identb", [128, 128], BF16)
    s1 = ExitStack()
    prm = sb("prm", [128, 16 + 512 + 128 + 256 + 128], F32, s1)
    P_GB, P_MNG, P_QKG, P_LAM, P_SUB = 0, 16, 528, 656, 912
    convT = sb("convT", [128, 40], F32, s1)
    maskF = sb("maskF", [128, 128], BF16, s1)
    maskB = sb("maskB", [128, 128], BF16, s1)
    small = sb("small", [128, 64], F32, s1)
    SM_NB, SM_NLAM = 0, 1
    ident = consts[:, C_IDENT:C_IDENT + 128]

    h1T = sb("h1T", [128, 8, S], BF16, s1)
    gm = sb("gm", [128, NT, 16], F32, s1)
    smod = ExitStack()
    mod = sb("mod", [128, 6 * D], F32, smod)
    modrow_d = nc.dram_tensor("modrow_d", [1, 6 * D], F32, kind="Internal").ap()
    CK = 'consts'
    sc.dma('sp', lambda e: e.dma_start(out=consts[:], in_=consts_d[:, :]), CK, writes=['consts'])
    sc.dma('sp', lambda e: e.dma_start(out=convT[:], in_=convT_d[:, :]), CK, writes=['convT'])
    for (off, src, n) in [(P_GB, gateb_d, 16), (P_MNG, mng_d, 512),
                          (P_QKG, qkg_d, 128), (P_LAM, lam_d, 256), (P_SUB, subg_d, 128)]:
        sc.dma('sp', lambda e, off=off, src=src, n=n: e.dma_start(
            out=prm[:, off:off + n], in_=src[0:1, :].partition_broadcast(128)), CK, writes=['prm'])
    sc.dma('sp', lambda e: e.dma_start(out=mod[:], in_=bada_d[0:1, :].partition_broadcast(128)), CK, writes=['mod'])

    with ExitStack() as p0:
        cT = sb("cT", [128, 8], F32, p0)
        prmA = sb("prmA", [128, 2 * D], F32, p0)
        sc.dma('sp', lambda e: e.dma_start(out=prmA[:, 0:D], in_=n1g_d[0:1, :].partition_broadcast(128)), CK, writes=['prmA'])
        sc.dma('sp', lambda e: e.dma_start(out=prmA[:, D:2 * D], in_=n2g_d[0:1, :].partition_broadcast(128)), CK, writes=['prmA'])
        scT = sb("scT", [128, 8], F32, p0)
        lc = sb("lc", [128, 8, 128], BF16, p0)
        wa = [sb("wa%d" % i, [128, 8, 512], BF16, p0) for i in range(2)]
        sc.dma('sp', lambda e: e.dma_start(out=cT[:], in_=cT_d[:, :]), 'cT', writes=['cT'])
        sc.op('act', lambda e: e.activation(out=scT[:], in_=cT[:], func=AF.Silu), reads=['cT'], writes=['scT'])
        sc.op('dve', lambda e: e.tensor_copy(out=lc[:], in_=scT[:].unsqueeze(2).broadcast_to([128, 8, 128])),
              reads=['scT'], writes=['lc'])
        sc.op('dve', lambda e: e.tensor_copy(out=identb[:], in_=ident), reads=['consts'], writes=['identb'])
        sc.op('dve', lambda e: e.tensor_copy(out=maskF[:], in_=consts[:, C_TL:C_TL + 128]), reads=['consts'], writes=['maskF'])
        sc.op('dve', lambda e: e.tensor_copy(out=maskB[:], in_=consts[:, C_TU:C_TU + 128]), reads=['consts'], writes=['maskB'])
        wada_v = wada_d.rearrange("(kc p) n -> p kc n", p=128)
        for n in range(12):
            w = wa[n % 2]
            wk = ('wa', n % 2)
            sc.dma('pool', lambda e, w=w, n=n: e.dma_start(out=w[:], in_=wada_v[:, :, n * 512:(n + 1) * 512]),
                   wk, writes=[wk])
            bank = ('ps', n % 2)

            def mm(e, w=w, n=n):
                for kc in range(8):
                    r = e.matmul(ps[:, n % 2, :], lhsT=lc[:, kc, :], rhs=w[:, kc, :], start=(kc == 0), stop=(kc == 7))
                return r
            sc.op('pe', mm, reads=['lc', wk], writes=[bank])
            sc.op('dve', lambda e, n=n: e.tensor_tensor(out=mod[:, n * 512:(n + 1) * 512], in0=ps[:, n % 2, :],
                                                        in1=mod[:, n * 512:(n + 1) * 512], op=ALU.add),
                  reads=[bank, 'mod'], writes=['mod'])
        sc.op('dve', lambda e: e.scalar_tensor_tensor(out=mod[:, D:2 * D], in0=mod[:, D:2 * D], scalar=1.0,
                                                      in1=prmA[:, 0:D], op0=ALU.add, op1=ALU.mult),
              reads=['mod', 'prmA'], writes=['mod'])
        sc.op('dve', lambda e: e.scalar_tensor_tensor(out=mod[:, 4 * D:5 * D], in0=mod[:, 4 * D:5 * D], scalar=1.0,
                                                      in1=prmA[:, D:2 * D], op0=ALU.add, op1=ALU.mult),
              reads=['mod', 'prmA'], writes=['mod'])
        if debug:
            o = dbg_out("dbg_mod", [128, 6 * D])
            sc.dma('sp', lambda e: e.dma_start(out=o[:, :], in_=mod[:]), 'dbgmod', reads=['mod'])
        sc.barrier()
    SHIFT1, GS1, GATE1, SHIFT2, GS2, GATE2 = [mod[:, i * D:(i + 1) * D] for i in range(6)]

    if stage <= 0:
        sc.finish()
        return nc, dbg

    with ExitStack() as p1:
        xt = [sb("xt%d" % i, [128, D], F32, p1) for i in range(2)]
        junk = sb("junk1", [128, D], BF16, p1)
        tmp = [sb("tmp1_%d" % i, [128, D], F32, p1) for i in range(2)]
        h1b = [sb("h1b%d" % i, [128, D], BF16, p1) for i in range(2)]
        st1 = sb("st1", [128, NT, 3], F32, p1)
        wgt = sb("wgt", [128, 8, 16], BF16, p1)
        win_v = win_d.rearrange("(kc p) n -> p kc n", p=128)
        sc.dma('pool', lambda e: e.dma_start(out=wgt[:], in_=win_v[:, :, 2048:2064]), 'wgt', writes=['wgt'])
        for i in range(NT):
            b = i % 2
            xk, tk, hk = ('xt', b), ('tmp1', b), ('h1b', b)
            sc.dma('sp', lambda e, i=i, b=b: e.dma_start(out=xt[b][:], in_=x_d[i * 128:(i + 1) * 128, :]), xk, writes=[xk])
            sc.op('act', lambda e, i=i, b=b: e.activation(out=junk[:], in_=xt[b][:], func=AF.Square,
                                                          accum_out=st1[:, i, 0:1]), reads=[xk], writes=['junk1', ('st1', i)])
            sc.op('act', lambda e, i=i: e.activation(out=st1[:, i, 1:2], in_=st1[:, i, 0:1], func=AF.Sqrt,
                                                     scale=1.0 / D, bias=EPS), reads=[('st1', i)], writes=[('st1', i)])
            sc.op('dve', lambda e, i=i: e.reciprocal(out=st1[:, i, 2:3], in_=st1[:, i, 1:2]),
                  reads=[('st1', i)], writes=[('st1', i)])
            sc.op('dve', lambda e, i=i, b=b: e.scalar_tensor_tensor(out=tmp[b][:], in0=xt[b][:], scalar=st1[:, i, 2:3],
                                                                    in1=GS1, op0=ALU.mult, op1=ALU.mult),
                  reads=[xk, ('st1', i), 'mod'], writes=[tk])
            sc.op('dve', lambda e, b=b: e.tensor_tensor(out=h1b[b][:], in0=tmp[b][:], in1=SHIFT1, op=ALU.add),
                  reads=[tk, 'mod'], writes=[hk])
            bank = ('ps', 2 + b)
            pb = ps[:, 2 + b, :].bitcast(BF16)

            def tr(e, b=b, pb=pb):
                for kc in range(8):
                    r = e.transpose(out=pb[:, kc * 128:(kc + 1) * 128], in_=h1b[b][:, kc * 128:(kc + 1) * 128], identity=identb[:])
                return r
            sc.op('pe', tr, reads=[hk, 'identb'], writes=[bank])
            sc.op('act', lambda e, i=i, pb=pb: e.copy(out=h1T[:, :, i * 128:(i + 1) * 128],
                                                      in_=pb.rearrange("p (k t) -> p k t", k=8)),
                  reads=[bank], writes=[('h1T', i)])
            gbank = ('ps', 4 + b)

            def gmm(e, i=i, b=b):
                for kc in range(8):
                    r = e.matmul(ps[:, 4 + b, 0:16], lhsT=h1T[:, kc, i * 128:(i + 1) * 128], rhs=wgt[:, kc, :],
                                 start=(kc == 0), stop=(kc == 7))
                return r
            sc.op('pe', gmm, reads=[('h1T', i), 'wgt'], writes=[gbank])
            sc.op('dve', lambda e, i=i, b=b: e.tensor_tensor(out=gm[:, i, :], in0=ps[:, 4 + b, 0:16],
                                                             in1=prm[:, P_GB:P_GB + 16], op=ALU.add),
                  reads=[gbank, 'prm'], writes=['gm'])
        if debug:
            o = dbg_out("dbg_h1T", [128, 8, S], BF16)
            sc.dma('sp', lambda e: e.dma_start(out=o[:, :, :], in_=h1T[:]), 'dbgh1T', reads=[('h1T', i) for i in range(NT)])
            o3 = dbg_out("dbg_st1", [128, NT, 3])
            sc.dma('sp', lambda e: e.dma_start(out=o3[:, :, :], in_=st1[:]), 'dbgst1', reads=[('st1', i) for i in range(NT)])
            o4 = dbg_out("dbg_tmp", [128, D])
            sc.dma('sp', lambda e: e.dma_start(out=o4[:, :], in_=tmp[1][:]), 'dbgtmp', reads=[('tmp1', 1)])
            o5 = dbg_out("dbg_h1b", [128, D], BF16)
            sc.dma('sp', lambda e: e.dma_start(out=o5[:, :], in_=h1b[1][:]), 'dbgh1b', reads=[('h1b', 1)])
            o2 = dbg_out("dbg_gm", [128, NT, 16])
            sc.dma('sp', lambda e: e.dma_start(out=o2[:, :, :], in_=gm[:]), 'dbggm', reads=['gm'])
        sc.barrier()

    sc.dma('sp', lambda e: e.dma_start(out=modrow_d[0:1, :], in_=mod[0:1, :]), 'modrow', reads=['mod'], writes=['modrow_d'])
    sc.barrier()
    smod.close()

    if stage <= 1:
        sc.finish()
        return nc, dbg

    LNK = -0.5 * math.log(128.0)
    cum = sb("cum", [128, 4, NT, 4], F32, s1)
    egs = sb("egs", [128, 4, NT, 4], F32, s1)
    tw = sb("tw", [128, 6, NT, 4], F32, s1)
    with ExitStack() as pg:
        lsg = sb("lsg", [128, 2, NT, 4], F32, pg)
        tg = sb("tg", [128, 2, NT, 4], F32, pg)
        for d, c0 in ((0, 4), (1, 12)):
            sc.op('act', lambda e, d=d, c0=c0: e.activation(out=tg[:, d, :, :], in_=gm[:, :, c0:c0 + 4], func=AF.Exp, scale=-1.0),
                  reads=['gm'], writes=['tg'])
        sc.op('act', lambda e: e.activation(out=tg[:], in_=tg[:], func=AF.Ln, bias=1.0), reads=['tg'], writes=['tg'])
        sc.op('dve', lambda e: e.tensor_scalar(out=lsg[:], in0=tg[:], scalar1=-1.0, scalar2=None, op0=ALU.mult),
              reads=['tg'], writes=['lsg'])
        lf = lsg[:, 0, :, :].rearrange("p t h -> p (t h)")
        lb = lsg[:, 1, :, :].rearrange("p t h -> p (t h)")
        specs = [(C_TL, lf), (C_TUS, lf), (C_TU, lb), (C_TLS, lb)]

        def cmm(e):
            for k, (co, rhs) in enumerate(specs):
                r = e.matmul(ps[:, 6, k * 128:(k + 1) * 128], lhsT=consts[:, co:co + 128], rhs=rhs, start=True, stop=True)
            return r
        sc.op('pe', cmm, reads=['lsg', 'consts'], writes=[('ps', 6)])
        sc.op('act', lambda e: e.copy(out=cum[:].rearrange("p a t h -> p (a t h)"), in_=ps[:, 6, :]),
              reads=[('ps', 6)], writes=['cum'])
        specs2 = [(C_INDA, lf), (C_INDB, lf), (C_INDA, lb), (C_INDB, lb)]

        def gmm2(e):
            for k, (co, rhs) in enumerate(specs2):
                r = e.matmul(ps[:, 7, k * 128:(k + 1) * 128], lhsT=consts[:, co:co + 128], rhs=rhs, start=True, stop=True)
            return r
        sc.op('pe', gmm2, reads=['lsg', 'consts'], writes=[('ps', 7)])
        sc.op('act', lambda e: e.activation(out=egs[:].rearrange("p a t h -> p (a t h)"), in_=ps[:, 7, :], func=AF.Exp),
              reads=[('ps', 7)], writes=['egs'])
        for d, ic in ((0, 0), (1, 8)):
            ig = gm[:, :, ic:ic + 4]
            a_ = cum[:, 2 * d, :, :]
            r_ = cum[:, 2 * d + 1, :, :]
            sc.op('dve', lambda e, d=d, ig=ig, a_=a_: e.tensor_tensor(out=tw[:, 3 * d, :, :], in0=ig, in1=a_, op=ALU.subtract),
                  reads=['gm', 'cum'], writes=['tw'])
            sc.op('dve', lambda e, d=d, ig=ig, r_=r_: e.tensor_tensor(out=tw[:, 3 * d + 1, :, :], in0=ig, in1=r_, op=ALU.add),
                  reads=['gm', 'cum'], writes=['tw'])
            sc.op('act', lambda e, d=d: e.activation(out=tw[:, 3 * d:3 * d + 2, :, :], in_=tw[:, 3 * d:3 * d + 2, :, :],
                                                     func=AF.Exp, bias=LNK), reads=['tw'], writes=['tw'])
            sc.op('act', lambda e, d=d, a_=a_: e.activation(out=tw[:, 3 * d + 2, :, :], in_=a_, func=AF.Exp),
                  reads=['cum'], writes=['tw'])
        if debug:
            o = dbg_out("dbg_tw", [128, 6, NT, 4])
            sc.dma('sp', lambda e: e.dma_start(out=o[:, :, :, :], in_=tw[:]), 'dbgtw', reads=['tw'])
            o = dbg_out("dbg_egs", [128, 4, NT, 4])
            sc.dma('sp', lambda e: e.dma_start(out=o[:, :, :, :], in_=egs[:]), 'dbgegs', reads=['egs'])
        sc.barrier()

    if stage <= 1.5:
        sc.finish()
        return nc, dbg

    win_v = win_d.rearrange("(kc p) n -> p kc n", p=128)
    yT_v = yT_d.rearrange("(c p) t -> p c t", p=128)

    def emit_y_tile(ph, h_chunk, i, ym, ymk, yst, ystk):
        g4 = i % 4
        bank = ('ps', 5)
        pb = ps[:, 5, :].bitcast(BF16)
        sc.op('pe', lambda e: e.transpose(out=pb[:, g4 * 128:(g4 + 1) * 128], in_=ym, identity=identb[:]),
              reads=[ymk, 'identb'], writes=[bank])
        sc.op('act', lambda e: e.copy(out=yst[:, g4 * 128:(g4 + 1) * 128], in_=pb[:, g4 * 128:(g4 + 1) * 128]),
              reads=[bank], writes=[ystk])
        if g4 == 3:
            t0 = (i - 3) * 128
            sc.dma('sp', lambda e: e.dma_start(out=yT_v[:, h_chunk, t0:t0 + 512], in_=yst[:]), ystk, reads=[ystk], writes=['yT_d'])

    n_mheads = 4 if stage >= 2.5 else 1
    sM = ExitStack()
    wm2 = [sb("wm%d" % k, [128, 8, 512], BF16, sM) for k in range(2)]
    for h in range(n_mheads):
        with ExitStack() as ph:
            wm = wm2[h % 2]
            WMK = ('wm', h % 2)
            qkT = sb("qkT", [128, 2, S], BF16, ph)
            pre = sb("pre", [128, 2, 516], F32, ph)
            acc = sb("acc", [128, 2, 512], F32, ph)
            vext = sb("vext", [128, NT, 129], BF16, ph)
            sgo = sb("sgo", [128, NT, 128], BF16, ph)
            ktok = sb("ktok", [128, NT, 128], BF16, ph)
            Cst = sb("Cst", [128, 2, 65, 129], BF16, ph)
            stt = sb("stt", [128, 2, 2, 129], F32, ph)
            vtmp = [sb("vtmp%d" % k, [128, 129], BF16, ph) for k in range(2)]
            smg = [sb("smg%d" % k, [128, 2, 4, 128], BF16, ph) for k in range(2)]
            vtg = [sb("vtg%d" % k, [128, 2, 4, 129], BF16, ph) for k in range(2)]
            pst = sb("pst", [128, 2, 48], F32, ph)
            hfg = [sb("hfg%d" % k, [128, 4, 128], F32, ph) for k in range(2)]
            hsg = [sb("hsg%d" % k, [128, 4, 128], F32, ph) for k in range(2)]
            ymg = [sb("ymg%d" % k, [128, 4, 128], BF16, ph) for k in range(2)]
            yst = [sb("yst%d" % k, [128, 512], BF16, ph) for k in range(2)]
            for hh in ([0, 1] if h == 0 else [h + 1]):
                if hh >= n_mheads:
                    continue
                for k, c0 in enumerate((hh * 128, 512 + hh * 128, 1024 + hh * 128, 1536 + hh * 128)):
                    sc.dma('pool', lambda e, k=k, c0=c0, hh=hh: e.dma_start(out=wm2[hh % 2][:, :, k * 128:(k + 1) * 128], in_=win_v[:, :, c0:c0 + 128]),
                           ('wm', hh % 2), writes=[('wm', hh % 2)])
            sc.op('pool', lambda e: e.memset(vext[:, :, 128:129], 1.0), writes=['vext1'])
            sc.op('pool', lambda e: e.memset(Cst[:, :, 0, :], 0.0), writes=[('Cst', 0, 0), ('Cst', 1, 0)])
            sc.op('pool', lambda e: e.memset(stt[:, :, 0, :], 0.0), writes=[('stt', 0, 0), ('stt', 1, 0)])
            sc.op('pool', lambda e: e.memset(pre[:, :, 0:4], 0.0), writes=['pre'])
            for g in range(NG + 1):
                if g < NG:
                    for qk in range(2):
                        bank = ('ps', qk)

                        def mm(e, qk=qk, g=g):
                            for kc in range(8):
                                r = e.matmul(ps[:, qk, :], lhsT=wm[:, kc, qk * 128:(qk + 1) * 128],
                                             rhs=h1T[:, kc, g * 512:(g + 1) * 512], start=(kc == 0), stop=(kc == 7))
                            return r
                        sc.op('pe', mm, reads=[WMK] + [('h1T', 4 * g + k) for k in range(4)], writes=[bank])
                        sc.op('act', lambda e, qk=qk: e.copy(out=pre[:, qk, 4:516], in_=ps[:, qk, :]), reads=[bank], writes=['pre'])
                else:
                    sc.op('pool', lambda e: e.memset(pre[:, :, 4:8], 0.0), writes=['pre'])
                W = 512 if g < NG else 2
                for qk in range(2):
                    cb = (4 * qk + h) * 5
                    sc.op('dve', lambda e, qk=qk, cb=cb, W=W: e.tensor_scalar(out=acc[:, qk, 0:W], in0=pre[:, qk, 0:W], scalar1=convT[:, cb:cb + 1],
                                                                            scalar2=None, op0=ALU.mult), reads=['pre', 'convT'], writes=[('acc', qk)])
                    for j in range(1, 5):
                        sc.op('dve', lambda e, qk=qk, cb=cb, j=j, W=W: e.scalar_tensor_tensor(
                            out=acc[:, qk, 0:W], in0=pre[:, qk, j:j + W], scalar=convT[:, cb + j:cb + j + 1], in1=acc[:, qk, 0:W],
                            op0=ALU.mult, op1=ALU.add), reads=['pre', 'convT', ('acc', qk)], writes=[('acc', qk)])
                    n0 = 2 if g == 0 else 0
                    t0 = 512 * g - 2 + n0
                    sc.op('act', lambda e, qk=qk, n0=n0, t0=t0, W=W: e.activation(out=qkT[:, qk, t0:t0 + W - n0], in_=acc[:, qk, n0:W], func=AF.Silu),
                          reads=[('acc', qk)], writes=[('qkT', qk)])
                if g < NG:
                    sc.op('pool', lambda e: e.tensor_copy(out=pre[:, :, 0:4], in_=pre[:, :, 512:516]), reads=['pre'], writes=['pre'])
                    for k in range(4):
                        i = 4 * g + k
                        bank = ('ps', 2 + (i % 2))

                        def mmvo(e, i=i):
                            for kc in range(8):
                                r = e.matmul(ps[:, 2 + (i % 2), 0:256], lhsT=h1T[:, kc, i * 128:(i + 1) * 128], rhs=wm[:, kc, 256:512],
                                             start=(kc == 0), stop=(kc == 7))
                            return r
                        sc.op('pe', mmvo, reads=[WMK, ('h1T', i)], writes=[bank])
                        sc.op('act', lambda e, i=i: e.copy(out=vext[:, i, 0:128], in_=ps[:, 2 + (i % 2), 0:128]), reads=[bank], writes=[('vext', i)])
                        sc.op('act', lambda e, i=i: e.activation(out=sgo[:, i, :], in_=ps[:, 2 + (i % 2), 128:256], func=AF.Sigmoid),
                              reads=[bank], writes=[('sgo', i)])
            for g in range(NG):
                bank = ('ps', 4)
                pb = ps[:, 4, :].bitcast(BF16)

                def trk(e, g=g, pb=pb):
                    for k in range(4):
                        i = 4 * g + k
                        r = e.transpose(out=pb[:, k * 128:(k + 1) * 128], in_=qkT[:, 1, i * 128:(i + 1) * 128], identity=identb[:])
                    return r
                sc.op('pe', trk, reads=[('qkT', 1), 'identb'], writes=[bank])
                sc.op('act', lambda e, g=g, pb=pb: e.copy(out=ktok[:, 4 * g:4 * g + 4, :], in_=pb[:, 0:512].rearrange("p (k d) -> p k d", k=4)),
                      reads=[bank], writes=['ktok'])
            nv = 0
            for step in range(64):
                for d in range(2):
                    c = step if d == 0 else 63 - step
                    i, half = c // 2, c % 2
                    first_of_tile = (half == 0) if d == 0 else (half == 1)
                    vk = ('vhat', d)
                    vb = vtmp[d]
                    if first_of_tile:
                        sc.op('act', lambda e, d=d, i=i, vb=vb: e.activation(out=vb[:], in_=vext[:, i, :], func=AF.Copy,
                                                                             scale=tw[:, 3 * d + 1, i, h:h + 1]),
                              reads=[('vext', i), 'vext1', 'tw'], writes=[vk])
                    slot = nv % 4
                    nv += 1
                    pbank = 4 + slot
                    pc = 0
                    pk = ('ps', pbank)
                    sc.op('pe', lambda e, i=i, half=half, vb=vb, pbank=pbank, pc=pc: e.matmul(
                        ps[:, pbank, pc:pc + 129], lhsT=ktok[half * 64:(half + 1) * 64, i, :], rhs=vb[half * 64:(half + 1) * 64, :],
                        start=True, stop=True), reads=['ktok', vk], writes=[pk])
                    cur, nxt = step % 2, (step + 1) % 2
                    sc.op('dve', lambda e, d=d, i=i, half=half, cur=cur, nxt=nxt, pbank=pbank, pc=pc: e.scalar_tensor_tensor(
                        out=stt[:, d, nxt, :], in0=stt[:, d, cur, :], scalar=egs[:, 2 * d + half, i, h:h + 1], in1=ps[:, pbank, pc:pc + 129],
                        op0=ALU.mult, op1=ALU.add), reads=[('stt', d, cur), 'egs', pk], writes=[('stt', d, nxt)])
                    sc.op('pool', lambda e, d=d, nxt=nxt, step=step: e.tensor_copy(out=Cst[:, d, step + 1, :], in_=stt[:, d, nxt, :]),
                          reads=[('stt', d, nxt)], writes=[('Cst', d, step + 1)])
            for g in range(NG):
                gb = g % 2
                sbank = ('ps', gb)

                def smm(e, g=g, gb=gb):
                    for t in range(4):
                        blk = slice((4 * g + t) * 128, (4 * g + t + 1) * 128)
                        r = e.matmul(ps[:, gb, t * 128:(t + 1) * 128], lhsT=qkT[:, 1, blk], rhs=qkT[:, 0, blk], start=True, stop=True)
                    return r
                sc.op('pe', smm, reads=[('qkT', 0), ('qkT', 1)], writes=[sbank])
                S4 = ps[:, gb, :].rearrange("p (t j) -> p t j", t=4)
                smF, smB = smg[gb][:, 0, :, :], smg[gb][:, 1, :, :]
                kF, kB = ('smg', gb, 0), ('smg', gb, 1)
                sc.op('dve', lambda e, S4=S4, smF=smF: e.tensor_tensor(out=smF, in0=S4, in1=maskF[:].unsqueeze(1).broadcast_to([128, 4, 128]), op=ALU.mult),
                      reads=[sbank, 'maskF'], writes=[kF])
                sc.op('dve', lambda e, S4=S4, smB=smB: e.tensor_tensor(out=smB, in0=S4, in1=maskB[:].unsqueeze(1).broadcast_to([128, 4, 128]), op=ALU.mult),
                      reads=[sbank, 'maskB'], writes=[kB])
                vtk = ('vtg', gb)
                for d in range(2):
                    sc.op('pool', lambda e, d=d, g=g, gb=gb: e.tensor_tensor(
                        out=vtg[gb][:, d, :, :], in0=vext[:, 4 * g:4 * g + 4, :],
                        in1=tw[:, 3 * d, 4 * g:4 * g + 4, h:h + 1].broadcast_to([128, 4, 129]), op=ALU.mult),
                        reads=[('vext', 4 * g + t) for t in range(4)] + ['vext1', 'tw'], writes=[(vtk, d)])
                ubanks = [('ps', 2 + t) for t in range(4)]

                def umm(e, g=g, gb=gb):
                    for t in range(4):
                        i = 4 * g + t
                        for d in range(2):
                            o0_ = d * 129
                            e.matmul(ps[:, 2 + t, o0_:o0_ + 129], lhsT=smg[gb][:, d, t, :], rhs=vtg[gb][:, d, t, :], start=True, stop=False)
                            for half in range(2):
                                c = 2 * i + half
                                sidx = c if d == 0 else 63 - c
                                t0 = i * 128 + half * 64
                                r = e.matmul(ps[half * 64:(half + 1) * 64, 2 + t, o0_:o0_ + 129], lhsT=qkT[:, 0, t0:t0 + 64],
                                             rhs=Cst[:, d, sidx, :], start=False, stop=True)
                    return r
                creads = []
                for t in range(4):
                    i = 4 * g + t
                    creads += [('Cst', 0, 2 * i), ('Cst', 0, 2 * i + 1), ('Cst', 1, 63 - 2 * i), ('Cst', 1, 62 - 2 * i)]
                sc.op('pe', umm, reads=[kF, kB, (vtk, 0), (vtk, 1), ('qkT', 0)] + creads, writes=ubanks)
                P = pst[:, gb, :]
                pk = ('pst', gb)
                U4 = ps[:, 2:6, 0:258].rearrange("p t (d v) -> p t d v", d=2)
                ea2 = tw[:, 2:6:3, 4 * g:4 * g + 4, h].rearrange("p d t -> p t d")
                Pv = lambda lo: P[:, lo:lo + 8].rearrange("p (t d) -> p t d", d=2)
                sc.op('dve', lambda e: e.tensor_tensor(out=Pv(0), in0=U4[:, :, :, 128], in1=ea2, op=ALU.mult), reads=ubanks + ['tw'], writes=[pk])
                sc.op('dve', lambda e: e.tensor_scalar(out=P[:, 16:24], in0=P[:, 0:8], scalar1=-1.0, scalar2=None, op0=ALU.mult), reads=[pk], writes=[pk])
                sc.op('dve', lambda e: e.scalar_tensor_tensor(out=P[:, 8:16], in0=P[:, 16:24], scalar=1.0, in1=P[:, 0:8], op0=ALU.max, op1=ALU.max),
                      reads=[pk], writes=[pk])
                sc.op('dve', lambda e: e.reciprocal(out=P[:, 16:24], in_=P[:, 8:16]), reads=[pk], writes=[pk])
                sc.op('dve', lambda e: e.tensor_tensor(out=Pv(24), in0=Pv(16), in1=ea2, op=ALU.mult), reads=[pk, 'tw'], writes=[pk])
                RR = Pv(24)
                hfb, hsb = hfg[gb], hsg[gb]
                sc.op('dve', lambda e: e.tensor_tensor(out=hfb[:], in0=U4[:, :, 0, 0:128], in1=RR[:, :, 0:1].broadcast_to([128, 4, 128]), op=ALU.mult),
                      reads=ubanks + [pk], writes=[('hfg', gb)])
                sc.op('dve', lambda e: e.tensor_tensor(out=hsb[:], in0=U4[:, :, 1, 0:128], in1=RR[:, :, 1:2].broadcast_to([128, 4, 128]), op=ALU.mult),
                      reads=ubanks + [pk], writes=[('hsg', gb)])
                sc.op('pool', lambda e: e.tensor_tensor(out=hsb[:], in0=hsb[:], in1=hfb[:], op=ALU.add), reads=[('hsg', gb), ('hfg', gb)], writes=[('hsg', gb)])
                sc.op('pool', lambda e: e.tensor_tensor(out=hfb[:], in0=hsb[:], in1=hsb[:], op=ALU.mult), reads=[('hsg', gb)], writes=[('hfg', gb)])
                sc.op('dve', lambda e: e.tensor_reduce(out=P[:, 32:36], in_=hfb[:], axis=AX.X, op=ALU.add), reads=[('hfg', gb)], writes=[pk])
                sc.op('act', lambda e: e.activation(out=P[:, 36:40], in_=P[:, 32:36], func=AF.Sqrt, scale=1.0 / 128, bias=EPS), reads=[pk], writes=[pk])
                sc.op('dve', lambda e: e.reciprocal(out=P[:, 40:44], in_=P[:, 36:40]), reads=[pk], writes=[pk])
                sc.op('dve', lambda e: e.tensor_tensor(out=hsb[:], in0=hsb[:], in1=P[:, 40:44].unsqueeze(2).broadcast_to([128, 4, 128]), op=ALU.mult),
                      reads=[('hsg', gb), pk], writes=[('hsg', gb)])
                sc.op('pool', lambda e: e.tensor_tensor(out=hsb[:], in0=hsb[:],
                                                        in1=prm[:, P_MNG + h * 128:P_MNG + (h + 1) * 128].unsqueeze(1).broadcast_to([128, 4, 128]), op=ALU.mult),
                      reads=[('hsg', gb), 'prm'], writes=[('hsg', gb)])
                ym = ymg[gb]
                ymk = ('ymg', gb)
                sc.op('dve', lambda e, g=g: e.tensor_tensor(out=ym[:], in0=hsb[:], in1=sgo[:, 4 * g:4 * g + 4, :], op=ALU.mult),
                      reads=[('hsg', gb)] + [('sgo', 4 * g + t) for t in range(4)], writes=[ymk])
                tbank = ('ps', 6)
                pb = ps[:, 6, :].bitcast(BF16)

                def ytr(e, ym=ym, pb=pb):
                    for t in range(4):
                        r = e.transpose(out=pb[:, t * 128:(t + 1) * 128], in_=ym[:, t, :], identity=identb[:])
                    return r
                sc.op('pe', ytr, reads=[ymk, 'identb'], writes=[tbank])
                ys = yst[gb]
                ysk = ('yst', gb)
                sc.op('act', lambda e, ys=ys, pb=pb: e.copy(out=ys[:], in_=pb[:, 0:512]), reads=[tbank], writes=[ysk])
                sc.dma('sp', lambda e, ys=ys, g=g: e.dma_start(out=yT_v[:, h, g * 512:(g + 1) * 512], in_=ys[:]), ysk, reads=[ysk], writes=['yT_d'])
            if debug and h == 0:
                o = dbg_out("dbg_qkT", [128, 2, S], BF16)
                sc.dma('sp', lambda e: e.dma_start(out=o[:, :, :], in_=qkT[:]), 'dbgqkT', reads=[('qkT', 0), ('qkT', 1)])
                o = dbg_out("dbg_Cst", [128, 2, 65, 129], BF16)
                sc.dma('sp', lambda e: e.dma_start(out=o[:, :, :, :], in_=Cst[:]), 'dbgCst',
                       reads=[('Cst', d, k) for d in range(2) for k in range(65)])
            sc.barrier()
    sM.close()

    if stage <= 2.5:
        sc.finish()
        return nc, dbg

    cosT = sb("cosT", [128, NT, 8], F32, s1)
    sinT = sb("sinT", [128, NT, 8], F32, s1)
    qkg4 = sb("qkg4", [128, 4, 64], F32, s1)
    subg8 = sb("subg8", [128, 128], F32, s1)
    TWO_PI = 6.28318
    with ExitStack() as pa:
        posi = sb("posi", [128, NT], I32, pa)
        posf = sb("posf", [128, NT], F32, pa)
        ut = sb("ut", [128, NT, 8], F32, pa)
        ui = sb("ui", [128, NT, 8], I32, pa)
        uf = sb("uf", [128, NT, 8], F32, pa)
        jl = sb("jl", [128, 64], F32, pa)
        sc.dma('sp', lambda e: e.dma_start(out=posi[:], in_=pos_d[:, :]), 'posi', writes=['posi'])
        sc.op('dve', lambda e: e.tensor_copy(out=posf[:], in_=posi[:]), reads=['posi'], writes=['posf'])
        sc.op('dve', lambda e: e.tensor_tensor(out=ut[:], in0=posf[:].unsqueeze(2).broadcast_to([128, NT, 8]),
                                               in1=consts[:, C_FREQ:C_FREQ + 8].unsqueeze(1).broadcast_to([128, NT, 8]), op=ALU.mult),
              reads=['posf', 'consts'], writes=['ut'])
        for tab, shift in ((sinT, 0.0), (cosT, 0.25)):
            if shift:
                sc.op('dve', lambda e, shift=shift: e.tensor_scalar(out=ut[:], in0=ut[:], scalar1=shift, scalar2=None, op0=ALU.add),
                      reads=['ut'], writes=['ut'])
            sc.op('dve', lambda e: e.tensor_copy(out=ui[:], in_=ut[:]), reads=['ut'], writes=['ui'])
            sc.op('dve', lambda e: e.tensor_copy(out=uf[:], in_=ui[:]), reads=['ui'], writes=['uf'])
            sc.op('dve', lambda e: e.tensor_tensor(out=uf[:], in0=ut[:], in1=uf[:], op=ALU.subtract), reads=['ut', 'uf'], writes=['uf'])
            sc.op('act', lambda e, tab=tab: e.activation(out=tab[:], in_=uf[:], func=AF.Sin, scale=TWO_PI), reads=['uf'], writes=['rot'])
        sc.op('dve', lambda e: e.tensor_reduce(out=small[:, 2:4], in_=prm[:, P_QKG:P_QKG + 128].rearrange("p (a d) -> p a d", a=2),
                                               axis=AX.X, op=ALU.max, apply_absolute_value=True), reads=['prm'], writes=['small'])
        sc.op('dve', lambda e: e.scalar_tensor_tensor(out=small[:, SM_NB:SM_NB + 1], in0=small[:, 2:3], scalar=-8.0, in1=small[:, 3:4],
                                                      op0=ALU.mult, op1=ALU.mult), reads=['small'], writes=['small'])
        for k in range(2):
            sc.op('dve', lambda e, k=k: e.tensor_tensor(out=jl[:], in0=prm[:, P_LAM + 128 * k:P_LAM + 128 * k + 64],
                                                        in1=prm[:, P_LAM + 128 * k + 64:P_LAM + 128 * k + 128], op=ALU.mult),
                  reads=['prm'], writes=['jl'])
            sc.op('dve', lambda e, k=k: e.tensor_reduce(out=small[:, 4 + k:5 + k], in_=jl[:], axis=AX.X, op=ALU.add), reads=['jl'], writes=['small'])
        sc.op('act', lambda e: e.activation(out=small[:, 6:8], in_=small[:, 4:6], func=AF.Exp), reads=['small'], writes=['small'])
        sc.op('dve', lambda e: e.scalar_tensor_tensor(out=small[:, SM_NLAM:SM_NLAM + 1], in0=small[:, 7:8], scalar=-LAM_INIT, in1=small[:, 6:7],
                                                      op0=ALU.add, op1=ALU.subtract), reads=['small'], writes=['small'])
        for a in range(4):
            o_ = P_QKG + (64 if a >= 2 else 0)
            sc.op('pool', lambda e, a=a, o_=o_: e.tensor_copy(out=qkg4[:, a, :], in_=prm[:, o_:o_ + 64]), reads=['prm'], writes=['qkg4'])
        sc.op('dve', lambda e: e.tensor_scalar(out=subg8[:], in0=prm[:, P_SUB:P_SUB + 128], scalar1=1.0 - LAM_INIT, scalar2=None, op0=ALU.mult),
              reads=['prm'], writes=['subg8'])
        if debug:
            o = dbg_out("dbg_small", [128, 64])
            sc.op('pool', lambda e: e.memset(small[:, 8:64], 0.0), writes=['small'])
            sc.dma('sp', lambda e: e.dma_start(out=o[:, :], in_=small[:]), 'dbgsmall', reads=['small'])
            o = dbg_out("dbg_cos", [128, NT, 8])
            sc.dma('sp', lambda e: e.dma_start(out=o[:, :, :], in_=cosT[:]), 'dbgcos', reads=['rot'])
        sc.barrier()

    n_aheads = 4 if stage >= 3.5 else 1
    sA = ExitStack()
    wa2 = [sb("waa%d" % k, [128, 8, 384], BF16, sA) for k in range(2)]
    for h in range(n_aheads):
        with ExitStack() as ph:
            wa_ = wa2[h % 2]
            WAK = ('waa', h % 2)
            qTa = sb("qTa", [128, 2, S], BF16, ph)
            kTa = sb("kTa", [128, S], BF16, ph)
            vxa = sb("vxa", [128, NT, 129], BF16, ph)
            xq = [sb("xq%d" % k, [128, 4, 4, 64], F32, ph) for k in range(2)]
            sq = sb("sq", [128, 4, 4, 64], F32, ph)
            rp = [sb("rp%d" % k, [128, 4, 4, 4, 8], F32, ph) for k in range(2)]
            st4 = sb("st4", [128, 2, 48], F32, ph)
            xr = [sb("xr%d" % k, [128, 4, 4, 64], BF16, ph) for k in range(2)]
            pt = [sb("pt%d" % k, [128, 2, 512], BF16, ph) for k in range(3)]
            o0 = sb("o0", [128, 4, 128], F32, ph)
            ob4 = [sb("ob4_%d" % k, [128, 4, 128], F32, ph) for k in range(2)]
            osq = sb("osq", [128, 4, 128], F32, ph)
            st5 = sb("st5", [128, 2, 24], F32, ph)
            ymt4 = [sb("ymt4_%d" % k, [128, 4, 128], BF16, ph) for k in range(2)]
            yst = [sb("ysta%d" % k, [128, 512], BF16, ph) for k in range(2)]
            for hh in ([0, 1] if h == 0 else [h + 1]):
                if hh >= n_aheads:
                    continue
                for k, c0 in enumerate((2064 + hh * 128, 2576 + hh * 128, 3088 + hh * 128)):
                    sc.dma('pool', lambda e, k=k, c0=c0, hh=hh: e.dma_start(out=wa2[hh % 2][:, :, k * 128:(k + 1) * 128], in_=win_v[:, :, c0:c0 + 128]),
                           ('waa', hh % 2), writes=[('waa', hh % 2)])
            sc.op('pool', lambda e: e.memset(vxa[:, :, 128:129], 1.0), writes=['vxa1'])
            sc.op('pool', lambda e: e.memset(qTa[64:128, 0, :], 0.0), writes=['qTa'])
            sc.op('pool', lambda e: e.memset(qTa[0:64, 1, :], 0.0), writes=['qTa'])
            for g in range(NG):
                b = g % 2
                banks = [('ps', k) for k in range(4)]

                def mmp(e, g=g):
                    for t in range(4):
                        i = 4 * g + t
                        for kc in range(8):
                            r = e.matmul(ps[:, t, 0:384], lhsT=h1T[:, kc, i * 128:(i + 1) * 128], rhs=wa_[:, kc, :], start=(kc == 0), stop=(kc == 7))
                    return r
                sc.op('pe', mmp, reads=[WAK] + [('h1T', 4 * g + t) for t in range(4)], writes=banks)
                xk, rk, sk, xrk = ('xq', b), ('rp', b), ('st4', b), ('xr', b)
                X = xq[b]
                X3 = X[:].rearrange("p t a d -> p t (a d)")
                X16 = X[:].rearrange("p t a d -> p (t a) d")
                sc.op('act', lambda e, X3=X3: e.copy(out=X3, in_=ps[:, 0:4, 0:256]), reads=banks, writes=[xk])
                sc.op('act', lambda e, g=g: e.copy(out=vxa[:, 4 * g:4 * g + 4, 0:128], in_=ps[:, 0:4, 256:384]), reads=banks,
                      writes=[('vxa', 4 * g + t) for t in range(4)])
                sc.op('dve', lambda e, X=X: e.tensor_tensor(out=sq[:], in0=X[:], in1=X[:], op=ALU.mult), reads=[xk], writes=['sq'])
                T4 = st4[:, b, :]
                sc.op('dve', lambda e, T4=T4: e.tensor_reduce(out=T4[:, 0:16], in_=sq[:].rearrange("p t a d -> p (t a) d"), axis=AX.X, op=ALU.add),
                      reads=['sq'], writes=[sk])
                sc.op('act', lambda e, T4=T4: e.activation(out=T4[:, 16:32], in_=T4[:, 0:16], func=AF.Sqrt, scale=1.0 / 64, bias=EPS), reads=[sk], writes=[sk])
                sc.op('dve', lambda e, T4=T4: e.reciprocal(out=T4[:, 32:48], in_=T4[:, 16:32]), reads=[sk], writes=[sk])
                sc.op('dve', lambda e, X16=X16, T4=T4: e.tensor_tensor(out=X16, in0=X16, in1=T4[:, 32:48].unsqueeze(2).broadcast_to([128, 16, 64]), op=ALU.mult),
                      reads=[xk, sk], writes=[xk])
                sc.op('dve', lambda e, X=X: e.tensor_tensor(out=X[:], in0=X[:], in1=qkg4[:].unsqueeze(1).broadcast_to([128, 4, 4, 64]), op=ALU.mult),
                      reads=[xk, 'qkg4'], writes=[xk])
                cs = cosT[:, 4 * g:4 * g + 4, :].unsqueeze(2).broadcast_to([128, 4, 4, 8])
                sn = sinT[:, 4 * g:4 * g + 4, :].unsqueeze(2).broadcast_to([128, 4, 4, 8])
                R = rp[b]
                for k, (tt, tr_) in enumerate(((X[:, :, :, 0:8], cs), (X[:, :, :, 8:16], sn), (X[:, :, :, 8:16], cs), (X[:, :, :, 0:8], sn))):
                    sc.op('pool', lambda e, k=k, tt=tt, tr_=tr_, R=R: e.tensor_tensor(out=R[:, k, :, :, :], in0=tt, in1=tr_, op=ALU.mult),
                          reads=[xk, 'rot'], writes=[rk])
                XR = xr[b]
                sc.op('act', lambda e, XR=XR, X=X: e.copy(out=XR[:], in_=X[:]), reads=[xk], writes=[xrk])
                sc.op('dve', lambda e, XR=XR, R=R: e.tensor_tensor(out=XR[:, :, :, 0:8], in0=R[:, 0, :, :, :], in1=R[:, 1, :, :, :], op=ALU.subtract),
                      reads=[rk, xrk], writes=[xrk])
                sc.op('dve', lambda e, XR=XR, R=R: e.tensor_tensor(out=XR[:, :, :, 8:16], in0=R[:, 2, :, :, :], in1=R[:, 3, :, :, :], op=ALU.add),
                      reads=[rk, xrk], writes=[xrk])
                tbanks = [('ps', 4), ('ps', 5)]
                pbq = ps[:, 4, :].bitcast(BF16)
                pbk = ps[:, 5, :].bitcast(BF16)
                XRf = XR[:].rearrange("p t a d -> p t (a d)")

                def trq(e, XRf=XRf, pbq=pbq, pbk=pbk):
                    for t in range(4):
                        e.transpose(out=pbq[:, t * 128:(t + 1) * 128], in_=XRf[:, t, 0:128], identity=identb[:])
                        r = e.transpose(out=pbk[:, t * 128:(t + 1) * 128], in_=XRf[:, t, 128:256], identity=identb[:])
                    return r
                sc.op('pe', trq, reads=[xrk, 'identb'], writes=tbanks)
                sc.op('act', lambda e, pbq=pbq, g=g: e.copy(out=qTa[0:64, 0, g * 512:(g + 1) * 512], in_=pbq[0:64, 0:512]), reads=tbanks, writes=['qTa'])
                sc.op('act', lambda e, pbq=pbq, g=g: e.copy(out=qTa[64:128, 1, g * 512:(g + 1) * 512], in_=pbq[64:128, 0:512]), reads=tbanks, writes=['qTa'])
                sc.op('act', lambda e, pbk=pbk, g=g: e.copy(out=kTa[:, g * 512:(g + 1) * 512], in_=pbk[:, 0:512]), reads=tbanks, writes=['kTa'])
            if debug and h == 0:
                o = dbg_out("dbg_qTa", [128, S], BF16)
                sc.dma('sp', lambda e: e.dma_start(out=o[0:64, :], in_=qTa[0:64, 0, :]), 'dbgqTa', reads=['qTa'])
                sc.dma('sp', lambda e: e.dma_start(out=o[64:128, :], in_=qTa[64:128, 1, :]), 'dbgqTa', reads=['qTa'])
                o = dbg_out("dbg_kTa", [128, S], BF16)
                sc.dma('sp', lambda e: e.dma_start(out=o[:, :], in_=kTa[:]), 'dbgkTa', reads=['kTa'])
            nqb = 8 if stage >= 3.2 else 1
            for qb in range(nqb):
                for p in range(2):
                    pr = slice(64 * p, 64 * p + 64)
                    def st_mm(j):
                        pb0 = 4 + 2 * (j % 2)

                        def f(e, j=j, pb0=pb0):
                            for u in range(2):
                                kt = 2 * j + u
                                r = e.matmul(ps[:, pb0 + u, :], lhsT=kTa[:, kt * 128:(kt + 1) * 128], rhs=qTa[:, p, qb * 512:(qb + 1) * 512],
                                             start=True, stop=True)
                            return r
                        sc.op('pe', f, reads=['qTa', 'kTa'], writes=[('ps', pb0), ('ps', pb0 + 1)])
                    st_mm(0)
                    for j in range(NT // 2):
                        pb0 = 4 + 2 * (j % 2)
                        sbanks = [('ps', pb0), ('ps', pb0 + 1)]
                        if j + 1 < NT // 2:
                            st_mm(j + 1)
                        ptk = ('pt', j % 3)
                        ptb = pt[j % 3]
                        sc.op('act', lambda e, pb0=pb0, ptb=ptb: e.activation(out=ptb[:], in_=ps[:, pb0:pb0 + 2, :], func=AF.Exp, scale=0.125,
                                                                              bias=small[:, SM_NB:SM_NB + 1]),
                              reads=sbanks + ['small'], writes=[ptk])

                        def pv(e, ptb=ptb, j=j):
                            for u in range(2):
                                kt = 2 * j + u
                                for qs in range(4):
                                    r = e.matmul(ps[:, qs, 0:129], lhsT=ptb[:, u, qs * 128:(qs + 1) * 128], rhs=vxa[:, kt, :],
                                                 start=(kt == 0), stop=(kt == NT - 1))
                            return r
                        sc.op('pe', pv, reads=[ptk, ('vxa', 2 * j), ('vxa', 2 * j + 1), 'vxa1'], writes=[('ps', 0), ('ps', 1), ('ps', 2), ('ps', 3)])
                    abanks = [('ps', k) for k in range(4)]
                    acc4 = ps[:, 0:4, 0:129]
                    T5 = st5[:, p, :]
                    tk5 = ('st5', p)
                    sc.op('dve', lambda e, T5=T5, acc4=acc4: e.reciprocal(out=T5[:, 0:4], in_=acc4[:, :, 128]), reads=abanks, writes=[tk5])
                    if p == 0:
                        sc.op('dve', lambda e, T5=T5, acc4=acc4: e.tensor_tensor(out=o0[:], in0=acc4[:, :, 0:128],
                                                                                  in1=T5[:, 0:4].unsqueeze(2).broadcast_to([128, 4, 128]), op=ALU.mult),
                              reads=abanks + [tk5], writes=['o0'])
                        continue
                    O = ob4[qb % 2]
                    okk = ('ob4', qb % 2)
                    sc.op('dve', lambda e, T5=T5: e.tensor_scalar(out=T5[:, 4:8], in0=T5[:, 0:4], scalar1=small[:, SM_NLAM:SM_NLAM + 1], scalar2=None, op0=ALU.mult),
                          reads=[tk5, 'small'], writes=[tk5])
                    sc.op('dve', lambda e, T5=T5, acc4=acc4, O=O: e.tensor_tensor(out=O[:], in0=acc4[:, :, 0:128],
                                                                                   in1=T5[:, 4:8].unsqueeze(2).broadcast_to([128, 4, 128]), op=ALU.mult),
                          reads=abanks + [tk5], writes=[okk])
                    sc.op('dve', lambda e, O=O: e.tensor_tensor(out=O[:], in0=O[:], in1=o0[:], op=ALU.add), reads=[okk, 'o0'], writes=[okk])
                    sc.op('pool', lambda e, O=O: e.tensor_tensor(out=osq[:], in0=O[:], in1=O[:], op=ALU.mult), reads=[okk], writes=['osq'])
                    sc.op('dve', lambda e, T5=T5: e.tensor_reduce(out=T5[:, 8:12], in_=osq[:], axis=AX.X, op=ALU.add), reads=['osq'], writes=[tk5])
                    sc.op('act', lambda e, T5=T5: e.activation(out=T5[:, 12:16], in_=T5[:, 8:12], func=AF.Sqrt, scale=1.0 / 128, bias=EPS), reads=[tk5], writes=[tk5])
                    sc.op('dve', lambda e, T5=T5: e.reciprocal(out=T5[:, 16:20], in_=T5[:, 12:16]), reads=[tk5], writes=[tk5])
                    sc.op('dve', lambda e, T5=T5, O=O: e.tensor_tensor(out=O[:], in0=O[:], in1=T5[:, 16:20].unsqueeze(2).broadcast_to([128, 4, 128]), op=ALU.mult),
                          reads=[okk, tk5], writes=[okk])
                    ym = ymt4[qb % 2]
                    ymk = ('ymt4', qb % 2)
                    sc.op('pool', lambda e, O=O, ym=ym: e.tensor_tensor(out=ym[:], in0=O[:], in1=subg8[:].unsqueeze(1).broadcast_to([128, 4, 128]), op=ALU.mult),
                          reads=[okk, 'subg8'], writes=[ymk])
                    tbank = ('ps', 7)
                    pb = ps[:, 7, :].bitcast(BF16)

                    def ytr(e, ym=ym, pb=pb):
                        for t in range(4):
                            r = e.transpose(out=pb[:, t * 128:(t + 1) * 128], in_=ym[:, t, :], identity=identb[:])
                        return r
                    sc.op('pe', ytr, reads=[ymk, 'identb'], writes=[tbank])
                    ys = yst[qb % 2]
                    ysk = ('ysta', qb % 2)
                    sc.op('act', lambda e, ys=ys, pb=pb: e.copy(out=ys[:], in_=pb[:, 0:512]), reads=[tbank], writes=[ysk])
                    sc.dma('sp', lambda e, ys=ys, qb=qb: e.dma_start(out=yT_v[:, 4 + h, qb * 512:(qb + 1) * 512], in_=ys[:]), ysk, reads=[ysk], writes=['yT_d'])
            sc.barrier()
    sA.close()

    if stage <= 3.5:
        sc.finish()
        return nc, dbg

    s1.close()

    sW = ExitStack()
    aff = sb("aff", [128, NT, NE], F32, sW)
    g2rep = sb("g2rep", [128, D], F32, sW)
    with ExitStack() as pw:
        woutb = sb("woutb", [128, 8, D], BF16, pw)
        wrt = sb("wrt", [128, 8, NE], F32, pw)
        ytl = [sb("ytl%d" % k, [128, 8, 512], BF16, pw) for k in range(2)]
        xt2 = [sb("xt2_%d" % k, [128, D], F32, pw) for k in range(2)]
        tmpw = [sb("tmpw%d" % k, [128, D], F32, pw) for k in range(2)]
        x1t = [sb("x1t%d" % k, [128, D], F32, pw) for k in range(2)]
        h2f = [sb("h2f%d" % k, [128, D], F32, pw) for k in range(2)]
        h2b = [sb("h2b%d" % k, [128, D], BF16, pw) for k in range(2)]
        h2T = [sb("h2T%d" % k, [128, 8, 128], F32, pw) for k in range(2)]
        junkw = sb("junkw", [128, D], BF16, pw)
        stw = sb("stw", [128, NT, 8], F32, pw)
        esm = sb("esm", [128, 2, NE], F32, pw)
        wout_v = wout_d.rearrange("(kc p) n -> p kc n", p=128)
        sc.dma('pool', lambda e: e.dma_start(out=woutb[:, 0:4, :], in_=wout_v[:, 0:4, :]), 'woutb', writes=['woutb'])
        sc.dma('pool', lambda e: e.dma_start(out=woutb[:, 4:8, :], in_=wout_v[:, 4:8, :]), 'woutb', writes=['woutb'])
        sc.dma('sp', lambda e: e.dma_start(out=wrt[:], in_=wr_d.rearrange("(kc p) n -> p kc n", p=128)), 'wrt', writes=['wrt'])
        modW = sb("modW", [128, 3 * D], F32, pw)
        sc.dma('sp', lambda e: e.dma_start(out=modW[:], in_=modrow_d[0:1, 2 * D:5 * D].partition_broadcast(128)), 'modW', reads=['modrow_d'], writes=['mod'])
        sc.dma('sp', lambda e: e.dma_start(out=g2rep[:], in_=modrow_d[0:1, 5 * D:6 * D].partition_broadcast(128)), 'g2rep', reads=['modrow_d'], writes=['g2rep'])
        GATE1, SHIFT2, GS2 = modW[:, 0:D], modW[:, D:2 * D], modW[:, 2 * D:3 * D]
        for i in range(NT):
            b = i % 2
            g, k4 = i // 4, i % 4
            yk = ('ytl', g % 2)
            if k4 == 0:
                sc.dma('sp', lambda e, g=g: e.dma_start(out=ytl[g % 2][:], in_=yT_v[:, :, g * 512:(g + 1) * 512]), yk, writes=[yk])
            xk = ('xt2', b)
            sc.dma('sp', lambda e, i=i, b=b: e.dma_start(out=xt2[b][:], in_=x_d[i * 128:(i + 1) * 128, :]), xk, writes=[xk])
            banks = [('ps', 2 * b), ('ps', 2 * b + 1)]

            def mmw(e, i=i, b=b, g=g, k4=k4):
                for dh in range(2):
                    for c in range(8):
                        r = e.matmul(ps[:, 2 * b + dh, :], lhsT=ytl[g % 2][:, c, k4 * 128:(k4 + 1) * 128], rhs=woutb[:, c, dh * 512:(dh + 1) * 512],
                                     start=(c == 0), stop=(c == 7))
                return r
            sc.op('pe', mmw, reads=[yk, 'woutb'], writes=banks)
            mixv = ps[:, 2 * b:2 * b + 2, :].rearrange("p a n -> p (a n)")
            sc.op('dve', lambda e, b=b, mixv=mixv: e.tensor_tensor(out=tmpw[b][:], in0=mixv, in1=GATE1, op=ALU.mult), reads=banks + ['mod'], writes=[('tmpw', b)])
            sc.op('dve', lambda e, b=b: e.tensor_tensor(out=x1t[b][:], in0=tmpw[b][:], in1=xt2[b][:], op=ALU.add),
                  reads=[('tmpw', b), xk], writes=[('x1t', b)])
            sc.dma('sp', lambda e, i=i, b=b: e.dma_start(out=out_d[i * 128:(i + 1) * 128, :], in_=x1t[b][:]), ('x1s', b), reads=[('x1t', b)], writes=[('outd', i)])
            T = stw[:, i, :]
            tk = ('stw', i)
            sc.op('act', lambda e, b=b, T=T: e.activation(out=junkw[:], in_=x1t[b][:], func=AF.Square, accum_out=T[:, 0:1]), reads=[('x1t', b)], writes=['junkw', tk])
            sc.op('act', lambda e, T=T: e.activation(out=T[:, 1:2], in_=T[:, 0:1], func=AF.Sqrt, scale=1.0 / D, bias=EPS), reads=[tk], writes=[tk])
            sc.op('dve', lambda e, T=T: e.reciprocal(out=T[:, 2:3], in_=T[:, 1:2]), reads=[tk], writes=[tk])
            sc.op('dve', lambda e, b=b, T=T: e.scalar_tensor_tensor(out=tmpw[b][:], in0=x1t[b][:], scalar=T[:, 2:3], in1=GS2, op0=ALU.mult, op1=ALU.mult),
                  reads=[('x1t', b), tk, 'mod'], writes=[('tmpw', b)])
            sc.op('dve', lambda e, b=b: e.tensor_tensor(out=h2f[b][:], in0=tmpw[b][:], in1=SHIFT2, op=ALU.add), reads=[('tmpw', b), 'mod'], writes=[('h2f', b)])
            sc.op('act', lambda e, b=b: e.copy(out=h2b[b][:], in_=h2f[b][:]), reads=[('h2f', b)], writes=[('h2b', b)])
            sc.dma('sp', lambda e, i=i, b=b: e.dma_start(out=h2_d[i * 128:(i + 1) * 128, :], in_=h2b[b][:]), ('h2s', b), reads=[('h2b', b)], writes=[('h2d', i)])
            tb = [('ps', 4), ('ps', 5)]

            def trh(e, b=b):
                for kc in range(8):
                    r = e.transpose(out=ps[:, 4 + kc // 4, (kc % 4) * 128:(kc % 4 + 1) * 128], in_=h2f[b][:, kc * 128:(kc + 1) * 128], identity=ident)
                return r
            sc.op('pe', trh, reads=[('h2f', b), 'consts'], writes=tb)
            sc.op('act', lambda e, b=b: e.copy(out=h2T[b][:].rearrange("p k t -> p (k t)"), in_=ps[:, 4:6, :].rearrange("p a n -> p (a n)")),
                  reads=tb, writes=[('h2T', b)])
            lb = ('ps', 6 + b)

            def mml(e, b=b):
                for kc in range(8):
                    r = e.matmul(ps[:, 6 + b, 0:NE], lhsT=h2T[b][:, kc, :], rhs=wrt[:, kc, :], start=(kc == 0), stop=(kc == 7))
                return r
            sc.op('pe', mml, reads=[('h2T', b), 'wrt'], writes=[lb])
            sc.op('dve', lambda e, b=b, T=T: e.tensor_reduce(out=T[:, 3:4], in_=ps[:, 6 + b, 0:NE], axis=AX.X, op=ALU.max), reads=[lb], writes=[tk])
            sc.op('dve', lambda e, T=T: e.tensor_scalar(out=T[:, 4:5], in0=T[:, 3:4], scalar1=-1.0, scalar2=None, op0=ALU.mult), reads=[tk], writes=[tk])
            sc.op('act', lambda e, b=b, T=T: e.activation(out=esm[:, b, :], in_=ps[:, 6 + b, 0:NE], func=AF.Exp, bias=T[:, 4:5], accum_out=T[:, 5:6]),
                  reads=[lb, tk], writes=[('esm', b), tk])
            sc.op('dve', lambda e, T=T: e.reciprocal(out=T[:, 6:7], in_=T[:, 5:6]), reads=[tk], writes=[tk])
            sc.op('dve', lambda e, b=b, i=i, T=T: e.tensor_scalar(out=aff[:, i, :], in0=esm[:, b, :], scalar1=T[:, 6:7], scalar2=None, op0=ALU.mult),
                  reads=[('esm', b), tk], writes=['aff'])
        if debug:
            o = dbg_out("dbg_aff", [128, NT, NE])
            sc.dma('sp', lambda e: e.dma_start(out=o[:, :, :], in_=aff[:]), 'dbgaff', reads=['aff'])
        sc.barrier()

    if stage <= 4:
        sc.finish()
        return nc, dbg

    rank_tok = sb("rank_tok", [128, NT, NE], F32, sW)
    R5 = sb("R5", [128, NT, NE, 5], BF16, sW)
    with ExitStack() as pr:
        affT = sb("affT", [NE, S], F32, pr)
        junkR = sb("junkR", [NE, S], F32, pr)
        mkT = sb("mkT", [NE, S], F32, pr)
        csT = sb("csT", [NE, S], F32, pr)
        bs = sb("bs", [NE, 4], F32, pr)
        r1 = sb("r1", [128, NT, NE], F32, pr)
        for rnd in range(2):
            banks = [('ps', k) for k in range(4)]

            def tra(e, rnd=rnd):
                for k in range(16):
                    i = rnd * 16 + k
                    r = e.transpose(out=ps[0:NE, k // 4, (k % 4) * 128:(k % 4 + 1) * 128], in_=aff[:, i, :], identity=ident)
                return r
            sc.op('pe', tra, reads=['aff', 'consts'], writes=banks)
            sc.op('act', lambda e, rnd=rnd: e.copy(out=affT[:, rnd * 2048:(rnd + 1) * 2048], in_=ps[0:NE, 0:4, :].rearrange("p a n -> p (a n)")),
                  reads=banks, writes=['affT'])
        sc.op('dve', lambda e: e.memset(bs[:, 0:1], 0.0), writes=['bs'])
        for n in range(NBIS):
            w = 2.0 ** (-(n + 1))
            sc.op('dve', lambda e, w=w: e.tensor_scalar(out=bs[:, 1:2], in0=bs[:, 0:1], scalar1=w, scalar2=None, op0=ALU.add), reads=['bs'], writes=['bs'])
            sc.op('dve', lambda e: e.tensor_scalar(out=junkR[:], in0=affT[:], scalar1=bs[:, 1:2], scalar2=None, op0=ALU.is_gt, op1=ALU.add,
                                                   accum_out=bs[:, 2:3]), reads=['affT', 'bs'], writes=['junkR', 'bs'])
            sc.op('dve', lambda e: e.tensor_scalar(out=bs[:, 3:4], in0=bs[:, 2:3], scalar1=CAP - 0.5, scalar2=None, op0=ALU.is_gt), reads=['bs'], writes=['bs'])
            sc.op('dve', lambda e, w=w: e.scalar_tensor_tensor(out=bs[:, 0:1], in0=bs[:, 3:4], scalar=w, in1=bs[:, 0:1], op0=ALU.mult, op1=ALU.add),
                  reads=['bs'], writes=['bs'])
        sc.op('dve', lambda e: e.tensor_scalar(out=mkT[:], in0=affT[:], scalar1=bs[:, 0:1], scalar2=None, op0=ALU.is_gt), reads=['affT', 'bs'], writes=['mkT'])
        sc.op('pool', lambda e: e.memset(junkR[:], 1.0), reads=[], writes=['junkR'])
        sc.op('dve', lambda e: e.tensor_tensor_scan(out=csT[:], data0=junkR[:], data1=mkT[:], initial=0.0, op0=ALU.mult, op1=ALU.add),
              reads=['junkR', 'mkT'], writes=['csT'])
        sc.op('dve', lambda e: e.tensor_tensor(out=csT[:], in0=csT[:], in1=mkT[:], op=ALU.mult), reads=['csT', 'mkT'], writes=['csT'])
        sc.op('dve', lambda e: e.tensor_scalar(out=csT[:], in0=csT[:], scalar1=-1.0, scalar2=None, op0=ALU.add), reads=['csT'], writes=['csT'])
        rb = ('ps', 4)

        def trr(e):
            for i in range(NT):
                r = e.transpose(out=ps[:, 4, i * NE:(i + 1) * NE], in_=csT[:, i * 128:(i + 1) * 128], identity=consts[0:NE, C_IDENT:C_IDENT + NE])
            return r
        sc.op('pe', trr, reads=['csT', 'consts'], writes=[rb])
        sc.op('act', lambda e: e.copy(out=rank_tok[:].rearrange("p t e -> p (t e)"), in_=ps[:, 4, :]), reads=[rb], writes=['rank_tok'])
        sc.op('pool', lambda e: e.tensor_copy(out=R5[:, :, :, 0], in_=consts[:, C_TLO:C_TLO + 1].unsqueeze(2).broadcast_to([128, NT, NE])),
              reads=['consts'], writes=['R5'])
        sc.op('pool', lambda e: e.tensor_copy(out=R5[:, :, :, 1], in_=consts[:, C_THI:C_THI + NT].unsqueeze(2).broadcast_to([128, NT, NE])),
              reads=['consts'], writes=['R5'])
        sc.op('dve', lambda e: e.tensor_copy(out=R5[:, :, :, 2], in_=aff[:]), reads=['aff'], writes=['R5'])
        sc.op('dve', lambda e: e.tensor_tensor(out=r1[:], in0=aff[:], in1=R5[:, :, :, 2], op=ALU.subtract), reads=['aff', 'R5'], writes=['r1'])
        sc.op('dve', lambda e: e.tensor_copy(out=R5[:, :, :, 3], in_=r1[:]), reads=['r1'], writes=['R5'])
        sc.op('dve', lambda e: e.tensor_tensor(out=r1[:], in0=r1[:], in1=R5[:, :, :, 3], op=ALU.subtract), reads=['r1', 'R5'], writes=['r1'])
        sc.op('dve', lambda e: e.tensor_copy(out=R5[:, :, :, 4], in_=r1[:]), reads=['r1'], writes=['R5'])
        if debug:
            o = dbg_out("dbg_rank", [128, NT, NE])
            sc.dma('sp', lambda e: e.dma_start(out=o[:, :, :], in_=rank_tok[:]), 'dbgrank', reads=['rank_tok'])
            o = dbg_out("dbg_thr", [NE, 4])
            sc.dma('sp', lambda e: e.dma_start(out=o[:, :], in_=bs[:]), 'dbgthr', reads=['bs'])
        sc.barrier()

    if stage <= 4.5:
        sc.finish()
        return nc, dbg

    n_exp = NE if stage >= 6 else int(round((stage - 5) * 10)) + 1
    with ExitStack() as pe_:
        NSLOT = 6
        ring = [sb("ring%d" % k, [128, 8, D], BF16, pe_) for k in range(NSLOT)]
        Pm = sb("Pm", [128, NT, 512], BF16, pe_)
        xe = sb("xe", [128, 4, D], BF16, pe_)
        xeT2 = [sb("xeT%d" % k, [128, 8, 512], BF16, pe_) for k in range(2)]
        aT = sb("aT", [128, 16, 512], BF16, pe_)
        sgt = [sb("sgt%d" % k, [128, 512], BF16, pe_) for k in range(2)]
        yv = sb("yv", [128, 4, D], F32, pe_)
        idf = sb("idf", [128, 2, 8], F32, pe_)
        idp = sb("idp", [128, 2, 32], F32, pe_)
        idxi = [sb("idxi%d" % k, [128, 4], I32, pe_) for k in range(2)]
        wg_v = wg_d.rearrange("(e kc p) n -> p e kc n", p=128, kc=8)
        wu_v = wu_d.rearrange("(e kc p) n -> p e kc n", p=128, kc=8)
        wd_v = wd_d.rearrange("(e fh fc p) n -> p e fh fc n", p=128, fc=8, fh=2)

        def load_piece(e_, k):
            slot = ring[k]
            key = ('ring', k)
            if k < 4:
                src = (wg_v if k % 2 == 0 else wu_v)[:, e_, :, (k // 2) * D:(k // 2 + 1) * D]
            else:
                src = wd_v[:, e_, k - 4, :, :]
            sc.dma('pool', lambda e, slot=slot, src=src: e.dma_start(out=slot[:], in_=src), key, writes=[key])

        def prep_a(e_):
            for i in range(NT):
                sc.op('dve', lambda e, i=i: e.tensor_scalar(out=Pm[:, i, :], in0=consts[:, C_IOTA:C_IOTA + 512], scalar1=rank_tok[:, i, e_:e_ + 1],
                                                            scalar2=None, op0=ALU.is_equal), reads=['consts', 'rank_tok'], writes=['Pm'])

        def prep_b(e_):
            b = e_ % 2
            ibank = ('ps', 7)

            def imm(e):
                for cc in range(4):
                    for i in range(NT):
                        r = e.matmul(ps[:, 7, cc * 8:cc * 8 + 5], lhsT=Pm[:, i, cc * 128:(cc + 1) * 128], rhs=R5[:, i, e_, :],
                                     start=(i == 0), stop=(i == NT - 1))
                return r
            sc.op('pe', imm, reads=['Pm', 'R5'], writes=[ibank])
            sc.op('act', lambda e, b=b: e.copy(out=idp[:, b, :].rearrange("p (c k) -> p c k", k=8)[:, :, 0:5], in_=ps[:, 7, 0:32].rearrange("p (c k) -> p c k", k=8)[:, :, 0:5]), reads=[ibank], writes=[('idp', b)])
            ibank = ('idp', b)
            I3 = idp[:, b, :].rearrange("p (c k) -> p c k", k=8)
            F_ = idf[:, b, :]
            fk = ('idf', b)
            sc.op('dve', lambda e, F_=F_, I3=I3: e.scalar_tensor_tensor(out=F_[:, 0:4].unsqueeze(2), in0=I3[:, :, 1:2], scalar=128.0, in1=I3[:, :, 0:1],
                                                                      op0=ALU.mult, op1=ALU.add), reads=[ibank], writes=[fk])
            sc.op('dve', lambda e, F_=F_, I3=I3: e.tensor_reduce(out=F_[:, 4:8], in_=I3[:, :, 2:5], axis=AX.X, op=ALU.add), reads=[ibank], writes=[fk])
            ik = ('idxi', b)
            sc.op('dve', lambda e, F_=F_, b=b: e.tensor_copy(out=idxi[b][:], in_=F_[:, 0:4]), reads=[fk], writes=[ik])
            for cc in range(4):
                sc.dma('pool', lambda e, cc=cc, b=b: e.indirect_dma_start(
                    out=xe[:, cc, :], out_offset=None, in_=h2_d[:, :],
                    in_offset=bass.IndirectOffsetOnAxis(ap=idxi[b][:, cc:cc + 1], axis=0)), 'xe', reads=[ik], writes=['xe'])

        def prep_c(e_):
            xeT = xeT2[e_ % 2]
            for kc in range(8):
                tb = 6
                tbank = ('ps', tb)
                pb = ps[:, tb, :].bitcast(BF16)

                def trx(e, kc=kc, pb=pb):
                    for cc in range(4):
                        r = e.transpose(out=pb[:, cc * 128:(cc + 1) * 128], in_=xe[:, cc, kc * 128:(kc + 1) * 128], identity=identb[:])
                    return r
                sc.op('pe', trx, reads=['xe', 'identb'], writes=[tbank])
                sc.op('act', lambda e, kc=kc, pb=pb, xeT=xeT: e.copy(out=xeT[:, kc, :], in_=pb[:, 0:512]), reads=[tbank], writes=[('xeT', e_ % 2)])

        for k in range(NSLOT):
            load_piece(0, k)
        prep_a(0)
        prep_b(0)
        prep_c(0)
        for e_ in range(n_exp):
            b = e_ % 2
            xeT = xeT2[b]
            xk_ = ('xeT', b)
            if e_ + 1 < n_exp:
                prep_a(e_ + 1)
            for fh in range(2):
                gk, uk = ('ring', 2 * fh), ('ring', 2 * fh + 1)
                Wg, Wu = ring[2 * fh], ring[2 * fh + 1]
                for fo in range(8):
                    gb, ubk = fo % 2, 2 + (fo % 2)

                    def mmg(e, Wg=Wg, fo=fo, gb=gb):
                        for kc in range(8):
                            r = e.matmul(ps[:, gb, :], lhsT=Wg[:, kc, fo * 128:(fo + 1) * 128], rhs=xeT[:, kc, :], start=(kc == 0), stop=(kc == 7))
                        return r

                    def mmu(e, Wu=Wu, fo=fo, ubk=ubk):
                        for kc in range(8):
                            r = e.matmul(ps[:, ubk, :], lhsT=Wu[:, kc, fo * 128:(fo + 1) * 128], rhs=xeT[:, kc, :], start=(kc == 0), stop=(kc == 7))
                        return r
                    sc.op('pe', mmg, reads=[gk, xk_], writes=[('ps', gb)])
                    sc.op('pe', mmu, reads=[uk, xk_], writes=[('ps', ubk)])
                    sg = sgt[fo % 2]
                    sk = ('sgt', fo % 2)
                    sc.op('act', lambda e, sg=sg, gb=gb: e.activation(out=sg[:], in_=ps[:, gb, :], func=AF.Silu), reads=[('ps', gb)], writes=[sk])
                    fidx = fh * 8 + fo
                    sc.op('dve', lambda e, sg=sg, ubk=ubk, fidx=fidx: e.tensor_tensor(out=aT[:, fidx, :], in0=ps[:, ubk, :], in1=sg[:], op=ALU.mult),
                          reads=[('ps', ubk), sk], writes=[('aT', fidx)])
                if e_ + 1 < n_exp:
                    load_piece(e_ + 1, 2 * fh)
                    load_piece(e_ + 1, 2 * fh + 1)
                    if fh == 0:
                        prep_b(e_ + 1)
                    else:
                        prep_c(e_ + 1)
            F_ = idf[:, b, :]
            for cc in range(4):
                for dh in range(2):
                    db = 4 + ((cc * 2 + dh) % 2)

                    def mmd(e, cc=cc, dh=dh, db=db):
                        for fc in range(16):
                            r = e.matmul(ps[:, db, :], lhsT=aT[:, fc, cc * 128:(cc + 1) * 128], rhs=ring[4 + fc // 8][:, fc % 8, dh * 512:(dh + 1) * 512],
                                         start=(fc == 0), stop=(fc == 15))
                        return r
                    sc.op('pe', mmd, reads=[('aT', f) for f in range(16)] + [('ring', 4), ('ring', 5)], writes=[('ps', db)])
                    sc.op('dve', lambda e, cc=cc, dh=dh, db=db, F_=F_: e.scalar_tensor_tensor(
                        out=yv[:, cc, dh * 512:(dh + 1) * 512], in0=ps[:, db, :], scalar=F_[:, 4 + cc:5 + cc], in1=g2rep[:, dh * 512:(dh + 1) * 512],
                        op0=ALU.mult, op1=ALU.mult), reads=[('ps', db), ('idf', b), 'g2rep'], writes=[('yv', cc)])
            if e_ + 1 < n_exp:
                load_piece(e_ + 1, 4)
                load_piece(e_ + 1, 5)
            for cc in range(4):
                sc.dma('pool', lambda e, cc=cc, b=b: e.indirect_dma_start(
                    out=out_d[:, :], out_offset=bass.IndirectOffsetOnAxis(ap=idxi[b][:, cc:cc + 1], axis=0),
                    in_=yv[:, cc, :], in_offset=None, compute_op=ALU.add), 'scat', reads=[('yv', cc), ('idxi', b)], writes=['outd'])
        sc.barrier()
    sW.close()
    sc.finish()
    return nc, dbg


def prep_inputs(inputs, b):
    f = lambda a: np.ascontiguousarray(a, dtype=np.float32)
    m = {
        "x": f(inputs["x"][b]),
        "cT": f(inputs["c"][b].reshape(8, 128).T),
        "pos": np.ascontiguousarray(inputs["positions"][b].reshape(NT, 128).T.astype(np.int32)),
        "norm1_g": f(inputs["norm1_g"][0:1]),
        "norm2_g": f(inputs["norm2_g"][0:1]),
        "w_ada": f(inputs["w_ada"][0]),
        "b_ada": f(inputs["b_ada"][0:1]),
        "w_in": f(inputs["w_in"][0]),
        "convT": f(inputs["mlstm_conv_w"][0].T.reshape(8, 128, 5).transpose(1, 0, 2).reshape(128, 40)),
        "gate_b": f(inputs["mlstm_gate_b"][0:1]),
        "mnorm_g": f(inputs["mlstm_norm_g"][0:1]),
        "qk_g": f(inputs["diff_qk_g"][0].reshape(1, 128)),
        "lam": f(inputs["diff_lambda"][0].reshape(1, 256)),
        "subln_g": f(inputs["diff_subln_g"][0:1]),
        "w_out": f(inputs["w_out"][0]),
        "w_router": f(inputs["w_router"][0]),
        "consts": make_consts(),
    }
    return m


def kernel(**inputs):
    nc, _ = build()
    shared = None
    in_maps = []
    for b in range(8):
        m = prep_inputs(inputs, b)
        if shared is None:
            shared = {
                "w_gate_e": np.ascontiguousarray(inputs["w_gate_e"][0].reshape(NE * D, 2 * D), dtype=np.float32),
                "w_up_e": np.ascontiguousarray(inputs["w_up_e"][0].reshape(NE * D, 2 * D), dtype=np.float32),
                "w_down_e": np.ascontiguousarray(inputs["w_down_e"][0].reshape(NE * 2 * D, D), dtype=np.float32),
            }
        m.update(shared)
        in_maps.append(m)
    res = run_bass_kernel_spmd(nc, in_maps, core_ids=list(range(8)))
    return np.stack([np.asarray(r["out"]) for r in res.results], axis=0).astype(np.float32)
```

```python
import math
from contextlib import ExitStack

import numpy as np
import concourse.bass as bass
import concourse.mybir as mybir
from concourse.bass_utils import run_bass_kernel_spmd

F32 = mybir.dt.float32
BF16 = mybir.dt.bfloat16
I32 = mybir.dt.int32
AF = mybir.ActivationFunctionType
ALU = mybir.AluOpType
AX = mybir.AxisListType

S = 4096
D = 1024
NT = 32
NG = 8
EPS = 1e-6
D_IN = 3600
NE = 16
CAP = 512
LAM_INIT = 0.8 - 0.6 * math.exp(-0.3 * 0)
NBIS = 27

SAME_ENG_SYNC = True

C_IDENT = 0
C_TL = 128
C_TU = 256
C_TLS = 384
C_TUS = 512
C_INDA = 640
C_INDB = 768
C_IOTA = 896
C_TLO = 1408
C_FREQ = 1409
C_THI = 1417
NCONST = 1449


def make_consts():
    c = np.zeros((128, NCONST), np.float32)
    p = np.arange(128)
    s = p[:, None]
    j = p[None, :]
    same = (s // 64) == (j // 64)
    c[:, C_IDENT:C_IDENT + 128] = (s == j)
    c[:, C_TL:C_TL + 128] = same & (s <= j)
    c[:, C_TU:C_TU + 128] = same & (s >= j)
    c[:, C_TLS:C_TLS + 128] = same & (s < j)
    c[:, C_TUS:C_TUS + 128] = same & (s > j)
    c[:, C_INDA:C_INDA + 128] = (s < 64) & (j >= 0)
    c[:, C_INDB:C_INDB + 128] = (s >= 64) & (j >= 0)
    c[:, C_IOTA:C_IOTA + 512] = np.arange(512)[None, :]
    c[:, C_TLO] = p
    inv_freq = (500000.0 ** (-np.arange(0, 16, 2, dtype=np.float32) / 16)).astype(np.float32)
    c[:, C_FREQ:C_FREQ + 8] = (inv_freq.astype(np.float64) / (2 * np.pi)).astype(np.float32)[None, :]
    c[:, C_THI:C_THI + 32] = np.arange(32)[None, :]
    return c


class Sched:
    def __init__(self, nc, es):
        self.nc = nc
        self.es = es
        self.E = {'pe': nc.tensor, 'act': nc.scalar, 'dve': nc.vector, 'pool': nc.gpsimd, 'sp': nc.sync}
        self.sem = {k: es.enter_context(nc.semaphore('s_' + k)) for k in self.E}
        self.cnt = {k: 0 for k in self.E}
        self.seen = {k: {} for k in self.E}
        self.reg = {}
        self.dsem = {}
        self.dsem_by_sid = {}
        self.ninstr = {k: 0 for k in self.E}

    def _deps(self, reads, writes):
        deps = {}

        def add(tok):
            if tok is None:
                return
            if tok[0].startswith('d_'):
                tok = (tok[0], tok[1], self.dsem_by_sid[tok[0]][1])
            if tok[0] not in deps or deps[tok[0]][2] < tok[2]:
                deps[tok[0]] = tok
        for r in reads:
            st = self.reg.get(r)
            if st:
                add(st[0])
        for w in writes:
            st = self.reg.get(w)
            if st:
                add(st[0])
                for t in st[1].values():
                    add(t)
        return deps

    def _wait(self, eng, deps):
        for sid, (_, h, v) in deps.items():
            if sid == 'e_' + eng and not SAME_ENG_SYNC:
                continue
            if self.seen[eng].get(sid, 0) >= v:
                continue
            self.E[eng].wait_ge(h, v)
            self.seen[eng][sid] = v

    def _commit(self, tok, reads, writes):
        for r in reads:
            st = self.reg.setdefault(r, [None, {}])
            st[1][tok[0]] = tok
        for w in writes:
            self.reg[w] = [tok, {}]

    def op(self, eng, fn, reads=(), writes=()):
        self._wait(eng, self._deps(reads, writes))
        ins = fn(self.E[eng])
        self.cnt[eng] += 1
        ins.then_inc(self.sem[eng], 1)
        self._commit(('e_' + eng, self.sem[eng], self.cnt[eng]), reads, writes)

    def dma(self, q, fn, semkey, reads=(), writes=()):
        self._wait(q, self._deps(reads, writes))
        ins = fn(self.E[q])
        d = self.dsem.get(semkey)
        if d is None:
            d = [self.es.enter_context(self.nc.semaphore('d%d' % len(self.dsem))), 0]
            self.dsem[semkey] = d
            self.dsem_by_sid['d_' + str(semkey)] = d
        d[1] += 16
        ins.then_inc(d[0], 16)
        self._commit(('d_' + str(semkey), d[0], d[1]), reads, writes)

    def barrier(self):
        toks = {}
        for k in self.E:
            if self.cnt[k]:
                toks['e_' + k] = ('e_' + k, self.sem[k], self.cnt[k])
        for key, d in self.dsem.items():
            if d[1]:
                toks['d_' + str(key)] = ('d_' + str(key), d[0], d[1])
        for k in self.E:
            self._wait(k, toks)

    def finish(self):
        toks = {}
        for k in self.E:
            if self.cnt[k]:
                toks['e_' + k] = ('e_' + k, self.sem[k], self.cnt[k])
        for key, d in self.dsem.items():
            if d[1]:
                toks['d_' + str(key)] = ('d_' + str(key), d[0], d[1])
        self._wait('sp', toks)


def build(stage=99, debug=False):
    nc = bass.Bass("TRN2", target_bir_lowering=False)
    es = ExitStack()
    sc = Sched(nc, es)

    def din(name, shape, dt=F32):
        return nc.dram_tensor(name, list(shape), dt, kind="ExternalInput").ap()

    x_d = din("x", [S, D])
    cT_d = din("cT", [128, 8])
    pos_d = din("pos", [128, NT], I32)
    n1g_d = din("norm1_g", [1, D])
    n2g_d = din("norm2_g", [1, D])
    wada_d = din("w_ada", [D, 6 * D])
    bada_d = din("b_ada", [1, 6 * D])
    win_d = din("w_in", [D, D_IN])
    convT_d = din("convT", [128, 40])
    gateb_d = din("gate_b", [1, 16])
    mng_d = din("mnorm_g", [1, 512])
    qkg_d = din("qk_g", [1, 128])
    lam_d = din("lam", [1, 256])
    subg_d = din("subln_g", [1, 128])
    wout_d = din("w_out", [D, D])
    wr_d = din("w_router", [D, NE])
    consts_d = din("consts", [128, NCONST])
    if stage >= 5:
        wg_d = din("w_gate_e", [NE * D, 2 * D])
        wu_d = din("w_up_e", [NE * D, 2 * D])
        wd_d = din("w_down_e", [NE * 2 * D, D])
    out_d = nc.dram_tensor("out", [S, D], F32, kind="ExternalOutput").ap()
    skind = "ExternalOutput" if debug else "Internal"
    yT_d = nc.dram_tensor("yT_d", [D, S], BF16, kind=skind).ap()
    h2_d = nc.dram_tensor("h2_d", [S, D], BF16, kind=skind).ap()
    dbg = {}

    def dbg_out(name, shape, dt=F32):
        if debug:
            dbg[name] = nc.dram_tensor(name, list(shape), dt, kind="ExternalOutput").ap()
            return dbg[name]
        return None

    uniq = [0]

    def sb(name, shape, dt=F32, stack=es):
        uniq[0] += 1
        return stack.enter_context(nc.sbuf_tensor("sb%d_%s" % (uniq[0], name), list(shape), dt))

    ps = es.enter_context(nc.psum_tensor("ps", [128, 8, 512], F32))
    consts = sb("consts", [128, NCONST])
    identb = sb("identb", [128, 128], BF16)
    s1 = ExitStack()
    prm = sb("prm", [128, 16 + 512 + 128 + 256 + 128], F32, s1)
    P_GB, P_MNG, P_QKG, P_LAM, P_SUB = 0, 16, 528, 656, 912
    convT = sb("convT", [128, 40], F32, s1)
    maskF = sb("maskF", [128, 128], BF16, s1)
    maskB = sb("maskB", [128, 128], BF16, s1)
    small = sb("small", [128, 64], F32, s1)
    SM_NB, SM_NLAM = 0, 1
    ident = consts[:, C_IDENT:C_IDENT + 128]

    h1T = sb("h1T", [128, 8, S], BF16, s1)
    gm = sb("gm", [128, NT, 16], F32, s1)
    smod = ExitStack()
    mod = sb("mod", [128, 6 * D], F32, smod)
    modrow_d = nc.dram_tensor("modrow_d", [1, 6 * D], F32, kind="Internal").ap()
    CK = 'consts'
    sc.dma('sp', lambda e: e.dma_start(out=consts[:], in_=consts_d[:, :]), CK, writes=['consts'])
    sc.dma('sp', lambda e: e.dma_start(out=convT[:], in_=convT_d[:, :]), CK, writes=['convT'])
    for (off, src, n) in [(P_GB, gateb_d, 16), (P_MNG, mng_d, 512),
                          (P_QKG, qkg_d, 128), (P_LAM, lam_d, 256), (P_SUB, subg_d, 128)]:
        sc.dma('sp', lambda e, off=off, src=src, n=n: e.dma_start(
            out=prm[:, off:off + n], in_=src[0:1, :].partition_broadcast(128)), CK, writes=['prm'])
    sc.dma('sp', lambda e: e.dma_start(out=mod[:], in_=bada_d[0:1, :].partition_broadcast(128)), CK, writes=['mod'])

    with ExitStack() as p0:
        cT = sb("cT", [128, 8], F32, p0)
        prmA = sb("prmA", [128, 2 * D], F32, p0)
        sc.dma('sp', lambda e: e.dma_start(out=prmA[:, 0:D], in_=n1g_d[0:1, :].partition_broadcast(128)), CK, writes=['prmA'])
        sc.dma('sp', lambda e: e.dma_start(out=prmA[:, D:2 * D], in_=n2g_d[0:1, :].partition_broadcast(128)), CK, writes=['prmA'])
        scT = sb("scT", [128, 8], F32, p0)
        lc = sb("lc", [128, 8, 128], BF16, p0)
        wa = [sb("wa%d" % i, [128, 8, 512], BF16, p0) for i in range(2)]
        sc.dma('sp', lambda e: e.dma_start(out=cT[:], in_=cT_d[:, :]), 'cT', writes=['cT'])
        sc.op('act', lambda e: e.activation(out=scT[:], in_=cT[:], func=AF.Silu), reads=['cT'], writes=['scT'])
        sc.op('dve', lambda e: e.tensor_copy(out=lc[:], in_=scT[:].unsqueeze(2).broadcast_to([128, 8, 128])),
              reads=['scT'], writes=['lc'])
        sc.op('dve', lambda e: e.tensor_copy(out=identb[:], in_=ident), reads=['consts'], writes=['identb'])
        sc.op('dve', lambda e: e.tensor_copy(out=maskF[:], in_=consts[:, C_TL:C_TL + 128]), reads=['consts'], writes=['maskF'])
        sc.op('dve', lambda e: e.tensor_copy(out=maskB[:], in_=consts[:, C_TU:C_TU + 128]), reads=['consts'], writes=['maskB'])
        wada_v = wada_d.rearrange("(kc p) n -> p kc n", p=128)
        for n in range(12):
            w = wa[n % 2]
            wk = ('wa', n % 2)
            sc.dma('pool', lambda e, w=w, n=n: e.dma_start(out=w[:], in_=wada_v[:, :, n * 512:(n + 1) * 512]),
                   wk, writes=[wk])
            bank = ('ps', n % 2)

            def mm(e, w=w, n=n):
                for kc in range(8):
                    r = e.matmul(ps[:, n % 2, :], lhsT=lc[:, kc, :], rhs=w[:, kc, :], start=(kc == 0), stop=(kc == 7))
                return r
            sc.op('pe', mm, reads=['lc', wk], writes=[bank])
            sc.op('dve', lambda e, n=n: e.tensor_tensor(out=mod[:, n * 512:(n + 1) * 512], in0=ps[:, n % 2, :],
                                                        in1=mod[:, n * 512:(n + 1) * 512], op=ALU.add),
                  reads=[bank, 'mod'], writes=['mod'])
        sc.op('dve', lambda e: e.scalar_tensor_tensor(out=mod[:, D:2 * D], in0=mod[:, D:2 * D], scalar=1.0,
                                                      in1=prmA[:, 0:D], op0=ALU.add, op1=ALU.mult),
              reads=['mod', 'prmA'], writes=['mod'])
        sc.op('dve', lambda e: e.scalar_tensor_tensor(out=mod[:, 4 * D:5 * D], in0=mod[:, 4 * D:5 * D], scalar=1.0,
                                                      in1=prmA[:, D:2 * D], op0=ALU.add, op1=ALU.mult),
              reads=['mod', 'prmA'], writes=['mod'])
        if debug:
            o = dbg_out("dbg_mod", [128, 6 * D])
            sc.dma('sp', lambda e: e.dma_start(out=o[:, :], in_=mod[:]), 'dbgmod', reads=['mod'])
        sc.barrier()
    SHIFT1, GS1, GATE1, SHIFT2, GS2, GATE2 = [mod[:, i * D:(i + 1) * D] for i in range(6)]

    if stage <= 0:
        sc.finish()
        return nc, dbg

    with ExitStack() as p1:
        xt = [sb("xt%d" % i, [128, D], F32, p1) for i in range(2)]
        junk = sb("junk1", [128, D], BF16, p1)
        tmp = [sb("tmp1_%d" % i, [128, D], F32, p1) for i in range(2)]
        h1b = [sb("h1b%d" % i, [128, D], BF16, p1) for i in range(2)]
        st1 = sb("st1", [128, NT, 3], F32, p1)
        wgt = sb("wgt", [128, 8, 16], BF16, p1)
        win_v = win_d.rearrange("(kc p) n -> p kc n", p=128)
        sc.dma('pool', lambda e: e.dma_start(out=wgt[:], in_=win_v[:, :, 2048:2064]), 'wgt', writes=['wgt'])
        def p1A(i):
            b = i % 2
            xk, tk, hk = ('xt', b), ('tmp1', b), ('h1b', b)
            sc.dma('sp', lambda e, i=i, b=b: e.dma_start(out=xt[b][:], in_=x_d[i * 128:(i + 1) * 128, :]), xk, writes=[xk])
            sc.op('act', lambda e, i=i, b=b: e.activation(out=junk[:], in_=xt[b][:], func=AF.Square,
                                                          accum_out=st1[:, i, 0:1]), reads=[xk], writes=['junk1', ('st1', i)])
            sc.op('act', lambda e, i=i: e.activation(out=st1[:, i, 1:2], in_=st1[:, i, 0:1], func=AF.Sqrt,
                                                     scale=1.0 / D, bias=EPS), reads=[('st1', i)], writes=[('st1', i)])
            sc.op('dve', lambda e, i=i: e.reciprocal(out=st1[:, i, 2:3], in_=st1[:, i, 1:2]),
                  reads=[('st1', i)], writes=[('st1', i)])
            sc.op('dve', lambda e, i=i, b=b: e.scalar_tensor_tensor(out=tmp[b][:], in0=xt[b][:], scalar=st1[:, i, 2:3],
                                                                    in1=GS1, op0=ALU.mult, op1=ALU.mult),
                  reads=[xk, ('st1', i), 'mod'], writes=[tk])
            sc.op('dve', lambda e, b=b: e.tensor_tensor(out=h1b[b][:], in0=tmp[b][:], in1=SHIFT1, op=ALU.add),
                  reads=[tk, 'mod'], writes=[hk])

        def p1B(i):
            b = i % 2
            xk, tk, hk = ('xt', b), ('tmp1', b), ('h1b', b)
            bank = ('ps', 2 + b)
            pb = ps[:, 2 + b, :].bitcast(BF16)

            def tr(e, b=b, pb=pb):
                for kc in range(8):
                    r = e.transpose(out=pb[:, kc * 128:(kc + 1) * 128], in_=h1b[b][:, kc * 128:(kc + 1) * 128], identity=identb[:])
                return r
            sc.op('pe', tr, reads=[hk, 'identb'], writes=[bank])
            sc.op('act', lambda e, i=i, pb=pb: e.copy(out=h1T[:, :, i * 128:(i + 1) * 128],
                                                      in_=pb.rearrange("p (k t) -> p k t", k=8)),
                  reads=[bank], writes=[('h1T', i)])
            gbank = ('ps', 4 + b)

            def gmm(e, i=i, b=b):
                for kc in range(8):
                    r = e.matmul(ps[:, 4 + b, 0:16], lhsT=h1T[:, kc, i * 128:(i + 1) * 128], rhs=wgt[:, kc, :],
                                 start=(kc == 0), stop=(kc == 7))
                return r
            sc.op('pe', gmm, reads=[('h1T', i), 'wgt'], writes=[gbank])
            sc.op('dve', lambda e, i=i, b=b: e.tensor_tensor(out=gm[:, i, :], in0=ps[:, 4 + b, 0:16],
                                                             in1=prm[:, P_GB:P_GB + 16], op=ALU.add),
                  reads=[gbank, 'prm'], writes=['gm'])

        p1A(0)
        for i in range(NT):
            if i + 1 < NT:
                p1A(i + 1)
            p1B(i)
        if debug:
            o = dbg_out("dbg_h1T", [128, 8, S], BF16)
            sc.dma('sp', lambda e: e.dma_start(out=o[:, :, :], in_=h1T[:]), 'dbgh1T', reads=[('h1T', i) for i in range(NT)])
            o3 = dbg_out("dbg_st1", [128, NT, 3])
            sc.dma('sp', lambda e: e.dma_start(out=o3[:, :, :], in_=st1[:]), 'dbgst1', reads=[('st1', i) for i in range(NT)])
            o4 = dbg_out("dbg_tmp", [128, D])
            sc.dma('sp', lambda e: e.dma_start(out=o4[:, :], in_=tmp[1][:]), 'dbgtmp', reads=[('tmp1', 1)])
            o5 = dbg_out("dbg_h1b", [128, D], BF16)
            sc.dma('sp', lambda e: e.dma_start(out=o5[:, :], in_=h1b[1][:]), 'dbgh1b', reads=[('h1b', 1)])
            o2 = dbg_out("dbg_gm", [128, NT, 16])
            sc.dma('sp', lambda e: e.dma_start(out=o2[:, :, :], in_=gm[:]), 'dbggm', reads=['gm'])
        sc.barrier()

    sc.dma('sp', lambda e: e.dma_start(out=modrow_d[0:1, :], in_=mod[0:1, :]), 'modrow', reads=['mod'], writes=['modrow_d'])
    sc.barrier()
    smod.close()

    if stage <= 1:
        sc.finish()
        return nc, dbg

    LNK = -0.5 * math.log(128.0)
    cum = sb("cum", [128, 4, NT, 4], F32, s1)
    egs = sb("egs", [128, 4, NT, 4], F32, s1)
    tw = sb("tw", [128, 6, NT, 4], F32, s1)
    with ExitStack() as pg:
        lsg = sb("lsg", [128, 2, NT, 4], F32, pg)
        tg = sb("tg", [128, 2, NT, 4], F32, pg)
        for d, c0 in ((0, 4), (1, 12)):
            sc.op('act', lambda e, d=d, c0=c0: e.activation(out=tg[:, d, :, :], in_=gm[:, :, c0:c0 + 4], func=AF.Exp, scale=-1.0),
                  reads=['gm'], writes=['tg'])
        sc.op('act', lambda e: e.activation(out=tg[:], in_=tg[:], func=AF.Ln, bias=1.0), reads=['tg'], writes=['tg'])
        sc.op('dve', lambda e: e.tensor_scalar(out=lsg[:], in0=tg[:], scalar1=-1.0, scalar2=None, op0=ALU.mult),
              reads=['tg'], writes=['lsg'])
        lf = lsg[:, 0, :, :].rearrange("p t h -> p (t h)")
        lb = lsg[:, 1, :, :].rearrange("p t h -> p (t h)")
        specs = [(C_TL, lf), (C_TUS, lf), (C_TU, lb), (C_TLS, lb)]

        def cmm(e):
            for k, (co, rhs) in enumerate(specs):
                r = e.matmul(ps[:, 6, k * 128:(k + 1) * 128], lhsT=consts[:, co:co + 128], rhs=rhs, start=True, stop=True)
            return r
        sc.op('pe', cmm, reads=['lsg', 'consts'], writes=[('ps', 6)])
        sc.op('act', lambda e: e.copy(out=cum[:].rearrange("p a t h -> p (a t h)"), in_=ps[:, 6, :]),
              reads=[('ps', 6)], writes=['cum'])
        specs2 = [(C_INDA, lf), (C_INDB, lf), (C_INDA, lb), (C_INDB, lb)]

        def gmm2(e):
            for k, (co, rhs) in enumerate(specs2):
                r = e.matmul(ps[:, 7, k * 128:(k + 1) * 128], lhsT=consts[:, co:co + 128], rhs=rhs, start=True, stop=True)
            return r
        sc.op('pe', gmm2, reads=['lsg', 'consts'], writes=[('ps', 7)])
        sc.op('act', lambda e: e.activation(out=egs[:].rearrange("p a t h -> p (a t h)"), in_=ps[:, 7, :], func=AF.Exp),
              reads=[('ps', 7)], writes=['egs'])
        for d, ic in ((0, 0), (1, 8)):
            ig = gm[:, :, ic:ic + 4]
            a_ = cum[:, 2 * d, :, :]
            r_ = cum[:, 2 * d + 1, :, :]
            sc.op('dve', lambda e, d=d, ig=ig, a_=a_: e.tensor_tensor(out=tw[:, 3 * d, :, :], in0=ig, in1=a_, op=ALU.subtract),
                  reads=['gm', 'cum'], writes=['tw'])
            sc.op('dve', lambda e, d=d, ig=ig, r_=r_: e.tensor_tensor(out=tw[:, 3 * d + 1, :, :], in0=ig, in1=r_, op=ALU.add),
                  reads=['gm', 'cum'], writes=['tw'])
            sc.op('act', lambda e, d=d: e.activation(out=tw[:, 3 * d:3 * d + 2, :, :], in_=tw[:, 3 * d:3 * d + 2, :, :],
                                                     func=AF.Exp, bias=LNK), reads=['tw'], writes=['tw'])
            sc.op('act', lambda e, d=d, a_=a_: e.activation(out=tw[:, 3 * d + 2, :, :], in_=a_, func=AF.Exp),
                  reads=['cum'], writes=['tw'])
        if debug:
            o = dbg_out("dbg_tw", [128, 6, NT, 4])
            sc.dma('sp', lambda e: e.dma_start(out=o[:, :, :, :], in_=tw[:]), 'dbgtw', reads=['tw'])
            o = dbg_out("dbg_egs", [128, 4, NT, 4])
            sc.dma('sp', lambda e: e.dma_start(out=o[:, :, :, :], in_=egs[:]), 'dbgegs', reads=['egs'])
        sc.barrier()

    if stage <= 1.5:
        sc.finish()
        return nc, dbg

    win_v = win_d.rearrange("(kc p) n -> p kc n", p=128)
    yT_v = yT_d.rearrange("(c p) t -> p c t", p=128)

    def emit_y_tile(ph, h_chunk, i, ym, ymk, yst, ystk):
        g4 = i % 4
        bank = ('ps', 5)
        pb = ps[:, 5, :].bitcast(BF16)
        sc.op('pe', lambda e: e.transpose(out=pb[:, g4 * 128:(g4 + 1) * 128], in_=ym, identity=identb[:]),
              reads=[ymk, 'identb'], writes=[bank])
        sc.op('act', lambda e: e.copy(out=yst[:, g4 * 128:(g4 + 1) * 128], in_=pb[:, g4 * 128:(g4 + 1) * 128]),
              reads=[bank], writes=[ystk])
        if g4 == 3:
            t0 = (i - 3) * 128
            sc.dma('sp', lambda e: e.dma_start(out=yT_v[:, h_chunk, t0:t0 + 512], in_=yst[:]), ystk, reads=[ystk], writes=['yT_d'])

    n_mheads = 4 if stage >= 2.5 else 1
    sM = ExitStack()
    wm2 = [sb("wm%d" % k, [128, 8, 512], BF16, sM) for k in range(2)]
    for h in range(n_mheads):
        with ExitStack() as ph:
            wm = wm2[h % 2]
            WMK = ('wm', h % 2)
            qkT = sb("qkT", [128, 2, S], BF16, ph)
            pre = sb("pre", [128, 2, 516], F32, ph)
            acc = sb("acc", [128, 2, 512], F32, ph)
            vext = sb("vext", [128, NT, 129], BF16, ph)
            sgo = sb("sgo", [128, NT, 128], BF16, ph)
            ktok = sb("ktok", [128, NT, 128], BF16, ph)
            Cst = sb("Cst", [128, 2, 65, 129], BF16, ph)
            stt = sb("stt", [128, 2, 2, 129], F32, ph)
            vtmp = [sb("vtmp%d" % k, [128, 129], BF16, ph) for k in range(2)]
            smg = [sb("smg%d" % k, [128, 2, 4, 128], BF16, ph) for k in range(2)]
            vtg = [sb("vtg%d" % k, [128, 2, 4, 129], BF16, ph) for k in range(2)]
            pst = sb("pst", [128, 2, 48], F32, ph)
            hfg = [sb("hfg%d" % k, [128, 4, 128], F32, ph) for k in range(2)]
            hsg = [sb("hsg%d" % k, [128, 4, 128], F32, ph) for k in range(2)]
            ymg = [sb("ymg%d" % k, [128, 4, 128], BF16, ph) for k in range(2)]
            yst = [sb("yst%d" % k, [128, 512], BF16, ph) for k in range(2)]
            for hh in ([0, 1] if h == 0 else [h + 1]):
                if hh >= n_mheads:
                    continue
                for k, c0 in enumerate((hh * 128, 512 + hh * 128, 1024 + hh * 128, 1536 + hh * 128)):
                    sc.dma('pool', lambda e, k=k, c0=c0, hh=hh: e.dma_start(out=wm2[hh % 2][:, :, k * 128:(k + 1) * 128], in_=win_v[:, :, c0:c0 + 128]),
                           ('wm', hh % 2), writes=[('wm', hh % 2)])
            sc.op('pool', lambda e: e.memset(vext[:, :, 128:129], 1.0), writes=['vext1'])
            sc.op('pool', lambda e: e.memset(Cst[:, :, 0, :], 0.0), writes=[('Cst', 0, 0), ('Cst', 1, 0)])
            sc.op('pool', lambda e: e.memset(stt[:, :, 0, :], 0.0), writes=[('stt', 0, 0), ('stt', 1, 0)])
            sc.op('pool', lambda e: e.memset(pre[:, :, 0:4], 0.0), writes=['pre'])
            for g in range(NG + 1):
                if g < NG:
                    for qk in range(2):
                        bank = ('ps', qk)

                        def mm(e, qk=qk, g=g):
                            for kc in range(8):
                                r = e.matmul(ps[:, qk, :], lhsT=wm[:, kc, qk * 128:(qk + 1) * 128],
                                             rhs=h1T[:, kc, g * 512:(g + 1) * 512], start=(kc == 0), stop=(kc == 7))
                            return r
                        sc.op('pe', mm, reads=[WMK] + [('h1T', 4 * g + k) for k in range(4)], writes=[bank])
                        sc.op('act', lambda e, qk=qk: e.copy(out=pre[:, qk, 4:516], in_=ps[:, qk, :]), reads=[bank], writes=['pre'])
                else:
                    sc.op('pool', lambda e: e.memset(pre[:, :, 4:8], 0.0), writes=['pre'])
                W = 512 if g < NG else 2
                for qk in range(2):
                    cb = (4 * qk + h) * 5
                    sc.op('dve', lambda e, qk=qk, cb=cb, W=W: e.tensor_scalar(out=acc[:, qk, 0:W], in0=pre[:, qk, 0:W], scalar1=convT[:, cb:cb + 1],
                                                                            scalar2=None, op0=ALU.mult), reads=['pre', 'convT'], writes=[('acc', qk)])
                    for j in range(1, 5):
                        sc.op('dve', lambda e, qk=qk, cb=cb, j=j, W=W: e.scalar_tensor_tensor(
                            out=acc[:, qk, 0:W], in0=pre[:, qk, j:j + W], scalar=convT[:, cb + j:cb + j + 1], in1=acc[:, qk, 0:W],
                            op0=ALU.mult, op1=ALU.add), reads=['pre', 'convT', ('acc', qk)], writes=[('acc', qk)])
                    n0 = 2 if g == 0 else 0
                    t0 = 512 * g - 2 + n0
                    sc.op('act', lambda e, qk=qk, n0=n0, t0=t0, W=W: e.activation(out=qkT[:, qk, t0:t0 + W - n0], in_=acc[:, qk, n0:W], func=AF.Silu),
                          reads=[('acc', qk)], writes=[('qkT', qk)])
                if g < NG:
                    sc.op('pool', lambda e: e.tensor_copy(out=pre[:, :, 0:4], in_=pre[:, :, 512:516]), reads=['pre'], writes=['pre'])
                    for k in range(4):
                        i = 4 * g + k
                        bank = ('ps', 2 + (i % 2))

                        def mmvo(e, i=i):
                            for kc in range(8):
                                r = e.matmul(ps[:, 2 + (i % 2), 0:256], lhsT=h1T[:, kc, i * 128:(i + 1) * 128], rhs=wm[:, kc, 256:512],
                                             start=(kc == 0), stop=(kc == 7))
                            return r
                        sc.op('pe', mmvo, reads=[WMK, ('h1T', i)], writes=[bank])
                        sc.op('act', lambda e, i=i: e.copy(out=vext[:, i, 0:128], in_=ps[:, 2 + (i % 2), 0:128]), reads=[bank], writes=[('vext', i)])
                        sc.op('act', lambda e, i=i: e.activation(out=sgo[:, i, :], in_=ps[:, 2 + (i % 2), 128:256], func=AF.Sigmoid),
                              reads=[bank], writes=[('sgo', i)])
            for g in range(NG):
                bank = ('ps', 4)
                pb = ps[:, 4, :].bitcast(BF16)

                def trk(e, g=g, pb=pb):
                    for k in range(4):
                        i = 4 * g + k
                        r = e.transpose(out=pb[:, k * 128:(k + 1) * 128], in_=qkT[:, 1, i * 128:(i + 1) * 128], identity=identb[:])
                    return r
                sc.op('pe', trk, reads=[('qkT', 1), 'identb'], writes=[bank])
                sc.op('act', lambda e, g=g, pb=pb: e.copy(out=ktok[:, 4 * g:4 * g + 4, :], in_=pb[:, 0:512].rearrange("p (k d) -> p k d", k=4)),
                      reads=[bank], writes=['ktok'])
            nv = 0
            for step in range(64):
                for d in range(2):
                    c = step if d == 0 else 63 - step
                    i, half = c // 2, c % 2
                    first_of_tile = (half == 0) if d == 0 else (half == 1)
                    vk = ('vhat', d)
                    vb = vtmp[d]
                    if first_of_tile:
                        sc.op('act', lambda e, d=d, i=i, vb=vb: e.activation(out=vb[:], in_=vext[:, i, :], func=AF.Copy,
                                                                             scale=tw[:, 3 * d + 1, i, h:h + 1]),
                              reads=[('vext', i), 'vext1', 'tw'], writes=[vk])
                    slot = nv % 4
                    nv += 1
                    pbank = 4 + slot
                    pc = 0
                    pk = ('ps', pbank)
                    sc.op('pe', lambda e, i=i, half=half, vb=vb, pbank=pbank, pc=pc: e.matmul(
                        ps[:, pbank, pc:pc + 129], lhsT=ktok[half * 64:(half + 1) * 64, i, :], rhs=vb[half * 64:(half + 1) * 64, :],
                        start=True, stop=True), reads=['ktok', vk], writes=[pk])
                    cur, nxt = step % 2, (step + 1) % 2
                    sc.op('dve', lambda e, d=d, i=i, half=half, cur=cur, nxt=nxt, pbank=pbank, pc=pc: e.scalar_tensor_tensor(
                        out=stt[:, d, nxt, :], in0=stt[:, d, cur, :], scalar=egs[:, 2 * d + half, i, h:h + 1], in1=ps[:, pbank, pc:pc + 129],
                        op0=ALU.mult, op1=ALU.add), reads=[('stt', d, cur), 'egs', pk], writes=[('stt', d, nxt)])
                    sc.op('pool', lambda e, d=d, nxt=nxt, step=step: e.tensor_copy(out=Cst[:, d, step + 1, :], in_=stt[:, d, nxt, :]),
                          reads=[('stt', d, nxt)], writes=[('Cst', d, step + 1)])
            for g in range(NG):
                gb = g % 2
                sbank = ('ps', gb)

                def smm(e, g=g, gb=gb):
                    for t in range(4):
                        blk = slice((4 * g + t) * 128, (4 * g + t + 1) * 128)
                        r = e.matmul(ps[:, gb, t * 128:(t + 1) * 128], lhsT=qkT[:, 1, blk], rhs=qkT[:, 0, blk], start=True, stop=True)
                    return r
                sc.op('pe', smm, reads=[('qkT', 0), ('qkT', 1)], writes=[sbank])
                S4 = ps[:, gb, :].rearrange("p (t j) -> p t j", t=4)
                smF, smB = smg[gb][:, 0, :, :], smg[gb][:, 1, :, :]
                kF, kB = ('smg', gb, 0), ('smg', gb, 1)
                sc.op('dve', lambda e, S4=S4, smF=smF: e.tensor_tensor(out=smF, in0=S4, in1=maskF[:].unsqueeze(1).broadcast_to([128, 4, 128]), op=ALU.mult),
                      reads=[sbank, 'maskF'], writes=[kF])
                sc.op('dve', lambda e, S4=S4, smB=smB: e.tensor_tensor(out=smB, in0=S4, in1=maskB[:].unsqueeze(1).broadcast_to([128, 4, 128]), op=ALU.mult),
                      reads=[sbank, 'maskB'], writes=[kB])
                vtk = ('vtg', gb)
                for d in range(2):
                    sc.op('pool', lambda e, d=d, g=g, gb=gb: e.tensor_tensor(
                        out=vtg[gb][:, d, :, :], in0=vext[:, 4 * g:4 * g + 4, :],
                        in1=tw[:, 3 * d, 4 * g:4 * g + 4, h:h + 1].broadcast_to([128, 4, 129]), op=ALU.mult),
                        reads=[('vext', 4 * g + t) for t in range(4)] + ['vext1', 'tw'], writes=[(vtk, d)])
                ubanks = [('ps', 2 + t) for t in range(4)]

                def umm(e, g=g, gb=gb):
                    for t in range(4):
                        i = 4 * g + t
                        for d in range(2):
                            o0_ = d * 129
                            e.matmul(ps[:, 2 + t, o0_:o0_ + 129], lhsT=smg[gb][:, d, t, :], rhs=vtg[gb][:, d, t, :], start=True, stop=False)
                            for half in range(2):
                                c = 2 * i + half
                                sidx = c if d == 0 else 63 - c
                                t0 = i * 128 + half * 64
                                r = e.matmul(ps[half * 64:(half + 1) * 64, 2 + t, o0_:o0_ + 129], lhsT=qkT[:, 0, t0:t0 + 64],
                                             rhs=Cst[:, d, sidx, :], start=False, stop=True)
                    return r
                creads = []
                for t in range(4):
                    i = 4 * g + t
                    creads += [('Cst', 0, 2 * i), ('Cst', 0, 2 * i + 1), ('Cst', 1, 63 - 2 * i), ('Cst', 1, 62 - 2 * i)]
                sc.op('pe', umm, reads=[kF, kB, (vtk, 0), (vtk, 1), ('qkT', 0)] + creads, writes=ubanks)
                P = pst[:, gb, :]
                pk = ('pst', gb)
                U4 = ps[:, 2:6, 0:258].rearrange("p t (d v) -> p t d v", d=2)
                ea2 = tw[:, 2:6:3, 4 * g:4 * g + 4, h].rearrange("p d t -> p t d")
                Pv = lambda lo: P[:, lo:lo + 8].rearrange("p (t d) -> p t d", d=2)
                sc.op('dve', lambda e: e.tensor_tensor(out=Pv(0), in0=U4[:, :, :, 128], in1=ea2, op=ALU.mult), reads=ubanks + ['tw'], writes=[pk])
                sc.op('dve', lambda e: e.tensor_scalar(out=P[:, 16:24], in0=P[:, 0:8], scalar1=-1.0, scalar2=None, op0=ALU.mult), reads=[pk], writes=[pk])
                sc.op('dve', lambda e: e.scalar_tensor_tensor(out=P[:, 8:16], in0=P[:, 16:24], scalar=1.0, in1=P[:, 0:8], op0=ALU.max, op1=ALU.max),
                      reads=[pk], writes=[pk])
                sc.op('dve', lambda e: e.reciprocal(out=P[:, 16:24], in_=P[:, 8:16]), reads=[pk], writes=[pk])
                sc.op('dve', lambda e: e.tensor_tensor(out=Pv(24), in0=Pv(16), in1=ea2, op=ALU.mult), reads=[pk, 'tw'], writes=[pk])
                RR = Pv(24)
                hfb, hsb = hfg[gb], hsg[gb]
                sc.op('dve', lambda e: e.tensor_tensor(out=hfb[:], in0=U4[:, :, 0, 0:128], in1=RR[:, :, 0:1].broadcast_to([128, 4, 128]), op=ALU.mult),
                      reads=ubanks + [pk], writes=[('hfg', gb)])
                sc.op('dve', lambda e: e.tensor_tensor(out=hsb[:], in0=U4[:, :, 1, 0:128], in1=RR[:, :, 1:2].broadcast_to([128, 4, 128]), op=ALU.mult),
                      reads=ubanks + [pk], writes=[('hsg', gb)])
                sc.op('pool', lambda e: e.tensor_tensor(out=hsb[:], in0=hsb[:], in1=hfb[:], op=ALU.add), reads=[('hsg', gb), ('hfg', gb)], writes=[('hsg', gb)])
                sc.op('pool', lambda e: e.tensor_tensor(out=hfb[:], in0=hsb[:], in1=hsb[:], op=ALU.mult), reads=[('hsg', gb)], writes=[('hfg', gb)])
                sc.op('dve', lambda e: e.tensor_reduce(out=P[:, 32:36], in_=hfb[:], axis=AX.X, op=ALU.add), reads=[('hfg', gb)], writes=[pk])
                sc.op('act', lambda e: e.activation(out=P[:, 36:40], in_=P[:, 32:36], func=AF.Sqrt, scale=1.0 / 128, bias=EPS), reads=[pk], writes=[pk])
                sc.op('dve', lambda e: e.reciprocal(out=P[:, 40:44], in_=P[:, 36:40]), reads=[pk], writes=[pk])
                sc.op('dve', lambda e: e.tensor_tensor(out=hsb[:], in0=hsb[:], in1=P[:, 40:44].unsqueeze(2).broadcast_to([128, 4, 128]), op=ALU.mult),
                      reads=[('hsg', gb), pk], writes=[('hsg', gb)])
                sc.op('pool', lambda e: e.tensor_tensor(out=hsb[:], in0=hsb[:],
                                                        in1=prm[:, P_MNG + h * 128:P_MNG + (h + 1) * 128].unsqueeze(1).broadcast_to([128, 4, 128]), op=ALU.mult),
                      reads=[('hsg', gb), 'prm'], writes=[('hsg', gb)])
                ym = ymg[gb]
                ymk = ('ymg', gb)
                sc.op('dve', lambda e, g=g: e.tensor_tensor(out=ym[:], in0=hsb[:], in1=sgo[:, 4 * g:4 * g + 4, :], op=ALU.mult),
                      reads=[('hsg', gb)] + [('sgo', 4 * g + t) for t in range(4)], writes=[ymk])
                tbank = ('ps', 6)
                pb = ps[:, 6, :].bitcast(BF16)

                def ytr(e, ym=ym, pb=pb):
                    for t in range(4):
                        r = e.transpose(out=pb[:, t * 128:(t + 1) * 128], in_=ym[:, t, :], identity=identb[:])
                    return r
                sc.op('pe', ytr, reads=[ymk, 'identb'], writes=[tbank])
                ys = yst[gb]
                ysk = ('yst', gb)
                sc.op('act', lambda e, ys=ys, pb=pb: e.copy(out=ys[:], in_=pb[:, 0:512]), reads=[tbank], writes=[ysk])
                sc.dma('sp', lambda e, ys=ys, g=g: e.dma_start(out=yT_v[:, h, g * 512:(g + 1) * 512], in_=ys[:]), ysk, reads=[ysk], writes=['yT_d'])
            if debug and h == 0:
                o = dbg_out("dbg_qkT", [128, 2, S], BF16)
                sc.dma('sp', lambda e: e.dma_start(out=o[:, :, :], in_=qkT[:]), 'dbgqkT', reads=[('qkT', 0), ('qkT', 1)])
                o = dbg_out("dbg_Cst", [128, 2, 65, 129], BF16)
                sc.dma('sp', lambda e: e.dma_start(out=o[:, :, :, :], in_=Cst[:]), 'dbgCst',
                       reads=[('Cst', d, k) for d in range(2) for k in range(65)])
            sc.barrier()
    sM.close()

    if stage <= 2.5:
        sc.finish()
        return nc, dbg

    cosT = sb("cosT", [128, NT, 8], F32, s1)
    sinT = sb("sinT", [128, NT, 8], F32, s1)
    qkg4 = sb("qkg4", [128, 4, 64], F32, s1)
    subg8 = sb("subg8", [128, 128], F32, s1)
    TWO_PI = 6.28318
    with ExitStack() as pa:
        posi = sb("posi", [128, NT], I32, pa)
        posf = sb("posf", [128, NT], F32, pa)
        ut = sb("ut", [128, NT, 8], F32, pa)
        ui = sb("ui", [128, NT, 8], I32, pa)
        uf = sb("uf", [128, NT, 8], F32, pa)
        jl = sb("jl", [128, 64], F32, pa)
        sc.dma('sp', lambda e: e.dma_start(out=posi[:], in_=pos_d[:, :]), 'posi', writes=['posi'])
        sc.op('dve', lambda e: e.tensor_copy(out=posf[:], in_=posi[:]), reads=['posi'], writes=['posf'])
        sc.op('dve', lambda e: e.tensor_tensor(out=ut[:], in0=posf[:].unsqueeze(2).broadcast_to([128, NT, 8]),
                                               in1=consts[:, C_FREQ:C_FREQ + 8].unsqueeze(1).broadcast_to([128, NT, 8]), op=ALU.mult),
              reads=['posf', 'consts'], writes=['ut'])
        for tab, shift in ((sinT, 0.0), (cosT, 0.25)):
            if shift:
                sc.op('dve', lambda e, shift=shift: e.tensor_scalar(out=ut[:], in0=ut[:], scalar1=shift, scalar2=None, op0=ALU.add),
                      reads=['ut'], writes=['ut'])
            sc.op('dve', lambda e: e.tensor_copy(out=ui[:], in_=ut[:]), reads=['ut'], writes=['ui'])
            sc.op('dve', lambda e: e.tensor_copy(out=uf[:], in_=ui[:]), reads=['ui'], writes=['uf'])
            sc.op('dve', lambda e: e.tensor_tensor(out=uf[:], in0=ut[:], in1=uf[:], op=ALU.subtract), reads=['ut', 'uf'], writes=['uf'])
            sc.op('act', lambda e, tab=tab: e.activation(out=tab[:], in_=uf[:], func=AF.Sin, scale=TWO_PI), reads=['uf'], writes=['rot'])
        sc.op('dve', lambda e: e.tensor_reduce(out=small[:, 2:4], in_=prm[:, P_QKG:P_QKG + 128].rearrange("p (a d) -> p a d", a=2),
                                               axis=AX.X, op=ALU.max, apply_absolute_value=True), reads=['prm'], writes=['small'])
        sc.op('dve', lambda e: e.scalar_tensor_tensor(out=small[:, SM_NB:SM_NB + 1], in0=small[:, 2:3], scalar=-8.0, in1=small[:, 3:4],
                                                      op0=ALU.mult, op1=ALU.mult), reads=['small'], writes=['small'])
        for k in range(2):
            sc.op('dve', lambda e, k=k: e.tensor_tensor(out=jl[:], in0=prm[:, P_LAM + 128 * k:P_LAM + 128 * k + 64],
                                                        in1=prm[:, P_LAM + 128 * k + 64:P_LAM + 128 * k + 128], op=ALU.mult),
                  reads=['prm'], writes=['jl'])
            sc.op('dve', lambda e, k=k: e.tensor_reduce(out=small[:, 4 + k:5 + k], in_=jl[:], axis=AX.X, op=ALU.add), reads=['jl'], writes=['small'])
        sc.op('act', lambda e: e.activation(out=small[:, 6:8], in_=small[:, 4:6], func=AF.Exp), reads=['small'], writes=['small'])
        sc.op('dve', lambda e: e.scalar_tensor_tensor(out=small[:, SM_NLAM:SM_NLAM + 1], in0=small[:, 7:8], scalar=-LAM_INIT, in1=small[:, 6:7],
                                                      op0=ALU.add, op1=ALU.subtract), reads=['small'], writes=['small'])
        for a in range(4):
            o_ = P_QKG + (64 if a >= 2 else 0)
            sc.op('pool', lambda e, a=a, o_=o_: e.tensor_copy(out=qkg4[:, a, :], in_=prm[:, o_:o_ + 64]), reads=['prm'], writes=['qkg4'])
        sc.op('dve', lambda e: e.tensor_scalar(out=subg8[:], in0=prm[:, P_SUB:P_SUB + 128], scalar1=1.0 - LAM_INIT, scalar2=None, op0=ALU.mult),
              reads=['prm'], writes=['subg8'])
        if debug:
            o = dbg_out("dbg_small", [128, 64])
            sc.op('pool', lambda e: e.memset(small[:, 8:64], 0.0), writes=['small'])
            sc.dma('sp', lambda e: e.dma_start(out=o[:, :], in_=small[:]), 'dbgsmall', reads=['small'])
            o = dbg_out("dbg_cos", [128, NT, 8])
            sc.dma('sp', lambda e: e.dma_start(out=o[:, :, :], in_=cosT[:]), 'dbgcos', reads=['rot'])
        sc.barrier()

    n_aheads = 4 if stage >= 3.5 else 1
    sA = ExitStack()
    wa2 = [sb("waa%d" % k, [128, 8, 384], BF16, sA) for k in range(2)]
    for h in range(n_aheads):
        with ExitStack() as ph:
            wa_ = wa2[h % 2]
            WAK = ('waa', h % 2)
            qTa = sb("qTa", [128, 2, S], BF16, ph)
            kTa = sb("kTa", [128, S], BF16, ph)
            vxa = sb("vxa", [128, NT, 129], BF16, ph)
            xq = [sb("xq%d" % k, [128, 4, 4, 64], F32, ph) for k in range(2)]
            sq = sb("sq", [128, 4, 4, 64], F32, ph)
            rp = [sb("rp%d" % k, [128, 4, 4, 4, 8], F32, ph) for k in range(2)]
            st4 = sb("st4", [128, 2, 48], F32, ph)
            xr = [sb("xr%d" % k, [128, 4, 4, 64], BF16, ph) for k in range(2)]
            pt = [sb("pt%d" % k, [128, 2, 512], BF16, ph) for k in range(3)]
            o0 = sb("o0", [128, 4, 128], F32, ph)
            ob4 = [sb("ob4_%d" % k, [128, 4, 128], F32, ph) for k in range(2)]
            osq = sb("osq", [128, 4, 128], F32, ph)
            st5 = sb("st5", [128, 2, 24], F32, ph)
            ymt4 = [sb("ymt4_%d" % k, [128, 4, 128], BF16, ph) for k in range(2)]
            yst = [sb("ysta%d" % k, [128, 512], BF16, ph) for k in range(2)]
            for hh in ([0, 1] if h == 0 else [h + 1]):
                if hh >= n_aheads:
                    continue
                for k, c0 in enumerate((2064 + hh * 128, 2576 + hh * 128, 3088 + hh * 128)):
                    sc.dma('pool', lambda e, k=k, c0=c0, hh=hh: e.dma_start(out=wa2[hh % 2][:, :, k * 128:(k + 1) * 128], in_=win_v[:, :, c0:c0 + 128]),
                           ('waa', hh % 2), writes=[('waa', hh % 2)])
            sc.op('pool', lambda e: e.memset(vxa[:, :, 128:129], 1.0), writes=['vxa1'])
            sc.op('pool', lambda e: e.memset(qTa[64:128, 0, :], 0.0), writes=['qTa'])
            sc.op('pool', lambda e: e.memset(qTa[0:64, 1, :], 0.0), writes=['qTa'])
            def projA(g):
                    b = g % 2
                    banks = [('ps', k) for k in range(4)]
                    xk, rk, sk, xrk = ('xq', b), ('rp', b), ('st4', b), ('xr', b)
                    X = xq[b]
                    X3 = X[:].rearrange("p t a d -> p t (a d)")
                    X16 = X[:].rearrange("p t a d -> p (t a) d")
                    T4 = st4[:, b, :]
                    R = rp[b]
                    b = g % 2
                    banks = [('ps', k) for k in range(4)]

                    def mmp(e, g=g):
                        for t in range(4):
                            i = 4 * g + t
                            for kc in range(8):
                                r = e.matmul(ps[:, t, 0:384], lhsT=h1T[:, kc, i * 128:(i + 1) * 128], rhs=wa_[:, kc, :], start=(kc == 0), stop=(kc == 7))
                        return r
                    sc.op('pe', mmp, reads=[WAK] + [('h1T', 4 * g + t) for t in range(4)], writes=banks)
                    xk, rk, sk, xrk = ('xq', b), ('rp', b), ('st4', b), ('xr', b)
                    X = xq[b]
                    X3 = X[:].rearrange("p t a d -> p t (a d)")
                    X16 = X[:].rearrange("p t a d -> p (t a) d")
                    sc.op('act', lambda e, X3=X3: e.copy(out=X3, in_=ps[:, 0:4, 0:256]), reads=banks, writes=[xk])
                    sc.op('act', lambda e, g=g: e.copy(out=vxa[:, 4 * g:4 * g + 4, 0:128], in_=ps[:, 0:4, 256:384]), reads=banks,
                          writes=[('vxa', 4 * g + t) for t in range(4)])

            def projB(g):
                    b = g % 2
                    banks = [('ps', k) for k in range(4)]
                    xk, rk, sk, xrk = ('xq', b), ('rp', b), ('st4', b), ('xr', b)
                    X = xq[b]
                    X3 = X[:].rearrange("p t a d -> p t (a d)")
                    X16 = X[:].rearrange("p t a d -> p (t a) d")
                    T4 = st4[:, b, :]
                    R = rp[b]
                    sc.op('dve', lambda e, X=X: e.tensor_tensor(out=sq[:], in0=X[:], in1=X[:], op=ALU.mult), reads=[xk], writes=['sq'])
                    T4 = st4[:, b, :]
                    sc.op('dve', lambda e, T4=T4: e.tensor_reduce(out=T4[:, 0:16], in_=sq[:].rearrange("p t a d -> p (t a) d"), axis=AX.X, op=ALU.add),
                          reads=['sq'], writes=[sk])
                    sc.op('act', lambda e, T4=T4: e.activation(out=T4[:, 16:32], in_=T4[:, 0:16], func=AF.Sqrt, scale=1.0 / 64, bias=EPS), reads=[sk], writes=[sk])
                    sc.op('dve', lambda e, T4=T4: e.reciprocal(out=T4[:, 32:48], in_=T4[:, 16:32]), reads=[sk], writes=[sk])
                    sc.op('dve', lambda e, X16=X16, T4=T4: e.tensor_tensor(out=X16, in0=X16, in1=T4[:, 32:48].unsqueeze(2).broadcast_to([128, 16, 64]), op=ALU.mult),
                          reads=[xk, sk], writes=[xk])
                    sc.op('dve', lambda e, X=X: e.tensor_tensor(out=X[:], in0=X[:], in1=qkg4[:].unsqueeze(1).broadcast_to([128, 4, 4, 64]), op=ALU.mult),
                          reads=[xk, 'qkg4'], writes=[xk])
                    cs = cosT[:, 4 * g:4 * g + 4, :].unsqueeze(2).broadcast_to([128, 4, 4, 8])
                    sn = sinT[:, 4 * g:4 * g + 4, :].unsqueeze(2).broadcast_to([128, 4, 4, 8])
                    R = rp[b]
                    for k, (tt, tr_) in enumerate(((X[:, :, :, 0:8], cs), (X[:, :, :, 8:16], sn), (X[:, :, :, 8:16], cs), (X[:, :, :, 0:8], sn))):
                        sc.op('pool', lambda e, k=k, tt=tt, tr_=tr_, R=R: e.tensor_tensor(out=R[:, k, :, :, :], in0=tt, in1=tr_, op=ALU.mult),
                              reads=[xk, 'rot'], writes=[rk])

            def projC(g):
                    b = g % 2
                    banks = [('ps', k) for k in range(4)]
                    xk, rk, sk, xrk = ('xq', b), ('rp', b), ('st4', b), ('xr', b)
                    X = xq[b]
                    X3 = X[:].rearrange("p t a d -> p t (a d)")
                    X16 = X[:].rearrange("p t a d -> p (t a) d")
                    T4 = st4[:, b, :]
                    R = rp[b]
                    XR = xr[b]
                    sc.op('act', lambda e, XR=XR, X=X: e.copy(out=XR[:], in_=X[:]), reads=[xk], writes=[xrk])
                    sc.op('dve', lambda e, XR=XR, R=R: e.tensor_tensor(out=XR[:, :, :, 0:8], in0=R[:, 0, :, :, :], in1=R[:, 1, :, :, :], op=ALU.subtract),
                          reads=[rk, xrk], writes=[xrk])
                    sc.op('dve', lambda e, XR=XR, R=R: e.tensor_tensor(out=XR[:, :, :, 8:16], in0=R[:, 2, :, :, :], in1=R[:, 3, :, :, :], op=ALU.add),
                          reads=[rk, xrk], writes=[xrk])
                    tbanks = [('ps', 4), ('ps', 5)]
                    pbq = ps[:, 4, :].bitcast(BF16)
                    pbk = ps[:, 5, :].bitcast(BF16)
                    XRf = XR[:].rearrange("p t a d -> p t (a d)")

                    def trq(e, XRf=XRf, pbq=pbq, pbk=pbk):
                        for t in range(4):
                            e.transpose(out=pbq[:, t * 128:(t + 1) * 128], in_=XRf[:, t, 0:128], identity=identb[:])
                            r = e.transpose(out=pbk[:, t * 128:(t + 1) * 128], in_=XRf[:, t, 128:256], identity=identb[:])
                        return r
                    sc.op('pe', trq, reads=[xrk, 'identb'], writes=tbanks)
                    sc.op('act', lambda e, pbq=pbq, g=g: e.copy(out=qTa[0:64, 0, g * 512:(g + 1) * 512], in_=pbq[0:64, 0:512]), reads=tbanks, writes=['qTa'])
                    sc.op('act', lambda e, pbq=pbq, g=g: e.copy(out=qTa[64:128, 1, g * 512:(g + 1) * 512], in_=pbq[64:128, 0:512]), reads=tbanks, writes=['qTa'])
                    sc.op('act', lambda e, pbk=pbk, g=g: e.copy(out=kTa[:, g * 512:(g + 1) * 512], in_=pbk[:, 0:512]), reads=tbanks, writes=['kTa'])

            projA(0)
            for g in range(NG):
                if g + 1 < NG:
                    projA(g + 1)
                projB(g)
                projC(g)
            if debug and h == 0:
                o = dbg_out("dbg_qTa", [128, S], BF16)
                sc.dma('sp', lambda e: e.dma_start(out=o[0:64, :], in_=qTa[0:64, 0, :]), 'dbgqTa', reads=['qTa'])
                sc.dma('sp', lambda e: e.dma_start(out=o[64:128, :], in_=qTa[64:128, 1, :]), 'dbgqTa', reads=['qTa'])
                o = dbg_out("dbg_kTa", [128, S], BF16)
                sc.dma('sp', lambda e: e.dma_start(out=o[:, :], in_=kTa[:]), 'dbgkTa', reads=['kTa'])
            nqb = 8 if stage >= 3.2 else 1
            for qb in range(nqb):
                for p in range(2):
                    pr = slice(64 * p, 64 * p + 64)
                    def st_mm(j):
                        pb0 = 4 + 2 * (j % 2)

                        def f(e, j=j, pb0=pb0):
                            for u in range(2):
                                kt = 2 * j + u
                                r = e.matmul(ps[:, pb0 + u, :], lhsT=kTa[:, kt * 128:(kt + 1) * 128], rhs=qTa[:, p, qb * 512:(qb + 1) * 512],
                                             start=True, stop=True)
                            return r
                        sc.op('pe', f, reads=['qTa', 'kTa'], writes=[('ps', pb0), ('ps', pb0 + 1)])
                    st_mm(0)
                    for j in range(NT // 2):
                        pb0 = 4 + 2 * (j % 2)
                        sbanks = [('ps', pb0), ('ps', pb0 + 1)]
                        if j + 1 < NT // 2:
                            st_mm(j + 1)
                        ptk = ('pt', j % 3)
                        ptb = pt[j % 3]
                        sc.op('act', lambda e, pb0=pb0, ptb=ptb: e.activation(out=ptb[:], in_=ps[:, pb0:pb0 + 2, :], func=AF.Exp, scale=0.125,
                                                                              bias=small[:, SM_NB:SM_NB + 1]),
                              reads=sbanks + ['small'], writes=[ptk])

                        def pv(e, ptb=ptb, j=j):
                            for u in range(2):
                                kt = 2 * j + u
                                for qs in range(4):
                                    r = e.matmul(ps[:, qs, 0:129], lhsT=ptb[:, u, qs * 128:(qs + 1) * 128], rhs=vxa[:, kt, :],
                                                 start=(kt == 0), stop=(kt == NT - 1))
                            return r
                        sc.op('pe', pv, reads=[ptk, ('vxa', 2 * j), ('vxa', 2 * j + 1), 'vxa1'], writes=[('ps', 0), ('ps', 1), ('ps', 2), ('ps', 3)])
                    abanks = [('ps', k) for k in range(4)]
                    acc4 = ps[:, 0:4, 0:129]
                    T5 = st5[:, p, :]
                    tk5 = ('st5', p)
                    sc.op('dve', lambda e, T5=T5, acc4=acc4: e.reciprocal(out=T5[:, 0:4], in_=acc4[:, :, 128]), reads=abanks, writes=[tk5])
                    if p == 0:
                        sc.op('dve', lambda e, T5=T5, acc4=acc4: e.tensor_tensor(out=o0[:], in0=acc4[:, :, 0:128],
                                                                                  in1=T5[:, 0:4].unsqueeze(2).broadcast_to([128, 4, 128]), op=ALU.mult),
                              reads=abanks + [tk5], writes=['o0'])
                        continue
                    O = ob4[qb % 2]
                    okk = ('ob4', qb % 2)
                    sc.op('dve', lambda e, T5=T5: e.tensor_scalar(out=T5[:, 4:8], in0=T5[:, 0:4], scalar1=small[:, SM_NLAM:SM_NLAM + 1], scalar2=None, op0=ALU.mult),
                          reads=[tk5, 'small'], writes=[tk5])
                    sc.op('dve', lambda e, T5=T5, acc4=acc4, O=O: e.tensor_tensor(out=O[:], in0=acc4[:, :, 0:128],
                                                                                   in1=T5[:, 4:8].unsqueeze(2).broadcast_to([128, 4, 128]), op=ALU.mult),
                          reads=abanks + [tk5], writes=[okk])
                    sc.op('dve', lambda e, O=O: e.tensor_tensor(out=O[:], in0=O[:], in1=o0[:], op=ALU.add), reads=[okk, 'o0'], writes=[okk])
                    sc.op('pool', lambda e, O=O: e.tensor_tensor(out=osq[:], in0=O[:], in1=O[:], op=ALU.mult), reads=[okk], writes=['osq'])
                    sc.op('dve', lambda e, T5=T5: e.tensor_reduce(out=T5[:, 8:12], in_=osq[:], axis=AX.X, op=ALU.add), reads=['osq'], writes=[tk5])
                    sc.op('act', lambda e, T5=T5: e.activation(out=T5[:, 12:16], in_=T5[:, 8:12], func=AF.Sqrt, scale=1.0 / 128, bias=EPS), reads=[tk5], writes=[tk5])
                    sc.op('dve', lambda e, T5=T5: e.reciprocal(out=T5[:, 16:20], in_=T5[:, 12:16]), reads=[tk5], writes=[tk5])
                    sc.op('dve', lambda e, T5=T5, O=O: e.tensor_tensor(out=O[:], in0=O[:], in1=T5[:, 16:20].unsqueeze(2).broadcast_to([128, 4, 128]), op=ALU.mult),
                          reads=[okk, tk5], writes=[okk])
                    ym = ymt4[qb % 2]
                    ymk = ('ymt4', qb % 2)
                    sc.op('pool', lambda e, O=O, ym=ym: e.tensor_tensor(out=ym[:], in0=O[:], in1=subg8[:].unsqueeze(1).broadcast_to([128, 4, 128]), op=ALU.mult),
                          reads=[okk, 'subg8'], writes=[ymk])
                    tbank = ('ps', 7)
                    pb = ps[:, 7, :].bitcast(BF16)

                    def ytr(e, ym=ym, pb=pb):
                        for t in range(4):
                            r = e.transpose(out=pb[:, t * 128:(t + 1) * 128], in_=ym[:, t, :], identity=identb[:])
                        return r
                    sc.op('pe', ytr, reads=[ymk, 'identb'], writes=[tbank])
                    ys = yst[qb % 2]
                    ysk = ('ysta', qb % 2)
                    sc.op('act', lambda e, ys=ys, pb=pb: e.copy(out=ys[:], in_=pb[:, 0:512]), reads=[tbank], writes=[ysk])
                    sc.dma('sp', lambda e, ys=ys, qb=qb: e.dma_start(out=yT_v[:, 4 + h, qb * 512:(qb + 1) * 512], in_=ys[:]), ysk, reads=[ysk], writes=['yT_d'])
            sc.barrier()
    sA.close()

    if stage <= 3.5:
        sc.finish()
        return nc, dbg

    s1.close()

    sW = ExitStack()
    aff = sb("aff", [128, NT, NE], F32, sW)
    g2rep = sb("g2rep", [128, D], F32, sW)
    with ExitStack() as pw:
        woutb = sb("woutb", [128, 8, D], BF16, pw)
        wrt = sb("wrt", [128, 8, NE], F32, pw)
        ytl = [sb("ytl%d" % k, [128, 8, 512], BF16, pw) for k in range(2)]
        xt2 = [sb("xt2_%d" % k, [128, D], F32, pw) for k in range(2)]
        tmpw = [sb("tmpw%d" % k, [128, D], F32, pw) for k in range(2)]
        x1t = [sb("x1t%d" % k, [128, D], F32, pw) for k in range(2)]
        h2f = [sb("h2f%d" % k, [128, D], F32, pw) for k in range(2)]
        h2b = [sb("h2b%d" % k, [128, D], BF16, pw) for k in range(2)]
        h2T = [sb("h2T%d" % k, [128, 8, 128], F32, pw) for k in range(2)]
        junkw = sb("junkw", [128, D], BF16, pw)
        stw = sb("stw", [128, NT, 8], F32, pw)
        esm = sb("esm", [128, 2, NE], F32, pw)
        wout_v = wout_d.rearrange("(kc p) n -> p kc n", p=128)
        sc.dma('pool', lambda e: e.dma_start(out=woutb[:, 0:4, :], in_=wout_v[:, 0:4, :]), 'woutb', writes=['woutb'])
        sc.dma('pool', lambda e: e.dma_start(out=woutb[:, 4:8, :], in_=wout_v[:, 4:8, :]), 'woutb', writes=['woutb'])
        sc.dma('sp', lambda e: e.dma_start(out=wrt[:], in_=wr_d.rearrange("(kc p) n -> p kc n", p=128)), 'wrt', writes=['wrt'])
        modW = sb("modW", [128, 3 * D], F32, pw)
        sc.dma('sp', lambda e: e.dma_start(out=modW[:], in_=modrow_d[0:1, 2 * D:5 * D].partition_broadcast(128)), 'modW', reads=['modrow_d'], writes=['mod'])
        sc.dma('sp', lambda e: e.dma_start(out=g2rep[:], in_=modrow_d[0:1, 5 * D:6 * D].partition_broadcast(128)), 'g2rep', reads=['modrow_d'], writes=['g2rep'])
        GATE1, SHIFT2, GS2 = modW[:, 0:D], modW[:, D:2 * D], modW[:, 2 * D:3 * D]
        def wA(i):
            b = i % 2
            g, k4 = i // 4, i % 4
            yk = ('ytl', g % 2)
            if k4 == 0:
                sc.dma('sp', lambda e, g=g: e.dma_start(out=ytl[g % 2][:], in_=yT_v[:, :, g * 512:(g + 1) * 512]), yk, writes=[yk])
            xk = ('xt2', b)
            sc.dma('sp', lambda e, i=i, b=b: e.dma_start(out=xt2[b][:], in_=x_d[i * 128:(i + 1) * 128, :]), xk, writes=[xk])
            banks = [('ps', 2 * b), ('ps', 2 * b + 1)]

            def mmw(e, i=i, b=b, g=g, k4=k4):
                for dh in range(2):
                    for c in range(8):
                        r = e.matmul(ps[:, 2 * b + dh, :], lhsT=ytl[g % 2][:, c, k4 * 128:(k4 + 1) * 128], rhs=woutb[:, c, dh * 512:(dh + 1) * 512],
                                     start=(c == 0), stop=(c == 7))
                return r
            sc.op('pe', mmw, reads=[yk, 'woutb'], writes=banks)

        def wB(i):
            b = i % 2
            g, k4 = i // 4, i % 4
            yk = ('ytl', g % 2)
            xk = ('xt2', b)
            banks = [('ps', 2 * b), ('ps', 2 * b + 1)]
            T = stw[:, i, :]
            tk = ('stw', i)
            mixv = ps[:, 2 * b:2 * b + 2, :].rearrange("p a n -> p (a n)")
            sc.op('dve', lambda e, b=b, mixv=mixv: e.tensor_tensor(out=tmpw[b][:], in0=mixv, in1=GATE1, op=ALU.mult), reads=banks + ['mod'], writes=[('tmpw', b)])
            sc.op('dve', lambda e, b=b: e.tensor_tensor(out=x1t[b][:], in0=tmpw[b][:], in1=xt2[b][:], op=ALU.add),
                  reads=[('tmpw', b), xk], writes=[('x1t', b)])
            sc.dma('sp', lambda e, i=i, b=b: e.dma_start(out=out_d[i * 128:(i + 1) * 128, :], in_=x1t[b][:]), ('x1s', b), reads=[('x1t', b)], writes=[('outd', i)])
            T = stw[:, i, :]
            tk = ('stw', i)
            sc.op('act', lambda e, b=b, T=T: e.activation(out=junkw[:], in_=x1t[b][:], func=AF.Square, accum_out=T[:, 0:1]), reads=[('x1t', b)], writes=['junkw', tk])
            sc.op('act', lambda e, T=T: e.activation(out=T[:, 1:2], in_=T[:, 0:1], func=AF.Sqrt, scale=1.0 / D, bias=EPS), reads=[tk], writes=[tk])
            sc.op('dve', lambda e, T=T: e.reciprocal(out=T[:, 2:3], in_=T[:, 1:2]), reads=[tk], writes=[tk])
            sc.op('dve', lambda e, b=b, T=T: e.scalar_tensor_tensor(out=tmpw[b][:], in0=x1t[b][:], scalar=T[:, 2:3], in1=GS2, op0=ALU.mult, op1=ALU.mult),
                  reads=[('x1t', b), tk, 'mod'], writes=[('tmpw', b)])
            sc.op('dve', lambda e, b=b: e.tensor_tensor(out=h2f[b][:], in0=tmpw[b][:], in1=SHIFT2, op=ALU.add), reads=[('tmpw', b), 'mod'], writes=[('h2f', b)])
            sc.op('act', lambda e, b=b: e.copy(out=h2b[b][:], in_=h2f[b][:]), reads=[('h2f', b)], writes=[('h2b', b)])
            sc.dma('sp', lambda e, i=i, b=b: e.dma_start(out=h2_d[i * 128:(i + 1) * 128, :], in_=h2b[b][:]), ('h2s', b), reads=[('h2b', b)], writes=[('h2d', i)])

        def wC(i):
            b = i % 2
            g, k4 = i // 4, i % 4
            yk = ('ytl', g % 2)
            xk = ('xt2', b)
            banks = [('ps', 2 * b), ('ps', 2 * b + 1)]
            T = stw[:, i, :]
            tk = ('stw', i)
            tb = [('ps', 4), ('ps', 5)]

            def trh(e, b=b):
                for kc in range(8):
                    r = e.transpose(out=ps[:, 4 + kc // 4, (kc % 4) * 128:(kc % 4 + 1) * 128], in_=h2f[b][:, kc * 128:(kc + 1) * 128], identity=ident)
                return r
            sc.op('pe', trh, reads=[('h2f', b), 'consts'], writes=tb)
            sc.op('act', lambda e, b=b: e.copy(out=h2T[b][:].rearrange("p k t -> p (k t)"), in_=ps[:, 4:6, :].rearrange("p a n -> p (a n)")),
                  reads=tb, writes=[('h2T', b)])
            lb = ('ps', 6 + b)

            def mml(e, b=b):
                for kc in range(8):
                    r = e.matmul(ps[:, 6 + b, 0:NE], lhsT=h2T[b][:, kc, :], rhs=wrt[:, kc, :], start=(kc == 0), stop=(kc == 7))
                return r
            sc.op('pe', mml, reads=[('h2T', b), 'wrt'], writes=[lb])
            sc.op('dve', lambda e, b=b, T=T: e.tensor_reduce(out=T[:, 3:4], in_=ps[:, 6 + b, 0:NE], axis=AX.X, op=ALU.max), reads=[lb], writes=[tk])
            sc.op('dve', lambda e, T=T: e.tensor_scalar(out=T[:, 4:5], in0=T[:, 3:4], scalar1=-1.0, scalar2=None, op0=ALU.mult), reads=[tk], writes=[tk])
            sc.op('act', lambda e, b=b, T=T: e.activation(out=esm[:, b, :], in_=ps[:, 6 + b, 0:NE], func=AF.Exp, bias=T[:, 4:5], accum_out=T[:, 5:6]),
                  reads=[lb, tk], writes=[('esm', b), tk])
            sc.op('dve', lambda e, T=T: e.reciprocal(out=T[:, 6:7], in_=T[:, 5:6]), reads=[tk], writes=[tk])
            sc.op('dve', lambda e, b=b, i=i, T=T: e.tensor_scalar(out=aff[:, i, :], in0=esm[:, b, :], scalar1=T[:, 6:7], scalar2=None, op0=ALU.mult),
                  reads=[('esm', b), tk], writes=['aff'])

        wA(0)
        for i in range(NT):
            if i + 1 < NT:
                wA(i + 1)
            wB(i)
            wC(i)
        if debug:
            o = dbg_out("dbg_aff", [128, NT, NE])
            sc.dma('sp', lambda e: e.dma_start(out=o[:, :, :], in_=aff[:]), 'dbgaff', reads=['aff'])
        sc.barrier()

    if stage <= 4:
        sc.finish()
        return nc, dbg

    rank_tok = sb("rank_tok", [128, NT, NE], F32, sW)
    R5 = sb("R5", [128, NT, NE, 5], BF16, sW)
    with ExitStack() as pr:
        affT = sb("affT", [NE, S], F32, pr)
        junkR = sb("junkR", [NE, S], F32, pr)
        mkT = sb("mkT", [NE, S], F32, pr)
        csT = sb("csT", [NE, S], F32, pr)
        bs = sb("bs", [NE, 4], F32, pr)
        r1 = sb("r1", [128, NT, NE], F32, pr)
        for rnd in range(2):
            banks = [('ps', k) for k in range(4)]

            def tra(e, rnd=rnd):
                for k in range(16):
                    i = rnd * 16 + k
                    r = e.transpose(out=ps[0:NE, k // 4, (k % 4) * 128:(k % 4 + 1) * 128], in_=aff[:, i, :], identity=ident)
                return r
            sc.op('pe', tra, reads=['aff', 'consts'], writes=banks)
            sc.op('act', lambda e, rnd=rnd: e.copy(out=affT[:, rnd * 2048:(rnd + 1) * 2048], in_=ps[0:NE, 0:4, :].rearrange("p a n -> p (a n)")),
                  reads=banks, writes=['affT'])
        sc.op('dve', lambda e: e.memset(bs[:, 0:1], 0.0), writes=['bs'])
        for n in range(NBIS):
            w = 2.0 ** (-(n + 1))
            sc.op('dve', lambda e, w=w: e.tensor_scalar(out=bs[:, 1:2], in0=bs[:, 0:1], scalar1=w, scalar2=None, op0=ALU.add), reads=['bs'], writes=['bs'])
            sc.op('dve', lambda e: e.tensor_scalar(out=junkR[:], in0=affT[:], scalar1=bs[:, 1:2], scalar2=None, op0=ALU.is_gt, op1=ALU.add,
                                                   accum_out=bs[:, 2:3]), reads=['affT', 'bs'], writes=['junkR', 'bs'])
            sc.op('dve', lambda e: e.tensor_scalar(out=bs[:, 3:4], in0=bs[:, 2:3], scalar1=CAP - 0.5, scalar2=None, op0=ALU.is_gt), reads=['bs'], writes=['bs'])
            sc.op('dve', lambda e, w=w: e.scalar_tensor_tensor(out=bs[:, 0:1], in0=bs[:, 3:4], scalar=w, in1=bs[:, 0:1], op0=ALU.mult, op1=ALU.add),
                  reads=['bs'], writes=['bs'])
        sc.op('dve', lambda e: e.tensor_scalar(out=mkT[:], in0=affT[:], scalar1=bs[:, 0:1], scalar2=None, op0=ALU.is_gt), reads=['affT', 'bs'], writes=['mkT'])
        sc.op('pool', lambda e: e.memset(junkR[:], 1.0), reads=[], writes=['junkR'])
        sc.op('dve', lambda e: e.tensor_tensor_scan(out=csT[:], data0=junkR[:], data1=mkT[:], initial=0.0, op0=ALU.mult, op1=ALU.add),
              reads=['junkR', 'mkT'], writes=['csT'])
        sc.op('dve', lambda e: e.tensor_tensor(out=csT[:], in0=csT[:], in1=mkT[:], op=ALU.mult), reads=['csT', 'mkT'], writes=['csT'])
        sc.op('dve', lambda e: e.tensor_scalar(out=csT[:], in0=csT[:], scalar1=-1.0, scalar2=None, op0=ALU.add), reads=['csT'], writes=['csT'])
        rb = ('ps', 4)

        def trr(e):
            for i in range(NT):
                r = e.transpose(out=ps[:, 4, i * NE:(i + 1) * NE], in_=csT[:, i * 128:(i + 1) * 128], identity=consts[0:NE, C_IDENT:C_IDENT + NE])
            return r
        sc.op('pe', trr, reads=['csT', 'consts'], writes=[rb])
        sc.op('act', lambda e: e.copy(out=rank_tok[:].rearrange("p t e -> p (t e)"), in_=ps[:, 4, :]), reads=[rb], writes=['rank_tok'])
        sc.op('pool', lambda e: e.tensor_copy(out=R5[:, :, :, 0], in_=consts[:, C_TLO:C_TLO + 1].unsqueeze(2).broadcast_to([128, NT, NE])),
              reads=['consts'], writes=['R5'])
        sc.op('pool', lambda e: e.tensor_copy(out=R5[:, :, :, 1], in_=consts[:, C_THI:C_THI + NT].unsqueeze(2).broadcast_to([128, NT, NE])),
              reads=['consts'], writes=['R5'])
        sc.op('dve', lambda e: e.tensor_copy(out=R5[:, :, :, 2], in_=aff[:]), reads=['aff'], writes=['R5'])
        sc.op('dve', lambda e: e.tensor_tensor(out=r1[:], in0=aff[:], in1=R5[:, :, :, 2], op=ALU.subtract), reads=['aff', 'R5'], writes=['r1'])
        sc.op('dve', lambda e: e.tensor_copy(out=R5[:, :, :, 3], in_=r1[:]), reads=['r1'], writes=['R5'])
        sc.op('dve', lambda e: e.tensor_tensor(out=r1[:], in0=r1[:], in1=R5[:, :, :, 3], op=ALU.subtract), reads=['r1', 'R5'], writes=['r1'])
        sc.op('dve', lambda e: e.tensor_copy(out=R5[:, :, :, 4], in_=r1[:]), reads=['r1'], writes=['R5'])
        if debug:
            o = dbg_out("dbg_rank", [128, NT, NE])
            sc.dma('sp', lambda e: e.dma_start(out=o[:, :, :], in_=rank_tok[:]), 'dbgrank', reads=['rank_tok'])
            o = dbg_out("dbg_thr", [NE, 4])
            sc.dma('sp', lambda e: e.dma_start(out=o[:, :], in_=bs[:]), 'dbgthr', reads=['bs'])
        sc.barrier()

    if stage <= 4.5:
        sc.finish()
        return nc, dbg

    n_exp = NE if stage >= 6 else int(round((stage - 5) * 10)) + 1
    with ExitStack() as pe_:
        NSLOT = 6
        ring = [sb("ring%d" % k, [128, 8, D], BF16, pe_) for k in range(NSLOT)]
        Pm = sb("Pm", [128, NT, 512], BF16, pe_)
        xe = sb("xe", [128, 4, D], BF16, pe_)
        xeT2 = [sb("xeT%d" % k, [128, 8, 512], BF16, pe_) for k in range(2)]
        aT = sb("aT", [128, 16, 512], BF16, pe_)
        sgt = [sb("sgt%d" % k, [128, 512], BF16, pe_) for k in range(2)]
        yv = sb("yv", [128, 4, D], F32, pe_)
        idf = sb("idf", [128, 2, 8], F32, pe_)
        idp = sb("idp", [128, 2, 32], F32, pe_)
        idxi = [sb("idxi%d" % k, [128, 4], I32, pe_) for k in range(2)]
        wg_v = wg_d.rearrange("(e kc p) n -> p e kc n", p=128, kc=8)
        wu_v = wu_d.rearrange("(e kc p) n -> p e kc n", p=128, kc=8)
        wd_v = wd_d.rearrange("(e fh fc p) n -> p e fh fc n", p=128, fc=8, fh=2)

        def load_piece(e_, k):
            slot = ring[k]
            key = ('ring', k)
            if k < 4:
                src = (wg_v if k % 2 == 0 else wu_v)[:, e_, :, (k // 2) * D:(k // 2 + 1) * D]
            else:
                src = wd_v[:, e_, k - 4, :, :]
            sc.dma('pool', lambda e, slot=slot, src=src: e.dma_start(out=slot[:], in_=src), key, writes=[key])

        def prep_a(e_):
            for i in range(NT):
                sc.op('dve', lambda e, i=i: e.tensor_scalar(out=Pm[:, i, :], in0=consts[:, C_IOTA:C_IOTA + 512], scalar1=rank_tok[:, i, e_:e_ + 1],
                                                            scalar2=None, op0=ALU.is_equal), reads=['consts', 'rank_tok'], writes=['Pm'])

        def prep_b(e_):
            b = e_ % 2
            ibank = ('ps', 7)

            def imm(e):
                for cc in range(4):
                    for i in range(NT):
                        r = e.matmul(ps[:, 7, cc * 8:cc * 8 + 5], lhsT=Pm[:, i, cc * 128:(cc + 1) * 128], rhs=R5[:, i, e_, :],
                                     start=(i == 0), stop=(i == NT - 1))
                return r
            sc.op('pe', imm, reads=['Pm', 'R5'], writes=[ibank])
            sc.op('act', lambda e, b=b: e.copy(out=idp[:, b, :].rearrange("p (c k) -> p c k", k=8)[:, :, 0:5], in_=ps[:, 7, 0:32].rearrange("p (c k) -> p c k", k=8)[:, :, 0:5]), reads=[ibank], writes=[('idp', b)])
            ibank = ('idp', b)
            I3 = idp[:, b, :].rearrange("p (c k) -> p c k", k=8)
            F_ = idf[:, b, :]
            fk = ('idf', b)
            sc.op('dve', lambda e, F_=F_, I3=I3: e.scalar_tensor_tensor(out=F_[:, 0:4].unsqueeze(2), in0=I3[:, :, 1:2], scalar=128.0, in1=I3[:, :, 0:1],
                                                                      op0=ALU.mult, op1=ALU.add), reads=[ibank], writes=[fk])
            sc.op('dve', lambda e, F_=F_, I3=I3: e.tensor_reduce(out=F_[:, 4:8], in_=I3[:, :, 2:5], axis=AX.X, op=ALU.add), reads=[ibank], writes=[fk])
            ik = ('idxi', b)
            sc.op('dve', lambda e, F_=F_, b=b: e.tensor_copy(out=idxi[b][:], in_=F_[:, 0:4]), reads=[fk], writes=[ik])
            for cc in range(4):
                sc.dma('pool', lambda e, cc=cc, b=b: e.indirect_dma_start(
                    out=xe[:, cc, :], out_offset=None, in_=h2_d[:, :],
                    in_offset=bass.IndirectOffsetOnAxis(ap=idxi[b][:, cc:cc + 1], axis=0)), 'xe', reads=[ik], writes=['xe'])

        def prep_c(e_):
            xeT = xeT2[e_ % 2]
            for kc in range(8):
                tb = 6
                tbank = ('ps', tb)
                pb = ps[:, tb, :].bitcast(BF16)

                def trx(e, kc=kc, pb=pb):
                    for cc in range(4):
                        r = e.transpose(out=pb[:, cc * 128:(cc + 1) * 128], in_=xe[:, cc, kc * 128:(kc + 1) * 128], identity=identb[:])
                    return r
                sc.op('pe', trx, reads=['xe', 'identb'], writes=[tbank])
                sc.op('act', lambda e, kc=kc, pb=pb, xeT=xeT: e.copy(out=xeT[:, kc, :], in_=pb[:, 0:512]), reads=[tbank], writes=[('xeT', e_ % 2)])

        for k in range(NSLOT):
            load_piece(0, k)
        prep_a(0)
        prep_b(0)
        prep_c(0)
        for e_ in range(n_exp):
            b = e_ % 2
            xeT = xeT2[b]
            xk_ = ('xeT', b)
            if e_ + 1 < n_exp:
                prep_a(e_ + 1)
            for fh in range(2):
                gk, uk = ('ring', 2 * fh), ('ring', 2 * fh + 1)
                Wg, Wu = ring[2 * fh], ring[2 * fh + 1]
                for fo in range(8):
                    gb, ubk = fo % 2, 2 + (fo % 2)

                    def mmg(e, Wg=Wg, fo=fo, gb=gb):
                        for kc in range(8):
                            r = e.matmul(ps[:, gb, :], lhsT=Wg[:, kc, fo * 128:(fo + 1) * 128], rhs=xeT[:, kc, :], start=(kc == 0), stop=(kc == 7))
                        return r

                    def mmu(e, Wu=Wu, fo=fo, ubk=ubk):
                        for kc in range(8):
                            r = e.matmul(ps[:, ubk, :], lhsT=Wu[:, kc, fo * 128:(fo + 1) * 128], rhs=xeT[:, kc, :], start=(kc == 0), stop=(kc == 7))
                        return r
                    sc.op('pe', mmg, reads=[gk, xk_], writes=[('ps', gb)])
                    sc.op('pe', mmu, reads=[uk, xk_], writes=[('ps', ubk)])
                    sg = sgt[fo % 2]
                    sk = ('sgt', fo % 2)
                    sc.op('act', lambda e, sg=sg, gb=gb: e.activation(out=sg[:], in_=ps[:, gb, :], func=AF.Silu), reads=[('ps', gb)], writes=[sk])
                    fidx = fh * 8 + fo
                    sc.op('dve', lambda e, sg=sg, ubk=ubk, fidx=fidx: e.tensor_tensor(out=aT[:, fidx, :], in0=ps[:, ubk, :], in1=sg[:], op=ALU.mult),
                          reads=[('ps', ubk), sk], writes=[('aT', fidx)])
                if e_ + 1 < n_exp:
                    load_piece(e_ + 1, 2 * fh)
                    load_piece(e_ + 1, 2 * fh + 1)
                    if fh == 0:
                        prep_b(e_ + 1)
                    else:
                        prep_c(e_ + 1)
            F_ = idf[:, b, :]
            for cc in range(4):
                for dh in range(2):
                    db = 4 + ((cc * 2 + dh) % 2)

                    def mmd(e, cc=cc, dh=dh, db=db):
                        for fc in range(16):
                            r = e.matmul(ps[:, db, :], lhsT=aT[:, fc, cc * 128:(cc + 1) * 128], rhs=ring[4 + fc // 8][:, fc % 8, dh * 512:(dh + 1) * 512],
                                         start=(fc == 0), stop=(fc == 15))
                        return r
                    sc.op('pe', mmd, reads=[('aT', f) for f in range(16)] + [('ring', 4), ('ring', 5)], writes=[('ps', db)])
                    sc.op('dve', lambda e, cc=cc, dh=dh, db=db, F_=F_: e.scalar_tensor_tensor(
                        out=yv[:, cc, dh * 512:(dh + 1) * 512], in0=ps[:, db, :], scalar=F_[:, 4 + cc:5 + cc], in1=g2rep[:, dh * 512:(dh + 1) * 512],
                        op0=ALU.mult, op1=ALU.mult), reads=[('ps', db), ('idf', b), 'g2rep'], writes=[('yv', cc)])
            if e_ + 1 < n_exp:
                load_piece(e_ + 1, 4)
                load_piece(e_ + 1, 5)
            for cc in range(4):
                sc.dma('pool', lambda e, cc=cc, b=b: e.indirect_dma_start(
                    out=out_d[:, :], out_offset=bass.IndirectOffsetOnAxis(ap=idxi[b][:, cc:cc + 1], axis=0),
                    in_=yv[:, cc, :], in_offset=None, compute_op=ALU.add), 'scat', reads=[('yv', cc), ('idxi', b)], writes=['outd'])
        sc.barrier()
    sW.close()
    sc.finish()
    return nc, dbg


def prep_inputs(inputs, b):
    f = lambda a: np.ascontiguousarray(a, dtype=np.float32)
    m = {
        "x": f(inputs["x"][b]),
        "cT": f(inputs["c"][b].reshape(8, 128).T),
        "pos": np.ascontiguousarray(inputs["positions"][b].reshape(NT, 128).T.astype(np.int32)),
        "norm1_g": f(inputs["norm1_g"][0:1]),
        "norm2_g": f(inputs["norm2_g"][0:1]),
        "w_ada": f(inputs["w_ada"][0]),
        "b_ada": f(inputs["b_ada"][0:1]),
        "w_in": f(inputs["w_in"][0]),
        "convT": f(inputs["mlstm_conv_w"][0].T.reshape(8, 128, 5).transpose(1, 0, 2).reshape(128, 40)),
        "gate_b": f(inputs["mlstm_gate_b"][0:1]),
        "mnorm_g": f(inputs["mlstm_norm_g"][0:1]),
        "qk_g": f(inputs["diff_qk_g"][0].reshape(1, 128)),
        "lam": f(inputs["diff_lambda"][0].reshape(1, 256)),
        "subln_g": f(inputs["diff_subln_g"][0:1]),
        "w_out": f(inputs["w_out"][0]),
        "w_router": f(inputs["w_router"][0]),
        "consts": make_consts(),
    }
    return m


def kernel(**inputs):
    nc, _ = build()
    shared = None
    in_maps = []
    for b in range(8):
        m = prep_inputs(inputs, b)
        if shared is None:
            shared = {
                "w_gate_e": np.ascontiguousarray(inputs["w_gate_e"][0].reshape(NE * D, 2 * D), dtype=np.float32),
                "w_up_e": np.ascontiguousarray(inputs["w_up_e"][0].reshape(NE * D, 2 * D), dtype=np.float32),
                "w_down_e": np.ascontiguousarray(inputs["w_down_e"][0].reshape(NE * 2 * D, D), dtype=np.float32),
            }
        m.update(shared)
        in_maps.append(m)
    res = run_bass_kernel_spmd(nc, in_maps, core_ids=list(range(8)))
    return np.stack([np.asarray(r["out"]) for r in res.results], axis=0).astype(np.float32)
```

```python
import math
from contextlib import ExitStack

import numpy as np
import concourse.bass as bass
import concourse.mybir as mybir
from concourse.bass_utils import run_bass_kernel_spmd

F32 = mybir.dt.float32
BF16 = mybir.dt.bfloat16
I32 = mybir.dt.int32
AF = mybir.ActivationFunctionType
ALU = mybir.AluOpType
AX = mybir.AxisListType

S = 4096
D = 1024
NT = 32
NG = 8
EPS = 1e-6
D_IN = 3600
NE = 16
CAP = 512
LAM_INIT = 0.8 - 0.6 * math.exp(-0.3 * 0)
NBIS = 27

SAME_ENG_SYNC = True

C_IDENT = 0
C_TL = 128
C_TU = 256
C_TLS = 384
C_TUS = 512
C_INDA = 640
C_INDB = 768
C_IOTA = 896
C_TLO = 1408
C_FREQ = 1409
C_THI = 1417
NCONST = 1449


def make_consts():
    c = np.zeros((128, NCONST), np.float32)
    p = np.arange(128)
    s = p[:, None]
    j = p[None, :]
    same = (s // 64) == (j // 64)
    c[:, C_IDENT:C_IDENT + 128] = (s == j)
    c[:, C_TL:C_TL + 128] = same & (s <= j)
    c[:, C_TU:C_TU + 128] = same & (s >= j)
    c[:, C_TLS:C_TLS + 128] = same & (s < j)
    c[:, C_TUS:C_TUS + 128] = same & (s > j)
    c[:, C_INDA:C_INDA + 128] = (s < 64) & (j >= 0)
    c[:, C_INDB:C_INDB + 128] = (s >= 64) & (j >= 0)
    c[:, C_IOTA:C_IOTA + 512] = np.arange(512)[None, :]
    c[:, C_TLO] = p
    inv_freq = (500000.0 ** (-np.arange(0, 16, 2, dtype=np.float32) / 16)).astype(np.float32)
    c[:, C_FREQ:C_FREQ + 8] = (inv_freq.astype(np.float64) / (2 * np.pi)).astype(np.float32)[None, :]
    c[:, C_THI:C_THI + 32] = np.arange(32)[None, :]
    return c


class Sched:
    def __init__(self, nc, es):
        self.nc = nc
        self.es = es
        self.E = {'pe': nc.tensor, 'act': nc.scalar, 'dve': nc.vector, 'pool': nc.gpsimd, 'sp': nc.sync}
        self.sem = {k: es.enter_context(nc.semaphore('s_' + k)) for k in self.E}
        self.cnt = {k: 0 for k in self.E}
        self.seen = {k: {} for k in self.E}
        self.reg = {}
        self.dsem = {}
        self.dsem_by_sid = {}
        self.ninstr = {k: 0 for k in self.E}

    def _deps(self, reads, writes):
        deps = {}

        def add(tok):
            if tok is None:
                return
            if tok[0].startswith('d_'):
                tok = (tok[0], tok[1], self.dsem_by_sid[tok[0]][1])
            if tok[0] not in deps or deps[tok[0]][2] < tok[2]:
                deps[tok[0]] = tok
        for r in reads:
            st = self.reg.get(r)
            if st:
                add(st[0])
        for w in writes:
            st = self.reg.get(w)
            if st:
                add(st[0])
                for t in st[1].values():
                    add(t)
        return deps

    def _wait(self, eng, deps):
        for sid, (_, h, v) in deps.items():
            if sid == 'e_' + eng and not SAME_ENG_SYNC:
                continue
            if self.seen[eng].get(sid, 0) >= v:
                continue
            self.E[eng].wait_ge(h, v)
            self.seen[eng][sid] = v

    def _commit(self, tok, reads, writes):
        for r in reads:
            st = self.reg.setdefault(r, [None, {}])
            st[1][tok[0]] = tok
        for w in writes:
            self.reg[w] = [tok, {}]

    def op(self, eng, fn, reads=(), writes=()):
        self._wait(eng, self._deps(reads, writes))
        ins = fn(self.E[eng])
        self.cnt[eng] += 1
        ins.then_inc(self.sem[eng], 1)
        self._commit(('e_' + eng, self.sem[eng], self.cnt[eng]), reads, writes)

    def dma(self, q, fn, semkey, reads=(), writes=()):
        self._wait(q, self._deps(reads, writes))
        ins = fn(self.E[q])
        d = self.dsem.get(semkey)
        if d is None:
            d = [self.es.enter_context(self.nc.semaphore('d%d' % len(self.dsem))), 0]
            self.dsem[semkey] = d
            self.dsem_by_sid['d_' + str(semkey)] = d
        d[1] += 16
        ins.then_inc(d[0], 16)
        self._commit(('d_' + str(semkey), d[0], d[1]), reads, writes)

    def barrier(self):
        toks = {}
        for k in self.E:
            if self.cnt[k]:
                toks['e_' + k] = ('e_' + k, self.sem[k], self.cnt[k])
        for key, d in self.dsem.items():
            if d[1]:
                toks['d_' + str(key)] = ('d_' + str(key), d[0], d[1])
        for k in self.E:
            self._wait(k, toks)

    def finish(self):
        toks = {}
        for k in self.E:
            if self.cnt[k]:
                toks['e_' + k] = ('e_' + k, self.sem[k], self.cnt[k])
        for key, d in self.dsem.items():
            if d[1]:
                toks['d_' + str(key)] = ('d_' + str(key), d[0], d[1])
        self._wait('sp', toks)


def build(stage=99, debug=False):
    nc = bass.Bass("TRN2", target_bir_lowering=False)
    es = ExitStack()
    sc = Sched(nc, es)

    def din(name, shape, dt=F32):
        return nc.dram_tensor(name, list(shape), dt, kind="ExternalInput").ap()

    x_d = din("x", [S, D])
    cT_d = din("cT", [128, 8])
    pos_d = din("pos", [128, NT], I32)
    n1g_d = din("norm1_g", [1, D])
    n2g_d = din("norm2_g", [1, D])
    wada_d = din("w_ada", [D, 6 * D])
    bada_d = din("b_ada", [1, 6 * D])
    win_d = din("w_in", [D, D_IN])
    convT_d = din("convT", [128, 40])
    gateb_d = din("gate_b", [1, 16])
    mng_d = din("mnorm_g", [1, 512])
    qkg_d = din("qk_g", [1, 128])
    lam_d = din("lam", [1, 256])
    subg_d = din("subln_g", [1, 128])
    wout_d = din("w_out", [D, D])
    wr_d = din("w_router", [D, NE])
    consts_d = din("consts", [128, NCONST])
    if stage >= 5:
        wg_d = din("w_gate_e", [NE * D, 2 * D])
        wu_d = din("w_up_e", [NE * D, 2 * D])
        wd_d = din("w_down_e", [NE * 2 * D, D])
    out_d = nc.dram_tensor("out", [S, D], F32, kind="ExternalOutput").ap()
    skind = "ExternalOutput" if debug else "Internal"
    yT_d = nc.dram_tensor("yT_d", [D, S], BF16, kind=skind).ap()
    h2_d = nc.dram_tensor("h2_d", [S, D], BF16, kind=skind).ap()
    dbg = {}

    def dbg_out(name, shape, dt=F32):
        if debug:
            dbg[name] = nc.dram_tensor(name, list(shape), dt, kind="ExternalOutput").ap()
            return dbg[name]
        return None

    uniq = [0]

    def sb(name, shape, dt=F32, stack=es):
        uniq[0] += 1
        return stack.enter_context(nc.sbuf_tensor("sb%d_%s" % (uniq[0], name), list(shape), dt))

    ps = es.enter_context(nc.psum_tensor("ps", [128, 8, 512], F32))
    consts = sb("consts", [128, NCONST])
    identb = sb("identb", [128, 128], BF16)
    s1 = ExitStack()
    prm = sb("prm", [128, 16 + 512 + 128 + 256 + 128], F32, s1)
    P_GB, P_MNG, P_QKG, P_LAM, P_SUB = 0, 16, 528, 656, 912
    convT = sb("convT", [128, 40], F32, s1)
    maskF = sb("maskF", [128, 128], BF16, s1)
    maskB = sb("maskB", [128, 128], BF16, s1)
    small = sb("small", [128, 64], F32, s1)
    SM_NB, SM_NLAM = 0, 1
    ident = consts[:, C_IDENT:C_IDENT + 128]

    h1T = sb("h1T", [128, 8, S], BF16, s1)
    gm = sb("gm", [128, NT, 16], F32, s1)
    smod = ExitStack()
    mod = sb("mod", [128, 6 * D], F32, smod)
    modrow_d = nc.dram_tensor("modrow_d", [1, 6 * D], F32, kind="Internal").ap()
    CK = 'consts'
    sc.dma('sp', lambda e: e.dma_start(out=consts[:], in_=consts_d[:, :]), CK, writes=['consts'])
    sc.dma('sp', lambda e: e.dma_start(out=convT[:], in_=convT_d[:, :]), CK, writes=['convT'])
    for (off, src, n) in [(P_GB, gateb_d, 16), (P_MNG, mng_d, 512),
                          (P_QKG, qkg_d, 128), (P_LAM, lam_d, 256), (P_SUB, subg_d, 128)]:
        sc.dma('sp', lambda e, off=off, src=src, n=n: e.dma_start(
            out=prm[:, off:off + n], in_=src[0:1, :].partition_broadcast(128)), CK, writes=['prm'])
    sc.dma('sp', lambda e: e.dma_start(out=mod[:], in_=bada_d[0:1, :].partition_broadcast(128)), CK, writes=['mod'])

    with ExitStack() as p0:
        cT = sb("cT", [128, 8], F32, p0)
        prmA = sb("prmA", [128, 2 * D], F32, p0)
        sc.dma('sp', lambda e: e.dma_start(out=prmA[:, 0:D], in_=n1g_d[0:1, :].partition_broadcast(128)), CK, writes=['prmA'])
        sc.dma('sp', lambda e: e.dma_start(out=prmA[:, D:2 * D], in_=n2g_d[0:1, :].partition_broadcast(128)), CK, writes=['prmA'])
        scT = sb("scT", [128, 8], F32, p0)
        lc = sb("lc", [128, 8, 128], BF16, p0)
        wa = [sb("wa%d" % i, [128, 8, 512], BF16, p0) for i in range(2)]
        sc.dma('sp', lambda e: e.dma_start(out=cT[:], in_=cT_d[:, :]), 'cT', writes=['cT'])
        sc.op('act', lambda e: e.activation(out=scT[:], in_=cT[:], func=AF.Silu), reads=['cT'], writes=['scT'])
        sc.op('dve', lambda e: e.tensor_copy(out=lc[:], in_=scT[:].unsqueeze(2).broadcast_to([128, 8, 128])),
              reads=['scT'], writes=['lc'])
        sc.op('dve', lambda e: e.tensor_copy(out=identb[:], in_=ident), reads=['consts'], writes=['identb'])
        sc.op('dve', lambda e: e.tensor_copy(out=maskF[:], in_=consts[:, C_TL:C_TL + 128]), reads=['consts'], writes=['maskF'])
        sc.op('dve', lambda e: e.tensor_copy(out=maskB[:], in_=consts[:, C_TU:C_TU + 128]), reads=['consts'], writes=['maskB'])
        wada_v = wada_d.rearrange("(kc p) n -> p kc n", p=128)
        for n in range(12):
            w = wa[n % 2]
            wk = ('wa', n % 2)
            sc.dma('pool', lambda e, w=w, n=n: e.dma_start(out=w[:], in_=wada_v[:, :, n * 512:(n + 1) * 512]),
                   wk, writes=[wk])
            bank = ('ps', n % 2)

            def mm(e, w=w, n=n):
                for kc in range(8):
                    r = e.matmul(ps[:, n % 2, :], lhsT=lc[:, kc, :], rhs=w[:, kc, :], start=(kc == 0), stop=(kc == 7))
                return r
            sc.op('pe', mm, reads=['lc', wk], writes=[bank])
            sc.op('dve', lambda e, n=n: e.tensor_tensor(out=mod[:, n * 512:(n + 1) * 512], in0=ps[:, n % 2, :],
                                                        in1=mod[:, n * 512:(n + 1) * 512], op=ALU.add),
                  reads=[bank, 'mod'], writes=['mod'])
        sc.op('dve', lambda e: e.scalar_tensor_tensor(out=mod[:, D:2 * D], in0=mod[:, D:2 * D], scalar=1.0,
                                                      in1=prmA[:, 0:D], op0=ALU.add, op1=ALU.mult),
              reads=['mod', 'prmA'], writes=['mod'])
        sc.op('dve', lambda e: e.scalar_tensor_tensor(out=mod[:, 4 * D:5 * D], in0=mod[:, 4 * D:5 * D], scalar=1.0,
                                                      in1=prmA[:, D:2 * D], op0=ALU.add, op1=ALU.mult),
              reads=['mod', 'prmA'], writes=['mod'])
        if debug:
            o = dbg_out("dbg_mod", [128, 6 * D])
            sc.dma('sp', lambda e: e.dma_start(out=o[:, :], in_=mod[:]), 'dbgmod', reads=['mod'])
        sc.barrier()
    SHIFT1, GS1, GATE1, SHIFT2, GS2, GATE2 = [mod[:, i * D:(i + 1) * D] for i in range(6)]

    if stage <= 0:
        sc.finish()
        return nc, dbg

    with ExitStack() as p1:
        xt = [sb("xt%d" % i, [128, D], F32, p1) for i in range(2)]
        junk = sb("junk1", [128, D], BF16, p1)
        tmp = [sb("tmp1_%d" % i, [128, D], F32, p1) for i in range(2)]
        h1b = [sb("h1b%d" % i, [128, D], BF16, p1) for i in range(2)]
        st1 = sb("st1", [128, NT, 3], F32, p1)
        wgt = sb("wgt", [128, 8, 16], BF16, p1)
        win_v = win_d.rearrange("(kc p) n -> p kc n", p=128)
        sc.dma('pool', lambda e: e.dma_start(out=wgt[:], in_=win_v[:, :, 2048:2064]), 'wgt', writes=['wgt'])
        def p1A(i):
            b = i % 2
            xk, tk, hk = ('xt', b), ('tmp1', b), ('h1b', b)
            sc.dma('sp', lambda e, i=i, b=b: e.dma_start(out=xt[b][:], in_=x_d[i * 128:(i + 1) * 128, :]), xk, writes=[xk])
            sc.op('act', lambda e, i=i, b=b: e.activation(out=junk[:], in_=xt[b][:], func=AF.Square,
                                                          accum_out=st1[:, i, 0:1]), reads=[xk], writes=['junk1', ('st1', i)])
            sc.op('act', lambda e, i=i: e.activation(out=st1[:, i, 1:2], in_=st1[:, i, 0:1], func=AF.Sqrt,
                                                     scale=1.0 / D, bias=EPS), reads=[('st1', i)], writes=[('st1', i)])
            sc.op('dve', lambda e, i=i: e.reciprocal(out=st1[:, i, 2:3], in_=st1[:, i, 1:2]),
                  reads=[('st1', i)], writes=[('st1', i)])
            sc.op('dve', lambda e, i=i, b=b: e.scalar_tensor_tensor(out=tmp[b][:], in0=xt[b][:], scalar=st1[:, i, 2:3],
                                                                    in1=GS1, op0=ALU.mult, op1=ALU.mult),
                  reads=[xk, ('st1', i), 'mod'], writes=[tk])
            sc.op('dve', lambda e, b=b: e.tensor_tensor(out=h1b[b][:], in0=tmp[b][:], in1=SHIFT1, op=ALU.add),
                  reads=[tk, 'mod'], writes=[hk])

        def p1B(i):
            b = i % 2
            xk, tk, hk = ('xt', b), ('tmp1', b), ('h1b', b)
            bank = ('ps', 2 + b)
            pb = ps[:, 2 + b, :].bitcast(BF16)

            def tr(e, b=b, pb=pb):
                for kc in range(8):
                    r = e.transpose(out=pb[:, kc * 128:(kc + 1) * 128], in_=h1b[b][:, kc * 128:(kc + 1) * 128], identity=identb[:])
                return r
            sc.op('pe', tr, reads=[hk, 'identb'], writes=[bank])
            sc.op('act', lambda e, i=i, pb=pb: e.copy(out=h1T[:, :, i * 128:(i + 1) * 128],
                                                      in_=pb.rearrange("p (k t) -> p k t", k=8)),
                  reads=[bank], writes=[('h1T', i)])
            gbank = ('ps', 4 + b)

            def gmm(e, i=i, b=b):
                for kc in range(8):
                    r = e.matmul(ps[:, 4 + b, 0:16], lhsT=h1T[:, kc, i * 128:(i + 1) * 128], rhs=wgt[:, kc, :],
                                 start=(kc == 0), stop=(kc == 7))
                return r
            sc.op('pe', gmm, reads=[('h1T', i), 'wgt'], writes=[gbank])
            sc.op('dve', lambda e, i=i, b=b: e.tensor_tensor(out=gm[:, i, :], in0=ps[:, 4 + b, 0:16],
                                                             in1=prm[:, P_GB:P_GB + 16], op=ALU.add),
                  reads=[gbank, 'prm'], writes=['gm'])

        p1A(0)
        for i in range(NT):
            if i + 1 < NT:
                p1A(i + 1)
            p1B(i)
        if debug:
            o = dbg_out("dbg_h1T", [128, 8, S], BF16)
            sc.dma('sp', lambda e: e.dma_start(out=o[:, :, :], in_=h1T[:]), 'dbgh1T', reads=[('h1T', i) for i in range(NT)])
            o3 = dbg_out("dbg_st1", [128, NT, 3])
            sc.dma('sp', lambda e: e.dma_start(out=o3[:, :, :], in_=st1[:]), 'dbgst1', reads=[('st1', i) for i in range(NT)])
            o4 = dbg_out("dbg_tmp", [128, D])
            sc.dma('sp', lambda e: e.dma_start(out=o4[:, :], in_=tmp[1][:]), 'dbgtmp', reads=[('tmp1', 1)])
            o5 = dbg_out("dbg_h1b", [128, D], BF16)
            sc.dma('sp', lambda e: e.dma_start(out=o5[:, :], in_=h1b[1][:]), 'dbgh1b', reads=[('h1b', 1)])
            o2 = dbg_out("dbg_gm", [128, NT, 16])
            sc.dma('sp', lambda e: e.dma_start(out=o2[:, :, :], in_=gm[:]), 'dbggm', reads=['gm'])
        sc.barrier()

    sc.dma('sp', lambda e: e.dma_start(out=modrow_d[0:1, :], in_=mod[0:1, :]), 'modrow', reads=['mod'], writes=['modrow_d'])
    sc.barrier()
    smod.close()

    if stage <= 1:
        sc.finish()
        return nc, dbg

    LNK = -0.5 * math.log(128.0)
    cum = sb("cum", [128, 4, NT, 4], F32, s1)
    egs = sb("egs", [128, 4, NT, 4], F32, s1)
    tw = sb("tw", [128, 6, NT, 4], F32, s1)
    with ExitStack() as pg:
        lsg = sb("lsg", [128, 2, NT, 4], F32, pg)
        tg = sb("tg", [128, 2, NT, 4], F32, pg)
        for d, c0 in ((0, 4), (1, 12)):
            sc.op('act', lambda e, d=d, c0=c0: e.activation(out=tg[:, d, :, :], in_=gm[:, :, c0:c0 + 4], func=AF.Exp, scale=-1.0),
                  reads=['gm'], writes=['tg'])
        sc.op('act', lambda e: e.activation(out=tg[:], in_=tg[:], func=AF.Ln, bias=1.0), reads=['tg'], writes=['tg'])
        sc.op('dve', lambda e: e.tensor_scalar(out=lsg[:], in0=tg[:], scalar1=-1.0, scalar2=None, op0=ALU.mult),
              reads=['tg'], writes=['lsg'])
        lf = lsg[:, 0, :, :].rearrange("p t h -> p (t h)")
        lb = lsg[:, 1, :, :].rearrange("p t h -> p (t h)")
        specs = [(C_TL, lf), (C_TUS, lf), (C_TU, lb), (C_TLS, lb)]

        def cmm(e):
            for k, (co, rhs) in enumerate(specs):
                r = e.matmul(ps[:, 6, k * 128:(k + 1) * 128], lhsT=consts[:, co:co + 128], rhs=rhs, start=True, stop=True)
            return r
        sc.op('pe', cmm, reads=['lsg', 'consts'], writes=[('ps', 6)])
        sc.op('act', lambda e: e.copy(out=cum[:].rearrange("p a t h -> p (a t h)"), in_=ps[:, 6, :]),
              reads=[('ps', 6)], writes=['cum'])
        specs2 = [(C_INDA, lf), (C_INDB, lf), (C_INDA, lb), (C_INDB, lb)]

        def gmm2(e):
            for k, (co, rhs) in enumerate(specs2):
                r = e.matmul(ps[:, 7, k * 128:(k + 1) * 128], lhsT=consts[:, co:co + 128], rhs=rhs, start=True, stop=True)
            return r
        sc.op('pe', gmm2, reads=['lsg', 'consts'], writes=[('ps', 7)])
        sc.op('act', lambda e: e.activation(out=egs[:].rearrange("p a t h -> p (a t h)"), in_=ps[:, 7, :], func=AF.Exp),
              reads=[('ps', 7)], writes=['egs'])
        for d, ic in ((0, 0), (1, 8)):
            ig = gm[:, :, ic:ic + 4]
            a_ = cum[:, 2 * d, :, :]
            r_ = cum[:, 2 * d + 1, :, :]
            sc.op('dve', lambda e, d=d, ig=ig, a_=a_: e.tensor_tensor(out=tw[:, 3 * d, :, :], in0=ig, in1=a_, op=ALU.subtract),
                  reads=['gm', 'cum'], writes=['tw'])
            sc.op('dve', lambda e, d=d, ig=ig, r_=r_: e.tensor_tensor(out=tw[:, 3 * d + 1, :, :], in0=ig, in1=r_, op=ALU.add),
                  reads=['gm', 'cum'], writes=['tw'])
            sc.op('act', lambda e, d=d: e.activation(out=tw[:, 3 * d:3 * d + 2, :, :], in_=tw[:, 3 * d:3 * d + 2, :, :],
                                                     func=AF.Exp, bias=LNK), reads=['tw'], writes=['tw'])
            sc.op('act', lambda e, d=d, a_=a_: e.activation(out=tw[:, 3 * d + 2, :, :], in_=a_, func=AF.Exp),
                  reads=['cum'], writes=['tw'])
        if debug:
            o = dbg_out("dbg_tw", [128, 6, NT, 4])
            sc.dma('sp', lambda e: e.dma_start(out=o[:, :, :, :], in_=tw[:]), 'dbgtw', reads=['tw'])
            o = dbg_out("dbg_egs", [128, 4, NT, 4])
            sc.dma('sp', lambda e: e.dma_start(out=o[:, :, :, :], in_=egs[:]), 'dbgegs', reads=['egs'])
        sc.barrier()

    if stage <= 1.5:
        sc.finish()
        return nc, dbg

    win_v = win_d.rearrange("(kc p) n -> p kc n", p=128)
    yT_v = yT_d.rearrange("(c p) t -> p c t", p=128)

    def emit_y_tile(ph, h_chunk, i, ym, ymk, yst, ystk):
        g4 = i % 4
        bank = ('ps', 5)
        pb = ps[:, 5, :].bitcast(BF16)
        sc.op('pe', lambda e: e.transpose(out=pb[:, g4 * 128:(g4 + 1) * 128], in_=ym, identity=identb[:]),
              reads=[ymk, 'identb'], writes=[bank])
        sc.op('act', lambda e: e.copy(out=yst[:, g4 * 128:(g4 + 1) * 128], in_=pb[:, g4 * 128:(g4 + 1) * 128]),
              reads=[bank], writes=[ystk])
        if g4 == 3:
            t0 = (i - 3) * 128
            sc.dma('sp', lambda e: e.dma_start(out=yT_v[:, h_chunk, t0:t0 + 512], in_=yst[:]), ystk, reads=[ystk], writes=['yT_d'])

    n_mheads = 4 if stage >= 2.5 else 1
    sM = ExitStack()
    wm2 = [sb("wm%d" % k, [128, 8, 512], BF16, sM) for k in range(2)]
    for h in range(n_mheads):
        with ExitStack() as ph:
            wm = wm2[h % 2]
            WMK = ('wm', h % 2)
            qkT = sb("qkT", [128, 2, S], BF16, ph)
            pre = sb("pre", [128, 2, 516], F32, ph)
            acc = sb("acc", [128, 2, 512], F32, ph)
            vext = sb("vext", [128, NT, 129], BF16, ph)
            sgo = sb("sgo", [128, NT, 128], BF16, ph)
            ktok = sb("ktok", [128, NT, 128], BF16, ph)
            Cst = sb("Cst", [128, 2, 65, 129], BF16, ph)
            stt = sb("stt", [128, 2, 2, 129], F32, ph)
            vtmp = [sb("vtmp%d" % k, [128, 129], BF16, ph) for k in range(2)]
            smg = [sb("smg%d" % k, [128, 2, 4, 128], BF16, ph) for k in range(2)]
            vtg = [sb("vtg%d" % k, [128, 2, 4, 129], BF16, ph) for k in range(2)]
            pst = sb("pst", [128, 2, 48], F32, ph)
            hfg = [sb("hfg%d" % k, [128, 4, 128], F32, ph) for k in range(2)]
            hsg = [sb("hsg%d" % k, [128, 4, 128], F32, ph) for k in range(2)]
            ymg = [sb("ymg%d" % k, [128, 4, 128], BF16, ph) for k in range(2)]
            yst = [sb("yst%d" % k, [128, 512], BF16, ph) for k in range(2)]
            for hh in ([0, 1] if h == 0 else [h + 1]):
                if hh >= n_mheads:
                    continue
                for k, c0 in enumerate((hh * 128, 512 + hh * 128, 1024 + hh * 128, 1536 + hh * 128)):
                    sc.dma('pool', lambda e, k=k, c0=c0, hh=hh: e.dma_start(out=wm2[hh % 2][:, :, k * 128:(k + 1) * 128], in_=win_v[:, :, c0:c0 + 128]),
                           ('wm', hh % 2), writes=[('wm', hh % 2)])
            sc.op('pool', lambda e: e.memset(vext[:, :, 128:129], 1.0), writes=['vext1'])
            sc.op('pool', lambda e: e.memset(Cst[:, :, 0, :], 0.0), writes=[('Cst', 0, 0), ('Cst', 1, 0)])
            sc.op('pool', lambda e: e.memset(stt[:, :, 0, :], 0.0), writes=[('stt', 0, 0), ('stt', 1, 0)])
            sc.op('pool', lambda e: e.memset(pre[:, :, 0:4], 0.0), writes=['pre'])
            for g in range(NG + 1):
                if g < NG:
                    for qk in range(2):
                        bank = ('ps', qk)

                        def mm(e, qk=qk, g=g):
                            for kc in range(8):
                                r = e.matmul(ps[:, qk, :], lhsT=wm[:, kc, qk * 128:(qk + 1) * 128],
                                             rhs=h1T[:, kc, g * 512:(g + 1) * 512], start=(kc == 0), stop=(kc == 7))
                            return r
                        sc.op('pe', mm, reads=[WMK] + [('h1T', 4 * g + k) for k in range(4)], writes=[bank])
                        sc.op('act', lambda e, qk=qk: e.copy(out=pre[:, qk, 4:516], in_=ps[:, qk, :]), reads=[bank], writes=['pre'])
                else:
                    sc.op('pool', lambda e: e.memset(pre[:, :, 4:8], 0.0), writes=['pre'])
                if g < NG:
                    for k in range(4):
                        i = 4 * g + k
                        bank = ('ps', 2 + (i % 2))

                        def mmvo(e, i=i):
                            for kc in range(8):
                                r = e.matmul(ps[:, 2 + (i % 2), 0:256], lhsT=h1T[:, kc, i * 128:(i + 1) * 128], rhs=wm[:, kc, 256:512],
                                             start=(kc == 0), stop=(kc == 7))
                            return r
                        sc.op('pe', mmvo, reads=[WMK, ('h1T', i)], writes=[bank])
                        sc.op('act', lambda e, i=i: e.copy(out=vext[:, i, 0:128], in_=ps[:, 2 + (i % 2), 0:128]), reads=[bank], writes=[('vext', i)])
                        sc.op('act', lambda e, i=i: e.activation(out=sgo[:, i, :], in_=ps[:, 2 + (i % 2), 128:256], func=AF.Sigmoid),
                              reads=[bank], writes=[('sgo', i)])
                W = 512 if g < NG else 2
                for qk in range(2):
                    cb = (4 * qk + h) * 5
                    sc.op('dve', lambda e, qk=qk, cb=cb, W=W: e.tensor_scalar(out=acc[:, qk, 0:W], in0=pre[:, qk, 0:W], scalar1=convT[:, cb:cb + 1],
                                                                            scalar2=None, op0=ALU.mult), reads=['pre', 'convT'], writes=[('acc', qk)])
                    for j in range(1, 5):
                        sc.op('dve', lambda e, qk=qk, cb=cb, j=j, W=W: e.scalar_tensor_tensor(
                            out=acc[:, qk, 0:W], in0=pre[:, qk, j:j + W], scalar=convT[:, cb + j:cb + j + 1], in1=acc[:, qk, 0:W],
                            op0=ALU.mult, op1=ALU.add), reads=['pre', 'convT', ('acc', qk)], writes=[('acc', qk)])
                    n0 = 2 if g == 0 else 0
                    t0 = 512 * g - 2 + n0
                    sc.op('act', lambda e, qk=qk, n0=n0, t0=t0, W=W: e.activation(out=qkT[:, qk, t0:t0 + W - n0], in_=acc[:, qk, n0:W], func=AF.Silu),
                          reads=[('acc', qk)], writes=[('qkT', qk)])
                if g < NG:
                    sc.op('pool', lambda e: e.tensor_copy(out=pre[:, :, 0:4], in_=pre[:, :, 512:516]), reads=['pre'], writes=['pre'])
            for g in range(NG):
                bank = ('ps', 4)
                pb = ps[:, 4, :].bitcast(BF16)

                def trk(e, g=g, pb=pb):
                    for k in range(4):
                        i = 4 * g + k
                        r = e.transpose(out=pb[:, k * 128:(k + 1) * 128], in_=qkT[:, 1, i * 128:(i + 1) * 128], identity=identb[:])
                    return r
                sc.op('pe', trk, reads=[('qkT', 1), 'identb'], writes=[bank])
                sc.op('act', lambda e, g=g, pb=pb: e.copy(out=ktok[:, 4 * g:4 * g + 4, :], in_=pb[:, 0:512].rearrange("p (k d) -> p k d", k=4)),
                      reads=[bank], writes=['ktok'])
            nv = 0
            for step in range(64):
                for d in range(2):
                    c = step if d == 0 else 63 - step
                    i, half = c // 2, c % 2
                    first_of_tile = (half == 0) if d == 0 else (half == 1)
                    vk = ('vhat', d)
                    vb = vtmp[d]
                    if first_of_tile:
                        sc.op('act', lambda e, d=d, i=i, vb=vb: e.activation(out=vb[:], in_=vext[:, i, :], func=AF.Copy,
                                                                             scale=tw[:, 3 * d + 1, i, h:h + 1]),
                              reads=[('vext', i), 'vext1', 'tw'], writes=[vk])
                    slot = nv % 4
                    nv += 1
                    pbank = 4 + slot
                    pc = 0
                    pk = ('ps', pbank)
                    sc.op('pe', lambda e, i=i, half=half, vb=vb, pbank=pbank, pc=pc: e.matmul(
                        ps[:, pbank, pc:pc + 129], lhsT=ktok[half * 64:(half + 1) * 64, i, :], rhs=vb[half * 64:(half + 1) * 64, :],
                        start=True, stop=True), reads=['ktok', vk], writes=[pk])
                    cur, nxt = step % 2, (step + 1) % 2
                    sc.op('dve', lambda e, d=d, i=i, half=half, cur=cur, nxt=nxt, pbank=pbank, pc=pc: e.scalar_tensor_tensor(
                        out=stt[:, d, nxt, :], in0=stt[:, d, cur, :], scalar=egs[:, 2 * d + half, i, h:h + 1], in1=ps[:, pbank, pc:pc + 129],
                        op0=ALU.mult, op1=ALU.add), reads=[('stt', d, cur), 'egs', pk], writes=[('stt', d, nxt)])
                    if d == 0:
                        sc.op('pool', lambda e, d=d, nxt=nxt, step=step: e.tensor_copy(out=Cst[:, d, step + 1, :], in_=stt[:, d, nxt, :]),
                              reads=[('stt', d, nxt)], writes=[('Cst', d, step + 1)])
                    else:
                        sc.op('act', lambda e, d=d, nxt=nxt, step=step: e.copy(out=Cst[:, d, step + 1, :], in_=stt[:, d, nxt, :]),
                              reads=[('stt', d, nxt)], writes=[('Cst', d, step + 1)])
            for g in range(NG):
                gb = g % 2
                sbank = ('ps', gb)

                def smm(e, g=g, gb=gb):
                    for t in range(4):
                        blk = slice((4 * g + t) * 128, (4 * g + t + 1) * 128)
                        r = e.matmul(ps[:, gb, t * 128:(t + 1) * 128], lhsT=qkT[:, 1, blk], rhs=qkT[:, 0, blk], start=True, stop=True)
                    return r
                sc.op('pe', smm, reads=[('qkT', 0), ('qkT', 1)], writes=[sbank])
                S4 = ps[:, gb, :].rearrange("p (t j) -> p t j", t=4)
                smF, smB = smg[gb][:, 0, :, :], smg[gb][:, 1, :, :]
                kF, kB = ('smg', gb, 0), ('smg', gb, 1)
                sc.op('dve', lambda e, S4=S4, smF=smF: e.tensor_tensor(out=smF, in0=S4, in1=maskF[:].unsqueeze(1).broadcast_to([128, 4, 128]), op=ALU.mult),
                      reads=[sbank, 'maskF'], writes=[kF])
                sc.op('dve', lambda e, S4=S4, smB=smB: e.tensor_tensor(out=smB, in0=S4, in1=maskB[:].unsqueeze(1).broadcast_to([128, 4, 128]), op=ALU.mult),
                      reads=[sbank, 'maskB'], writes=[kB])
                vtk = ('vtg', gb)
                for d in range(2):
                    sc.op('pool', lambda e, d=d, g=g, gb=gb: e.tensor_tensor(
                        out=vtg[gb][:, d, :, :], in0=vext[:, 4 * g:4 * g + 4, :],
                        in1=tw[:, 3 * d, 4 * g:4 * g + 4, h:h + 1].broadcast_to([128, 4, 129]), op=ALU.mult),
                        reads=[('vext', 4 * g + t) for t in range(4)] + ['vext1', 'tw'], writes=[(vtk, d)])
                ubanks = [('ps', 2 + t) for t in range(4)]

                def umm(e, g=g, gb=gb):
                    for t in range(4):
                        i = 4 * g + t
                        for d in range(2):
                            o0_ = d * 129
                            e.matmul(ps[:, 2 + t, o0_:o0_ + 129], lhsT=smg[gb][:, d, t, :], rhs=vtg[gb][:, d, t, :], start=True, stop=False)
                            for half in range(2):
                                c = 2 * i + half
                                sidx = c if d == 0 else 63 - c
                                t0 = i * 128 + half * 64
                                r = e.matmul(ps[half * 64:(half + 1) * 64, 2 + t, o0_:o0_ + 129], lhsT=qkT[:, 0, t0:t0 + 64],
                                             rhs=Cst[:, d, sidx, :], start=False, stop=True)
                    return r
                creads = []
                for t in range(4):
                    i = 4 * g + t
                    creads += [('Cst', 0, 2 * i), ('Cst', 0, 2 * i + 1), ('Cst', 1, 63 - 2 * i), ('Cst', 1, 62 - 2 * i)]
                sc.op('pe', umm, reads=[kF, kB, (vtk, 0), (vtk, 1), ('qkT', 0)] + creads, writes=ubanks)
                P = pst[:, gb, :]
                pk = ('pst', gb)
                U4 = ps[:, 2:6, 0:258].rearrange("p t (d v) -> p t d v", d=2)
                ea2 = tw[:, 2:6:3, 4 * g:4 * g + 4, h].rearrange("p d t -> p t d")
                Pv = lambda lo: P[:, lo:lo + 8].rearrange("p (t d) -> p t d", d=2)
                sc.op('dve', lambda e: e.tensor_tensor(out=Pv(0), in0=U4[:, :, :, 128], in1=ea2, op=ALU.mult), reads=ubanks + ['tw'], writes=[pk])
                sc.op('dve', lambda e: e.tensor_scalar(out=P[:, 16:24], in0=P[:, 0:8], scalar1=-1.0, scalar2=None, op0=ALU.mult), reads=[pk], writes=[pk])
                sc.op('dve', lambda e: e.scalar_tensor_tensor(out=P[:, 8:16], in0=P[:, 16:24], scalar=1.0, in1=P[:, 0:8], op0=ALU.max, op1=ALU.max),
                      reads=[pk], writes=[pk])
                sc.op('dve', lambda e: e.reciprocal(out=P[:, 16:24], in_=P[:, 8:16]), reads=[pk], writes=[pk])
                sc.op('dve', lambda e: e.tensor_tensor(out=Pv(24), in0=Pv(16), in1=ea2, op=ALU.mult), reads=[pk, 'tw'], writes=[pk])
                RR = Pv(24)
                hfb, hsb = hfg[gb], hsg[gb]
                sc.op('dve', lambda e: e.tensor_tensor(out=hfb[:], in0=U4[:, :, 0, 0:128], in1=RR[:, :, 0:1].broadcast_to([128, 4, 128]), op=ALU.mult),
                      reads=ubanks + [pk], writes=[('hfg', gb)])
                sc.op('dve', lambda e: e.tensor_tensor(out=hsb[:], in0=U4[:, :, 1, 0:128], in1=RR[:, :, 1:2].broadcast_to([128, 4, 128]), op=ALU.mult),
                      reads=ubanks + [pk], writes=[('hsg', gb)])
                sc.op('pool', lambda e: e.tensor_tensor(out=hsb[:], in0=hsb[:], in1=hfb[:], op=ALU.add), reads=[('hsg', gb), ('hfg', gb)], writes=[('hsg', gb)])
                sc.op('pool', lambda e: e.tensor_tensor(out=hfb[:], in0=hsb[:], in1=hsb[:], op=ALU.mult), reads=[('hsg', gb)], writes=[('hfg', gb)])
                sc.op('dve', lambda e: e.tensor_reduce(out=P[:, 32:36], in_=hfb[:], axis=AX.X, op=ALU.add), reads=[('hfg', gb)], writes=[pk])
                sc.op('act', lambda e: e.activation(out=P[:, 36:40], in_=P[:, 32:36], func=AF.Sqrt, scale=1.0 / 128, bias=EPS), reads=[pk], writes=[pk])
                sc.op('dve', lambda e: e.reciprocal(out=P[:, 40:44], in_=P[:, 36:40]), reads=[pk], writes=[pk])
                sc.op('dve', lambda e: e.tensor_tensor(out=hsb[:], in0=hsb[:], in1=P[:, 40:44].unsqueeze(2).broadcast_to([128, 4, 128]), op=ALU.mult),
                      reads=[('hsg', gb), pk], writes=[('hsg', gb)])
                sc.op('pool', lambda e: e.tensor_tensor(out=hsb[:], in0=hsb[:],
                                                        in1=prm[:, P_MNG + h * 128:P_MNG + (h + 1) * 128].unsqueeze(1).broadcast_to([128, 4, 128]), op=ALU.mult),
                      reads=[('hsg', gb), 'prm'], writes=[('hsg', gb)])
                ym = ymg[gb]
                ymk = ('ymg', gb)
                sc.op('dve', lambda e, g=g: e.tensor_tensor(out=ym[:], in0=hsb[:], in1=sgo[:, 4 * g:4 * g + 4, :], op=ALU.mult),
                      reads=[('hsg', gb)] + [('sgo', 4 * g + t) for t in range(4)], writes=[ymk])
                tbank = ('ps', 6)
                pb = ps[:, 6, :].bitcast(BF16)

                def ytr(e, ym=ym, pb=pb):
                    for t in range(4):
                        r = e.transpose(out=pb[:, t * 128:(t + 1) * 128], in_=ym[:, t, :], identity=identb[:])
                    return r
                sc.op('pe', ytr, reads=[ymk, 'identb'], writes=[tbank])
                ys = yst[gb]
                ysk = ('yst', gb)
                sc.op('act', lambda e, ys=ys, pb=pb: e.copy(out=ys[:], in_=pb[:, 0:512]), reads=[tbank], writes=[ysk])
                sc.dma('sp', lambda e, ys=ys, g=g: e.dma_start(out=yT_v[:, h, g * 512:(g + 1) * 512], in_=ys[:]), ysk, reads=[ysk], writes=['yT_d'])
            if debug and h == 0:
                o = dbg_out("dbg_qkT", [128, 2, S], BF16)
                sc.dma('sp', lambda e: e.dma_start(out=o[:, :, :], in_=qkT[:]), 'dbgqkT', reads=[('qkT', 0), ('qkT', 1)])
                o = dbg_out("dbg_Cst", [128, 2, 65, 129], BF16)
                sc.dma('sp', lambda e: e.dma_start(out=o[:, :, :, :], in_=Cst[:]), 'dbgCst',
                       reads=[('Cst', d, k) for d in range(2) for k in range(65)])
            sc.barrier()
    sM.close()

    if stage <= 2.5:
        sc.finish()
        return nc, dbg

    cosT = sb("cosT", [128, NT, 8], F32, s1)
    sinT = sb("sinT", [128, NT, 8], F32, s1)
    qkg4 = sb("qkg4", [128, 4, 64], F32, s1)
    subg8 = sb("subg8", [128, 128], F32, s1)
    TWO_PI = 6.28318
    with ExitStack() as pa:
        posi = sb("posi", [128, NT], I32, pa)
        posf = sb("posf", [128, NT], F32, pa)
        ut = sb("ut", [128, NT, 8], F32, pa)
        ui = sb("ui", [128, NT, 8], I32, pa)
        uf = sb("uf", [128, NT, 8], F32, pa)
        jl = sb("jl", [128, 64], F32, pa)
        sc.dma('sp', lambda e: e.dma_start(out=posi[:], in_=pos_d[:, :]), 'posi', writes=['posi'])
        sc.op('dve', lambda e: e.tensor_copy(out=posf[:], in_=posi[:]), reads=['posi'], writes=['posf'])
        sc.op('dve', lambda e: e.tensor_tensor(out=ut[:], in0=posf[:].unsqueeze(2).broadcast_to([128, NT, 8]),
                                               in1=consts[:, C_FREQ:C_FREQ + 8].unsqueeze(1).broadcast_to([128, NT, 8]), op=ALU.mult),
              reads=['posf', 'consts'], writes=['ut'])
        for tab, shift in ((sinT, 0.0), (cosT, 0.25)):
            if shift:
                sc.op('dve', lambda e, shift=shift: e.tensor_scalar(out=ut[:], in0=ut[:], scalar1=shift, scalar2=None, op0=ALU.add),
                      reads=['ut'], writes=['ut'])
            sc.op('dve', lambda e: e.tensor_copy(out=ui[:], in_=ut[:]), reads=['ut'], writes=['ui'])
            sc.op('dve', lambda e: e.tensor_copy(out=uf[:], in_=ui[:]), reads=['ui'], writes=['uf'])
            sc.op('dve', lambda e: e.tensor_tensor(out=uf[:], in0=ut[:], in1=uf[:], op=ALU.subtract), reads=['ut', 'uf'], writes=['uf'])
            sc.op('act', lambda e, tab=tab: e.activation(out=tab[:], in_=uf[:], func=AF.Sin, scale=TWO_PI), reads=['uf'], writes=['rot'])
        sc.op('dve', lambda e: e.tensor_reduce(out=small[:, 2:4], in_=prm[:, P_QKG:P_QKG + 128].rearrange("p (a d) -> p a d", a=2),
                                               axis=AX.X, op=ALU.max, apply_absolute_value=True), reads=['prm'], writes=['small'])
        sc.op('dve', lambda e: e.scalar_tensor_tensor(out=small[:, SM_NB:SM_NB + 1], in0=small[:, 2:3], scalar=-8.0, in1=small[:, 3:4],
                                                      op0=ALU.mult, op1=ALU.mult), reads=['small'], writes=['small'])
        for k in range(2):
            sc.op('dve', lambda e, k=k: e.tensor_tensor(out=jl[:], in0=prm[:, P_LAM + 128 * k:P_LAM + 128 * k + 64],
                                                        in1=prm[:, P_LAM + 128 * k + 64:P_LAM + 128 * k + 128], op=ALU.mult),
                  reads=['prm'], writes=['jl'])
            sc.op('dve', lambda e, k=k: e.tensor_reduce(out=small[:, 4 + k:5 + k], in_=jl[:], axis=AX.X, op=ALU.add), reads=['jl'], writes=['small'])
        sc.op('act', lambda e: e.activation(out=small[:, 6:8], in_=small[:, 4:6], func=AF.Exp), reads=['small'], writes=['small'])
        sc.op('dve', lambda e: e.scalar_tensor_tensor(out=small[:, SM_NLAM:SM_NLAM + 1], in0=small[:, 7:8], scalar=-LAM_INIT, in1=small[:, 6:7],
                                                      op0=ALU.add, op1=ALU.subtract), reads=['small'], writes=['small'])
        for a in range(4):
            o_ = P_QKG + (64 if a >= 2 else 0)
            sc.op('pool', lambda e, a=a, o_=o_: e.tensor_copy(out=qkg4[:, a, :], in_=prm[:, o_:o_ + 64]), reads=['prm'], writes=['qkg4'])
        sc.op('dve', lambda e: e.tensor_scalar(out=subg8[:], in0=prm[:, P_SUB:P_SUB + 128], scalar1=1.0 - LAM_INIT, scalar2=None, op0=ALU.mult),
              reads=['prm'], writes=['subg8'])
        if debug:
            o = dbg_out("dbg_small", [128, 64])
            sc.op('pool', lambda e: e.memset(small[:, 8:64], 0.0), writes=['small'])
            sc.dma('sp', lambda e: e.dma_start(out=o[:, :], in_=small[:]), 'dbgsmall', reads=['small'])
            o = dbg_out("dbg_cos", [128, NT, 8])
            sc.dma('sp', lambda e: e.dma_start(out=o[:, :, :], in_=cosT[:]), 'dbgcos', reads=['rot'])
        sc.barrier()

    n_aheads = 4 if stage >= 3.5 else 1
    sA = ExitStack()
    wa2 = [sb("waa%d" % k, [128, 8, 384], BF16, sA) for k in range(2)]
    for h in range(n_aheads):
        with ExitStack() as ph:
            wa_ = wa2[h % 2]
            WAK = ('waa', h % 2)
            qTa = sb("qTa", [128, 2, S], BF16, ph)
            kTa = sb("kTa", [128, S], BF16, ph)
            vxa = sb("vxa", [128, NT, 129], BF16, ph)
            xq = [sb("xq%d" % k, [128, 4, 4, 64], F32, ph) for k in range(2)]
            sq = sb("sq", [128, 4, 4, 64], F32, ph)
            rp = [sb("rp%d" % k, [128, 4, 4, 4, 8], F32, ph) for k in range(2)]
            st4 = sb("st4", [128, 2, 48], F32, ph)
            xr = [sb("xr%d" % k, [128, 4, 4, 64], BF16, ph) for k in range(2)]
            pt = [sb("pt%d" % k, [128, 2, 512], BF16, ph) for k in range(3)]
            o0 = sb("o0", [128, 4, 128], F32, ph)
            ob4 = [sb("ob4_%d" % k, [128, 4, 128], F32, ph) for k in range(2)]
            osq = sb("osq", [128, 4, 128], F32, ph)
            st5 = sb("st5", [128, 2, 24], F32, ph)
            ymt4 = [sb("ymt4_%d" % k, [128, 4, 128], BF16, ph) for k in range(2)]
            yst = [sb("ysta%d" % k, [128, 512], BF16, ph) for k in range(2)]
            for hh in ([0, 1] if h == 0 else [h + 1]):
                if hh >= n_aheads:
                    continue
                for k, c0 in enumerate((2064 + hh * 128, 2576 + hh * 128, 3088 + hh * 128)):
                    sc.dma('pool', lambda e, k=k, c0=c0, hh=hh: e.dma_start(out=wa2[hh % 2][:, :, k * 128:(k + 1) * 128], in_=win_v[:, :, c0:c0 + 128]),
                           ('waa', hh % 2), writes=[('waa', hh % 2)])
            sc.op('pool', lambda e: e.memset(vxa[:, :, 128:129], 1.0), writes=['vxa1'])
            sc.op('pool', lambda e: e.memset(qTa[64:128, 0, :], 0.0), writes=['qTa'])
            sc.op('pool', lambda e: e.memset(qTa[0:64, 1, :], 0.0), writes=['qTa'])
            def projA(g):
                    b = g % 2
                    banks = [('ps', k) for k in range(4)]
                    xk, rk, sk, xrk = ('xq', b), ('rp', b), ('st4', b), ('xr', b)
                    X = xq[b]
                    X3 = X[:].rearrange("p t a d -> p t (a d)")
                    X16 = X[:].rearrange("p t a d -> p (t a) d")
                    T4 = st4[:, b, :]
                    R = rp[b]
                    b = g % 2
                    banks = [('ps', k) for k in range(4)]

                    def mmp(e, g=g):
                        for t in range(4):
                            i = 4 * g + t
                            for kc in range(8):
                                r = e.matmul(ps[:, t, 0:384], lhsT=h1T[:, kc, i * 128:(i + 1) * 128], rhs=wa_[:, kc, :], start=(kc == 0), stop=(kc == 7))
                        return r
                    sc.op('pe', mmp, reads=[WAK] + [('h1T', 4 * g + t) for t in range(4)], writes=banks)
                    xk, rk, sk, xrk = ('xq', b), ('rp', b), ('st4', b), ('xr', b)
                    X = xq[b]
                    X3 = X[:].rearrange("p t a d -> p t (a d)")
                    X16 = X[:].rearrange("p t a d -> p (t a) d")
                    sc.op('act', lambda e, X3=X3: e.copy(out=X3, in_=ps[:, 0:4, 0:256]), reads=banks, writes=[xk])
                    sc.op('act', lambda e, g=g: e.copy(out=vxa[:, 4 * g:4 * g + 4, 0:128], in_=ps[:, 0:4, 256:384]), reads=banks,
                          writes=[('vxa', 4 * g + t) for t in range(4)])

            def projB(g):
                    b = g % 2
                    banks = [('ps', k) for k in range(4)]
                    xk, rk, sk, xrk = ('xq', b), ('rp', b), ('st4', b), ('xr', b)
                    X = xq[b]
                    X3 = X[:].rearrange("p t a d -> p t (a d)")
                    X16 = X[:].rearrange("p t a d -> p (t a) d")
                    T4 = st4[:, b, :]
                    R = rp[b]
                    sc.op('dve', lambda e, X=X: e.tensor_tensor(out=sq[:], in0=X[:], in1=X[:], op=ALU.mult), reads=[xk], writes=['sq'])
                    T4 = st4[:, b, :]
                    sc.op('dve', lambda e, T4=T4: e.tensor_reduce(out=T4[:, 0:16], in_=sq[:].rearrange("p t a d -> p (t a) d"), axis=AX.X, op=ALU.add),
                          reads=['sq'], writes=[sk])
                    sc.op('act', lambda e, T4=T4: e.activation(out=T4[:, 16:32], in_=T4[:, 0:16], func=AF.Sqrt, scale=1.0 / 64, bias=EPS), reads=[sk], writes=[sk])
                    sc.op('dve', lambda e, T4=T4: e.reciprocal(out=T4[:, 32:48], in_=T4[:, 16:32]), reads=[sk], writes=[sk])
                    sc.op('dve', lambda e, X16=X16, T4=T4: e.tensor_tensor(out=X16, in0=X16, in1=T4[:, 32:48].unsqueeze(2).broadcast_to([128, 16, 64]), op=ALU.mult),
                          reads=[xk, sk], writes=[xk])
                    sc.op('dve', lambda e, X=X: e.tensor_tensor(out=X[:], in0=X[:], in1=qkg4[:].unsqueeze(1).broadcast_to([128, 4, 4, 64]), op=ALU.mult),
                          reads=[xk, 'qkg4'], writes=[xk])
                    cs = cosT[:, 4 * g:4 * g + 4, :].unsqueeze(2).broadcast_to([128, 4, 4, 8])
                    sn = sinT[:, 4 * g:4 * g + 4, :].unsqueeze(2).broadcast_to([128, 4, 4, 8])
                    R = rp[b]
                    for k, (tt, tr_) in enumerate(((X[:, :, :, 0:8], cs), (X[:, :, :, 8:16], sn), (X[:, :, :, 8:16], cs), (X[:, :, :, 0:8], sn))):
                        sc.op('pool', lambda e, k=k, tt=tt, tr_=tr_, R=R: e.tensor_tensor(out=R[:, k, :, :, :], in0=tt, in1=tr_, op=ALU.mult),
                              reads=[xk, 'rot'], writes=[rk])

            def projC(g):
                    b = g % 2
                    banks = [('ps', k) for k in range(4)]
                    xk, rk, sk, xrk = ('xq', b), ('rp', b), ('st4', b), ('xr', b)
                    X = xq[b]
                    X3 = X[:].rearrange("p t a d -> p t (a d)")
                    X16 = X[:].rearrange("p t a d -> p (t a) d")
                    T4 = st4[:, b, :]
                    R = rp[b]
                    XR = xr[b]
                    sc.op('act', lambda e, XR=XR, X=X: e.copy(out=XR[:], in_=X[:]), reads=[xk], writes=[xrk])
                    sc.op('dve', lambda e, XR=XR, R=R: e.tensor_tensor(out=XR[:, :, :, 0:8], in0=R[:, 0, :, :, :], in1=R[:, 1, :, :, :], op=ALU.subtract),
                          reads=[rk, xrk], writes=[xrk])
                    sc.op('dve', lambda e, XR=XR, R=R: e.tensor_tensor(out=XR[:, :, :, 8:16], in0=R[:, 2, :, :, :], in1=R[:, 3, :, :, :], op=ALU.add),
                          reads=[rk, xrk], writes=[xrk])
                    tbanks = [('ps', 4), ('ps', 5)]
                    pbq = ps[:, 4, :].bitcast(BF16)
                    pbk = ps[:, 5, :].bitcast(BF16)
                    XRf = XR[:].rearrange("p t a d -> p t (a d)")

                    def trq(e, XRf=XRf, pbq=pbq, pbk=pbk):
                        for t in range(4):
                            e.transpose(out=pbq[:, t * 128:(t + 1) * 128], in_=XRf[:, t, 0:128], identity=identb[:])
                            r = e.transpose(out=pbk[:, t * 128:(t + 1) * 128], in_=XRf[:, t, 128:256], identity=identb[:])
                        return r
                    sc.op('pe', trq, reads=[xrk, 'identb'], writes=tbanks)
                    sc.op('act', lambda e, pbq=pbq, g=g: e.copy(out=qTa[0:64, 0, g * 512:(g + 1) * 512], in_=pbq[0:64, 0:512]), reads=tbanks, writes=['qTa'])
                    sc.op('act', lambda e, pbq=pbq, g=g: e.copy(out=qTa[64:128, 1, g * 512:(g + 1) * 512], in_=pbq[64:128, 0:512]), reads=tbanks, writes=['qTa'])
                    sc.op('act', lambda e, pbk=pbk, g=g: e.copy(out=kTa[:, g * 512:(g + 1) * 512], in_=pbk[:, 0:512]), reads=tbanks, writes=['kTa'])

            projA(0)
            for g in range(NG):
                if g + 1 < NG:
                    projA(g + 1)
                projB(g)
                projC(g)
            if debug and h == 0:
                o = dbg_out("dbg_qTa", [128, S], BF16)
                sc.dma('sp', lambda e: e.dma_start(out=o[0:64, :], in_=qTa[0:64, 0, :]), 'dbgqTa', reads=['qTa'])
                sc.dma('sp', lambda e: e.dma_start(out=o[64:128, :], in_=qTa[64:128, 1, :]), 'dbgqTa', reads=['qTa'])
                o = dbg_out("dbg_kTa", [128, S], BF16)
                sc.dma('sp', lambda e: e.dma_start(out=o[:, :], in_=kTa[:]), 'dbgkTa', reads=['kTa'])
            nqb = 8 if stage >= 3.2 else 1
            for qb in range(nqb):
                for p in range(2):
                    pr = slice(64 * p, 64 * p + 64)
                    def st_mm(j):
                        pb0 = 4 + 2 * (j % 2)

                        def f(e, j=j, pb0=pb0):
                            for u in range(2):
                                kt = 2 * j + u
                                r = e.matmul(ps[:, pb0 + u, :], lhsT=kTa[:, kt * 128:(kt + 1) * 128], rhs=qTa[:, p, qb * 512:(qb + 1) * 512],
                                             start=True, stop=True)
                            return r
                        sc.op('pe', f, reads=['qTa', 'kTa'], writes=[('ps', pb0), ('ps', pb0 + 1)])
                    st_mm(0)
                    for j in range(NT // 2):
                        pb0 = 4 + 2 * (j % 2)
                        sbanks = [('ps', pb0), ('ps', pb0 + 1)]
                        if j + 1 < NT // 2:
                            st_mm(j + 1)
                        ptk = ('pt', j % 3)
                        ptb = pt[j % 3]
                        sc.op('act', lambda e, pb0=pb0, ptb=ptb: e.activation(out=ptb[:], in_=ps[:, pb0:pb0 + 2, :], func=AF.Exp, scale=0.125,
                                                                              bias=small[:, SM_NB:SM_NB + 1]),
                              reads=sbanks + ['small'], writes=[ptk])

                        def pv(e, ptb=ptb, j=j):
                            for u in range(2):
                                kt = 2 * j + u
                                for qs in range(4):
                                    r = e.matmul(ps[:, qs, 0:129], lhsT=ptb[:, u, qs * 128:(qs + 1) * 128], rhs=vxa[:, kt, :],
                                                 start=(kt == 0), stop=(kt == NT - 1))
                            return r
                        sc.op('pe', pv, reads=[ptk, ('vxa', 2 * j), ('vxa', 2 * j + 1), 'vxa1'], writes=[('ps', 0), ('ps', 1), ('ps', 2), ('ps', 3)])
                    abanks = [('ps', k) for k in range(4)]
                    acc4 = ps[:, 0:4, 0:129]
                    T5 = st5[:, p, :]
                    tk5 = ('st5', p)
                    sc.op('dve', lambda e, T5=T5, acc4=acc4: e.reciprocal(out=T5[:, 0:4], in_=acc4[:, :, 128]), reads=abanks, writes=[tk5])
                    if p == 0:
                        sc.op('dve', lambda e, T5=T5, acc4=acc4: e.tensor_tensor(out=o0[:], in0=acc4[:, :, 0:128],
                                                                                  in1=T5[:, 0:4].unsqueeze(2).broadcast_to([128, 4, 128]), op=ALU.mult),
                              reads=abanks + [tk5], writes=['o0'])
                        continue
                    O = ob4[qb % 2]
                    okk = ('ob4', qb % 2)
                    sc.op('dve', lambda e, T5=T5: e.tensor_scalar(out=T5[:, 4:8], in0=T5[:, 0:4], scalar1=small[:, SM_NLAM:SM_NLAM + 1], scalar2=None, op0=ALU.mult),
                          reads=[tk5, 'small'], writes=[tk5])
                    sc.op('dve', lambda e, T5=T5, acc4=acc4, O=O: e.tensor_tensor(out=O[:], in0=acc4[:, :, 0:128],
                                                                                   in1=T5[:, 4:8].unsqueeze(2).broadcast_to([128, 4, 128]), op=ALU.mult),
                          reads=abanks + [tk5], writes=[okk])
                    sc.op('dve', lambda e, O=O: e.tensor_tensor(out=O[:], in0=O[:], in1=o0[:], op=ALU.add), reads=[okk, 'o0'], writes=[okk])
                    sc.op('pool', lambda e, O=O: e.tensor_tensor(out=osq[:], in0=O[:], in1=O[:], op=ALU.mult), reads=[okk], writes=['osq'])
                    sc.op('dve', lambda e, T5=T5: e.tensor_reduce(out=T5[:, 8:12], in_=osq[:], axis=AX.X, op=ALU.add), reads=['osq'], writes=[tk5])
                    sc.op('act', lambda e, T5=T5: e.activation(out=T5[:, 12:16], in_=T5[:, 8:12], func=AF.Sqrt, scale=1.0 / 128, bias=EPS), reads=[tk5], writes=[tk5])
                    sc.op('dve', lambda e, T5=T5: e.reciprocal(out=T5[:, 16:20], in_=T5[:, 12:16]), reads=[tk5], writes=[tk5])
                    sc.op('dve', lambda e, T5=T5, O=O: e.tensor_tensor(out=O[:], in0=O[:], in1=T5[:, 16:20].unsqueeze(2).broadcast_to([128, 4, 128]), op=ALU.mult),
                          reads=[okk, tk5], writes=[okk])
                    ym = ymt4[qb % 2]
                    ymk = ('ymt4', qb % 2)
                    sc.op('pool', lambda e, O=O, ym=ym: e.tensor_tensor(out=ym[:], in0=O[:], in1=subg8[:].unsqueeze(1).broadcast_to([128, 4, 128]), op=ALU.mult),
                          reads=[okk, 'subg8'], writes=[ymk])
                    tbank = ('ps', 7)
                    pb = ps[:, 7, :].bitcast(BF16)

                    def ytr(e, ym=ym, pb=pb):
                        for t in range(4):
                            r = e.transpose(out=pb[:, t * 128:(t + 1) * 128], in_=ym[:, t, :], identity=identb[:])
                        return r
                    sc.op('pe', ytr, reads=[ymk, 'identb'], writes=[tbank])
                    ys = yst[qb % 2]
                    ysk = ('ysta', qb % 2)
                    sc.op('act', lambda e, ys=ys, pb=pb: e.copy(out=ys[:], in_=pb[:, 0:512]), reads=[tbank], writes=[ysk])
                    sc.dma('sp', lambda e, ys=ys, qb=qb: e.dma_start(out=yT_v[:, 4 + h, qb * 512:(qb + 1) * 512], in_=ys[:]), ysk, reads=[ysk], writes=['yT_d'])
            sc.barrier()
    sA.close()

    if stage <= 3.5:
        sc.finish()
        return nc, dbg

    s1.close()

    sW = ExitStack()
    aff = sb("aff", [128, NT, NE], F32, sW)
    g2rep = sb("g2rep", [128, D], F32, sW)
    with ExitStack() as pw:
        woutb = sb("woutb", [128, 8, D], BF16, pw)
        wrt = sb("wrt", [128, 8, NE], F32, pw)
        ytl = [sb("ytl%d" % k, [128, 8, 512], BF16, pw) for k in range(2)]
        xt2 = [sb("xt2_%d" % k, [128, D], F32, pw) for k in range(2)]
        tmpw = [sb("tmpw%d" % k, [128, D], F32, pw) for k in range(2)]
        x1t = [sb("x1t%d" % k, [128, D], F32, pw) for k in range(2)]
        h2f = [sb("h2f%d" % k, [128, D], F32, pw) for k in range(2)]
        h2b = [sb("h2b%d" % k, [128, D], BF16, pw) for k in range(2)]
        h2T = [sb("h2T%d" % k, [128, 8, 128], F32, pw) for k in range(2)]
        junkw = sb("junkw", [128, D], BF16, pw)
        stw = sb("stw", [128, NT, 8], F32, pw)
        esm = sb("esm", [128, 2, NE], F32, pw)
        wout_v = wout_d.rearrange("(kc p) n -> p kc n", p=128)
        sc.dma('pool', lambda e: e.dma_start(out=woutb[:, 0:4, :], in_=wout_v[:, 0:4, :]), 'woutb', writes=['woutb'])
        sc.dma('pool', lambda e: e.dma_start(out=woutb[:, 4:8, :], in_=wout_v[:, 4:8, :]), 'woutb', writes=['woutb'])
        sc.dma('sp', lambda e: e.dma_start(out=wrt[:], in_=wr_d.rearrange("(kc p) n -> p kc n", p=128)), 'wrt', writes=['wrt'])
        modW = sb("modW", [128, 3 * D], F32, pw)
        sc.dma('sp', lambda e: e.dma_start(out=modW[:], in_=modrow_d[0:1, 2 * D:5 * D].partition_broadcast(128)), 'modW', reads=['modrow_d'], writes=['mod'])
        sc.dma('sp', lambda e: e.dma_start(out=g2rep[:], in_=modrow_d[0:1, 5 * D:6 * D].partition_broadcast(128)), 'g2rep', reads=['modrow_d'], writes=['g2rep'])
        GATE1, SHIFT2, GS2 = modW[:, 0:D], modW[:, D:2 * D], modW[:, 2 * D:3 * D]
        def wA(i):
            b = i % 2
            g, k4 = i // 4, i % 4
            yk = ('ytl', g % 2)
            if k4 == 0:
                sc.dma('sp', lambda e, g=g: e.dma_start(out=ytl[g % 2][:], in_=yT_v[:, :, g * 512:(g + 1) * 512]), yk, writes=[yk])
            xk = ('xt2', b)
            sc.dma('sp', lambda e, i=i, b=b: e.dma_start(out=xt2[b][:], in_=x_d[i * 128:(i + 1) * 128, :]), xk, writes=[xk])
            banks = [('ps', 2 * b), ('ps', 2 * b + 1)]

            def mmw(e, i=i, b=b, g=g, k4=k4):
                for dh in range(2):
                    for c in range(8):
                        r = e.matmul(ps[:, 2 * b + dh, :], lhsT=ytl[g % 2][:, c, k4 * 128:(k4 + 1) * 128], rhs=woutb[:, c, dh * 512:(dh + 1) * 512],
                                     start=(c == 0), stop=(c == 7))
                return r
            sc.op('pe', mmw, reads=[yk, 'woutb'], writes=banks)

        def wB(i):
            b = i % 2
            g, k4 = i // 4, i % 4
            yk = ('ytl', g % 2)
            xk = ('xt2', b)
            banks = [('ps', 2 * b), ('ps', 2 * b + 1)]
            T = stw[:, i, :]
            tk = ('stw', i)
            mixv = ps[:, 2 * b:2 * b + 2, :].rearrange("p a n -> p (a n)")
            sc.op('dve', lambda e, b=b, mixv=mixv: e.tensor_tensor(out=tmpw[b][:], in0=mixv, in1=GATE1, op=ALU.mult), reads=banks + ['mod'], writes=[('tmpw', b)])
            sc.op('dve', lambda e, b=b: e.tensor_tensor(out=x1t[b][:], in0=tmpw[b][:], in1=xt2[b][:], op=ALU.add),
                  reads=[('tmpw', b), xk], writes=[('x1t', b)])
            sc.dma('sp', lambda e, i=i, b=b: e.dma_start(out=out_d[i * 128:(i + 1) * 128, :], in_=x1t[b][:]), ('x1s', b), reads=[('x1t', b)], writes=[('outd', i)])
            T = stw[:, i, :]
            tk = ('stw', i)
            sc.op('act', lambda e, b=b, T=T: e.activation(out=junkw[:], in_=x1t[b][:], func=AF.Square, accum_out=T[:, 0:1]), reads=[('x1t', b)], writes=['junkw', tk])
            sc.op('act', lambda e, T=T: e.activation(out=T[:, 1:2], in_=T[:, 0:1], func=AF.Sqrt, scale=1.0 / D, bias=EPS), reads=[tk], writes=[tk])
            sc.op('dve', lambda e, T=T: e.reciprocal(out=T[:, 2:3], in_=T[:, 1:2]), reads=[tk], writes=[tk])
            sc.op('dve', lambda e, b=b, T=T: e.scalar_tensor_tensor(out=tmpw[b][:], in0=x1t[b][:], scalar=T[:, 2:3], in1=GS2, op0=ALU.mult, op1=ALU.mult),
                  reads=[('x1t', b), tk, 'mod'], writes=[('tmpw', b)])
            sc.op('dve', lambda e, b=b: e.tensor_tensor(out=h2f[b][:], in0=tmpw[b][:], in1=SHIFT2, op=ALU.add), reads=[('tmpw', b), 'mod'], writes=[('h2f', b)])
            sc.op('act', lambda e, b=b: e.copy(out=h2b[b][:], in_=h2f[b][:]), reads=[('h2f', b)], writes=[('h2b', b)])
            sc.dma('sp', lambda e, i=i, b=b: e.dma_start(out=h2_d[i * 128:(i + 1) * 128, :], in_=h2b[b][:]), ('h2s', b), reads=[('h2b', b)], writes=[('h2d', i)])

        def wC(i):
            b = i % 2
            g, k4 = i // 4, i % 4
            yk = ('ytl', g % 2)
            xk = ('xt2', b)
            banks = [('ps', 2 * b), ('ps', 2 * b + 1)]
            T = stw[:, i, :]
            tk = ('stw', i)
            tb = [('ps', 4), ('ps', 5)]

            def trh(e, b=b):
                for kc in range(8):
                    r = e.transpose(out=ps[:, 4 + kc // 4, (kc % 4) * 128:(kc % 4 + 1) * 128], in_=h2f[b][:, kc * 128:(kc + 1) * 128], identity=ident)
                return r
            sc.op('pe', trh, reads=[('h2f', b), 'consts'], writes=tb)
            sc.op('act', lambda e, b=b: e.copy(out=h2T[b][:].rearrange("p k t -> p (k t)"), in_=ps[:, 4:6, :].rearrange("p a n -> p (a n)")),
                  reads=tb, writes=[('h2T', b)])
            lb = ('ps', 6 + b)

            def mml(e, b=b):
                for kc in range(8):
                    r = e.matmul(ps[:, 6 + b, 0:NE], lhsT=h2T[b][:, kc, :], rhs=wrt[:, kc, :], start=(kc == 0), stop=(kc == 7))
                return r
            sc.op('pe', mml, reads=[('h2T', b), 'wrt'], writes=[lb])
            sc.op('dve', lambda e, b=b, T=T: e.tensor_reduce(out=T[:, 3:4], in_=ps[:, 6 + b, 0:NE], axis=AX.X, op=ALU.max), reads=[lb], writes=[tk])
            sc.op('dve', lambda e, T=T: e.tensor_scalar(out=T[:, 4:5], in0=T[:, 3:4], scalar1=-1.0, scalar2=None, op0=ALU.mult), reads=[tk], writes=[tk])
            sc.op('act', lambda e, b=b, T=T: e.activation(out=esm[:, b, :], in_=ps[:, 6 + b, 0:NE], func=AF.Exp, bias=T[:, 4:5], accum_out=T[:, 5:6]),
                  reads=[lb, tk], writes=[('esm', b), tk])
            sc.op('dve', lambda e, T=T: e.reciprocal(out=T[:, 6:7], in_=T[:, 5:6]), reads=[tk], writes=[tk])
            sc.op('dve', lambda e, b=b, i=i, T=T: e.tensor_scalar(out=aff[:, i, :], in0=esm[:, b, :], scalar1=T[:, 6:7], scalar2=None, op0=ALU.mult),
                  reads=[('esm', b), tk], writes=['aff'])

        wA(0)
        for i in range(NT):
            if i + 1 < NT:
                wA(i + 1)
            wB(i)
            wC(i)
        if debug:
            o = dbg_out("dbg_aff", [128, NT, NE])
            sc.dma('sp', lambda e: e.dma_start(out=o[:, :, :], in_=aff[:]), 'dbgaff', reads=['aff'])
        sc.barrier()

    if stage <= 4:
        sc.finish()
        return nc, dbg

    rank_tok = sb("rank_tok", [128, NT, NE], F32, sW)
    R5 = sb("R5", [128, NT, NE, 5], BF16, sW)
    with ExitStack() as pr:
        affT = sb("affT", [NE, S], F32, pr)
        junkR = sb("junkR", [NE, S], F32, pr)
        mkT = sb("mkT", [NE, S], F32, pr)
        csT = sb("csT", [NE, S], F32, pr)
        bs = sb("bs", [NE, 4], F32, pr)
        r1 = sb("r1", [128, NT, NE], F32, pr)
        for rnd in range(2):
            banks = [('ps', k) for k in range(4)]

            def tra(e, rnd=rnd):
                for k in range(16):
                    i = rnd * 16 + k
                    r = e.transpose(out=ps[0:NE, k // 4, (k % 4) * 128:(k % 4 + 1) * 128], in_=aff[:, i, :], identity=ident)
                return r
            sc.op('pe', tra, reads=['aff', 'consts'], writes=banks)
            sc.op('act', lambda e, rnd=rnd: e.copy(out=affT[:, rnd * 2048:(rnd + 1) * 2048], in_=ps[0:NE, 0:4, :].rearrange("p a n -> p (a n)")),
                  reads=banks, writes=['affT'])
        sc.op('dve', lambda e: e.memset(bs[:, 0:1], 0.0), writes=['bs'])
        for n in range(NBIS):
            w = 2.0 ** (-(n + 1))
            sc.op('dve', lambda e, w=w: e.tensor_scalar(out=bs[:, 1:2], in0=bs[:, 0:1], scalar1=w, scalar2=None, op0=ALU.add), reads=['bs'], writes=['bs'])
            sc.op('dve', lambda e: e.tensor_scalar(out=junkR[:], in0=affT[:], scalar1=bs[:, 1:2], scalar2=None, op0=ALU.is_gt, op1=ALU.add,
                                                   accum_out=bs[:, 2:3]), reads=['affT', 'bs'], writes=['junkR', 'bs'])
            sc.op('dve', lambda e: e.tensor_scalar(out=bs[:, 3:4], in0=bs[:, 2:3], scalar1=CAP - 0.5, scalar2=None, op0=ALU.is_gt), reads=['bs'], writes=['bs'])
            sc.op('dve', lambda e, w=w: e.scalar_tensor_tensor(out=bs[:, 0:1], in0=bs[:, 3:4], scalar=w, in1=bs[:, 0:1], op0=ALU.mult, op1=ALU.add),
                  reads=['bs'], writes=['bs'])
        sc.op('dve', lambda e: e.tensor_scalar(out=mkT[:], in0=affT[:], scalar1=bs[:, 0:1], scalar2=None, op0=ALU.is_gt), reads=['affT', 'bs'], writes=['mkT'])
        sc.op('pool', lambda e: e.memset(junkR[:], 1.0), reads=[], writes=['junkR'])
        sc.op('dve', lambda e: e.tensor_tensor_scan(out=csT[:], data0=junkR[:], data1=mkT[:], initial=0.0, op0=ALU.mult, op1=ALU.add),
              reads=['junkR', 'mkT'], writes=['csT'])
        sc.op('dve', lambda e: e.tensor_tensor(out=csT[:], in0=csT[:], in1=mkT[:], op=ALU.mult), reads=['csT', 'mkT'], writes=['csT'])
        sc.op('dve', lambda e: e.tensor_scalar(out=csT[:], in0=csT[:], scalar1=-1.0, scalar2=None, op0=ALU.add), reads=['csT'], writes=['csT'])
        rb = ('ps', 4)

        def trr(e):
            for i in range(NT):
                r = e.transpose(out=ps[:, 4, i * NE:(i + 1) * NE], in_=csT[:, i * 128:(i + 1) * 128], identity=consts[0:NE, C_IDENT:C_IDENT + NE])
            return r
        sc.op('pe', trr, reads=['csT', 'consts'], writes=[rb])
        sc.op('act', lambda e: e.copy(out=rank_tok[:].rearrange("p t e -> p (t e)"), in_=ps[:, 4, :]), reads=[rb], writes=['rank_tok'])
        sc.op('pool', lambda e: e.tensor_copy(out=R5[:, :, :, 0], in_=consts[:, C_TLO:C_TLO + 1].unsqueeze(2).broadcast_to([128, NT, NE])),
              reads=['consts'], writes=['R5'])
        sc.op('pool', lambda e: e.tensor_copy(out=R5[:, :, :, 1], in_=consts[:, C_THI:C_THI + NT].unsqueeze(2).broadcast_to([128, NT, NE])),
              reads=['consts'], writes=['R5'])
        sc.op('dve', lambda e: e.tensor_copy(out=R5[:, :, :, 2], in_=aff[:]), reads=['aff'], writes=['R5'])
        sc.op('dve', lambda e: e.tensor_tensor(out=r1[:], in0=aff[:], in1=R5[:, :, :, 2], op=ALU.subtract), reads=['aff', 'R5'], writes=['r1'])
        sc.op('dve', lambda e: e.tensor_copy(out=R5[:, :, :, 3], in_=r1[:]), reads=['r1'], writes=['R5'])
        sc.op('dve', lambda e: e.tensor_tensor(out=r1[:], in0=r1[:], in1=R5[:, :, :, 3], op=ALU.subtract), reads=['r1', 'R5'], writes=['r1'])
        sc.op('dve', lambda e: e.tensor_copy(out=R5[:, :, :, 4], in_=r1[:]), reads=['r1'], writes=['R5'])
        if debug:
            o = dbg_out("dbg_rank", [128, NT, NE])
            sc.dma('sp', lambda e: e.dma_start(out=o[:, :, :], in_=rank_tok[:]), 'dbgrank', reads=['rank_tok'])
            o = dbg_out("dbg_thr", [NE, 4])
            sc.dma('sp', lambda e: e.dma_start(out=o[:, :], in_=bs[:]), 'dbgthr', reads=['bs'])
        sc.barrier()

    if stage <= 4.5:
        sc.finish()
        return nc, dbg

    n_exp = NE if stage >= 6 else int(round((stage - 5) * 10)) + 1
    with ExitStack() as pe_:
        NSLOT = 6
        ring = [sb("ring%d" % k, [128, 8, D], BF16, pe_) for k in range(NSLOT)]
        Pm = sb("Pm", [128, NT, 512], BF16, pe_)
        xe = sb("xe", [128, 4, D], BF16, pe_)
        xeT2 = [sb("xeT%d" % k, [128, 8, 512], BF16, pe_) for k in range(2)]
        aT = sb("aT", [128, 16, 512], BF16, pe_)
        sgt = [sb("sgt%d" % k, [128, 512], BF16, pe_) for k in range(2)]
        yv = sb("yv", [128, 4, D], F32, pe_)
        idf = sb("idf", [128, 2, 8], F32, pe_)
        idp = sb("idp", [128, 2, 32], F32, pe_)
        idxi = [sb("idxi%d" % k, [128, 4], I32, pe_) for k in range(2)]
        wg_v = wg_d.rearrange("(e kc p) n -> p e kc n", p=128, kc=8)
        wu_v = wu_d.rearrange("(e kc p) n -> p e kc n", p=128, kc=8)
        wd_v = wd_d.rearrange("(e fh fc p) n -> p e fh fc n", p=128, fc=8, fh=2)

        def load_piece(e_, k):
            slot = ring[k]
            key = ('ring', k)
            if k < 4:
                src = (wg_v if k % 2 == 0 else wu_v)[:, e_, :, (k // 2) * D:(k // 2 + 1) * D]
            else:
                src = wd_v[:, e_, k - 4, :, :]
            sc.dma('pool', lambda e, slot=slot, src=src: e.dma_start(out=slot[:], in_=src), key, writes=[key])

        def prep_a(e_):
            for i in range(NT):
                sc.op('dve', lambda e, i=i: e.tensor_scalar(out=Pm[:, i, :], in0=consts[:, C_IOTA:C_IOTA + 512], scalar1=rank_tok[:, i, e_:e_ + 1],
                                                            scalar2=None, op0=ALU.is_equal), reads=['consts', 'rank_tok'], writes=['Pm'])

        def prep_b(e_):
            b = e_ % 2
            ibank = ('ps', 7)

            def imm(e):
                for cc in range(4):
                    for i in range(NT):
                        r = e.matmul(ps[:, 7, cc * 8:cc * 8 + 5], lhsT=Pm[:, i, cc * 128:(cc + 1) * 128], rhs=R5[:, i, e_, :],
                                     start=(i == 0), stop=(i == NT - 1))
                return r
            sc.op('pe', imm, reads=['Pm', 'R5'], writes=[ibank])
            sc.op('act', lambda e, b=b: e.copy(out=idp[:, b, :].rearrange("p (c k) -> p c k", k=8)[:, :, 0:5], in_=ps[:, 7, 0:32].rearrange("p (c k) -> p c k", k=8)[:, :, 0:5]), reads=[ibank], writes=[('idp', b)])
            ibank = ('idp', b)
            I3 = idp[:, b, :].rearrange("p (c k) -> p c k", k=8)
            F_ = idf[:, b, :]
            fk = ('idf', b)
            sc.op('dve', lambda e, F_=F_, I3=I3: e.scalar_tensor_tensor(out=F_[:, 0:4].unsqueeze(2), in0=I3[:, :, 1:2], scalar=128.0, in1=I3[:, :, 0:1],
                                                                      op0=ALU.mult, op1=ALU.add), reads=[ibank], writes=[fk])
            sc.op('dve', lambda e, F_=F_, I3=I3: e.tensor_reduce(out=F_[:, 4:8], in_=I3[:, :, 2:5], axis=AX.X, op=ALU.add), reads=[ibank], writes=[fk])
            ik = ('idxi', b)
            sc.op('dve', lambda e, F_=F_, b=b: e.tensor_copy(out=idxi[b][:], in_=F_[:, 0:4]), reads=[fk], writes=[ik])
            for cc in range(4):
                sc.dma('pool', lambda e, cc=cc, b=b: e.indirect_dma_start(
                    out=xe[:, cc, :], out_offset=None, in_=h2_d[:, :],
                    in_offset=bass.IndirectOffsetOnAxis(ap=idxi[b][:, cc:cc + 1], axis=0)), 'xe', reads=[ik], writes=['xe'])

        def prep_c(e_):
            xeT = xeT2[e_ % 2]
            for kc in range(8):
                tb = 6
                tbank = ('ps', tb)
                pb = ps[:, tb, :].bitcast(BF16)

                def trx(e, kc=kc, pb=pb):
                    for cc in range(4):
                        r = e.transpose(out=pb[:, cc * 128:(cc + 1) * 128], in_=xe[:, cc, kc * 128:(kc + 1) * 128], identity=identb[:])
                    return r
                sc.op('pe', trx, reads=['xe', 'identb'], writes=[tbank])
                sc.op('act', lambda e, kc=kc, pb=pb, xeT=xeT: e.copy(out=xeT[:, kc, :], in_=pb[:, 0:512]), reads=[tbank], writes=[('xeT', e_ % 2)])

        for k in range(NSLOT):
            load_piece(0, k)
        prep_a(0)
        prep_b(0)
        prep_c(0)
        for e_ in range(n_exp):
            b = e_ % 2
            xeT = xeT2[b]
            xk_ = ('xeT', b)
            if e_ + 1 < n_exp:
                prep_a(e_ + 1)
            for fh in range(2):
                gk, uk = ('ring', 2 * fh), ('ring', 2 * fh + 1)
                Wg, Wu = ring[2 * fh], ring[2 * fh + 1]
                for fo in range(8):
                    gb, ubk = fo % 2, 2 + (fo % 2)

                    def mmg(e, Wg=Wg, fo=fo, gb=gb):
                        for kc in range(8):
                            r = e.matmul(ps[:, gb, :], lhsT=Wg[:, kc, fo * 128:(fo + 1) * 128], rhs=xeT[:, kc, :], start=(kc == 0), stop=(kc == 7))
                        return r

                    def mmu(e, Wu=Wu, fo=fo, ubk=ubk):
                        for kc in range(8):
                            r = e.matmul(ps[:, ubk, :], lhsT=Wu[:, kc, fo * 128:(fo + 1) * 128], rhs=xeT[:, kc, :], start=(kc == 0), stop=(kc == 7))
                        return r
                    sc.op('pe', mmg, reads=[gk, xk_], writes=[('ps', gb)])
                    sc.op('pe', mmu, reads=[uk, xk_], writes=[('ps', ubk)])
                    sg = sgt[fo % 2]
                    sk = ('sgt', fo % 2)
                    sc.op('act', lambda e, sg=sg, gb=gb: e.activation(out=sg[:], in_=ps[:, gb, :], func=AF.Silu), reads=[('ps', gb)], writes=[sk])
                    fidx = fh * 8 + fo
                    sc.op('dve', lambda e, sg=sg, ubk=ubk, fidx=fidx: e.tensor_tensor(out=aT[:, fidx, :], in0=ps[:, ubk, :], in1=sg[:], op=ALU.mult),
                          reads=[('ps', ubk), sk], writes=[('aT', fidx)])
                if e_ + 1 < n_exp:
                    load_piece(e_ + 1, 2 * fh)
                    load_piece(e_ + 1, 2 * fh + 1)
                    if fh == 0:
                        prep_b(e_ + 1)
                    else:
                        prep_c(e_ + 1)
            F_ = idf[:, b, :]
            for cc in range(4):
                for dh in range(2):
                    db = 4 + ((cc * 2 + dh) % 2)

                    def mmd(e, cc=cc, dh=dh, db=db):
                        for fc in range(16):
                            r = e.matmul(ps[:, db, :], lhsT=aT[:, fc, cc * 128:(cc + 1) * 128], rhs=ring[4 + fc // 8][:, fc % 8, dh * 512:(dh + 1) * 512],
                                         start=(fc == 0), stop=(fc == 15))
                        return r
                    sc.op('pe', mmd, reads=[('aT', f) for f in range(16)] + [('ring', 4), ('ring', 5)], writes=[('ps', db)])
                    sc.op('dve', lambda e, cc=cc, dh=dh, db=db, F_=F_: e.scalar_tensor_tensor(
                        out=yv[:, cc, dh * 512:(dh + 1) * 512], in0=ps[:, db, :], scalar=F_[:, 4 + cc:5 + cc], in1=g2rep[:, dh * 512:(dh + 1) * 512],
                        op0=ALU.mult, op1=ALU.mult), reads=[('ps', db), ('idf', b), 'g2rep'], writes=[('yv', cc)])
            if e_ + 1 < n_exp:
                load_piece(e_ + 1, 4)
                load_piece(e_ + 1, 5)
            for cc in range(4):
                sc.dma('pool', lambda e, cc=cc, b=b: e.indirect_dma_start(
                    out=out_d[:, :], out_offset=bass.IndirectOffsetOnAxis(ap=idxi[b][:, cc:cc + 1], axis=0),
                    in_=yv[:, cc, :], in_offset=None, compute_op=ALU.add), 'scat', reads=[('yv', cc), ('idxi', b)], writes=['outd'])
        sc.barrier()
    sW.close()
    sc.finish()
    return nc, dbg


def prep_inputs(inputs, b):
    f = lambda a: np.ascontiguousarray(a, dtype=np.float32)
    m = {
        "x": f(inputs["x"][b]),
        "cT": f(inputs["c"][b].reshape(8, 128).T),
        "pos": np.ascontiguousarray(inputs["positions"][b].reshape(NT, 128).T.astype(np.int32)),
        "norm1_g": f(inputs["norm1_g"][0:1]),
        "norm2_g": f(inputs["norm2_g"][0:1]),
        "w_ada": f(inputs["w_ada"][0]),
        "b_ada": f(inputs["b_ada"][0:1]),
        "w_in": f(inputs["w_in"][0]),
        "convT": f(inputs["mlstm_conv_w"][0].T.reshape(8, 128, 5).transpose(1, 0, 2).reshape(128, 40)),
        "gate_b": f(inputs["mlstm_gate_b"][0:1]),
        "mnorm_g": f(inputs["mlstm_norm_g"][0:1]),
        "qk_g": f(inputs["diff_qk_g"][0].reshape(1, 128)),
        "lam": f(inputs["diff_lambda"][0].reshape(1, 256)),
        "subln_g": f(inputs["diff_subln_g"][0:1]),
        "w_out": f(inputs["w_out"][0]),
        "w_router": f(inputs["w_router"][0]),
        "consts": make_consts(),
    }
    return m


def kernel(**inputs):
    nc, _ = build()
    shared = None
    in_maps = []
    for b in range(8):
        m = prep_inputs(inputs, b)
        if shared is None:
            shared = {
                "w_gate_e": np.ascontiguousarray(inputs["w_gate_e"][0].reshape(NE * D, 2 * D), dtype=np.float32),
                "w_up_e": np.ascontiguousarray(inputs["w_up_e"][0].reshape(NE * D, 2 * D), dtype=np.float32),
                "w_down_e": np.ascontiguousarray(inputs["w_down_e"][0].reshape(NE * 2 * D, D), dtype=np.float32),
            }
        m.update(shared)
        in_maps.append(m)
    res = run_bass_kernel_spmd(nc, in_maps, core_ids=list(range(8)))
    return np.stack([np.asarray(r["out"]) for r in res.results], axis=0).astype(np.float32)
```

```python
import math
from contextlib import ExitStack

import numpy as np
import concourse.bass as bass
import concourse.mybir as mybir
from concourse.bass_utils import run_bass_kernel_spmd

F32 = mybir.dt.float32
BF16 = mybir.dt.bfloat16
I32 = mybir.dt.int32
AF = mybir.ActivationFunctionType
ALU = mybir.AluOpType
AX = mybir.AxisListType

S = 4096
D = 1024
NT = 32
NG = 8
EPS = 1e-6
D_IN = 3600
NE = 16
CAP = 512
LAM_INIT = 0.8 - 0.6 * math.exp(-0.3 * 0)
NBIS = 27

SAME_ENG_SYNC = True

C_IDENT = 0
C_TL = 128
C_TU = 256
C_TLS = 384
C_TUS = 512
C_INDA = 640
C_INDB = 768
C_IOTA = 896
C_TLO = 1408
C_FREQ = 1409
C_THI = 1417
NCONST = 1449


def make_consts():
    c = np.zeros((128, NCONST), np.float32)
    p = np.arange(128)
    s = p[:, None]
    j = p[None, :]
    same = (s // 64) == (j // 64)
    c[:, C_IDENT:C_IDENT + 128] = (s == j)
    c[:, C_TL:C_TL + 128] = same & (s <= j)
    c[:, C_TU:C_TU + 128] = same & (s >= j)
    c[:, C_TLS:C_TLS + 128] = same & (s < j)
    c[:, C_TUS:C_TUS + 128] = same & (s > j)
    c[:, C_INDA:C_INDA + 128] = (s < 64) & (j >= 0)
    c[:, C_INDB:C_INDB + 128] = (s >= 64) & (j >= 0)
    c[:, C_IOTA:C_IOTA + 512] = np.arange(512)[None, :]
    c[:, C_TLO] = p
    inv_freq = (500000.0 ** (-np.arange(0, 16, 2, dtype=np.float32) / 16)).astype(np.float32)
    c[:, C_FREQ:C_FREQ + 8] = (inv_freq.astype(np.float64) / (2 * np.pi)).astype(np.float32)[None, :]
    c[:, C_THI:C_THI + 32] = np.arange(32)[None, :]
    return c


class Sched:
    def __init__(self, nc, es):
        self.nc = nc
        self.es = es
        self.E = {'pe': nc.tensor, 'act': nc.scalar, 'dve': nc.vector, 'pool': nc.gpsimd, 'sp': nc.sync}
        self.sem = {k: es.enter_context(nc.semaphore('s_' + k)) for k in self.E}
        self.cnt = {k: 0 for k in self.E}
        self.seen = {k: {} for k in self.E}
        self.reg = {}
        self.dsem = {}
        self.dsem_by_sid = {}
        self.ninstr = {k: 0 for k in self.E}

    def _deps(self, reads, writes):
        deps = {}

        def add(tok):
            if tok is None:
                return
            if tok[0].startswith('d_'):
                tok = (tok[0], tok[1], self.dsem_by_sid[tok[0]][1])
            if tok[0] not in deps or deps[tok[0]][2] < tok[2]:
                deps[tok[0]] = tok
        for r in reads:
            st = self.reg.get(r)
            if st:
                add(st[0])
        for w in writes:
            st = self.reg.get(w)
            if st:
                add(st[0])
                for t in st[1].values():
                    add(t)
        return deps

    def _wait(self, eng, deps):
        for sid, (_, h, v) in deps.items():
            if sid == 'e_' + eng and not SAME_ENG_SYNC:
                continue
            if self.seen[eng].get(sid, 0) >= v:
                continue
            self.E[eng].wait_ge(h, v)
            self.seen[eng][sid] = v

    def _commit(self, tok, reads, writes):
        for r in reads:
            st = self.reg.setdefault(r, [None, {}])
            st[1][tok[0]] = tok
        for w in writes:
            self.reg[w] = [tok, {}]

    def op(self, eng, fn, reads=(), writes=()):
        self._wait(eng, self._deps(reads, writes))
        ins = fn(self.E[eng])
        self.cnt[eng] += 1
        ins.then_inc(self.sem[eng], 1)
        self._commit(('e_' + eng, self.sem[eng], self.cnt[eng]), reads, writes)

    def dma(self, q, fn, semkey, reads=(), writes=()):
        self._wait(q, self._deps(reads, writes))
        ins = fn(self.E[q])
        d = self.dsem.get(semkey)
        if d is None:
            d = [self.es.enter_context(self.nc.semaphore('d%d' % len(self.dsem))), 0]
            self.dsem[semkey] = d
            self.dsem_by_sid['d_' + str(semkey)] = d
        d[1] += 16
        ins.then_inc(d[0], 16)
        self._commit(('d_' + str(semkey), d[0], d[1]), reads, writes)

    def barrier(self):
        toks = {}
        for k in self.E:
            if self.cnt[k]:
                toks['e_' + k] = ('e_' + k, self.sem[k], self.cnt[k])
        for key, d in self.dsem.items():
            if d[1]:
                toks['d_' + str(key)] = ('d_' + str(key), d[0], d[1])
        for k in self.E:
            self._wait(k, toks)

    def finish(self):
        toks = {}
        for k in self.E:
            if self.cnt[k]:
                toks['e_' + k] = ('e_' + k, self.sem[k], self.cnt[k])
        for key, d in self.dsem.items():
            if d[1]:
                toks['d_' + str(key)] = ('d_' + str(key), d[0], d[1])
        self._wait('sp', toks)


def build(stage=99, debug=False):
    nc = bass.Bass("TRN2", target_bir_lowering=False)
    es = ExitStack()
    sc = Sched(nc, es)

    def din(name, shape, dt=F32):
        return nc.dram_tensor(name, list(shape), dt, kind="ExternalInput").ap()

    x_d = din("x", [S, D])
    cT_d = din("cT", [128, 8])
    pos_d = din("pos", [128, NT], I32)
    n1g_d = din("norm1_g", [1, D])
    n2g_d = din("norm2_g", [1, D])
    wada_d = din("w_ada", [D, 6 * D])
    bada_d = din("b_ada", [1, 6 * D])
    win_d = din("w_in", [D, D_IN])
    convT_d = din("convT", [128, 40])
    gateb_d = din("gate_b", [1, 16])
    mng_d = din("mnorm_g", [1, 512])
    qkg_d = din("qk_g", [1, 128])
    lam_d = din("lam", [1, 256])
    subg_d = din("subln_g", [1, 128])
    wout_d = din("w_out", [D, D])
    wr_d = din("w_router", [D, NE])
    consts_d = din("consts", [128, NCONST])
    if stage >= 5:
        wg_d = din("w_gate_e", [NE * D, 2 * D])
        wu_d = din("w_up_e", [NE * D, 2 * D])
        wd_d = din("w_down_e", [NE * 2 * D, D])
    out_d = nc.dram_tensor("out", [S, D], F32, kind="ExternalOutput").ap()
    skind = "ExternalOutput" if debug else "Internal"
    yT_d = nc.dram_tensor("yT_d", [D, S], BF16, kind=skind).ap()
    h2_d = nc.dram_tensor("h2_d", [S, D], BF16, kind=skind).ap()
    dbg = {}

    def dbg_out(name, shape, dt=F32):
        if debug:
            dbg[name] = nc.dram_tensor(name, list(shape), dt, kind="ExternalOutput").ap()
            return dbg[name]
        return None

    uniq = [0]

    def sb(name, shape, dt=F32, stack=es):
        uniq[0] += 1
        return stack.enter_context(nc.sbuf_tensor("sb%d_%s" % (uniq[0], name), list(shape), dt))

    ps = es.enter_context(nc.psum_tensor("ps", [128, 8, 512], F32))
    consts = sb("consts", [128, NCONST])
    identb = sb("identb", [128, 128], BF16)
    s1 = ExitStack()
    prm = sb("prm", [128, 16 + 512 + 128 + 256 + 128], F32, s1)
    P_GB, P_MNG, P_QKG, P_LAM, P_SUB = 0, 16, 528, 656, 912
    convT = sb("convT", [128, 40], F32, s1)
    maskF = sb("maskF", [128, 128], BF16, s1)
    maskB = sb("maskB", [128, 128], BF16, s1)
    small = sb("small", [128, 64], F32, s1)
    SM_NB, SM_NLAM = 0, 1
    ident = consts[:, C_IDENT:C_IDENT + 128]

    h1T = sb("h1T", [128, 8, S], BF16, s1)
    gm = sb("gm", [128, NT, 16], F32, s1)
    smod = ExitStack()
    mod = sb("mod", [128, 6 * D], F32, smod)
    modrow_d = nc.dram_tensor("modrow_d", [1, 6 * D], F32, kind="Internal").ap()
    CK = 'consts'
    sc.dma('sp', lambda e: e.dma_start(out=consts[:], in_=consts_d[:, :]), CK, writes=['consts'])
    sc.dma('sp', lambda e: e.dma_start(out=convT[:], in_=convT_d[:, :]), CK, writes=['convT'])
    for (off, src, n) in [(P_GB, gateb_d, 16), (P_MNG, mng_d, 512),
                          (P_QKG, qkg_d, 128), (P_LAM, lam_d, 256), (P_SUB, subg_d, 128)]:
        sc.dma('sp', lambda e, off=off, src=src, n=n: e.dma_start(
            out=prm[:, off:off + n], in_=src[0:1, :].partition_broadcast(128)), CK, writes=['prm'])
    sc.dma('sp', lambda e: e.dma_start(out=mod[:], in_=bada_d[0:1, :].partition_broadcast(128)), CK, writes=['mod'])

    with ExitStack() as p0:
        cT = sb("cT", [128, 8], F32, p0)
        prmA = sb("prmA", [128, 2 * D], F32, p0)
        sc.dma('sp', lambda e: e.dma_start(out=prmA[:, 0:D], in_=n1g_d[0:1, :].partition_broadcast(128)), CK, writes=['prmA'])
        sc.dma('sp', lambda e: e.dma_start(out=prmA[:, D:2 * D], in_=n2g_d[0:1, :].partition_broadcast(128)), CK, writes=['prmA'])
        scT = sb("scT", [128, 8], F32, p0)
        lc = sb("lc", [128, 8, 128], BF16, p0)
        wa = [sb("wa%d" % i, [128, 8, 512], BF16, p0) for i in range(2)]
        sc.dma('sp', lambda e: e.dma_start(out=cT[:], in_=cT_d[:, :]), 'cT', writes=['cT'])
        sc.op('act', lambda e: e.activation(out=scT[:], in_=cT[:], func=AF.Silu), reads=['cT'], writes=['scT'])
        sc.op('dve', lambda e: e.tensor_copy(out=lc[:], in_=scT[:].unsqueeze(2).broadcast_to([128, 8, 128])),
              reads=['scT'], writes=['lc'])
        sc.op('dve', lambda e: e.tensor_copy(out=identb[:], in_=ident), reads=['consts'], writes=['identb'])
        sc.op('dve', lambda e: e.tensor_copy(out=maskF[:], in_=consts[:, C_TL:C_TL + 128]), reads=['consts'], writes=['maskF'])
        sc.op('dve', lambda e: e.tensor_copy(out=maskB[:], in_=consts[:, C_TU:C_TU + 128]), reads=['consts'], writes=['maskB'])
        wada_v = wada_d.rearrange("(kc p) n -> p kc n", p=128)
        for n in range(12):
            w = wa[n % 2]
            wk = ('wa', n % 2)
            sc.dma('pool', lambda e, w=w, n=n: e.dma_start(out=w[:], in_=wada_v[:, :, n * 512:(n + 1) * 512]),
                   wk, writes=[wk])
            bank = ('ps', n % 2)

            def mm(e, w=w, n=n):
                for kc in range(8):
                    r = e.matmul(ps[:, n % 2, :], lhsT=lc[:, kc, :], rhs=w[:, kc, :], start=(kc == 0), stop=(kc == 7))
                return r
            sc.op('pe', mm, reads=['lc', wk], writes=[bank])
            sc.op('dve', lambda e, n=n: e.tensor_tensor(out=mod[:, n * 512:(n + 1) * 512], in0=ps[:, n % 2, :],
                                                        in1=mod[:, n * 512:(n + 1) * 512], op=ALU.add),
                  reads=[bank, 'mod'], writes=['mod'])
        sc.op('dve', lambda e: e.scalar_tensor_tensor(out=mod[:, D:2 * D], in0=mod[:, D:2 * D], scalar=1.0,
                                                      in1=prmA[:, 0:D], op0=ALU.add, op1=ALU.mult),
              reads=['mod', 'prmA'], writes=['mod'])
        sc.op('dve', lambda e: e.scalar_tensor_tensor(out=mod[:, 4 * D:5 * D], in0=mod[:, 4 * D:5 * D], scalar=1.0,
                                                      in1=prmA[:, D:2 * D], op0=ALU.add, op1=ALU.mult),
              reads=['mod', 'prmA'], writes=['mod'])
        if debug:
            o = dbg_out("dbg_mod", [128, 6 * D])
            sc.dma('sp', lambda e: e.dma_start(out=o[:, :], in_=mod[:]), 'dbgmod', reads=['mod'])
        sc.barrier()
    SHIFT1, GS1, GATE1, SHIFT2, GS2, GATE2 = [mod[:, i * D:(i + 1) * D] for i in range(6)]

    if stage <= 0:
        sc.finish()
        return nc, dbg

    with ExitStack() as p1:
        xt = [sb("xt%d" % i, [128, D], F32, p1) for i in range(2)]
        junk = sb("junk1", [128, D], BF16, p1)
        tmp = [sb("tmp1_%d" % i, [128, D], F32, p1) for i in range(2)]
        h1b = [sb("h1b%d" % i, [128, D], BF16, p1) for i in range(2)]
        st1 = sb("st1", [128, NT, 3], F32, p1)
        wgt = sb("wgt", [128, 8, 16], BF16, p1)
        win_v = win_d.rearrange("(kc p) n -> p kc n", p=128)
        sc.dma('pool', lambda e: e.dma_start(out=wgt[:], in_=win_v[:, :, 2048:2064]), 'wgt', writes=['wgt'])
        def p1A(i):
            b = i % 2
            xk, tk, hk = ('xt', b), ('tmp1', b), ('h1b', b)
            sc.dma('sp', lambda e, i=i, b=b: e.dma_start(out=xt[b][:], in_=x_d[i * 128:(i + 1) * 128, :]), xk, writes=[xk])
            sc.op('act', lambda e, i=i, b=b: e.activation(out=junk[:], in_=xt[b][:], func=AF.Square,
                                                          accum_out=st1[:, i, 0:1]), reads=[xk], writes=['junk1', ('st1', i)])
            sc.op('act', lambda e, i=i: e.activation(out=st1[:, i, 1:2], in_=st1[:, i, 0:1], func=AF.Sqrt,
                                                     scale=1.0 / D, bias=EPS), reads=[('st1', i)], writes=[('st1', i)])
            sc.op('dve', lambda e, i=i: e.reciprocal(out=st1[:, i, 2:3], in_=st1[:, i, 1:2]),
                  reads=[('st1', i)], writes=[('st1', i)])
            sc.op('dve', lambda e, i=i, b=b: e.scalar_tensor_tensor(out=tmp[b][:], in0=xt[b][:], scalar=st1[:, i, 2:3],
                                                                    in1=GS1, op0=ALU.mult, op1=ALU.mult),
                  reads=[xk, ('st1', i), 'mod'], writes=[tk])
            sc.op('dve', lambda e, b=b: e.tensor_tensor(out=h1b[b][:], in0=tmp[b][:], in1=SHIFT1, op=ALU.add),
                  reads=[tk, 'mod'], writes=[hk])

        def p1B(i):
            b = i % 2
            xk, tk, hk = ('xt', b), ('tmp1', b), ('h1b', b)
            bank = ('ps', 2 + b)
            pb = ps[:, 2 + b, :].bitcast(BF16)

            def tr(e, b=b, pb=pb):
                for kc in range(8):
                    r = e.transpose(out=pb[:, kc * 128:(kc + 1) * 128], in_=h1b[b][:, kc * 128:(kc + 1) * 128], identity=identb[:])
                return r
            sc.op('pe', tr, reads=[hk, 'identb'], writes=[bank])
            sc.op('act', lambda e, i=i, pb=pb: e.copy(out=h1T[:, :, i * 128:(i + 1) * 128],
                                                      in_=pb.rearrange("p (k t) -> p k t", k=8)),
                  reads=[bank], writes=[('h1T', i)])
            gbank = ('ps', 4 + b)

            def gmm(e, i=i, b=b):
                for kc in range(8):
                    r = e.matmul(ps[:, 4 + b, 0:16], lhsT=h1T[:, kc, i * 128:(i + 1) * 128], rhs=wgt[:, kc, :],
                                 start=(kc == 0), stop=(kc == 7))
                return r
            sc.op('pe', gmm, reads=[('h1T', i), 'wgt'], writes=[gbank])
            sc.op('dve', lambda e, i=i, b=b: e.tensor_tensor(out=gm[:, i, :], in0=ps[:, 4 + b, 0:16],
                                                             in1=prm[:, P_GB:P_GB + 16], op=ALU.add),
                  reads=[gbank, 'prm'], writes=['gm'])

        p1A(0)
        for i in range(NT):
            if i + 1 < NT:
                p1A(i + 1)
            p1B(i)
        if debug:
            o = dbg_out("dbg_h1T", [128, 8, S], BF16)
            sc.dma('sp', lambda e: e.dma_start(out=o[:, :, :], in_=h1T[:]), 'dbgh1T', reads=[('h1T', i) for i in range(NT)])
            o3 = dbg_out("dbg_st1", [128, NT, 3])
            sc.dma('sp', lambda e: e.dma_start(out=o3[:, :, :], in_=st1[:]), 'dbgst1', reads=[('st1', i) for i in range(NT)])
            o4 = dbg_out("dbg_tmp", [128, D])
            sc.dma('sp', lambda e: e.dma_start(out=o4[:, :], in_=tmp[1][:]), 'dbgtmp', reads=[('tmp1', 1)])
            o5 = dbg_out("dbg_h1b", [128, D], BF16)
            sc.dma('sp', lambda e: e.dma_start(out=o5[:, :], in_=h1b[1][:]), 'dbgh1b', reads=[('h1b', 1)])
            o2 = dbg_out("dbg_gm", [128, NT, 16])
            sc.dma('sp', lambda e: e.dma_start(out=o2[:, :, :], in_=gm[:]), 'dbggm', reads=['gm'])
        sc.barrier()

    sc.dma('sp', lambda e: e.dma_start(out=modrow_d[0:1, :], in_=mod[0:1, :]), 'modrow', reads=['mod'], writes=['modrow_d'])
    sc.barrier()
    smod.close()

    if stage <= 1:
        sc.finish()
        return nc, dbg

    LNK = -0.5 * math.log(128.0)
    cum = sb("cum", [128, 4, NT, 4], F32, s1)
    egs = sb("egs", [128, 4, NT, 4], F32, s1)
    tw = sb("tw", [128, 6, NT, 4], F32, s1)
    with ExitStack() as pg:
        lsg = sb("lsg", [128, 2, NT, 4], F32, pg)
        tg = sb("tg", [128, 2, NT, 4], F32, pg)
        for d, c0 in ((0, 4), (1, 12)):
            sc.op('act', lambda e, d=d, c0=c0: e.activation(out=tg[:, d, :, :], in_=gm[:, :, c0:c0 + 4], func=AF.Exp, scale=-1.0),
                  reads=['gm'], writes=['tg'])
        sc.op('act', lambda e: e.activation(out=tg[:], in_=tg[:], func=AF.Ln, bias=1.0), reads=['tg'], writes=['tg'])
        sc.op('dve', lambda e: e.tensor_scalar(out=lsg[:], in0=tg[:], scalar1=-1.0, scalar2=None, op0=ALU.mult),
              reads=['tg'], writes=['lsg'])
        lf = lsg[:, 0, :, :].rearrange("p t h -> p (t h)")
        lb = lsg[:, 1, :, :].rearrange("p t h -> p (t h)")
        specs = [(C_TL, lf), (C_TUS, lf), (C_TU, lb), (C_TLS, lb)]

        def cmm(e):
            for k, (co, rhs) in enumerate(specs):
                r = e.matmul(ps[:, 6, k * 128:(k + 1) * 128], lhsT=consts[:, co:co + 128], rhs=rhs, start=True, stop=True)
            return r
        sc.op('pe', cmm, reads=['lsg', 'consts'], writes=[('ps', 6)])
        sc.op('act', lambda e: e.copy(out=cum[:].rearrange("p a t h -> p (a t h)"), in_=ps[:, 6, :]),
              reads=[('ps', 6)], writes=['cum'])
        specs2 = [(C_INDA, lf), (C_INDB, lf), (C_INDA, lb), (C_INDB, lb)]

        def gmm2(e):
            for k, (co, rhs) in enumerate(specs2):
                r = e.matmul(ps[:, 7, k * 128:(k + 1) * 128], lhsT=consts[:, co:co + 128], rhs=rhs, start=True, stop=True)
            return r
        sc.op('pe', gmm2, reads=['lsg', 'consts'], writes=[('ps', 7)])
        sc.op('act', lambda e: e.activation(out=egs[:].rearrange("p a t h -> p (a t h)"), in_=ps[:, 7, :], func=AF.Exp),
              reads=[('ps', 7)], writes=['egs'])
        for d, ic in ((0, 0), (1, 8)):
            ig = gm[:, :, ic:ic + 4]
            a_ = cum[:, 2 * d, :, :]
            r_ = cum[:, 2 * d + 1, :, :]
            sc.op('dve', lambda e, d=d, ig=ig, a_=a_: e.tensor_tensor(out=tw[:, 3 * d, :, :], in0=ig, in1=a_, op=ALU.subtract),
                  reads=['gm', 'cum'], writes=['tw'])
            sc.op('dve', lambda e, d=d, ig=ig, r_=r_: e.tensor_tensor(out=tw[:, 3 * d + 1, :, :], in0=ig, in1=r_, op=ALU.add),
                  reads=['gm', 'cum'], writes=['tw'])
            sc.op('act', lambda e, d=d: e.activation(out=tw[:, 3 * d:3 * d + 2, :, :], in_=tw[:, 3 * d:3 * d + 2, :, :],
                                                     func=AF.Exp, bias=LNK), reads=['tw'], writes=['tw'])
            sc.op('act', lambda e, d=d, a_=a_: e.activation(out=tw[:, 3 * d + 2, :, :], in_=a_, func=AF.Exp),
                  reads=['cum'], writes=['tw'])
        if debug:
            o = dbg_out("dbg_tw", [128, 6, NT, 4])
            sc.dma('sp', lambda e: e.dma_start(out=o[:, :, :, :], in_=tw[:]), 'dbgtw', reads=['tw'])
            o = dbg_out("dbg_egs", [128, 4, NT, 4])
            sc.dma('sp', lambda e: e.dma_start(out=o[:, :, :, :], in_=egs[:]), 'dbgegs', reads=['egs'])
        sc.barrier()

    if stage <= 1.5:
        sc.finish()
        return nc, dbg

    win_v = win_d.rearrange("(kc p) n -> p kc n", p=128)
    yT_v = yT_d.rearrange("(c p) t -> p c t", p=128)

    def emit_y_tile(ph, h_chunk, i, ym, ymk, yst, ystk):
        g4 = i % 4
        bank = ('ps', 5)
        pb = ps[:, 5, :].bitcast(BF16)
        sc.op('pe', lambda e: e.transpose(out=pb[:, g4 * 128:(g4 + 1) * 128], in_=ym, identity=identb[:]),
              reads=[ymk, 'identb'], writes=[bank])
        sc.op('act', lambda e: e.copy(out=yst[:, g4 * 128:(g4 + 1) * 128], in_=pb[:, g4 * 128:(g4 + 1) * 128]),
              reads=[bank], writes=[ystk])
        if g4 == 3:
            t0 = (i - 3) * 128
            sc.dma('sp', lambda e: e.dma_start(out=yT_v[:, h_chunk, t0:t0 + 512], in_=yst[:]), ystk, reads=[ystk], writes=['yT_d'])

    n_mheads = 4 if stage >= 2.5 else 1
    sM = ExitStack()
    wm2 = [sb("wm%d" % k, [128, 8, 512], BF16, sM) for k in range(2)]
    for h in range(n_mheads):
        with ExitStack() as ph:
            wm = wm2[h % 2]
            WMK = ('wm', h % 2)
            qkT = sb("qkT", [128, 2, S], BF16, ph)
            pre = sb("pre", [128, 2, 516], F32, ph)
            acc = sb("acc", [128, 2, 512], F32, ph)
            vext = sb("vext", [128, NT, 129], BF16, ph)
            sgo = sb("sgo", [128, NT, 128], BF16, ph)
            ktok = sb("ktok", [128, NT, 128], BF16, ph)
            Cst = sb("Cst", [128, 2, 65, 129], BF16, ph)
            stt = sb("stt", [128, 2, 2, 129], F32, ph)
            vtmp = [sb("vtmp%d" % k, [128, 129], BF16, ph) for k in range(2)]
            smg = [sb("smg%d" % k, [128, 2, 4, 128], BF16, ph) for k in range(2)]
            vtg = [sb("vtg%d" % k, [128, 2, 4, 129], BF16, ph) for k in range(2)]
            pst = sb("pst", [128, 2, 48], F32, ph)
            hfg = [sb("hfg%d" % k, [128, 4, 128], F32, ph) for k in range(2)]
            hsg = [sb("hsg%d" % k, [128, 4, 128], F32, ph) for k in range(2)]
            ymg = [sb("ymg%d" % k, [128, 4, 128], BF16, ph) for k in range(2)]
            yst = [sb("yst%d" % k, [128, 512], BF16, ph) for k in range(2)]
            for hh in ([0, 1] if h == 0 else [h + 1]):
                if hh >= n_mheads:
                    continue
                for k, c0 in enumerate((hh * 128, 512 + hh * 128, 1024 + hh * 128, 1536 + hh * 128)):
                    sc.dma('pool', lambda e, k=k, c0=c0, hh=hh: e.dma_start(out=wm2[hh % 2][:, :, k * 128:(k + 1) * 128], in_=win_v[:, :, c0:c0 + 128]),
                           ('wm', hh % 2), writes=[('wm', hh % 2)])
            sc.op('pool', lambda e: e.memset(vext[:, :, 128:129], 1.0), writes=['vext1'])
            sc.op('pool', lambda e: e.memset(Cst[:, :, 0, :], 0.0), writes=[('Cst', 0, 0), ('Cst', 1, 0)])
            sc.op('pool', lambda e: e.memset(stt[:, :, 0, :], 0.0), writes=[('stt', 0, 0), ('stt', 1, 0)])
            sc.op('pool', lambda e: e.memset(pre[:, :, 0:4], 0.0), writes=['pre'])
            for g in range(NG + 1):
                if g < NG:
                    for qk in range(2):
                        bank = ('ps', qk)

                        def mm(e, qk=qk, g=g):
                            for kc in range(8):
                                r = e.matmul(ps[:, qk, :], lhsT=wm[:, kc, qk * 128:(qk + 1) * 128],
                                             rhs=h1T[:, kc, g * 512:(g + 1) * 512], start=(kc == 0), stop=(kc == 7))
                            return r
                        sc.op('pe', mm, reads=[WMK] + [('h1T', 4 * g + k) for k in range(4)], writes=[bank])
                        sc.op('act', lambda e, qk=qk: e.copy(out=pre[:, qk, 4:516], in_=ps[:, qk, :]), reads=[bank], writes=['pre'])
                else:
                    sc.op('pool', lambda e: e.memset(pre[:, :, 4:8], 0.0), writes=['pre'])
                if g < NG:
                    for k in range(4):
                        i = 4 * g + k
                        bank = ('ps', 2 + (i % 2))

                        def mmvo(e, i=i):
                            for kc in range(8):
                                r = e.matmul(ps[:, 2 + (i % 2), 0:256], lhsT=h1T[:, kc, i * 128:(i + 1) * 128], rhs=wm[:, kc, 256:512],
                                             start=(kc == 0), stop=(kc == 7))
                            return r
                        sc.op('pe', mmvo, reads=[WMK, ('h1T', i)], writes=[bank])
                        sc.op('act', lambda e, i=i: e.copy(out=vext[:, i, 0:128], in_=ps[:, 2 + (i % 2), 0:128]), reads=[bank], writes=[('vext', i)])
                        sc.op('act', lambda e, i=i: e.activation(out=sgo[:, i, :], in_=ps[:, 2 + (i % 2), 128:256], func=AF.Sigmoid),
                              reads=[bank], writes=[('sgo', i)])
                W = 512 if g < NG else 2
                for qk in range(2):
                    cb = (4 * qk + h) * 5
                    sc.op('dve', lambda e, qk=qk, cb=cb, W=W: e.tensor_scalar(out=acc[:, qk, 0:W], in0=pre[:, qk, 0:W], scalar1=convT[:, cb:cb + 1],
                                                                            scalar2=None, op0=ALU.mult), reads=['pre', 'convT'], writes=[('acc', qk)])
                    for j in range(1, 5):
                        sc.op('dve', lambda e, qk=qk, cb=cb, j=j, W=W: e.scalar_tensor_tensor(
                            out=acc[:, qk, 0:W], in0=pre[:, qk, j:j + W], scalar=convT[:, cb + j:cb + j + 1], in1=acc[:, qk, 0:W],
                            op0=ALU.mult, op1=ALU.add), reads=['pre', 'convT', ('acc', qk)], writes=[('acc', qk)])
                    n0 = 2 if g == 0 else 0
                    t0 = 512 * g - 2 + n0
                    sc.op('act', lambda e, qk=qk, n0=n0, t0=t0, W=W: e.activation(out=qkT[:, qk, t0:t0 + W - n0], in_=acc[:, qk, n0:W], func=AF.Silu),
                          reads=[('acc', qk)], writes=[('qkT', qk)])
                if g < NG:
                    sc.op('pool', lambda e: e.tensor_copy(out=pre[:, :, 0:4], in_=pre[:, :, 512:516]), reads=['pre'], writes=['pre'])
            for g in range(NG):
                bank = ('ps', 4)
                pb = ps[:, 4, :].bitcast(BF16)

                def trk(e, g=g, pb=pb):
                    for k in range(4):
                        i = 4 * g + k
                        r = e.transpose(out=pb[:, k * 128:(k + 1) * 128], in_=qkT[:, 1, i * 128:(i + 1) * 128], identity=identb[:])
                    return r
                sc.op('pe', trk, reads=[('qkT', 1), 'identb'], writes=[bank])
                sc.op('act', lambda e, g=g, pb=pb: e.copy(out=ktok[:, 4 * g:4 * g + 4, :], in_=pb[:, 0:512].rearrange("p (k d) -> p k d", k=4)),
                      reads=[bank], writes=['ktok'])
            nv = 0
            for step in range(64):
                for d in range(2):
                    c = step if d == 0 else 63 - step
                    i, half = c // 2, c % 2
                    first_of_tile = (half == 0) if d == 0 else (half == 1)
                    vk = ('vhat', d)
                    vb = vtmp[d]
                    if first_of_tile:
                        sc.op('act', lambda e, d=d, i=i, vb=vb: e.activation(out=vb[:], in_=vext[:, i, :], func=AF.Copy,
                                                                             scale=tw[:, 3 * d + 1, i, h:h + 1]),
                              reads=[('vext', i), 'vext1', 'tw'], writes=[vk])
                    slot = nv % 4
                    nv += 1
                    pbank = 4 + slot
                    pc = 0
                    pk = ('ps', pbank)
                    sc.op('pe', lambda e, i=i, half=half, vb=vb, pbank=pbank, pc=pc: e.matmul(
                        ps[:, pbank, pc:pc + 129], lhsT=ktok[half * 64:(half + 1) * 64, i, :], rhs=vb[half * 64:(half + 1) * 64, :],
                        start=True, stop=True), reads=['ktok', vk], writes=[pk])
                    cur, nxt = step % 2, (step + 1) % 2
                    sc.op('dve', lambda e, d=d, i=i, half=half, cur=cur, nxt=nxt, pbank=pbank, pc=pc: e.scalar_tensor_tensor(
                        out=stt[:, d, nxt, :], in0=stt[:, d, cur, :], scalar=egs[:, 2 * d + half, i, h:h + 1], in1=ps[:, pbank, pc:pc + 129],
                        op0=ALU.mult, op1=ALU.add), reads=[('stt', d, cur), 'egs', pk], writes=[('stt', d, nxt)])
                    if d == 0:
                        sc.op('pool', lambda e, d=d, nxt=nxt, step=step: e.tensor_copy(out=Cst[:, d, step + 1, :], in_=stt[:, d, nxt, :]),
                              reads=[('stt', d, nxt)], writes=[('Cst', d, step + 1)])
                    else:
                        sc.op('act', lambda e, d=d, nxt=nxt, step=step: e.copy(out=Cst[:, d, step + 1, :], in_=stt[:, d, nxt, :]),
                              reads=[('stt', d, nxt)], writes=[('Cst', d, step + 1)])
            def s3X1(g):
                    gb = g % 2
                    sbank = ('ps', gb)
                    kF, kB = ('smg', gb, 0), ('smg', gb, 1)
                    vtk = ('vtg', gb)
                    ubanks = [('ps', 2 + t) for t in range(4)]
                    P = pst[:, gb, :]
                    pk = ('pst', gb)
                    U4 = ps[:, 2:6, 0:258].rearrange("p t (d v) -> p t d v", d=2)
                    ea2 = tw[:, 2:6:3, 4 * g:4 * g + 4, h].rearrange("p d t -> p t d")
                    Pv = lambda lo: P[:, lo:lo + 8].rearrange("p (t d) -> p t d", d=2)
                    RR = Pv(24)
                    hfb, hsb = hfg[gb], hsg[gb]
                    gb = g % 2
                    sbank = ('ps', gb)

                    def smm(e, g=g, gb=gb):
                        for t in range(4):
                            blk = slice((4 * g + t) * 128, (4 * g + t + 1) * 128)
                            r = e.matmul(ps[:, gb, t * 128:(t + 1) * 128], lhsT=qkT[:, 1, blk], rhs=qkT[:, 0, blk], start=True, stop=True)
                        return r
                    sc.op('pe', smm, reads=[('qkT', 0), ('qkT', 1)], writes=[sbank])
                    S4 = ps[:, gb, :].rearrange("p (t j) -> p t j", t=4)
                    smF, smB = smg[gb][:, 0, :, :], smg[gb][:, 1, :, :]
                    kF, kB = ('smg', gb, 0), ('smg', gb, 1)
                    sc.op('dve', lambda e, S4=S4, smF=smF: e.tensor_tensor(out=smF, in0=S4, in1=maskF[:].unsqueeze(1).broadcast_to([128, 4, 128]), op=ALU.mult),
                          reads=[sbank, 'maskF'], writes=[kF])
                    sc.op('dve', lambda e, S4=S4, smB=smB: e.tensor_tensor(out=smB, in0=S4, in1=maskB[:].unsqueeze(1).broadcast_to([128, 4, 128]), op=ALU.mult),
                          reads=[sbank, 'maskB'], writes=[kB])
                    vtk = ('vtg', gb)
                    for d in range(2):
                        sc.op('pool', lambda e, d=d, g=g, gb=gb: e.tensor_tensor(
                            out=vtg[gb][:, d, :, :], in0=vext[:, 4 * g:4 * g + 4, :],
                            in1=tw[:, 3 * d, 4 * g:4 * g + 4, h:h + 1].broadcast_to([128, 4, 129]), op=ALU.mult),
                            reads=[('vext', 4 * g + t) for t in range(4)] + ['vext1', 'tw'], writes=[(vtk, d)])

            def s3X2(g):
                    gb = g % 2
                    sbank = ('ps', gb)
                    kF, kB = ('smg', gb, 0), ('smg', gb, 1)
                    vtk = ('vtg', gb)
                    ubanks = [('ps', 2 + t) for t in range(4)]
                    P = pst[:, gb, :]
                    pk = ('pst', gb)
                    U4 = ps[:, 2:6, 0:258].rearrange("p t (d v) -> p t d v", d=2)
                    ea2 = tw[:, 2:6:3, 4 * g:4 * g + 4, h].rearrange("p d t -> p t d")
                    Pv = lambda lo: P[:, lo:lo + 8].rearrange("p (t d) -> p t d", d=2)
                    RR = Pv(24)
                    hfb, hsb = hfg[gb], hsg[gb]
                    ubanks = [('ps', 2 + t) for t in range(4)]

                    def umm(e, g=g, gb=gb):
                        for t in range(4):
                            i = 4 * g + t
                            for d in range(2):
                                o0_ = d * 129
                                e.matmul(ps[:, 2 + t, o0_:o0_ + 129], lhsT=smg[gb][:, d, t, :], rhs=vtg[gb][:, d, t, :], start=True, stop=False)
                                for half in range(2):
                                    c = 2 * i + half
                                    sidx = c if d == 0 else 63 - c
                                    t0 = i * 128 + half * 64
                                    r = e.matmul(ps[half * 64:(half + 1) * 64, 2 + t, o0_:o0_ + 129], lhsT=qkT[:, 0, t0:t0 + 64],
                                                 rhs=Cst[:, d, sidx, :], start=False, stop=True)
                        return r
                    creads = []
                    for t in range(4):
                        i = 4 * g + t
                        creads += [('Cst', 0, 2 * i), ('Cst', 0, 2 * i + 1), ('Cst', 1, 63 - 2 * i), ('Cst', 1, 62 - 2 * i)]
                    sc.op('pe', umm, reads=[kF, kB, (vtk, 0), (vtk, 1), ('qkT', 0)] + creads, writes=ubanks)

            def s3Y1(g):
                    gb = g % 2
                    sbank = ('ps', gb)
                    kF, kB = ('smg', gb, 0), ('smg', gb, 1)
                    vtk = ('vtg', gb)
                    ubanks = [('ps', 2 + t) for t in range(4)]
                    P = pst[:, gb, :]
                    pk = ('pst', gb)
                    U4 = ps[:, 2:6, 0:258].rearrange("p t (d v) -> p t d v", d=2)
                    ea2 = tw[:, 2:6:3, 4 * g:4 * g + 4, h].rearrange("p d t -> p t d")
                    Pv = lambda lo: P[:, lo:lo + 8].rearrange("p (t d) -> p t d", d=2)
                    RR = Pv(24)
                    hfb, hsb = hfg[gb], hsg[gb]
                    P = pst[:, gb, :]
                    pk = ('pst', gb)
                    U4 = ps[:, 2:6, 0:258].rearrange("p t (d v) -> p t d v", d=2)
                    ea2 = tw[:, 2:6:3, 4 * g:4 * g + 4, h].rearrange("p d t -> p t d")
                    Pv = lambda lo: P[:, lo:lo + 8].rearrange("p (t d) -> p t d", d=2)
                    sc.op('dve', lambda e: e.tensor_tensor(out=Pv(0), in0=U4[:, :, :, 128], in1=ea2, op=ALU.mult), reads=ubanks + ['tw'], writes=[pk])
                    sc.op('dve', lambda e: e.tensor_scalar(out=P[:, 16:24], in0=P[:, 0:8], scalar1=-1.0, scalar2=None, op0=ALU.mult), reads=[pk], writes=[pk])
                    sc.op('dve', lambda e: e.scalar_tensor_tensor(out=P[:, 8:16], in0=P[:, 16:24], scalar=1.0, in1=P[:, 0:8], op0=ALU.max, op1=ALU.max),
                          reads=[pk], writes=[pk])
                    sc.op('dve', lambda e: e.reciprocal(out=P[:, 16:24], in_=P[:, 8:16]), reads=[pk], writes=[pk])
                    sc.op('dve', lambda e: e.tensor_tensor(out=Pv(24), in0=Pv(16), in1=ea2, op=ALU.mult), reads=[pk, 'tw'], writes=[pk])
                    RR = Pv(24)
                    hfb, hsb = hfg[gb], hsg[gb]
                    sc.op('dve', lambda e: e.tensor_tensor(out=hfb[:], in0=U4[:, :, 0, 0:128], in1=RR[:, :, 0:1].broadcast_to([128, 4, 128]), op=ALU.mult),
                          reads=ubanks + [pk], writes=[('hfg', gb)])
                    sc.op('dve', lambda e: e.tensor_tensor(out=hsb[:], in0=U4[:, :, 1, 0:128], in1=RR[:, :, 1:2].broadcast_to([128, 4, 128]), op=ALU.mult),
                          reads=ubanks + [pk], writes=[('hsg', gb)])

            def s3Y2(g):
                    gb = g % 2
                    sbank = ('ps', gb)
                    kF, kB = ('smg', gb, 0), ('smg', gb, 1)
                    vtk = ('vtg', gb)
                    ubanks = [('ps', 2 + t) for t in range(4)]
                    P = pst[:, gb, :]
                    pk = ('pst', gb)
                    U4 = ps[:, 2:6, 0:258].rearrange("p t (d v) -> p t d v", d=2)
                    ea2 = tw[:, 2:6:3, 4 * g:4 * g + 4, h].rearrange("p d t -> p t d")
                    Pv = lambda lo: P[:, lo:lo + 8].rearrange("p (t d) -> p t d", d=2)
                    RR = Pv(24)
                    hfb, hsb = hfg[gb], hsg[gb]
                    sc.op('pool', lambda e: e.tensor_tensor(out=hsb[:], in0=hsb[:], in1=hfb[:], op=ALU.add), reads=[('hsg', gb), ('hfg', gb)], writes=[('hsg', gb)])
                    sc.op('pool', lambda e: e.tensor_tensor(out=hfb[:], in0=hsb[:], in1=hsb[:], op=ALU.mult), reads=[('hsg', gb)], writes=[('hfg', gb)])
                    sc.op('dve', lambda e: e.tensor_reduce(out=P[:, 32:36], in_=hfb[:], axis=AX.X, op=ALU.add), reads=[('hfg', gb)], writes=[pk])
                    sc.op('act', lambda e: e.activation(out=P[:, 36:40], in_=P[:, 32:36], func=AF.Sqrt, scale=1.0 / 128, bias=EPS), reads=[pk], writes=[pk])
                    sc.op('dve', lambda e: e.reciprocal(out=P[:, 40:44], in_=P[:, 36:40]), reads=[pk], writes=[pk])
                    sc.op('dve', lambda e: e.tensor_tensor(out=hsb[:], in0=hsb[:], in1=P[:, 40:44].unsqueeze(2).broadcast_to([128, 4, 128]), op=ALU.mult),
                          reads=[('hsg', gb), pk], writes=[('hsg', gb)])
                    sc.op('pool', lambda e: e.tensor_tensor(out=hsb[:], in0=hsb[:],
                                                            in1=prm[:, P_MNG + h * 128:P_MNG + (h + 1) * 128].unsqueeze(1).broadcast_to([128, 4, 128]), op=ALU.mult),
                          reads=[('hsg', gb), 'prm'], writes=[('hsg', gb)])
                    ym = ymg[gb]
                    ymk = ('ymg', gb)
                    sc.op('dve', lambda e, g=g: e.tensor_tensor(out=ym[:], in0=hsb[:], in1=sgo[:, 4 * g:4 * g + 4, :], op=ALU.mult),
                          reads=[('hsg', gb)] + [('sgo', 4 * g + t) for t in range(4)], writes=[ymk])
                    tbank = ('ps', 6)
                    pb = ps[:, 6, :].bitcast(BF16)

                    def ytr(e, ym=ym, pb=pb):
                        for t in range(4):
                            r = e.transpose(out=pb[:, t * 128:(t + 1) * 128], in_=ym[:, t, :], identity=identb[:])
                        return r
                    sc.op('pe', ytr, reads=[ymk, 'identb'], writes=[tbank])
                    ys = yst[gb]
                    ysk = ('yst', gb)
                    sc.op('act', lambda e, ys=ys, pb=pb: e.copy(out=ys[:], in_=pb[:, 0:512]), reads=[tbank], writes=[ysk])
                    sc.dma('sp', lambda e, ys=ys, g=g: e.dma_start(out=yT_v[:, h, g * 512:(g + 1) * 512], in_=ys[:]), ysk, reads=[ysk], writes=['yT_d'])

            s3X1(0)
            s3X2(0)
            for g in range(NG):
                if g + 1 < NG:
                    s3X1(g + 1)
                s3Y1(g)
                if g + 1 < NG:
                    s3X2(g + 1)
                s3Y2(g)
            if debug and h == 0:
                o = dbg_out("dbg_qkT", [128, 2, S], BF16)
                sc.dma('sp', lambda e: e.dma_start(out=o[:, :, :], in_=qkT[:]), 'dbgqkT', reads=[('qkT', 0), ('qkT', 1)])
                o = dbg_out("dbg_Cst", [128, 2, 65, 129], BF16)
                sc.dma('sp', lambda e: e.dma_start(out=o[:, :, :, :], in_=Cst[:]), 'dbgCst',
                       reads=[('Cst', d, k) for d in range(2) for k in range(65)])
            sc.barrier()
    sM.close()

    if stage <= 2.5:
        sc.finish()
        return nc, dbg

    cosT = sb("cosT", [128, NT, 8], F32, s1)
    sinT = sb("sinT", [128, NT, 8], F32, s1)
    qkg4 = sb("qkg4", [128, 4, 64], F32, s1)
    subg8 = sb("subg8", [128, 128], F32, s1)
    TWO_PI = 6.28318
    with ExitStack() as pa:
        posi = sb("posi", [128, NT], I32, pa)
        posf = sb("posf", [128, NT], F32, pa)
        ut = sb("ut", [128, NT, 8], F32, pa)
        ui = sb("ui", [128, NT, 8], I32, pa)
        uf = sb("uf", [128, NT, 8], F32, pa)
        jl = sb("jl", [128, 64], F32, pa)
        sc.dma('sp', lambda e: e.dma_start(out=posi[:], in_=pos_d[:, :]), 'posi', writes=['posi'])
        sc.op('dve', lambda e: e.tensor_copy(out=posf[:], in_=posi[:]), reads=['posi'], writes=['posf'])
        sc.op('dve', lambda e: e.tensor_tensor(out=ut[:], in0=posf[:].unsqueeze(2).broadcast_to([128, NT, 8]),
                                               in1=consts[:, C_FREQ:C_FREQ + 8].unsqueeze(1).broadcast_to([128, NT, 8]), op=ALU.mult),
              reads=['posf', 'consts'], writes=['ut'])
        for tab, shift in ((sinT, 0.0), (cosT, 0.25)):
            if shift:
                sc.op('dve', lambda e, shift=shift: e.tensor_scalar(out=ut[:], in0=ut[:], scalar1=shift, scalar2=None, op0=ALU.add),
                      reads=['ut'], writes=['ut'])
            sc.op('dve', lambda e: e.tensor_copy(out=ui[:], in_=ut[:]), reads=['ut'], writes=['ui'])
            sc.op('dve', lambda e: e.tensor_copy(out=uf[:], in_=ui[:]), reads=['ui'], writes=['uf'])
            sc.op('dve', lambda e: e.tensor_tensor(out=uf[:], in0=ut[:], in1=uf[:], op=ALU.subtract), reads=['ut', 'uf'], writes=['uf'])
            sc.op('act', lambda e, tab=tab: e.activation(out=tab[:], in_=uf[:], func=AF.Sin, scale=TWO_PI), reads=['uf'], writes=['rot'])
        sc.op('dve', lambda e: e.tensor_reduce(out=small[:, 2:4], in_=prm[:, P_QKG:P_QKG + 128].rearrange("p (a d) -> p a d", a=2),
                                               axis=AX.X, op=ALU.max, apply_absolute_value=True), reads=['prm'], writes=['small'])
        sc.op('dve', lambda e: e.scalar_tensor_tensor(out=small[:, SM_NB:SM_NB + 1], in0=small[:, 2:3], scalar=-8.0, in1=small[:, 3:4],
                                                      op0=ALU.mult, op1=ALU.mult), reads=['small'], writes=['small'])
        for k in range(2):
            sc.op('dve', lambda e, k=k: e.tensor_tensor(out=jl[:], in0=prm[:, P_LAM + 128 * k:P_LAM + 128 * k + 64],
                                                        in1=prm[:, P_LAM + 128 * k + 64:P_LAM + 128 * k + 128], op=ALU.mult),
                  reads=['prm'], writes=['jl'])
            sc.op('dve', lambda e, k=k: e.tensor_reduce(out=small[:, 4 + k:5 + k], in_=jl[:], axis=AX.X, op=ALU.add), reads=['jl'], writes=['small'])
        sc.op('act', lambda e: e.activation(out=small[:, 6:8], in_=small[:, 4:6], func=AF.Exp), reads=['small'], writes=['small'])
        sc.op('dve', lambda e: e.scalar_tensor_tensor(out=small[:, SM_NLAM:SM_NLAM + 1], in0=small[:, 7:8], scalar=-LAM_INIT, in1=small[:, 6:7],
                                                      op0=ALU.add, op1=ALU.subtract), reads=['small'], writes=['small'])
        for a in range(4):
            o_ = P_QKG + (64 if a >= 2 else 0)
            sc.op('pool', lambda e, a=a, o_=o_: e.tensor_copy(out=qkg4[:, a, :], in_=prm[:, o_:o_ + 64]), reads=['prm'], writes=['qkg4'])
        sc.op('dve', lambda e: e.tensor_scalar(out=subg8[:], in0=prm[:, P_SUB:P_SUB + 128], scalar1=1.0 - LAM_INIT, scalar2=None, op0=ALU.mult),
              reads=['prm'], writes=['subg8'])
        if debug:
            o = dbg_out("dbg_small", [128, 64])
            sc.op('pool', lambda e: e.memset(small[:, 8:64], 0.0), writes=['small'])
            sc.dma('sp', lambda e: e.dma_start(out=o[:, :], in_=small[:]), 'dbgsmall', reads=['small'])
            o = dbg_out("dbg_cos", [128, NT, 8])
            sc.dma('sp', lambda e: e.dma_start(out=o[:, :, :], in_=cosT[:]), 'dbgcos', reads=['rot'])
        sc.barrier()

    n_aheads = 4 if stage >= 3.5 else 1
    sA = ExitStack()
    wa2 = [sb("waa%d" % k, [128, 8, 384], BF16, sA) for k in range(2)]
    for h in range(n_aheads):
        with ExitStack() as ph:
            wa_ = wa2[h % 2]
            WAK = ('waa', h % 2)
            qTa = sb("qTa", [128, 2, S], BF16, ph)
            kTa = sb("kTa", [128, S], BF16, ph)
            vxa = sb("vxa", [128, NT, 129], BF16, ph)
            xq = [sb("xq%d" % k, [128, 4, 4, 64], F32, ph) for k in range(2)]
            sq = sb("sq", [128, 4, 4, 64], F32, ph)
            rp = [sb("rp%d" % k, [128, 4, 4, 4, 8], F32, ph) for k in range(2)]
            st4 = sb("st4", [128, 2, 48], F32, ph)
            xr = [sb("xr%d" % k, [128, 4, 4, 64], BF16, ph) for k in range(2)]
            pt = [sb("pt%d" % k, [128, 2, 512], BF16, ph) for k in range(3)]
            o0 = sb("o0", [128, 4, 128], F32, ph)
            ob4 = [sb("ob4_%d" % k, [128, 4, 128], F32, ph) for k in range(2)]
            osq = sb("osq", [128, 4, 128], F32, ph)
            st5 = sb("st5", [128, 2, 24], F32, ph)
            ymt4 = [sb("ymt4_%d" % k, [128, 4, 128], BF16, ph) for k in range(2)]
            yst = [sb("ysta%d" % k, [128, 512], BF16, ph) for k in range(2)]
            for hh in ([0, 1] if h == 0 else [h + 1]):
                if hh >= n_aheads:
                    continue
                for k, c0 in enumerate((2064 + hh * 128, 2576 + hh * 128, 3088 + hh * 128)):
                    sc.dma('pool', lambda e, k=k, c0=c0, hh=hh: e.dma_start(out=wa2[hh % 2][:, :, k * 128:(k + 1) * 128], in_=win_v[:, :, c0:c0 + 128]),
                           ('waa', hh % 2), writes=[('waa', hh % 2)])
            sc.op('pool', lambda e: e.memset(vxa[:, :, 128:129], 1.0), writes=['vxa1'])
            sc.op('pool', lambda e: e.memset(qTa[64:128, 0, :], 0.0), writes=['qTa'])
            sc.op('pool', lambda e: e.memset(qTa[0:64, 1, :], 0.0), writes=['qTa'])
            def projA(g):
                    b = g % 2
                    banks = [('ps', k) for k in range(4)]
                    xk, rk, sk, xrk = ('xq', b), ('rp', b), ('st4', b), ('xr', b)
                    X = xq[b]
                    X3 = X[:].rearrange("p t a d -> p t (a d)")
                    X16 = X[:].rearrange("p t a d -> p (t a) d")
                    T4 = st4[:, b, :]
                    R = rp[b]
                    b = g % 2
                    banks = [('ps', k) for k in range(4)]

                    def mmp(e, g=g):
                        for t in range(4):
                            i = 4 * g + t
                            for kc in range(8):
                                r = e.matmul(ps[:, t, 0:384], lhsT=h1T[:, kc, i * 128:(i + 1) * 128], rhs=wa_[:, kc, :], start=(kc == 0), stop=(kc == 7))
                        return r
                    sc.op('pe', mmp, reads=[WAK] + [('h1T', 4 * g + t) for t in range(4)], writes=banks)
                    xk, rk, sk, xrk = ('xq', b), ('rp', b), ('st4', b), ('xr', b)
                    X = xq[b]
                    X3 = X[:].rearrange("p t a d -> p t (a d)")
                    X16 = X[:].rearrange("p t a d -> p (t a) d")
                    sc.op('act', lambda e, X3=X3: e.copy(out=X3, in_=ps[:, 0:4, 0:256]), reads=banks, writes=[xk])
                    sc.op('act', lambda e, g=g: e.copy(out=vxa[:, 4 * g:4 * g + 4, 0:128], in_=ps[:, 0:4, 256:384]), reads=banks,
                          writes=[('vxa', 4 * g + t) for t in range(4)])

            def projB(g):
                    b = g % 2
                    banks = [('ps', k) for k in range(4)]
                    xk, rk, sk, xrk = ('xq', b), ('rp', b), ('st4', b), ('xr', b)
                    X = xq[b]
                    X3 = X[:].rearrange("p t a d -> p t (a d)")
                    X16 = X[:].rearrange("p t a d -> p (t a) d")
                    T4 = st4[:, b, :]
                    R = rp[b]
                    sc.op('dve', lambda e, X=X: e.tensor_tensor(out=sq[:], in0=X[:], in1=X[:], op=ALU.mult), reads=[xk], writes=['sq'])
                    T4 = st4[:, b, :]
                    sc.op('dve', lambda e, T4=T4: e.tensor_reduce(out=T4[:, 0:16], in_=sq[:].rearrange("p t a d -> p (t a) d"), axis=AX.X, op=ALU.add),
                          reads=['sq'], writes=[sk])
                    sc.op('act', lambda e, T4=T4: e.activation(out=T4[:, 16:32], in_=T4[:, 0:16], func=AF.Sqrt, scale=1.0 / 64, bias=EPS), reads=[sk], writes=[sk])
                    sc.op('dve', lambda e, T4=T4: e.reciprocal(out=T4[:, 32:48], in_=T4[:, 16:32]), reads=[sk], writes=[sk])
                    sc.op('dve', lambda e, X16=X16, T4=T4: e.tensor_tensor(out=X16, in0=X16, in1=T4[:, 32:48].unsqueeze(2).broadcast_to([128, 16, 64]), op=ALU.mult),
                          reads=[xk, sk], writes=[xk])
                    sc.op('dve', lambda e, X=X: e.tensor_tensor(out=X[:], in0=X[:], in1=qkg4[:].unsqueeze(1).broadcast_to([128, 4, 4, 64]), op=ALU.mult),
                          reads=[xk, 'qkg4'], writes=[xk])
                    cs = cosT[:, 4 * g:4 * g + 4, :].unsqueeze(2).broadcast_to([128, 4, 4, 8])
                    sn = sinT[:, 4 * g:4 * g + 4, :].unsqueeze(2).broadcast_to([128, 4, 4, 8])
                    R = rp[b]
                    for k, (tt, tr_) in enumerate(((X[:, :, :, 0:8], cs), (X[:, :, :, 8:16], sn), (X[:, :, :, 8:16], cs), (X[:, :, :, 0:8], sn))):
                        sc.op('pool', lambda e, k=k, tt=tt, tr_=tr_, R=R: e.tensor_tensor(out=R[:, k, :, :, :], in0=tt, in1=tr_, op=ALU.mult),
                              reads=[xk, 'rot'], writes=[rk])

            def projC(g):
                    b = g % 2
                    banks = [('ps', k) for k in range(4)]
                    xk, rk, sk, xrk = ('xq', b), ('rp', b), ('st4', b), ('xr', b)
                    X = xq[b]
                    X3 = X[:].rearrange("p t a d -> p t (a d)")
                    X16 = X[:].rearrange("p t a d -> p (t a) d")
                    T4 = st4[:, b, :]
                    R = rp[b]
                    XR = xr[b]
                    sc.op('act', lambda e, XR=XR, X=X: e.copy(out=XR[:], in_=X[:]), reads=[xk], writes=[xrk])
                    sc.op('dve', lambda e, XR=XR, R=R: e.tensor_tensor(out=XR[:, :, :, 0:8], in0=R[:, 0, :, :, :], in1=R[:, 1, :, :, :], op=ALU.subtract),
                          reads=[rk, xrk], writes=[xrk])
                    sc.op('dve', lambda e, XR=XR, R=R: e.tensor_tensor(out=XR[:, :, :, 8:16], in0=R[:, 2, :, :, :], in1=R[:, 3, :, :, :], op=ALU.add),
                          reads=[rk, xrk], writes=[xrk])
                    tbanks = [('ps', 4), ('ps', 5)]
                    pbq = ps[:, 4, :].bitcast(BF16)
                    pbk = ps[:, 5, :].bitcast(BF16)
                    XRf = XR[:].rearrange("p t a d -> p t (a d)")

                    def trq(e, XRf=XRf, pbq=pbq, pbk=pbk):
                        for t in range(4):
                            e.transpose(out=pbq[:, t * 128:(t + 1) * 128], in_=XRf[:, t, 0:128], identity=identb[:])
                            r = e.transpose(out=pbk[:, t * 128:(t + 1) * 128], in_=XRf[:, t, 128:256], identity=identb[:])
                        return r
                    sc.op('pe', trq, reads=[xrk, 'identb'], writes=tbanks)
                    sc.op('act', lambda e, pbq=pbq, g=g: e.copy(out=qTa[0:64, 0, g * 512:(g + 1) * 512], in_=pbq[0:64, 0:512]), reads=tbanks, writes=['qTa'])
                    sc.op('act', lambda e, pbq=pbq, g=g: e.copy(out=qTa[64:128, 1, g * 512:(g + 1) * 512], in_=pbq[64:128, 0:512]), reads=tbanks, writes=['qTa'])
                    sc.op('act', lambda e, pbk=pbk, g=g: e.copy(out=kTa[:, g * 512:(g + 1) * 512], in_=pbk[:, 0:512]), reads=tbanks, writes=['kTa'])

            projA(0)
            for g in range(NG):
                if g + 1 < NG:
                    projA(g + 1)
                projB(g)
                projC(g)
            if debug and h == 0:
                o = dbg_out("dbg_qTa", [128, S], BF16)
                sc.dma('sp', lambda e: e.dma_start(out=o[0:64, :], in_=qTa[0:64, 0, :]), 'dbgqTa', reads=['qTa'])
                sc.dma('sp', lambda e: e.dma_start(out=o[64:128, :], in_=qTa[64:128, 1, :]), 'dbgqTa', reads=['qTa'])
                o = dbg_out("dbg_kTa", [128, S], BF16)
                sc.dma('sp', lambda e: e.dma_start(out=o[:, :], in_=kTa[:]), 'dbgkTa', reads=['kTa'])
            nqb = 8 if stage >= 3.2 else 1
            for qb in range(nqb):
                for p in range(2):
                    pr = slice(64 * p, 64 * p + 64)
                    def st_mm(j):
                        pb0 = 4 + 2 * (j % 2)

                        def f(e, j=j, pb0=pb0):
                            for u in range(2):
                                kt = 2 * j + u
                                r = e.matmul(ps[:, pb0 + u, :], lhsT=kTa[:, kt * 128:(kt + 1) * 128], rhs=qTa[:, p, qb * 512:(qb + 1) * 512],
                                             start=True, stop=True)
                            return r
                        sc.op('pe', f, reads=['qTa', 'kTa'], writes=[('ps', pb0), ('ps', pb0 + 1)])
                    st_mm(0)
                    for j in range(NT // 2):
                        pb0 = 4 + 2 * (j % 2)
                        sbanks = [('ps', pb0), ('ps', pb0 + 1)]
                        if j + 1 < NT // 2:
                            st_mm(j + 1)
                        ptk = ('pt', j % 3)
                        ptb = pt[j % 3]
                        sc.op('act', lambda e, pb0=pb0, ptb=ptb: e.activation(out=ptb[:], in_=ps[:, pb0:pb0 + 2, :], func=AF.Exp, scale=0.125,
                                                                              bias=small[:, SM_NB:SM_NB + 1]),
                              reads=sbanks + ['small'], writes=[ptk])

                        def pv(e, ptb=ptb, j=j):
                            for u in range(2):
                                kt = 2 * j + u
                                for qs in range(4):
                                    r = e.matmul(ps[:, qs, 0:129], lhsT=ptb[:, u, qs * 128:(qs + 1) * 128], rhs=vxa[:, kt, :],
                                                 start=(kt == 0), stop=(kt == NT - 1))
                            return r
                        sc.op('pe', pv, reads=[ptk, ('vxa', 2 * j), ('vxa', 2 * j + 1), 'vxa1'], writes=[('ps', 0), ('ps', 1), ('ps', 2), ('ps', 3)])
                    abanks = [('ps', k) for k in range(4)]
                    acc4 = ps[:, 0:4, 0:129]
                    T5 = st5[:, p, :]
                    tk5 = ('st5', p)
                    sc.op('dve', lambda e, T5=T5, acc4=acc4: e.reciprocal(out=T5[:, 0:4], in_=acc4[:, :, 128]), reads=abanks, writes=[tk5])
                    if p == 0:
                        sc.op('dve', lambda e, T5=T5, acc4=acc4: e.tensor_tensor(out=o0[:], in0=acc4[:, :, 0:128],
                                                                                  in1=T5[:, 0:4].unsqueeze(2).broadcast_to([128, 4, 128]), op=ALU.mult),
                              reads=abanks + [tk5], writes=['o0'])
                        continue
                    O = ob4[qb % 2]
                    okk = ('ob4', qb % 2)
                    sc.op('dve', lambda e, T5=T5: e.tensor_scalar(out=T5[:, 4:8], in0=T5[:, 0:4], scalar1=small[:, SM_NLAM:SM_NLAM + 1], scalar2=None, op0=ALU.mult),
                          reads=[tk5, 'small'], writes=[tk5])
                    sc.op('dve', lambda e, T5=T5, acc4=acc4, O=O: e.tensor_tensor(out=O[:], in0=acc4[:, :, 0:128],
                                                                                   in1=T5[:, 4:8].unsqueeze(2).broadcast_to([128, 4, 128]), op=ALU.mult),
                          reads=abanks + [tk5], writes=[okk])
                    sc.op('dve', lambda e, O=O: e.tensor_tensor(out=O[:], in0=O[:], in1=o0[:], op=ALU.add), reads=[okk, 'o0'], writes=[okk])
                    sc.op('pool', lambda e, O=O: e.tensor_tensor(out=osq[:], in0=O[:], in1=O[:], op=ALU.mult), reads=[okk], writes=['osq'])
                    sc.op('dve', lambda e, T5=T5: e.tensor_reduce(out=T5[:, 8:12], in_=osq[:], axis=AX.X, op=ALU.add), reads=['osq'], writes=[tk5])
                    sc.op('act', lambda e, T5=T5: e.activation(out=T5[:, 12:16], in_=T5[:, 8:12], func=AF.Sqrt, scale=1.0 / 128, bias=EPS), reads=[tk5], writes=[tk5])
                    sc.op('dve', lambda e, T5=T5: e.reciprocal(out=T5[:, 16:20], in_=T5[:, 12:16]), reads=[tk5], writes=[tk5])
                    sc.op('dve', lambda e, T5=T5, O=O: e.tensor_tensor(out=O[:], in0=O[:], in1=T5[:, 16:20].unsqueeze(2).broadcast_to([128, 4, 128]), op=ALU.mult),
                          reads=[okk, tk5], writes=[okk])
                    ym = ymt4[qb % 2]
                    ymk = ('ymt4', qb % 2)
                    sc.op('pool', lambda e, O=O, ym=ym: e.tensor_tensor(out=ym[:], in0=O[:], in1=subg8[:].unsqueeze(1).broadcast_to([128, 4, 128]), op=ALU.mult),
                          reads=[okk, 'subg8'], writes=[ymk])
                    tbank = ('ps', 7)
                    pb = ps[:, 7, :].bitcast(BF16)

                    def ytr(e, ym=ym, pb=pb):
                        for t in range(4):
                            r = e.transpose(out=pb[:, t * 128:(t + 1) * 128], in_=ym[:, t, :], identity=identb[:])
                        return r
                    sc.op('pe', ytr, reads=[ymk, 'identb'], writes=[tbank])
                    ys = yst[qb % 2]
                    ysk = ('ysta', qb % 2)
                    sc.op('act', lambda e, ys=ys, pb=pb: e.copy(out=ys[:], in_=pb[:, 0:512]), reads=[tbank], writes=[ysk])
                    sc.dma('sp', lambda e, ys=ys, qb=qb: e.dma_start(out=yT_v[:, 4 + h, qb * 512:(qb + 1) * 512], in_=ys[:]), ysk, reads=[ysk], writes=['yT_d'])
            sc.barrier()
    sA.close()

    if stage <= 3.5:
        sc.finish()
        return nc, dbg

    s1.close()

    sW = ExitStack()
    aff = sb("aff", [128, NT, NE], F32, sW)
    g2rep = sb("g2rep", [128, D], F32, sW)
    NSLOT = 6
    rank_tok = sb("rank_tok", [128, NT, NE], F32, sW)
    R5 = sb("R5", [128, NT, NE, 5], BF16, sW)
    if stage >= 5:
        ring = [sb("ring%d" % k, [128, 8, D], BF16, sW) for k in range(NSLOT)]
        wg_v = wg_d.rearrange("(e kc p) n -> p e kc n", p=128, kc=8)
        wu_v = wu_d.rearrange("(e kc p) n -> p e kc n", p=128, kc=8)
        wd_v = wd_d.rearrange("(e fh fc p) n -> p e fh fc n", p=128, fc=8, fh=2)

        def load_piece(e_, k):
            slot = ring[k]
            key = ('ring', k)
            if k < 4:
                src = (wg_v if k % 2 == 0 else wu_v)[:, e_, :, (k // 2) * D:(k // 2 + 1) * D]
            else:
                src = wd_v[:, e_, k - 4, :, :]
            sc.dma('pool', lambda e, slot=slot, src=src: e.dma_start(out=slot[:], in_=src), key, writes=[key])
        for k in range(NSLOT):
            load_piece(0, k)
    with ExitStack() as pw:
        woutb = sb("woutb", [128, 8, D], BF16, pw)
        wrt = sb("wrt", [128, 8, NE], F32, pw)
        ytl = [sb("ytl%d" % k, [128, 8, 512], BF16, pw) for k in range(2)]
        xt2 = [sb("xt2_%d" % k, [128, D], F32, pw) for k in range(2)]
        tmpw = [sb("tmpw%d" % k, [128, D], F32, pw) for k in range(2)]
        x1t = [sb("x1t%d" % k, [128, D], F32, pw) for k in range(2)]
        h2f = [sb("h2f%d" % k, [128, D], F32, pw) for k in range(2)]
        h2b = [sb("h2b%d" % k, [128, D], BF16, pw) for k in range(2)]
        h2T = [sb("h2T%d" % k, [128, 8, 128], F32, pw) for k in range(2)]
        junkw = sb("junkw", [128, D], BF16, pw)
        stw = sb("stw", [128, NT, 8], F32, pw)
        esm = sb("esm", [128, 2, NE], F32, pw)
        wout_v = wout_d.rearrange("(kc p) n -> p kc n", p=128)
        sc.dma('pool', lambda e: e.dma_start(out=woutb[:, 0:4, :], in_=wout_v[:, 0:4, :]), 'woutb', writes=['woutb'])
        sc.dma('pool', lambda e: e.dma_start(out=woutb[:, 4:8, :], in_=wout_v[:, 4:8, :]), 'woutb', writes=['woutb'])
        sc.dma('sp', lambda e: e.dma_start(out=wrt[:], in_=wr_d.rearrange("(kc p) n -> p kc n", p=128)), 'wrt', writes=['wrt'])
        modW = sb("modW", [128, 3 * D], F32, pw)
        sc.dma('sp', lambda e: e.dma_start(out=modW[:], in_=modrow_d[0:1, 2 * D:5 * D].partition_broadcast(128)), 'modW', reads=['modrow_d'], writes=['mod'])
        sc.dma('sp', lambda e: e.dma_start(out=g2rep[:], in_=modrow_d[0:1, 5 * D:6 * D].partition_broadcast(128)), 'g2rep', reads=['modrow_d'], writes=['g2rep'])
        GATE1, SHIFT2, GS2 = modW[:, 0:D], modW[:, D:2 * D], modW[:, 2 * D:3 * D]
        def wA(i):
            b = i % 2
            g, k4 = i // 4, i % 4
            yk = ('ytl', g % 2)
            if k4 == 0:
                sc.dma('sp', lambda e, g=g: e.dma_start(out=ytl[g % 2][:], in_=yT_v[:, :, g * 512:(g + 1) * 512]), yk, writes=[yk])
            xk = ('xt2', b)
            sc.dma('sp', lambda e, i=i, b=b: e.dma_start(out=xt2[b][:], in_=x_d[i * 128:(i + 1) * 128, :]), xk, writes=[xk])
            banks = [('ps', 2 * b), ('ps', 2 * b + 1)]

            def mmw(e, i=i, b=b, g=g, k4=k4):
                for dh in range(2):
                    for c in range(8):
                        r = e.matmul(ps[:, 2 * b + dh, :], lhsT=ytl[g % 2][:, c, k4 * 128:(k4 + 1) * 128], rhs=woutb[:, c, dh * 512:(dh + 1) * 512],
                                     start=(c == 0), stop=(c == 7))
                return r
            sc.op('pe', mmw, reads=[yk, 'woutb'], writes=banks)

        def wB(i):
            b = i % 2
            g, k4 = i // 4, i % 4
            yk = ('ytl', g % 2)
            xk = ('xt2', b)
            banks = [('ps', 2 * b), ('ps', 2 * b + 1)]
            T = stw[:, i, :]
            tk = ('stw', i)
            mixv = ps[:, 2 * b:2 * b + 2, :].rearrange("p a n -> p (a n)")
            sc.op('dve', lambda e, b=b, mixv=mixv: e.tensor_tensor(out=tmpw[b][:], in0=mixv, in1=GATE1, op=ALU.mult), reads=banks + ['mod'], writes=[('tmpw', b)])
            sc.op('dve', lambda e, b=b: e.tensor_tensor(out=x1t[b][:], in0=tmpw[b][:], in1=xt2[b][:], op=ALU.add),
                  reads=[('tmpw', b), xk], writes=[('x1t', b)])
            sc.dma('sp', lambda e, i=i, b=b: e.dma_start(out=out_d[i * 128:(i + 1) * 128, :], in_=x1t[b][:]), ('x1s', b), reads=[('x1t', b)], writes=[('outd', i)])
            T = stw[:, i, :]
            tk = ('stw', i)
            sc.op('act', lambda e, b=b, T=T: e.activation(out=junkw[:], in_=x1t[b][:], func=AF.Square, accum_out=T[:, 0:1]), reads=[('x1t', b)], writes=['junkw', tk])
            sc.op('act', lambda e, T=T: e.activation(out=T[:, 1:2], in_=T[:, 0:1], func=AF.Sqrt, scale=1.0 / D, bias=EPS), reads=[tk], writes=[tk])
            sc.op('dve', lambda e, T=T: e.reciprocal(out=T[:, 2:3], in_=T[:, 1:2]), reads=[tk], writes=[tk])
            sc.op('dve', lambda e, b=b, T=T: e.scalar_tensor_tensor(out=tmpw[b][:], in0=x1t[b][:], scalar=T[:, 2:3], in1=GS2, op0=ALU.mult, op1=ALU.mult),
                  reads=[('x1t', b), tk, 'mod'], writes=[('tmpw', b)])
            sc.op('dve', lambda e, b=b: e.tensor_tensor(out=h2f[b][:], in0=tmpw[b][:], in1=SHIFT2, op=ALU.add), reads=[('tmpw', b), 'mod'], writes=[('h2f', b)])
            sc.op('act', lambda e, b=b: e.copy(out=h2b[b][:], in_=h2f[b][:]), reads=[('h2f', b)], writes=[('h2b', b)])
            sc.dma('sp', lambda e, i=i, b=b: e.dma_start(out=h2_d[i * 128:(i + 1) * 128, :], in_=h2b[b][:]), ('h2s', b), reads=[('h2b', b)], writes=[('h2d', i)])

        def wC(i):
            b = i % 2
            g, k4 = i // 4, i % 4
            yk = ('ytl', g % 2)
            xk = ('xt2', b)
            banks = [('ps', 2 * b), ('ps', 2 * b + 1)]
            T = stw[:, i, :]
            tk = ('stw', i)
            tb = [('ps', 4), ('ps', 5)]

            def trh(e, b=b):
                for kc in range(8):
                    r = e.transpose(out=ps[:, 4 + kc // 4, (kc % 4) * 128:(kc % 4 + 1) * 128], in_=h2f[b][:, kc * 128:(kc + 1) * 128], identity=ident)
                return r
            sc.op('pe', trh, reads=[('h2f', b), 'consts'], writes=tb)
            sc.op('act', lambda e, b=b: e.copy(out=h2T[b][:].rearrange("p k t -> p (k t)"), in_=ps[:, 4:6, :].rearrange("p a n -> p (a n)")),
                  reads=tb, writes=[('h2T', b)])
            lb = ('ps', 6 + b)

            def mml(e, b=b):
                for kc in range(8):
                    r = e.matmul(ps[:, 6 + b, 0:NE], lhsT=h2T[b][:, kc, :], rhs=wrt[:, kc, :], start=(kc == 0), stop=(kc == 7))
                return r
            sc.op('pe', mml, reads=[('h2T', b), 'wrt'], writes=[lb])
            sc.op('dve', lambda e, b=b, T=T: e.tensor_reduce(out=T[:, 3:4], in_=ps[:, 6 + b, 0:NE], axis=AX.X, op=ALU.max), reads=[lb], writes=[tk])
            sc.op('dve', lambda e, T=T: e.tensor_scalar(out=T[:, 4:5], in0=T[:, 3:4], scalar1=-1.0, scalar2=None, op0=ALU.mult), reads=[tk], writes=[tk])
            sc.op('act', lambda e, b=b, T=T: e.activation(out=esm[:, b, :], in_=ps[:, 6 + b, 0:NE], func=AF.Exp, bias=T[:, 4:5], accum_out=T[:, 5:6]),
                  reads=[lb, tk], writes=[('esm', b), tk])
            sc.op('dve', lambda e, T=T: e.reciprocal(out=T[:, 6:7], in_=T[:, 5:6]), reads=[tk], writes=[tk])
            sc.op('dve', lambda e, b=b, i=i, T=T: e.tensor_scalar(out=aff[:, i, :], in0=esm[:, b, :], scalar1=T[:, 6:7], scalar2=None, op0=ALU.mult),
                  reads=[('esm', b), tk], writes=['aff'])

        wA(0)
        for i in range(NT):
            if i + 1 < NT:
                wA(i + 1)
            wB(i)
            wC(i)
        if debug:
            o = dbg_out("dbg_aff", [128, NT, NE])
            sc.dma('sp', lambda e: e.dma_start(out=o[:, :, :], in_=aff[:]), 'dbgaff', reads=['aff'])
        sc.barrier()

    if stage <= 4:
        sc.finish()
        return nc, dbg

    with ExitStack() as pr:
        affT = sb("affT", [NE, S], F32, pr)
        junkR = sb("junkR", [NE, S], F32, pr)
        mkT = sb("mkT", [NE, S], F32, pr)
        csT = sb("csT", [NE, S], F32, pr)
        bs = sb("bs", [NE, 4], F32, pr)
        r1 = sb("r1", [128, NT, NE], F32, pr)
        for rnd in range(2):
            banks = [('ps', k) for k in range(4)]

            def tra(e, rnd=rnd):
                for k in range(16):
                    i = rnd * 16 + k
                    r = e.transpose(out=ps[0:NE, k // 4, (k % 4) * 128:(k % 4 + 1) * 128], in_=aff[:, i, :], identity=ident)
                return r
            sc.op('pe', tra, reads=['aff', 'consts'], writes=banks)
            sc.op('act', lambda e, rnd=rnd: e.copy(out=affT[:, rnd * 2048:(rnd + 1) * 2048], in_=ps[0:NE, 0:4, :].rearrange("p a n -> p (a n)")),
                  reads=banks, writes=['affT'])
        sc.op('dve', lambda e: e.memset(bs[:, 0:1], 0.0), writes=['bs'])
        for n in range(NBIS):
            w = 2.0 ** (-(n + 1))
            sc.op('dve', lambda e, w=w: e.tensor_scalar(out=bs[:, 1:2], in0=bs[:, 0:1], scalar1=w, scalar2=None, op0=ALU.add), reads=['bs'], writes=['bs'])
            sc.op('dve', lambda e: e.tensor_scalar(out=junkR[:], in0=affT[:], scalar1=bs[:, 1:2], scalar2=None, op0=ALU.is_gt, op1=ALU.add,
                                                   accum_out=bs[:, 2:3]), reads=['affT', 'bs'], writes=['junkR', 'bs'])
            sc.op('dve', lambda e: e.tensor_scalar(out=bs[:, 3:4], in0=bs[:, 2:3], scalar1=CAP - 0.5, scalar2=None, op0=ALU.is_gt), reads=['bs'], writes=['bs'])
            sc.op('dve', lambda e, w=w: e.scalar_tensor_tensor(out=bs[:, 0:1], in0=bs[:, 3:4], scalar=w, in1=bs[:, 0:1], op0=ALU.mult, op1=ALU.add),
                  reads=['bs'], writes=['bs'])
        sc.op('dve', lambda e: e.tensor_scalar(out=mkT[:], in0=affT[:], scalar1=bs[:, 0:1], scalar2=None, op0=ALU.is_gt), reads=['affT', 'bs'], writes=['mkT'])
        sc.op('pool', lambda e: e.memset(junkR[:], 1.0), reads=[], writes=['junkR'])
        sc.op('dve', lambda e: e.tensor_tensor_scan(out=csT[:], data0=junkR[:], data1=mkT[:], initial=0.0, op0=ALU.mult, op1=ALU.add),
              reads=['junkR', 'mkT'], writes=['csT'])
        sc.op('dve', lambda e: e.tensor_tensor(out=csT[:], in0=csT[:], in1=mkT[:], op=ALU.mult), reads=['csT', 'mkT'], writes=['csT'])
        sc.op('dve', lambda e: e.tensor_scalar(out=csT[:], in0=csT[:], scalar1=-1.0, scalar2=None, op0=ALU.add), reads=['csT'], writes=['csT'])
        rb = ('ps', 4)

        def trr(e):
            for i in range(NT):
                r = e.transpose(out=ps[:, 4, i * NE:(i + 1) * NE], in_=csT[:, i * 128:(i + 1) * 128], identity=consts[0:NE, C_IDENT:C_IDENT + NE])
            return r
        sc.op('pe', trr, reads=['csT', 'consts'], writes=[rb])
        sc.op('act', lambda e: e.copy(out=rank_tok[:].rearrange("p t e -> p (t e)"), in_=ps[:, 4, :]), reads=[rb], writes=['rank_tok'])
        sc.op('pool', lambda e: e.tensor_copy(out=R5[:, :, :, 0], in_=consts[:, C_TLO:C_TLO + 1].unsqueeze(2).broadcast_to([128, NT, NE])),
              reads=['consts'], writes=['R5'])
        sc.op('pool', lambda e: e.tensor_copy(out=R5[:, :, :, 1], in_=consts[:, C_THI:C_THI + NT].unsqueeze(2).broadcast_to([128, NT, NE])),
              reads=['consts'], writes=['R5'])
        sc.op('dve', lambda e: e.tensor_copy(out=R5[:, :, :, 2], in_=aff[:]), reads=['aff'], writes=['R5'])
        sc.op('dve', lambda e: e.tensor_tensor(out=r1[:], in0=aff[:], in1=R5[:, :, :, 2], op=ALU.subtract), reads=['aff', 'R5'], writes=['r1'])
        sc.op('dve', lambda e: e.tensor_copy(out=R5[:, :, :, 3], in_=r1[:]), reads=['r1'], writes=['R5'])
        sc.op('dve', lambda e: e.tensor_tensor(out=r1[:], in0=r1[:], in1=R5[:, :, :, 3], op=ALU.subtract), reads=['r1', 'R5'], writes=['r1'])
        sc.op('dve', lambda e: e.tensor_copy(out=R5[:, :, :, 4], in_=r1[:]), reads=['r1'], writes=['R5'])
        if debug:
            o = dbg_out("dbg_rank", [128, NT, NE])
            sc.dma('sp', lambda e: e.dma_start(out=o[:, :, :], in_=rank_tok[:]), 'dbgrank', reads=['rank_tok'])
            o = dbg_out("dbg_thr", [NE, 4])
            sc.dma('sp', lambda e: e.dma_start(out=o[:, :], in_=bs[:]), 'dbgthr', reads=['bs'])
        sc.barrier()

    if stage <= 4.5:
        sc.finish()
        return nc, dbg

    n_exp = NE if stage >= 6 else int(round((stage - 5) * 10)) + 1
    with ExitStack() as pe_:
        Pm = sb("Pm", [128, NT, 512], BF16, pe_)
        xe = sb("xe", [128, 4, D], BF16, pe_)
        xeT2 = [sb("xeT%d" % k, [128, 8, 512], BF16, pe_) for k in range(2)]
        aT = sb("aT", [128, 16, 512], BF16, pe_)
        sgt = [sb("sgt%d" % k, [128, 512], BF16, pe_) for k in range(2)]
        yv = sb("yv", [128, 4, D], F32, pe_)
        idf = sb("idf", [128, 2, 8], F32, pe_)
        idp = sb("idp", [128, 2, 32], F32, pe_)
        idxi = [sb("idxi%d" % k, [128, 4], I32, pe_) for k in range(2)]
        def prep_a(e_):
            for i in range(NT):
                sc.op('dve', lambda e, i=i: e.tensor_scalar(out=Pm[:, i, :], in0=consts[:, C_IOTA:C_IOTA + 512], scalar1=rank_tok[:, i, e_:e_ + 1],
                                                            scalar2=None, op0=ALU.is_equal), reads=['consts', 'rank_tok'], writes=['Pm'])

        def prep_b(e_):
            b = e_ % 2
            ibank = ('ps', 7)

            def imm(e):
                for cc in range(4):
                    for i in range(NT):
                        r = e.matmul(ps[:, 7, cc * 8:cc * 8 + 5], lhsT=Pm[:, i, cc * 128:(cc + 1) * 128], rhs=R5[:, i, e_, :],
                                     start=(i == 0), stop=(i == NT - 1))
                return r
            sc.op('pe', imm, reads=['Pm', 'R5'], writes=[ibank])
            sc.op('act', lambda e, b=b: e.copy(out=idp[:, b, :].rearrange("p (c k) -> p c k", k=8)[:, :, 0:5], in_=ps[:, 7, 0:32].rearrange("p (c k) -> p c k", k=8)[:, :, 0:5]), reads=[ibank], writes=[('idp', b)])
            ibank = ('idp', b)
            I3 = idp[:, b, :].rearrange("p (c k) -> p c k", k=8)
            F_ = idf[:, b, :]
            fk = ('idf', b)
            sc.op('dve', lambda e, F_=F_, I3=I3: e.scalar_tensor_tensor(out=F_[:, 0:4].unsqueeze(2), in0=I3[:, :, 1:2], scalar=128.0, in1=I3[:, :, 0:1],
                                                                      op0=ALU.mult, op1=ALU.add), reads=[ibank], writes=[fk])
            sc.op('dve', lambda e, F_=F_, I3=I3: e.tensor_reduce(out=F_[:, 4:8], in_=I3[:, :, 2:5], axis=AX.X, op=ALU.add), reads=[ibank], writes=[fk])
            ik = ('idxi', b)
            sc.op('dve', lambda e, F_=F_, b=b: e.tensor_copy(out=idxi[b][:], in_=F_[:, 0:4]), reads=[fk], writes=[ik])
            for cc in range(4):
                sc.dma('pool', lambda e, cc=cc, b=b: e.indirect_dma_start(
                    out=xe[:, cc, :], out_offset=None, in_=h2_d[:, :],
                    in_offset=bass.IndirectOffsetOnAxis(ap=idxi[b][:, cc:cc + 1], axis=0)), 'xe', reads=[ik], writes=['xe'])

        def prep_c(e_):
            xeT = xeT2[e_ % 2]
            for kc in range(8):
                tb = 6
                tbank = ('ps', tb)
                pb = ps[:, tb, :].bitcast(BF16)

                def trx(e, kc=kc, pb=pb):
                    for cc in range(4):
                        r = e.transpose(out=pb[:, cc * 128:(cc + 1) * 128], in_=xe[:, cc, kc * 128:(kc + 1) * 128], identity=identb[:])
                    return r
                sc.op('pe', trx, reads=['xe', 'identb'], writes=[tbank])
                sc.op('act', lambda e, kc=kc, pb=pb, xeT=xeT: e.copy(out=xeT[:, kc, :], in_=pb[:, 0:512]), reads=[tbank], writes=[('xeT', e_ % 2)])

        prep_a(0)
        prep_b(0)
        prep_c(0)
        for e_ in range(n_exp):
            b = e_ % 2
            xeT = xeT2[b]
            xk_ = ('xeT', b)
            if e_ + 1 < n_exp:
                prep_a(e_ + 1)
            for fh in range(2):
                gk, uk = ('ring', 2 * fh), ('ring', 2 * fh + 1)
                Wg, Wu = ring[2 * fh], ring[2 * fh + 1]
                for fo in range(8):
                    gb, ubk = fo % 2, 2 + (fo % 2)

                    def mmg(e, Wg=Wg, fo=fo, gb=gb):
                        for kc in range(8):
                            r = e.matmul(ps[:, gb, :], lhsT=Wg[:, kc, fo * 128:(fo + 1) * 128], rhs=xeT[:, kc, :], start=(kc == 0), stop=(kc == 7))
                        return r

                    def mmu(e, Wu=Wu, fo=fo, ubk=ubk):
                        for kc in range(8):
                            r = e.matmul(ps[:, ubk, :], lhsT=Wu[:, kc, fo * 128:(fo + 1) * 128], rhs=xeT[:, kc, :], start=(kc == 0), stop=(kc == 7))
                        return r
                    sc.op('pe', mmg, reads=[gk, xk_], writes=[('ps', gb)])
                    sc.op('pe', mmu, reads=[uk, xk_], writes=[('ps', ubk)])
                    sg = sgt[fo % 2]
                    sk = ('sgt', fo % 2)
                    sc.op('act', lambda e, sg=sg, gb=gb: e.activation(out=sg[:], in_=ps[:, gb, :], func=AF.Silu), reads=[('ps', gb)], writes=[sk])
                    fidx = fh * 8 + fo
                    sc.op('dve', lambda e, sg=sg, ubk=ubk, fidx=fidx: e.tensor_tensor(out=aT[:, fidx, :], in0=ps[:, ubk, :], in1=sg[:], op=ALU.mult),
                          reads=[('ps', ubk), sk], writes=[('aT', fidx)])
                if e_ + 1 < n_exp:
                    load_piece(e_ + 1, 2 * fh)
                    load_piece(e_ + 1, 2 * fh + 1)
                    if fh == 0:
                        prep_b(e_ + 1)
                    else:
                        prep_c(e_ + 1)
            F_ = idf[:, b, :]
            for cc in range(4):
                for dh in range(2):
                    db = 4 + ((cc * 2 + dh) % 2)

                    def mmd(e, cc=cc, dh=dh, db=db):
                        for fc in range(16):
                            r = e.matmul(ps[:, db, :], lhsT=aT[:, fc, cc * 128:(cc + 1) * 128], rhs=ring[4 + fc // 8][:, fc % 8, dh * 512:(dh + 1) * 512],
                                         start=(fc == 0), stop=(fc == 15))
                        return r
                    sc.op('pe', mmd, reads=[('aT', f) for f in range(16)] + [('ring', 4), ('ring', 5)], writes=[('ps', db)])
                    sc.op('dve', lambda e, cc=cc, dh=dh, db=db, F_=F_: e.scalar_tensor_tensor(
                        out=yv[:, cc, dh * 512:(dh + 1) * 512], in0=ps[:, db, :], scalar=F_[:, 4 + cc:5 + cc], in1=g2rep[:, dh * 512:(dh + 1) * 512],
                        op0=ALU.mult, op1=ALU.mult), reads=[('ps', db), ('idf', b), 'g2rep'], writes=[('yv', cc)])
            if e_ + 1 < n_exp:
                load_piece(e_ + 1, 4)
                load_piece(e_ + 1, 5)
            for cc in range(4):
                sc.dma('pool', lambda e, cc=cc, b=b: e.indirect_dma_start(
                    out=out_d[:, :], out_offset=bass.IndirectOffsetOnAxis(ap=idxi[b][:, cc:cc + 1], axis=0),
                    in_=yv[:, cc, :], in_offset=None, compute_op=ALU.add), 'scat', reads=[('yv', cc), ('idxi', b)], writes=['outd'])
        sc.barrier()
    sW.close()
    sc.finish()
    return nc, dbg


def prep_inputs(inputs, b):
    f = lambda a: np.ascontiguousarray(a, dtype=np.float32)
    m = {
        "x": f(inputs["x"][b]),
        "cT": f(inputs["c"][b].reshape(8, 128).T),
        "pos": np.ascontiguousarray(inputs["positions"][b].reshape(NT, 128).T.astype(np.int32)),
        "norm1_g": f(inputs["norm1_g"][0:1]),
        "norm2_g": f(inputs["norm2_g"][0:1]),
        "w_ada": f(inputs["w_ada"][0]),
        "b_ada": f(inputs["b_ada"][0:1]),
        "w_in": f(inputs["w_in"][0]),
        "convT": f(inputs["mlstm_conv_w"][0].T.reshape(8, 128, 5).transpose(1, 0, 2).reshape(128, 40)),
        "gate_b": f(inputs["mlstm_gate_b"][0:1]),
        "mnorm_g": f(inputs["mlstm_norm_g"][0:1]),
        "qk_g": f(inputs["diff_qk_g"][0].reshape(1, 128)),
        "lam": f(inputs["diff_lambda"][0].reshape(1, 256)),
        "subln_g": f(inputs["diff_subln_g"][0:1]),
        "w_out": f(inputs["w_out"][0]),
        "w_router": f(inputs["w_router"][0]),
        "consts": make_consts(),
    }
    return m


def kernel(**inputs):
    nc, _ = build()
    shared = None
    in_maps = []
    for b in range(8):
        m = prep_inputs(inputs, b)
        if shared is None:
            shared = {
                "w_gate_e": np.ascontiguousarray(inputs["w_gate_e"][0].reshape(NE * D, 2 * D), dtype=np.float32),
                "w_up_e": np.ascontiguousarray(inputs["w_up_e"][0].reshape(NE * D, 2 * D), dtype=np.float32),
                "w_down_e": np.ascontiguousarray(inputs["w_down_e"][0].reshape(NE * 2 * D, D), dtype=np.float32),
            }
        m.update(shared)
        in_maps.append(m)
    res = run_bass_kernel_spmd(nc, in_maps, core_ids=list(range(8)))
    return np.stack([np.asarray(r["out"]) for r in res.results], axis=0).astype(np.float32)
```

```python
import math
from contextlib import ExitStack

import numpy as np
import concourse.bass as bass
import concourse.mybir as mybir
from concourse.bass_utils import run_bass_kernel_spmd

F32 = mybir.dt.float32
BF16 = mybir.dt.bfloat16
I32 = mybir.dt.int32
AF = mybir.ActivationFunctionType
ALU = mybir.AluOpType
AX = mybir.AxisListType

S = 4096
D = 1024
NT = 32
NG = 8
EPS = 1e-6
D_IN = 3600
NE = 16
CAP = 512
LAM_INIT = 0.8 - 0.6 * math.exp(-0.3 * 0)
NBIS = 27

SAME_ENG_SYNC = True

C_IDENT = 0
C_TL = 128
C_TU = 256
C_TLS = 384
C_TUS = 512
C_INDA = 640
C_INDB = 768
C_IOTA = 896
C_TLO = 1408
C_FREQ = 1409
C_THI = 1417
NCONST = 1449


def make_consts():
    c = np.zeros((128, NCONST), np.float32)
    p = np.arange(128)
    s = p[:, None]
    j = p[None, :]
    same = (s // 64) == (j // 64)
    c[:, C_IDENT:C_IDENT + 128] = (s == j)
    c[:, C_TL:C_TL + 128] = same & (s <= j)
    c[:, C_TU:C_TU + 128] = same & (s >= j)
    c[:, C_TLS:C_TLS + 128] = same & (s < j)
    c[:, C_TUS:C_TUS + 128] = same & (s > j)
    c[:, C_INDA:C_INDA + 128] = (s < 64) & (j >= 0)
    c[:, C_INDB:C_INDB + 128] = (s >= 64) & (j >= 0)
    c[:, C_IOTA:C_IOTA + 512] = np.arange(512)[None, :]
    c[:, C_TLO] = p
    inv_freq = (500000.0 ** (-np.arange(0, 16, 2, dtype=np.float32) / 16)).astype(np.float32)
    c[:, C_FREQ:C_FREQ + 8] = (inv_freq.astype(np.float64) / (2 * np.pi)).astype(np.float32)[None, :]
    c[:, C_THI:C_THI + 32] = np.arange(32)[None, :]
    return c


class Sched:
    def __init__(self, nc, es):
        self.nc = nc
        self.es = es
        self.E = {'pe': nc.tensor, 'act': nc.scalar, 'dve': nc.vector, 'pool': nc.gpsimd, 'sp': nc.sync}
        self.sem = {k: es.enter_context(nc.semaphore('s_' + k)) for k in self.E}
        self.cnt = {k: 0 for k in self.E}
        self.seen = {k: {} for k in self.E}
        self.reg = {}
        self.dsem = {}
        self.dsem_by_sid = {}
        self.ninstr = {k: 0 for k in self.E}

    def _deps(self, reads, writes):
        deps = {}

        def add(tok):
            if tok is None:
                return
            if tok[0].startswith('d_'):
                tok = (tok[0], tok[1], self.dsem_by_sid[tok[0]][1])
            if tok[0] not in deps or deps[tok[0]][2] < tok[2]:
                deps[tok[0]] = tok
        for r in reads:
            st = self.reg.get(r)
            if st:
                add(st[0])
        for w in writes:
            st = self.reg.get(w)
            if st:
                add(st[0])
                for t in st[1].values():
                    add(t)
        return deps

    def _wait(self, eng, deps):
        for sid, (_, h, v) in deps.items():
            if sid == 'e_' + eng and not SAME_ENG_SYNC:
                continue
            if self.seen[eng].get(sid, 0) >= v:
                continue
            self.E[eng].wait_ge(h, v)
            self.seen[eng][sid] = v

    def _commit(self, tok, reads, writes):
        for r in reads:
            st = self.reg.setdefault(r, [None, {}])
            st[1][tok[0]] = tok
        for w in writes:
            self.reg[w] = [tok, {}]

    def op(self, eng, fn, reads=(), writes=()):
        self._wait(eng, self._deps(reads, writes))
        ins = fn(self.E[eng])
        self.cnt[eng] += 1
        ins.then_inc(self.sem[eng], 1)
        self._commit(('e_' + eng, self.sem[eng], self.cnt[eng]), reads, writes)

    def dma(self, q, fn, semkey, reads=(), writes=()):
        self._wait(q, self._deps(reads, writes))
        ins = fn(self.E[q])
        d = self.dsem.get(semkey)
        if d is None:
            d = [self.es.enter_context(self.nc.semaphore('d%d' % len(self.dsem))), 0]
            self.dsem[semkey] = d
            self.dsem_by_sid['d_' + str(semkey)] = d
        d[1] += 16
        ins.then_inc(d[0], 16)
        self._commit(('d_' + str(semkey), d[0], d[1]), reads, writes)

    def barrier(self):
        toks = {}
        for k in self.E:
            if self.cnt[k]:
                toks['e_' + k] = ('e_' + k, self.sem[k], self.cnt[k])
        for key, d in self.dsem.items():
            if d[1]:
                toks['d_' + str(key)] = ('d_' + str(key), d[0], d[1])
        for k in self.E:
            self._wait(k, toks)

    def finish(self):
        toks = {}
        for k in self.E:
            if self.cnt[k]:
                toks['e_' + k] = ('e_' + k, self.sem[k], self.cnt[k])
        for key, d in self.dsem.items():
            if d[1]:
                toks['d_' + str(key)] = ('d_' + str(key), d[0], d[1])
        self._wait('sp', toks)


def build(stage=99, debug=False):
    nc = bass.Bass("TRN2", target_bir_lowering=False)
    es = ExitStack()
    sc = Sched(nc, es)

    def din(name, shape, dt=F32):
        return nc.dram_tensor(name, list(shape), dt, kind="ExternalInput").ap()

    x_d = din("x", [S, D])
    cT_d = din("cT", [128, 8])
    pos_d = din("pos", [128, NT], I32)
    n1g_d = din("norm1_g", [1, D])
    n2g_d = din("norm2_g", [1, D])
    wada_d = din("w_ada", [D, 6 * D])
    bada_d = din("b_ada", [1, 6 * D])
    win_d = din("w_in", [D, D_IN])
    convT_d = din("convT", [128, 40])
    gateb_d = din("gate_b", [1, 16])
    mng_d = din("mnorm_g", [1, 512])
    qkg_d = din("qk_g", [1, 128])
    lam_d = din("lam", [1, 256])
    subg_d = din("subln_g", [1, 128])
    wout_d = din("w_out", [D, D])
    wr_d = din("w_router", [D, NE])
    consts_d = din("consts", [128, NCONST])
    if stage >= 5:
        wg_d = din("w_gate_e", [NE * D, 2 * D])
        wu_d = din("w_up_e", [NE * D, 2 * D])
        wd_d = din("w_down_e", [NE * 2 * D, D])
    out_d = nc.dram_tensor("out", [S, D], F32, kind="ExternalOutput").ap()
    skind = "ExternalOutput" if debug else "Internal"
    yT_d = nc.dram_tensor("yT_d", [D, S], BF16, kind=skind).ap()
    h2_d = nc.dram_tensor("h2_d", [S, D], BF16, kind=skind).ap()
    dbg = {}

    def dbg_out(name, shape, dt=F32):
        if debug:
            dbg[name] = nc.dram_tensor(name, list(shape), dt, kind="ExternalOutput").ap()
            return dbg[name]
        return None

    uniq = [0]

    def sb(name, shape, dt=F32, stack=es):
        uniq[0] += 1
        return stack.enter_context(nc.sbuf_tensor("sb%d_%s" % (uniq[0], name), list(shape), dt))

    ps = es.enter_context(nc.psum_tensor("ps", [128, 8, 512], F32))
    consts = sb("consts", [128, NCONST])
    identb = sb("identb", [128, 128], BF16)
    s1 = ExitStack()
    prm = sb("prm", [128, 16 + 512 + 128 + 256 + 128], F32, s1)
    P_GB, P_MNG, P_QKG, P_LAM, P_SUB = 0, 16, 528, 656, 912
    convT = sb("convT", [128, 40], F32, s1)
    maskF = sb("maskF", [128, 128], BF16, s1)
    maskB = sb("maskB", [128, 128], BF16, s1)
    small = sb("small", [128, 64], F32, s1)
    SM_NB, SM_NLAM = 0, 1
    ident = consts[:, C_IDENT:C_IDENT + 128]

    h1T = sb("h1T", [128, 8, S], BF16, s1)
    gm = sb("gm", [128, NT, 16], F32, s1)
    smod = ExitStack()
    mod = sb("mod", [128, 6 * D], F32, smod)
    modrow_d = nc.dram_tensor("modrow_d", [1, 6 * D], F32, kind="Internal").ap()
    CK = 'consts'
    sc.dma('sp', lambda e: e.dma_start(out=consts[:], in_=consts_d[:, :]), CK, writes=['consts'])
    sc.dma('sp', lambda e: e.dma_start(out=convT[:], in_=convT_d[:, :]), CK, writes=['convT'])
    for (off, src, n) in [(P_GB, gateb_d, 16), (P_MNG, mng_d, 512),
                          (P_QKG, qkg_d, 128), (P_LAM, lam_d, 256), (P_SUB, subg_d, 128)]:
        sc.dma('sp', lambda e, off=off, src=src, n=n: e.dma_start(
            out=prm[:, off:off + n], in_=src[0:1, :].partition_broadcast(128)), CK, writes=['prm'])
    sc.dma('sp', lambda e: e.dma_start(out=mod[:], in_=bada_d[0:1, :].partition_broadcast(128)), CK, writes=['mod'])

    with ExitStack() as p0:
        cT = sb("cT", [128, 8], F32, p0)
        prmA = sb("prmA", [128, 2 * D], F32, p0)
        sc.dma('sp', lambda e: e.dma_start(out=prmA[:, 0:D], in_=n1g_d[0:1, :].partition_broadcast(128)), CK, writes=['prmA'])
        sc.dma('sp', lambda e: e.dma_start(out=prmA[:, D:2 * D], in_=n2g_d[0:1, :].partition_broadcast(128)), CK, writes=['prmA'])
        scT = sb("scT", [128, 8], F32, p0)
        lc = sb("lc", [128, 8, 128], BF16, p0)
        wa = [sb("wa%d" % i, [128, 8, 512], BF16, p0) for i in range(2)]
        sc.dma('sp', lambda e: e.dma_start(out=cT[:], in_=cT_d[:, :]), 'cT', writes=['cT'])
        sc.op('act', lambda e: e.activation(out=scT[:], in_=cT[:], func=AF.Silu), reads=['cT'], writes=['scT'])
        sc.op('dve', lambda e: e.tensor_copy(out=lc[:], in_=scT[:].unsqueeze(2).broadcast_to([128, 8, 128])),
              reads=['scT'], writes=['lc'])
        sc.op('dve', lambda e: e.tensor_copy(out=identb[:], in_=ident), reads=['consts'], writes=['identb'])
        sc.op('dve', lambda e: e.tensor_copy(out=maskF[:], in_=consts[:, C_TL:C_TL + 128]), reads=['consts'], writes=['maskF'])
        sc.op('dve', lambda e: e.tensor_copy(out=maskB[:], in_=consts[:, C_TU:C_TU + 128]), reads=['consts'], writes=['maskB'])
        wada_v = wada_d.rearrange("(kc p) n -> p kc n", p=128)
        for n in range(12):
            w = wa[n % 2]
            wk = ('wa', n % 2)
            sc.dma('pool', lambda e, w=w, n=n: e.dma_start(out=w[:], in_=wada_v[:, :, n * 512:(n + 1) * 512]),
                   wk, writes=[wk])
            bank = ('ps', n % 2)

            def mm(e, w=w, n=n):
                for kc in range(8):
                    r = e.matmul(ps[:, n % 2, :], lhsT=lc[:, kc, :], rhs=w[:, kc, :], start=(kc == 0), stop=(kc == 7))
                return r
            sc.op('pe', mm, reads=['lc', wk], writes=[bank])
            sc.op('dve', lambda e, n=n: e.tensor_tensor(out=mod[:, n * 512:(n + 1) * 512], in0=ps[:, n % 2, :],
                                                        in1=mod[:, n * 512:(n + 1) * 512], op=ALU.add),
                  reads=[bank, 'mod'], writes=['mod'])
        sc.op('dve', lambda e: e.scalar_tensor_tensor(out=mod[:, D:2 * D], in0=mod[:, D:2 * D], scalar=1.0,
                                                      in1=prmA[:, 0:D], op0=ALU.add, op1=ALU.mult),
              reads=['mod', 'prmA'], writes=['mod'])
        sc.op('dve', lambda e: e.scalar_tensor_tensor(out=mod[:, 4 * D:5 * D], in0=mod[:, 4 * D:5 * D], scalar=1.0,
                                                      in1=prmA[:, D:2 * D], op0=ALU.add, op1=ALU.mult),
              reads=['mod', 'prmA'], writes=['mod'])
        if debug:
            o = dbg_out("dbg_mod", [128, 6 * D])
            sc.dma('sp', lambda e: e.dma_start(out=o[:, :], in_=mod[:]), 'dbgmod', reads=['mod'])
        sc.barrier()
    SHIFT1, GS1, GATE1, SHIFT2, GS2, GATE2 = [mod[:, i * D:(i + 1) * D] for i in range(6)]

    if stage <= 0:
        sc.finish()
        return nc, dbg

    with ExitStack() as p1:
        xt = [sb("xt%d" % i, [128, D], F32, p1) for i in range(2)]
        junk = sb("junk1", [128, D], BF16, p1)
        tmp = [sb("tmp1_%d" % i, [128, D], F32, p1) for i in range(2)]
        h1b = [sb("h1b%d" % i, [128, D], BF16, p1) for i in range(2)]
        st1 = sb("st1", [128, NT, 3], F32, p1)
        wgt = sb("wgt", [128, 8, 16], BF16, p1)
        win_v = win_d.rearrange("(kc p) n -> p kc n", p=128)
        sc.dma('pool', lambda e: e.dma_start(out=wgt[:], in_=win_v[:, :, 2048:2064]), 'wgt', writes=['wgt'])
        def p1A(i):
            b = i % 2
            xk, tk, hk = ('xt', b), ('tmp1', b), ('h1b', b)
            sc.dma('sp', lambda e, i=i, b=b: e.dma_start(out=xt[b][:], in_=x_d[i * 128:(i + 1) * 128, :]), xk, writes=[xk])
            sc.op('act', lambda e, i=i, b=b: e.activation(out=junk[:], in_=xt[b][:], func=AF.Square,
                                                          accum_out=st1[:, i, 0:1]), reads=[xk], writes=['junk1', ('st1', i)])
            sc.op('act', lambda e, i=i: e.activation(out=st1[:, i, 1:2], in_=st1[:, i, 0:1], func=AF.Sqrt,
                                                     scale=1.0 / D, bias=EPS), reads=[('st1', i)], writes=[('st1', i)])
            sc.op('dve', lambda e, i=i: e.reciprocal(out=st1[:, i, 2:3], in_=st1[:, i, 1:2]),
                  reads=[('st1', i)], writes=[('st1', i)])
            sc.op('dve', lambda e, i=i, b=b: e.scalar_tensor_tensor(out=tmp[b][:], in0=xt[b][:], scalar=st1[:, i, 2:3],
                                                                    in1=GS1, op0=ALU.mult, op1=ALU.mult),
                  reads=[xk, ('st1', i), 'mod'], writes=[tk])
            sc.op('dve', lambda e, b=b: e.tensor_tensor(out=h1b[b][:], in0=tmp[b][:], in1=SHIFT1, op=ALU.add),
                  reads=[tk, 'mod'], writes=[hk])

        def p1B(i):
            b = i % 2
            xk, tk, hk = ('xt', b), ('tmp1', b), ('h1b', b)
            bank = ('ps', 2 + b)
            pb = ps[:, 2 + b, :].bitcast(BF16)

            def tr(e, b=b, pb=pb):
                for kc in range(8):
                    r = e.transpose(out=pb[:, kc * 128:(kc + 1) * 128], in_=h1b[b][:, kc * 128:(kc + 1) * 128], identity=identb[:])
                return r
            sc.op('pe', tr, reads=[hk, 'identb'], writes=[bank])
            sc.op('act', lambda e, i=i, pb=pb: e.copy(out=h1T[:, :, i * 128:(i + 1) * 128],
                                                      in_=pb.rearrange("p (k t) -> p k t", k=8)),
                  reads=[bank], writes=[('h1T', i)])
            gbank = ('ps', 4 + b)

            def gmm(e, i=i, b=b):
                for kc in range(8):
                    r = e.matmul(ps[:, 4 + b, 0:16], lhsT=h1T[:, kc, i * 128:(i + 1) * 128], rhs=wgt[:, kc, :],
                                 start=(kc == 0), stop=(kc == 7))
                return r
            sc.op('pe', gmm, reads=[('h1T', i), 'wgt'], writes=[gbank])
            sc.op('dve', lambda e, i=i, b=b: e.tensor_tensor(out=gm[:, i, :], in0=ps[:, 4 + b, 0:16],
                                                             in1=prm[:, P_GB:P_GB + 16], op=ALU.add),
                  reads=[gbank, 'prm'], writes=['gm'])

        p1A(0)
        for i in range(NT):
            if i + 1 < NT:
                p1A(i + 1)
            p1B(i)
        if debug:
            o = dbg_out("dbg_h1T", [128, 8, S], BF16)
            sc.dma('sp', lambda e: e.dma_start(out=o[:, :, :], in_=h1T[:]), 'dbgh1T', reads=[('h1T', i) for i in range(NT)])
            o3 = dbg_out("dbg_st1", [128, NT, 3])
            sc.dma('sp', lambda e: e.dma_start(out=o3[:, :, :], in_=st1[:]), 'dbgst1', reads=[('st1', i) for i in range(NT)])
            o4 = dbg_out("dbg_tmp", [128, D])
            sc.dma('sp', lambda e: e.dma_start(out=o4[:, :], in_=tmp[1][:]), 'dbgtmp', reads=[('tmp1', 1)])
            o5 = dbg_out("dbg_h1b", [128, D], BF16)
            sc.dma('sp', lambda e: e.dma_start(out=o5[:, :], in_=h1b[1][:]), 'dbgh1b', reads=[('h1b', 1)])
            o2 = dbg_out("dbg_gm", [128, NT, 16])
            sc.dma('sp', lambda e: e.dma_start(out=o2[:, :, :], in_=gm[:]), 'dbggm', reads=['gm'])
        sc.barrier()

    sc.dma('sp', lambda e: e.dma_start(out=modrow_d[0:1, :], in_=mod[0:1, :]), 'modrow', reads=['mod'], writes=['modrow_d'])
    sc.barrier()
    smod.close()

    if stage <= 1:
        sc.finish()
        return nc, dbg

    LNK = -0.5 * math.log(128.0)
    cum = sb("cum", [128, 4, NT, 4], F32, s1)
    egs = sb("egs", [128, 4, NT, 4], F32, s1)
    tw = sb("tw", [128, 6, NT, 4], F32, s1)
    with ExitStack() as pg:
        lsg = sb("lsg", [128, 2, NT, 4], F32, pg)
        tg = sb("tg", [128, 2, NT, 4], F32, pg)
        for d, c0 in ((0, 4), (1, 12)):
            sc.op('act', lambda e, d=d, c0=c0: e.activation(out=tg[:, d, :, :], in_=gm[:, :, c0:c0 + 4], func=AF.Exp, scale=-1.0),
                  reads=['gm'], writes=['tg'])
        sc.op('act', lambda e: e.activation(out=tg[:], in_=tg[:], func=AF.Ln, bias=1.0), reads=['tg'], writes=['tg'])
        sc.op('dve', lambda e: e.tensor_scalar(out=lsg[:], in0=tg[:], scalar1=-1.0, scalar2=None, op0=ALU.mult),
              reads=['tg'], writes=['lsg'])
        lf = lsg[:, 0, :, :].rearrange("p t h -> p (t h)")
        lb = lsg[:, 1, :, :].rearrange("p t h -> p (t h)")
        specs = [(C_TL, lf), (C_TUS, lf), (C_TU, lb), (C_TLS, lb)]

        def cmm(e):
            for k, (co, rhs) in enumerate(specs):
                r = e.matmul(ps[:, 6, k * 128:(k + 1) * 128], lhsT=consts[:, co:co + 128], rhs=rhs, start=True, stop=True)
            return r
        sc.op('pe', cmm, reads=['lsg', 'consts'], writes=[('ps', 6)])
        sc.op('act', lambda e: e.copy(out=cum[:].rearrange("p a t h -> p (a t h)"), in_=ps[:, 6, :]),
              reads=[('ps', 6)], writes=['cum'])
        specs2 = [(C_INDA, lf), (C_INDB, lf), (C_INDA, lb), (C_INDB, lb)]

        def gmm2(e):
            for k, (co, rhs) in enumerate(specs2):
                r = e.matmul(ps[:, 7, k * 128:(k + 1) * 128], lhsT=consts[:, co:co + 128], rhs=rhs, start=True, stop=True)
            return r
        sc.op('pe', gmm2, reads=['lsg', 'consts'], writes=[('ps', 7)])
        sc.op('act', lambda e: e.activation(out=egs[:].rearrange("p a t h -> p (a t h)"), in_=ps[:, 7, :], func=AF.Exp),
              reads=[('ps', 7)], writes=['egs'])
        for d, ic in ((0, 0), (1, 8)):
            ig = gm[:, :, ic:ic + 4]
            a_ = cum[:, 2 * d, :, :]
            r_ = cum[:, 2 * d + 1, :, :]
            sc.op('dve', lambda e, d=d, ig=ig, a_=a_: e.tensor_tensor(out=tw[:, 3 * d, :, :], in0=ig, in1=a_, op=ALU.subtract),
                  reads=['gm', 'cum'], writes=['tw'])
            sc.op('dve', lambda e, d=d, ig=ig, r_=r_: e.tensor_tensor(out=tw[:, 3 * d + 1, :, :], in0=ig, in1=r_, op=ALU.add),
                  reads=['gm', 'cum'], writes=['tw'])
            sc.op('act', lambda e, d=d: e.activation(out=tw[:, 3 * d:3 * d + 2, :, :], in_=tw[:, 3 * d:3 * d + 2, :, :],
                                                     func=AF.Exp, bias=LNK), reads=['tw'], writes=['tw'])
            sc.op('act', lambda e, d=d, a_=a_: e.activation(out=tw[:, 3 * d + 2, :, :], in_=a_, func=AF.Exp),
                  reads=['cum'], writes=['tw'])
        if debug:
            o = dbg_out("dbg_tw", [128, 6, NT, 4])
            sc.dma('sp', lambda e: e.dma_start(out=o[:, :, :, :], in_=tw[:]), 'dbgtw', reads=['tw'])
            o = dbg_out("dbg_egs", [128, 4, NT, 4])
            sc.dma('sp', lambda e: e.dma_start(out=o[:, :, :, :], in_=egs[:]), 'dbgegs', reads=['egs'])
        sc.barrier()

    if stage <= 1.5:
        sc.finish()
        return nc, dbg

    win_v = win_d.rearrange("(kc p) n -> p kc n", p=128)
    yT_v = yT_d.rearrange("(c p) t -> p c t", p=128)

    def emit_y_tile(ph, h_chunk, i, ym, ymk, yst, ystk):
        g4 = i % 4
        bank = ('ps', 5)
        pb = ps[:, 5, :].bitcast(BF16)
        sc.op('pe', lambda e: e.transpose(out=pb[:, g4 * 128:(g4 + 1) * 128], in_=ym, identity=identb[:]),
              reads=[ymk, 'identb'], writes=[bank])
        sc.op('act', lambda e: e.copy(out=yst[:, g4 * 128:(g4 + 1) * 128], in_=pb[:, g4 * 128:(g4 + 1) * 128]),
              reads=[bank], writes=[ystk])
        if g4 == 3:
            t0 = (i - 3) * 128
            sc.dma('sp', lambda e: e.dma_start(out=yT_v[:, h_chunk, t0:t0 + 512], in_=yst[:]), ystk, reads=[ystk], writes=['yT_d'])

    n_mheads = 4 if stage >= 2.5 else 1
    sM = ExitStack()
    wm2 = [sb("wm%d" % k, [128, 8, 512], BF16, sM) for k in range(2)]
    for h in range(n_mheads):
        with ExitStack() as ph:
            wm = wm2[h % 2]
            WMK = ('wm', h % 2)
            qkT = sb("qkT", [128, 2, S], BF16, ph)
            pre = sb("pre", [128, 2, 516], F32, ph)
            acc = sb("acc", [128, 2, 512], F32, ph)
            vext = sb("vext", [128, NT, 129], BF16, ph)
            sgo = sb("sgo", [128, NT, 128], BF16, ph)
            ktok = sb("ktok", [128, NT, 128], BF16, ph)
            Cst = sb("Cst", [128, 2, 65, 129], BF16, ph)
            stt = sb("stt", [128, 2, 2, 129], F32, ph)
            vtmp = [sb("vtmp%d" % k, [128, 129], BF16, ph) for k in range(2)]
            smg = [sb("smg%d" % k, [128, 2, 4, 128], BF16, ph) for k in range(2)]
            vtg = [sb("vtg%d" % k, [128, 2, 4, 129], BF16, ph) for k in range(2)]
            pst = sb("pst", [128, 2, 48], F32, ph)
            hfg = [sb("hfg%d" % k, [128, 4, 128], F32, ph) for k in range(2)]
            hsg = [sb("hsg%d" % k, [128, 4, 128], F32, ph) for k in range(2)]
            ymg = [sb("ymg%d" % k, [128, 4, 128], BF16, ph) for k in range(2)]
            yst = [sb("yst%d" % k, [128, 512], BF16, ph) for k in range(2)]
            for hh in ([0, 1] if h == 0 else [h + 1]):
                if hh >= n_mheads:
                    continue
                for k, c0 in enumerate((hh * 128, 512 + hh * 128, 1024 + hh * 128, 1536 + hh * 128)):
                    sc.dma('pool', lambda e, k=k, c0=c0, hh=hh: e.dma_start(out=wm2[hh % 2][:, :, k * 128:(k + 1) * 128], in_=win_v[:, :, c0:c0 + 128]),
                           ('wm', hh % 2), writes=[('wm', hh % 2)])
            sc.op('pool', lambda e: e.memset(vext[:, :, 128:129], 1.0), writes=['vext1'])
            sc.op('pool', lambda e: e.memset(Cst[:, :, 0, :], 0.0), writes=[('Cst', 0, 0), ('Cst', 1, 0)])
            sc.op('pool', lambda e: e.memset(stt[:, :, 0, :], 0.0), writes=[('stt', 0, 0), ('stt', 1, 0)])
            sc.op('pool', lambda e: e.memset(pre[:, :, 0:4], 0.0), writes=['pre'])
            for g in range(NG + 1):
                if g < NG:
                    for qk in range(2):
                        bank = ('ps', qk)

                        def mm(e, qk=qk, g=g):
                            for kc in range(8):
                                r = e.matmul(ps[:, qk, :], lhsT=wm[:, kc, qk * 128:(qk + 1) * 128],
                                             rhs=h1T[:, kc, g * 512:(g + 1) * 512], start=(kc == 0), stop=(kc == 7))
                            return r
                        sc.op('pe', mm, reads=[WMK] + [('h1T', 4 * g + k) for k in range(4)], writes=[bank])
                        sc.op('act', lambda e, qk=qk: e.copy(out=pre[:, qk, 4:516], in_=ps[:, qk, :]), reads=[bank], writes=['pre'])
                else:
                    sc.op('pool', lambda e: e.memset(pre[:, :, 4:8], 0.0), writes=['pre'])
                if g < NG:
                    for k in range(4):
                        i = 4 * g + k
                        bank = ('ps', 2 + (i % 2))

                        def mmvo(e, i=i):
                            for kc in range(8):
                                r = e.matmul(ps[:, 2 + (i % 2), 0:256], lhsT=h1T[:, kc, i * 128:(i + 1) * 128], rhs=wm[:, kc, 256:512],
                                             start=(kc == 0), stop=(kc == 7))
                            return r
                        sc.op('pe', mmvo, reads=[WMK, ('h1T', i)], writes=[bank])
                        sc.op('act', lambda e, i=i: e.copy(out=vext[:, i, 0:128], in_=ps[:, 2 + (i % 2), 0:128]), reads=[bank], writes=[('vext', i)])
                        sc.op('act', lambda e, i=i: e.activation(out=sgo[:, i, :], in_=ps[:, 2 + (i % 2), 128:256], func=AF.Sigmoid),
                              reads=[bank], writes=[('sgo', i)])
                W = 512 if g < NG else 2
                for qk in range(2):
                    cb = (4 * qk + h) * 5
                    sc.op('dve', lambda e, qk=qk, cb=cb, W=W: e.tensor_scalar(out=acc[:, qk, 0:W], in0=pre[:, qk, 0:W], scalar1=convT[:, cb:cb + 1],
                                                                            scalar2=None, op0=ALU.mult), reads=['pre', 'convT'], writes=[('acc', qk)])
                    for j in range(1, 5):
                        sc.op('dve', lambda e, qk=qk, cb=cb, j=j, W=W: e.scalar_tensor_tensor(
                            out=acc[:, qk, 0:W], in0=pre[:, qk, j:j + W], scalar=convT[:, cb + j:cb + j + 1], in1=acc[:, qk, 0:W],
                            op0=ALU.mult, op1=ALU.add), reads=['pre', 'convT', ('acc', qk)], writes=[('acc', qk)])
                    n0 = 2 if g == 0 else 0
                    t0 = 512 * g - 2 + n0
                    sc.op('act', lambda e, qk=qk, n0=n0, t0=t0, W=W: e.activation(out=qkT[:, qk, t0:t0 + W - n0], in_=acc[:, qk, n0:W], func=AF.Silu),
                          reads=[('acc', qk)], writes=[('qkT', qk)])
                if g < NG:
                    sc.op('pool', lambda e: e.tensor_copy(out=pre[:, :, 0:4], in_=pre[:, :, 512:516]), reads=['pre'], writes=['pre'])
            for g in range(NG):
                bank = ('ps', 4)
                pb = ps[:, 4, :].bitcast(BF16)

                def trk(e, g=g, pb=pb):
                    for k in range(4):
                        i = 4 * g + k
                        r = e.transpose(out=pb[:, k * 128:(k + 1) * 128], in_=qkT[:, 1, i * 128:(i + 1) * 128], identity=identb[:])
                    return r
                sc.op('pe', trk, reads=[('qkT', 1), 'identb'], writes=[bank])
                sc.op('act', lambda e, g=g, pb=pb: e.copy(out=ktok[:, 4 * g:4 * g + 4, :], in_=pb[:, 0:512].rearrange("p (k d) -> p k d", k=4)),
                      reads=[bank], writes=['ktok'])
            nv = 0
            for step in range(64):
                for d in range(2):
                    c = step if d == 0 else 63 - step
                    i, half = c // 2, c % 2
                    first_of_tile = (half == 0) if d == 0 else (half == 1)
                    vk = ('vhat', d)
                    vb = vtmp[d]
                    if first_of_tile:
                        sc.op('act', lambda e, d=d, i=i, vb=vb: e.activation(out=vb[:], in_=vext[:, i, :], func=AF.Copy,
                                                                             scale=tw[:, 3 * d + 1, i, h:h + 1]),
                              reads=[('vext', i), 'vext1', 'tw'], writes=[vk])
                    slot = nv % 4
                    nv += 1
                    pbank = 4 + slot
                    pc = 0
                    pk = ('ps', pbank)
                    sc.op('pe', lambda e, i=i, half=half, vb=vb, pbank=pbank, pc=pc: e.matmul(
                        ps[:, pbank, pc:pc + 129], lhsT=ktok[half * 64:(half + 1) * 64, i, :], rhs=vb[half * 64:(half + 1) * 64, :],
                        start=True, stop=True), reads=['ktok', vk], writes=[pk])
                    cur, nxt = step % 2, (step + 1) % 2
                    sc.op('dve', lambda e, d=d, i=i, half=half, cur=cur, nxt=nxt, pbank=pbank, pc=pc: e.scalar_tensor_tensor(
                        out=stt[:, d, nxt, :], in0=stt[:, d, cur, :], scalar=egs[:, 2 * d + half, i, h:h + 1], in1=ps[:, pbank, pc:pc + 129],
                        op0=ALU.mult, op1=ALU.add), reads=[('stt', d, cur), 'egs', pk], writes=[('stt', d, nxt)])
                    if d == 0:
                        sc.op('pool', lambda e, d=d, nxt=nxt, step=step: e.tensor_copy(out=Cst[:, d, step + 1, :], in_=stt[:, d, nxt, :]),
                              reads=[('stt', d, nxt)], writes=[('Cst', d, step + 1)])
                    else:
                        sc.op('act', lambda e, d=d, nxt=nxt, step=step: e.copy(out=Cst[:, d, step + 1, :], in_=stt[:, d, nxt, :]),
                              reads=[('stt', d, nxt)], writes=[('Cst', d, step + 1)])
            def s3X1(g):
                    gb = g % 2
                    sbank = ('ps', gb)
                    kF, kB = ('smg', gb, 0), ('smg', gb, 1)
                    vtk = ('vtg', gb)
                    ubanks = [('ps', 2 + t) for t in range(4)]
                    P = pst[:, gb, :]
                    pk = ('pst', gb)
                    U4 = ps[:, 2:6, 0:258].rearrange("p t (d v) -> p t d v", d=2)
                    ea2 = tw[:, 2:6:3, 4 * g:4 * g + 4, h].rearrange("p d t -> p t d")
                    Pv = lambda lo: P[:, lo:lo + 8].rearrange("p (t d) -> p t d", d=2)
                    RR = Pv(24)
                    hfb, hsb = hfg[gb], hsg[gb]
                    gb = g % 2
                    sbank = ('ps', gb)

                    def smm(e, g=g, gb=gb):
                        for t in range(4):
                            blk = slice((4 * g + t) * 128, (4 * g + t + 1) * 128)
                            r = e.matmul(ps[:, gb, t * 128:(t + 1) * 128], lhsT=qkT[:, 1, blk], rhs=qkT[:, 0, blk], start=True, stop=True)
                        return r
                    sc.op('pe', smm, reads=[('qkT', 0), ('qkT', 1)], writes=[sbank])
                    S4 = ps[:, gb, :].rearrange("p (t j) -> p t j", t=4)
                    smF, smB = smg[gb][:, 0, :, :], smg[gb][:, 1, :, :]
                    kF, kB = ('smg', gb, 0), ('smg', gb, 1)
                    sc.op('dve', lambda e, S4=S4, smF=smF: e.tensor_tensor(out=smF, in0=S4, in1=maskF[:].unsqueeze(1).broadcast_to([128, 4, 128]), op=ALU.mult),
                          reads=[sbank, 'maskF'], writes=[kF])
                    sc.op('dve', lambda e, S4=S4, smB=smB: e.tensor_tensor(out=smB, in0=S4, in1=maskB[:].unsqueeze(1).broadcast_to([128, 4, 128]), op=ALU.mult),
                          reads=[sbank, 'maskB'], writes=[kB])
                    vtk = ('vtg', gb)
                    for d in range(2):
                        sc.op('pool', lambda e, d=d, g=g, gb=gb: e.tensor_tensor(
                            out=vtg[gb][:, d, :, :], in0=vext[:, 4 * g:4 * g + 4, :],
                            in1=tw[:, 3 * d, 4 * g:4 * g + 4, h:h + 1].broadcast_to([128, 4, 129]), op=ALU.mult),
                            reads=[('vext', 4 * g + t) for t in range(4)] + ['vext1', 'tw'], writes=[(vtk, d)])

            def s3X2(g):
                    gb = g % 2
                    sbank = ('ps', gb)
                    kF, kB = ('smg', gb, 0), ('smg', gb, 1)
                    vtk = ('vtg', gb)
                    ubanks = [('ps', 2 + t) for t in range(4)]
                    P = pst[:, gb, :]
                    pk = ('pst', gb)
                    U4 = ps[:, 2:6, 0:258].rearrange("p t (d v) -> p t d v", d=2)
                    ea2 = tw[:, 2:6:3, 4 * g:4 * g + 4, h].rearrange("p d t -> p t d")
                    Pv = lambda lo: P[:, lo:lo + 8].rearrange("p (t d) -> p t d", d=2)
                    RR = Pv(24)
                    hfb, hsb = hfg[gb], hsg[gb]
                    ubanks = [('ps', 2 + t) for t in range(4)]

                    def umm(e, g=g, gb=gb):
                        for t in range(4):
                            i = 4 * g + t
                            for d in range(2):
                                o0_ = d * 129
                                e.matmul(ps[:, 2 + t, o0_:o0_ + 129], lhsT=smg[gb][:, d, t, :], rhs=vtg[gb][:, d, t, :], start=True, stop=False)
                                for half in range(2):
                                    c = 2 * i + half
                                    sidx = c if d == 0 else 63 - c
                                    t0 = i * 128 + half * 64
                                    r = e.matmul(ps[half * 64:(half + 1) * 64, 2 + t, o0_:o0_ + 129], lhsT=qkT[:, 0, t0:t0 + 64],
                                                 rhs=Cst[:, d, sidx, :], start=False, stop=True)
                        return r
                    creads = []
                    for t in range(4):
                        i = 4 * g + t
                        creads += [('Cst', 0, 2 * i), ('Cst', 0, 2 * i + 1), ('Cst', 1, 63 - 2 * i), ('Cst', 1, 62 - 2 * i)]
                    sc.op('pe', umm, reads=[kF, kB, (vtk, 0), (vtk, 1), ('qkT', 0)] + creads, writes=ubanks)

            def s3Y1(g):
                    gb = g % 2
                    sbank = ('ps', gb)
                    kF, kB = ('smg', gb, 0), ('smg', gb, 1)
                    vtk = ('vtg', gb)
                    ubanks = [('ps', 2 + t) for t in range(4)]
                    P = pst[:, gb, :]
                    pk = ('pst', gb)
                    U4 = ps[:, 2:6, 0:258].rearrange("p t (d v) -> p t d v", d=2)
                    ea2 = tw[:, 2:6:3, 4 * g:4 * g + 4, h].rearrange("p d t -> p t d")
                    Pv = lambda lo: P[:, lo:lo + 8].rearrange("p (t d) -> p t d", d=2)
                    RR = Pv(24)
                    hfb, hsb = hfg[gb], hsg[gb]
                    P = pst[:, gb, :]
                    pk = ('pst', gb)
                    U4 = ps[:, 2:6, 0:258].rearrange("p t (d v) -> p t d v", d=2)
                    ea2 = tw[:, 2:6:3, 4 * g:4 * g + 4, h].rearrange("p d t -> p t d")
                    Pv = lambda lo: P[:, lo:lo + 8].rearrange("p (t d) -> p t d", d=2)
                    sc.op('dve', lambda e: e.tensor_tensor(out=Pv(0), in0=U4[:, :, :, 128], in1=ea2, op=ALU.mult), reads=ubanks + ['tw'], writes=[pk])
                    sc.op('dve', lambda e: e.tensor_scalar(out=P[:, 16:24], in0=P[:, 0:8], scalar1=-1.0, scalar2=None, op0=ALU.mult), reads=[pk], writes=[pk])
                    sc.op('dve', lambda e: e.scalar_tensor_tensor(out=P[:, 8:16], in0=P[:, 16:24], scalar=1.0, in1=P[:, 0:8], op0=ALU.max, op1=ALU.max),
                          reads=[pk], writes=[pk])
                    sc.op('dve', lambda e: e.reciprocal(out=P[:, 16:24], in_=P[:, 8:16]), reads=[pk], writes=[pk])
                    sc.op('dve', lambda e: e.tensor_tensor(out=Pv(24), in0=Pv(16), in1=ea2, op=ALU.mult), reads=[pk, 'tw'], writes=[pk])
                    RR = Pv(24)
                    hfb, hsb = hfg[gb], hsg[gb]
                    sc.op('dve', lambda e: e.tensor_tensor(out=hfb[:], in0=U4[:, :, 0, 0:128], in1=RR[:, :, 0:1].broadcast_to([128, 4, 128]), op=ALU.mult),
                          reads=ubanks + [pk], writes=[('hfg', gb)])
                    sc.op('dve', lambda e: e.tensor_tensor(out=hsb[:], in0=U4[:, :, 1, 0:128], in1=RR[:, :, 1:2].broadcast_to([128, 4, 128]), op=ALU.mult),
                          reads=ubanks + [pk], writes=[('hsg', gb)])

            def s3Y2(g):
                    gb = g % 2
                    sbank = ('ps', gb)
                    kF, kB = ('smg', gb, 0), ('smg', gb, 1)
                    vtk = ('vtg', gb)
                    ubanks = [('ps', 2 + t) for t in range(4)]
                    P = pst[:, gb, :]
                    pk = ('pst', gb)
                    U4 = ps[:, 2:6, 0:258].rearrange("p t (d v) -> p t d v", d=2)
                    ea2 = tw[:, 2:6:3, 4 * g:4 * g + 4, h].rearrange("p d t -> p t d")
                    Pv = lambda lo: P[:, lo:lo + 8].rearrange("p (t d) -> p t d", d=2)
                    RR = Pv(24)
                    hfb, hsb = hfg[gb], hsg[gb]
                    sc.op('pool', lambda e: e.tensor_tensor(out=hsb[:], in0=hsb[:], in1=hfb[:], op=ALU.add), reads=[('hsg', gb), ('hfg', gb)], writes=[('hsg', gb)])
                    sc.op('pool', lambda e: e.tensor_tensor(out=hfb[:], in0=hsb[:], in1=hsb[:], op=ALU.mult), reads=[('hsg', gb)], writes=[('hfg', gb)])
                    sc.op('dve', lambda e: e.tensor_reduce(out=P[:, 32:36], in_=hfb[:], axis=AX.X, op=ALU.add), reads=[('hfg', gb)], writes=[pk])
                    sc.op('act', lambda e: e.activation(out=P[:, 36:40], in_=P[:, 32:36], func=AF.Sqrt, scale=1.0 / 128, bias=EPS), reads=[pk], writes=[pk])
                    sc.op('dve', lambda e: e.reciprocal(out=P[:, 40:44], in_=P[:, 36:40]), reads=[pk], writes=[pk])
                    sc.op('dve', lambda e: e.tensor_tensor(out=hsb[:], in0=hsb[:], in1=P[:, 40:44].unsqueeze(2).broadcast_to([128, 4, 128]), op=ALU.mult),
                          reads=[('hsg', gb), pk], writes=[('hsg', gb)])
                    sc.op('pool', lambda e: e.tensor_tensor(out=hsb[:], in0=hsb[:],
                                                            in1=prm[:, P_MNG + h * 128:P_MNG + (h + 1) * 128].unsqueeze(1).broadcast_to([128, 4, 128]), op=ALU.mult),
                          reads=[('hsg', gb), 'prm'], writes=[('hsg', gb)])
                    ym = ymg[gb]
                    ymk = ('ymg', gb)
                    sc.op('dve', lambda e, g=g: e.tensor_tensor(out=ym[:], in0=hsb[:], in1=sgo[:, 4 * g:4 * g + 4, :], op=ALU.mult),
                          reads=[('hsg', gb)] + [('sgo', 4 * g + t) for t in range(4)], writes=[ymk])
                    tbank = ('ps', 6)
                    pb = ps[:, 6, :].bitcast(BF16)

                    def ytr(e, ym=ym, pb=pb):
                        for t in range(4):
                            r = e.transpose(out=pb[:, t * 128:(t + 1) * 128], in_=ym[:, t, :], identity=identb[:])
                        return r
                    sc.op('pe', ytr, reads=[ymk, 'identb'], writes=[tbank])
                    ys = yst[gb]
                    ysk = ('yst', gb)
                    sc.op('act', lambda e, ys=ys, pb=pb: e.copy(out=ys[:], in_=pb[:, 0:512]), reads=[tbank], writes=[ysk])
                    sc.dma('sp', lambda e, ys=ys, g=g: e.dma_start(out=yT_v[:, h, g * 512:(g + 1) * 512], in_=ys[:]), ysk, reads=[ysk], writes=['yT_d'])

            s3X1(0)
            s3X2(0)
            for g in range(NG):
                if g + 1 < NG:
                    s3X1(g + 1)
                s3Y1(g)
                if g + 1 < NG:
                    s3X2(g + 1)
                s3Y2(g)
            if debug and h == 0:
                o = dbg_out("dbg_qkT", [128, 2, S], BF16)
                sc.dma('sp', lambda e: e.dma_start(out=o[:, :, :], in_=qkT[:]), 'dbgqkT', reads=[('qkT', 0), ('qkT', 1)])
                o = dbg_out("dbg_Cst", [128, 2, 65, 129], BF16)
                sc.dma('sp', lambda e: e.dma_start(out=o[:, :, :, :], in_=Cst[:]), 'dbgCst',
                       reads=[('Cst', d, k) for d in range(2) for k in range(65)])
            sc.barrier()
    sM.close()

    if stage <= 2.5:
        sc.finish()
        return nc, dbg

    cosT = sb("cosT", [128, NT, 8], F32, s1)
    sinT = sb("sinT", [128, NT, 8], F32, s1)
    qkg4 = sb("qkg4", [128, 4, 64], F32, s1)
    subg8 = sb("subg8", [128, 128], F32, s1)
    TWO_PI = 6.28318
    with ExitStack() as pa:
        posi = sb("posi", [128, NT], I32, pa)
        posf = sb("posf", [128, NT], F32, pa)
        ut = sb("ut", [128, NT, 8], F32, pa)
        ui = sb("ui", [128, NT, 8], I32, pa)
        uf = sb("uf", [128, NT, 8], F32, pa)
        jl = sb("jl", [128, 64], F32, pa)
        sc.dma('sp', lambda e: e.dma_start(out=posi[:], in_=pos_d[:, :]), 'posi', writes=['posi'])
        sc.op('dve', lambda e: e.tensor_copy(out=posf[:], in_=posi[:]), reads=['posi'], writes=['posf'])
        sc.op('dve', lambda e: e.tensor_tensor(out=ut[:], in0=posf[:].unsqueeze(2).broadcast_to([128, NT, 8]),
                                               in1=consts[:, C_FREQ:C_FREQ + 8].unsqueeze(1).broadcast_to([128, NT, 8]), op=ALU.mult),
              reads=['posf', 'consts'], writes=['ut'])
        for tab, shift in ((sinT, 0.0), (cosT, 0.25)):
            if shift:
                sc.op('dve', lambda e, shift=shift: e.tensor_scalar(out=ut[:], in0=ut[:], scalar1=shift, scalar2=None, op0=ALU.add),
                      reads=['ut'], writes=['ut'])
            sc.op('dve', lambda e: e.tensor_copy(out=ui[:], in_=ut[:]), reads=['ut'], writes=['ui'])
            sc.op('dve', lambda e: e.tensor_copy(out=uf[:], in_=ui[:]), reads=['ui'], writes=['uf'])
            sc.op('dve', lambda e: e.tensor_tensor(out=uf[:], in0=ut[:], in1=uf[:], op=ALU.subtract), reads=['ut', 'uf'], writes=['uf'])
            sc.op('act', lambda e, tab=tab: e.activation(out=tab[:], in_=uf[:], func=AF.Sin, scale=TWO_PI), reads=['uf'], writes=['rot'])
        sc.op('dve', lambda e: e.tensor_reduce(out=small[:, 2:4], in_=prm[:, P_QKG:P_QKG + 128].rearrange("p (a d) -> p a d", a=2),
                                               axis=AX.X, op=ALU.max, apply_absolute_value=True), reads=['prm'], writes=['small'])
        sc.op('dve', lambda e: e.scalar_tensor_tensor(out=small[:, SM_NB:SM_NB + 1], in0=small[:, 2:3], scalar=-8.0, in1=small[:, 3:4],
                                                      op0=ALU.mult, op1=ALU.mult), reads=['small'], writes=['small'])
        for k in range(2):
            sc.op('dve', lambda e, k=k: e.tensor_tensor(out=jl[:], in0=prm[:, P_LAM + 128 * k:P_LAM + 128 * k + 64],
                                                        in1=prm[:, P_LAM + 128 * k + 64:P_LAM + 128 * k + 128], op=ALU.mult),
                  reads=['prm'], writes=['jl'])
            sc.op('dve', lambda e, k=k: e.tensor_reduce(out=small[:, 4 + k:5 + k], in_=jl[:], axis=AX.X, op=ALU.add), reads=['jl'], writes=['small'])
        sc.op('act', lambda e: e.activation(out=small[:, 6:8], in_=small[:, 4:6], func=AF.Exp), reads=['small'], writes=['small'])
        sc.op('dve', lambda e: e.scalar_tensor_tensor(out=small[:, SM_NLAM:SM_NLAM + 1], in0=small[:, 7:8], scalar=-LAM_INIT, in1=small[:, 6:7],
                                                      op0=ALU.add, op1=ALU.subtract), reads=['small'], writes=['small'])
        for a in range(4):
            o_ = P_QKG + (64 if a >= 2 else 0)
            sc.op('pool', lambda e, a=a, o_=o_: e.tensor_copy(out=qkg4[:, a, :], in_=prm[:, o_:o_ + 64]), reads=['prm'], writes=['qkg4'])
        sc.op('dve', lambda e: e.tensor_scalar(out=subg8[:], in0=prm[:, P_SUB:P_SUB + 128], scalar1=1.0 - LAM_INIT, scalar2=None, op0=ALU.mult),
              reads=['prm'], writes=['subg8'])
        if debug:
            o = dbg_out("dbg_small", [128, 64])
            sc.op('pool', lambda e: e.memset(small[:, 8:64], 0.0), writes=['small'])
            sc.dma('sp', lambda e: e.dma_start(out=o[:, :], in_=small[:]), 'dbgsmall', reads=['small'])
            o = dbg_out("dbg_cos", [128, NT, 8])
            sc.dma('sp', lambda e: e.dma_start(out=o[:, :, :], in_=cosT[:]), 'dbgcos', reads=['rot'])
        sc.barrier()

    n_aheads = 4 if stage >= 3.5 else 1
    sA = ExitStack()
    wa2 = [sb("waa%d" % k, [128, 8, 384], BF16, sA) for k in range(2)]
    for h in range(n_aheads):
        with ExitStack() as ph:
            wa_ = wa2[h % 2]
            WAK = ('waa', h % 2)
            qTa = sb("qTa", [128, 2, S], BF16, ph)
            kTa = sb("kTa", [128, S], BF16, ph)
            vxa = sb("vxa", [128, NT, 129], BF16, ph)
            xq = [sb("xq%d" % k, [128, 4, 4, 64], F32, ph) for k in range(2)]
            sq = sb("sq", [128, 4, 4, 64], F32, ph)
            rp = [sb("rp%d" % k, [128, 4, 4, 4, 8], F32, ph) for k in range(2)]
            st4 = sb("st4", [128, 2, 48], F32, ph)
            xr = [sb("xr%d" % k, [128, 4, 4, 64], BF16, ph) for k in range(2)]
            pt = [sb("pt%d" % k, [128, 2, 512], BF16, ph) for k in range(3)]
            o0 = sb("o0", [128, 4, 128], F32, ph)
            ob4 = [sb("ob4_%d" % k, [128, 4, 128], F32, ph) for k in range(2)]
            osq = sb("osq", [128, 4, 128], F32, ph)
            st5 = sb("st5", [128, 2, 24], F32, ph)
            ymt4 = [sb("ymt4_%d" % k, [128, 4, 128], BF16, ph) for k in range(2)]
            yst = [sb("ysta%d" % k, [128, 512], BF16, ph) for k in range(2)]
            for hh in ([0, 1] if h == 0 else [h + 1]):
                if hh >= n_aheads:
                    continue
                for k, c0 in enumerate((2064 + hh * 128, 2576 + hh * 128, 3088 + hh * 128)):
                    sc.dma('pool', lambda e, k=k, c0=c0, hh=hh: e.dma_start(out=wa2[hh % 2][:, :, k * 128:(k + 1) * 128], in_=win_v[:, :, c0:c0 + 128]),
                           ('waa', hh % 2), writes=[('waa', hh % 2)])
            sc.op('pool', lambda e: e.memset(vxa[:, :, 128:129], 1.0), writes=['vxa1'])
            sc.op('pool', lambda e: e.memset(qTa[64:128, 0, :], 0.0), writes=['qTa'])
            sc.op('pool', lambda e: e.memset(qTa[0:64, 1, :], 0.0), writes=['qTa'])
            def projA(g):
                    b = g % 2
                    banks = [('ps', k) for k in range(4)]
                    xk, rk, sk, xrk = ('xq', b), ('rp', b), ('st4', b), ('xr', b)
                    X = xq[b]
                    X3 = X[:].rearrange("p t a d -> p t (a d)")
                    X16 = X[:].rearrange("p t a d -> p (t a) d")
                    T4 = st4[:, b, :]
                    R = rp[b]
                    b = g % 2
                    banks = [('ps', k) for k in range(4)]

                    def mmp(e, g=g):
                        for t in range(4):
                            i = 4 * g + t
                            for kc in range(8):
                                r = e.matmul(ps[:, t, 0:384], lhsT=h1T[:, kc, i * 128:(i + 1) * 128], rhs=wa_[:, kc, :], start=(kc == 0), stop=(kc == 7))
                        return r
                    sc.op('pe', mmp, reads=[WAK] + [('h1T', 4 * g + t) for t in range(4)], writes=banks)
                    xk, rk, sk, xrk = ('xq', b), ('rp', b), ('st4', b), ('xr', b)
                    X = xq[b]
                    X3 = X[:].rearrange("p t a d -> p t (a d)")
                    X16 = X[:].rearrange("p t a d -> p (t a) d")
                    sc.op('act', lambda e, X3=X3: e.copy(out=X3, in_=ps[:, 0:4, 0:256]), reads=banks, writes=[xk])
                    sc.op('act', lambda e, g=g: e.copy(out=vxa[:, 4 * g:4 * g + 4, 0:128], in_=ps[:, 0:4, 256:384]), reads=banks,
                          writes=[('vxa', 4 * g + t) for t in range(4)])

            def projB(g):
                    b = g % 2
                    banks = [('ps', k) for k in range(4)]
                    xk, rk, sk, xrk = ('xq', b), ('rp', b), ('st4', b), ('xr', b)
                    X = xq[b]
                    X3 = X[:].rearrange("p t a d -> p t (a d)")
                    X16 = X[:].rearrange("p t a d -> p (t a) d")
                    T4 = st4[:, b, :]
                    R = rp[b]
                    sc.op('dve', lambda e, X=X: e.tensor_tensor(out=sq[:], in0=X[:], in1=X[:], op=ALU.mult), reads=[xk], writes=['sq'])
                    T4 = st4[:, b, :]
                    sc.op('dve', lambda e, T4=T4: e.tensor_reduce(out=T4[:, 0:16], in_=sq[:].rearrange("p t a d -> p (t a) d"), axis=AX.X, op=ALU.add),
                          reads=['sq'], writes=[sk])
                    sc.op('act', lambda e, T4=T4: e.activation(out=T4[:, 16:32], in_=T4[:, 0:16], func=AF.Sqrt, scale=1.0 / 64, bias=EPS), reads=[sk], writes=[sk])
                    sc.op('dve', lambda e, T4=T4: e.reciprocal(out=T4[:, 32:48], in_=T4[:, 16:32]), reads=[sk], writes=[sk])
                    sc.op('dve', lambda e, X16=X16, T4=T4: e.tensor_tensor(out=X16, in0=X16, in1=T4[:, 32:48].unsqueeze(2).broadcast_to([128, 16, 64]), op=ALU.mult),
                          reads=[xk, sk], writes=[xk])
                    sc.op('dve', lambda e, X=X: e.tensor_tensor(out=X[:], in0=X[:], in1=qkg4[:].unsqueeze(1).broadcast_to([128, 4, 4, 64]), op=ALU.mult),
                          reads=[xk, 'qkg4'], writes=[xk])
                    cs = cosT[:, 4 * g:4 * g + 4, :].unsqueeze(2).broadcast_to([128, 4, 4, 8])
                    sn = sinT[:, 4 * g:4 * g + 4, :].unsqueeze(2).broadcast_to([128, 4, 4, 8])
                    R = rp[b]
                    for k, (tt, tr_) in enumerate(((X[:, :, :, 0:8], cs), (X[:, :, :, 8:16], sn), (X[:, :, :, 8:16], cs), (X[:, :, :, 0:8], sn))):
                        sc.op('pool', lambda e, k=k, tt=tt, tr_=tr_, R=R: e.tensor_tensor(out=R[:, k, :, :, :], in0=tt, in1=tr_, op=ALU.mult),
                              reads=[xk, 'rot'], writes=[rk])

            def projC(g):
                    b = g % 2
                    banks = [('ps', k) for k in range(4)]
                    xk, rk, sk, xrk = ('xq', b), ('rp', b), ('st4', b), ('xr', b)
                    X = xq[b]
                    X3 = X[:].rearrange("p t a d -> p t (a d)")
                    X16 = X[:].rearrange("p t a d -> p (t a) d")
                    T4 = st4[:, b, :]
                    R = rp[b]
                    XR = xr[b]
                    sc.op('act', lambda e, XR=XR, X=X: e.copy(out=XR[:], in_=X[:]), reads=[xk], writes=[xrk])
                    sc.op('dve', lambda e, XR=XR, R=R: e.tensor_tensor(out=XR[:, :, :, 0:8], in0=R[:, 0, :, :, :], in1=R[:, 1, :, :, :], op=ALU.subtract),
                          reads=[rk, xrk], writes=[xrk])
                    sc.op('dve', lambda e, XR=XR, R=R: e.tensor_tensor(out=XR[:, :, :, 8:16], in0=R[:, 2, :, :, :], in1=R[:, 3, :, :, :], op=ALU.add),
                          reads=[rk, xrk], writes=[xrk])
                    tbanks = [('ps', 4), ('ps', 5)]
                    pbq = ps[:, 4, :].bitcast(BF16)
                    pbk = ps[:, 5, :].bitcast(BF16)
                    XRf = XR[:].rearrange("p t a d -> p t (a d)")

                    def trq(e, XRf=XRf, pbq=pbq, pbk=pbk):
                        for t in range(4):
                            e.transpose(out=pbq[:, t * 128:(t + 1) * 128], in_=XRf[:, t, 0:128], identity=identb[:])
                            r = e.transpose(out=pbk[:, t * 128:(t + 1) * 128], in_=XRf[:, t, 128:256], identity=identb[:])
                        return r
                    sc.op('pe', trq, reads=[xrk, 'identb'], writes=tbanks)
                    sc.op('act', lambda e, pbq=pbq, g=g: e.copy(out=qTa[0:64, 0, g * 512:(g + 1) * 512], in_=pbq[0:64, 0:512]), reads=tbanks, writes=['qTa'])
                    sc.op('act', lambda e, pbq=pbq, g=g: e.copy(out=qTa[64:128, 1, g * 512:(g + 1) * 512], in_=pbq[64:128, 0:512]), reads=tbanks, writes=['qTa'])
                    sc.op('act', lambda e, pbk=pbk, g=g: e.copy(out=kTa[:, g * 512:(g + 1) * 512], in_=pbk[:, 0:512]), reads=tbanks, writes=['kTa'])

            projA(0)
            for g in range(NG):
                if g + 1 < NG:
                    projA(g + 1)
                projB(g)
                projC(g)
            if debug and h == 0:
                o = dbg_out("dbg_qTa", [128, S], BF16)
                sc.dma('sp', lambda e: e.dma_start(out=o[0:64, :], in_=qTa[0:64, 0, :]), 'dbgqTa', reads=['qTa'])
                sc.dma('sp', lambda e: e.dma_start(out=o[64:128, :], in_=qTa[64:128, 1, :]), 'dbgqTa', reads=['qTa'])
                o = dbg_out("dbg_kTa", [128, S], BF16)
                sc.dma('sp', lambda e: e.dma_start(out=o[:, :], in_=kTa[:]), 'dbgkTa', reads=['kTa'])
            nqb = 8 if stage >= 3.2 else 1
            for qb in range(nqb):
                for p in range(2):
                    pr = slice(64 * p, 64 * p + 64)
                    def st_mm(j, qb=qb, p=p):
                        pb0 = 4 + 2 * (j % 2)

                        def f(e, j=j, pb0=pb0, qb=qb, p=p):
                            for u in range(2):
                                kt = 2 * j + u
                                r = e.matmul(ps[:, pb0 + u, :], lhsT=kTa[:, kt * 128:(kt + 1) * 128], rhs=qTa[:, p, qb * 512:(qb + 1) * 512],
                                             start=True, stop=True)
                            return r
                        sc.op('pe', f, reads=['qTa', 'kTa'], writes=[('ps', pb0), ('ps', pb0 + 1)])
                    if qb == 0 and p == 0:
                        st_mm(0)
                    for j in range(NT // 2):
                        pb0 = 4 + 2 * (j % 2)
                        sbanks = [('ps', pb0), ('ps', pb0 + 1)]
                        if j + 1 < NT // 2:
                            st_mm(j + 1)
                        elif not (qb == nqb - 1 and p == 1):
                            nqb_, np_ = (qb, 1) if p == 0 else (qb + 1, 0)
                            st_mm(0, nqb_, np_)
                        ptk = ('pt', j % 3)
                        ptb = pt[j % 3]
                        sc.op('act', lambda e, pb0=pb0, ptb=ptb: e.activation(out=ptb[:], in_=ps[:, pb0:pb0 + 2, :], func=AF.Exp, scale=0.125,
                                                                              bias=small[:, SM_NB:SM_NB + 1]),
                              reads=sbanks + ['small'], writes=[ptk])

                        def pv(e, ptb=ptb, j=j):
                            for u in range(2):
                                kt = 2 * j + u
                                for qs in range(4):
                                    r = e.matmul(ps[:, qs, 0:129], lhsT=ptb[:, u, qs * 128:(qs + 1) * 128], rhs=vxa[:, kt, :],
                                                 start=(kt == 0), stop=(kt == NT - 1))
                            return r
                        sc.op('pe', pv, reads=[ptk, ('vxa', 2 * j), ('vxa', 2 * j + 1), 'vxa1'], writes=[('ps', 0), ('ps', 1), ('ps', 2), ('ps', 3)])
                    abanks = [('ps', k) for k in range(4)]
                    acc4 = ps[:, 0:4, 0:129]
                    T5 = st5[:, p, :]
                    tk5 = ('st5', p)
                    sc.op('dve', lambda e, T5=T5, acc4=acc4: e.reciprocal(out=T5[:, 0:4], in_=acc4[:, :, 128]), reads=abanks, writes=[tk5])
                    if p == 0:
                        sc.op('dve', lambda e, T5=T5, acc4=acc4: e.tensor_tensor(out=o0[:], in0=acc4[:, :, 0:128],
                                                                                  in1=T5[:, 0:4].unsqueeze(2).broadcast_to([128, 4, 128]), op=ALU.mult),
                              reads=abanks + [tk5], writes=['o0'])
                        continue
                    O = ob4[qb % 2]
                    okk = ('ob4', qb % 2)
                    sc.op('dve', lambda e, T5=T5: e.tensor_scalar(out=T5[:, 4:8], in0=T5[:, 0:4], scalar1=small[:, SM_NLAM:SM_NLAM + 1], scalar2=None, op0=ALU.mult),
                          reads=[tk5, 'small'], writes=[tk5])
                    sc.op('dve', lambda e, T5=T5, acc4=acc4, O=O: e.tensor_tensor(out=O[:], in0=acc4[:, :, 0:128],
                                                                                   in1=T5[:, 4:8].unsqueeze(2).broadcast_to([128, 4, 128]), op=ALU.mult),
                          reads=abanks + [tk5], writes=[okk])
                    sc.op('dve', lambda e, O=O: e.tensor_tensor(out=O[:], in0=O[:], in1=o0[:], op=ALU.add), reads=[okk, 'o0'], writes=[okk])
                    sc.op('pool', lambda e, O=O: e.tensor_tensor(out=osq[:], in0=O[:], in1=O[:], op=ALU.mult), reads=[okk], writes=['osq'])
                    sc.op('dve', lambda e, T5=T5: e.tensor_reduce(out=T5[:, 8:12], in_=osq[:], axis=AX.X, op=ALU.add), reads=['osq'], writes=[tk5])
                    sc.op('act', lambda e, T5=T5: e.activation(out=T5[:, 12:16], in_=T5[:, 8:12], func=AF.Sqrt, scale=1.0 / 128, bias=EPS), reads=[tk5], writes=[tk5])
                    sc.op('dve', lambda e, T5=T5: e.reciprocal(out=T5[:, 16:20], in_=T5[:, 12:16]), reads=[tk5], writes=[tk5])
                    sc.op('dve', lambda e, T5=T5, O=O: e.tensor_tensor(out=O[:], in0=O[:], in1=T5[:, 16:20].unsqueeze(2).broadcast_to([128, 4, 128]), op=ALU.mult),
                          reads=[okk, tk5], writes=[okk])
                    ym = ymt4[qb % 2]
                    ymk = ('ymt4', qb % 2)
                    sc.op('pool', lambda e, O=O, ym=ym: e.tensor_tensor(out=ym[:], in0=O[:], in1=subg8[:].unsqueeze(1).broadcast_to([128, 4, 128]), op=ALU.mult),
                          reads=[okk, 'subg8'], writes=[ymk])
                    tbank = ('ps', 7)
                    pb = ps[:, 7, :].bitcast(BF16)

                    def ytr(e, ym=ym, pb=pb):
                        for t in range(4):
                            r = e.transpose(out=pb[:, t * 128:(t + 1) * 128], in_=ym[:, t, :], identity=identb[:])
                        return r
                    sc.op('pe', ytr, reads=[ymk, 'identb'], writes=[tbank])
                    ys = yst[qb % 2]
                    ysk = ('ysta', qb % 2)
                    sc.op('act', lambda e, ys=ys, pb=pb: e.copy(out=ys[:], in_=pb[:, 0:512]), reads=[tbank], writes=[ysk])
                    sc.dma('sp', lambda e, ys=ys, qb=qb: e.dma_start(out=yT_v[:, 4 + h, qb * 512:(qb + 1) * 512], in_=ys[:]), ysk, reads=[ysk], writes=['yT_d'])
            sc.barrier()
    sA.close()

    if stage <= 3.5:
        sc.finish()
        return nc, dbg

    s1.close()

    sW = ExitStack()
    aff = sb("aff", [128, NT, NE], F32, sW)
    g2rep = sb("g2rep", [128, D], F32, sW)
    NSLOT = 6
    rank_tok = sb("rank_tok", [128, NT, NE], F32, sW)
    R5 = sb("R5", [128, NT, NE, 5], BF16, sW)
    if stage >= 5:
        ring = [sb("ring%d" % k, [128, 8, D], BF16, sW) for k in range(NSLOT)]
        wg_v = wg_d.rearrange("(e kc p) n -> p e kc n", p=128, kc=8)
        wu_v = wu_d.rearrange("(e kc p) n -> p e kc n", p=128, kc=8)
        wd_v = wd_d.rearrange("(e fh fc p) n -> p e fh fc n", p=128, fc=8, fh=2)

        def load_piece(e_, k):
            slot = ring[k]
            key = ('ring', k)
            if k < 4:
                src = (wg_v if k % 2 == 0 else wu_v)[:, e_, :, (k // 2) * D:(k // 2 + 1) * D]
            else:
                src = wd_v[:, e_, k - 4, :, :]
            sc.dma('pool', lambda e, slot=slot, src=src: e.dma_start(out=slot[:], in_=src), key, writes=[key])
        for k in range(NSLOT):
            load_piece(0, k)
    with ExitStack() as pw:
        woutb = sb("woutb", [128, 8, D], BF16, pw)
        wrt = sb("wrt", [128, 8, NE], F32, pw)
        ytl = [sb("ytl%d" % k, [128, 8, 512], BF16, pw) for k in range(2)]
        xt2 = [sb("xt2_%d" % k, [128, D], F32, pw) for k in range(2)]
        tmpw = [sb("tmpw%d" % k, [128, D], F32, pw) for k in range(2)]
        x1t = [sb("x1t%d" % k, [128, D], F32, pw) for k in range(2)]
        h2f = [sb("h2f%d" % k, [128, D], F32, pw) for k in range(2)]
        h2b = [sb("h2b%d" % k, [128, D], BF16, pw) for k in range(2)]
        h2T = [sb("h2T%d" % k, [128, 8, 128], F32, pw) for k in range(2)]
        junkw = sb("junkw", [128, D], BF16, pw)
        stw = sb("stw", [128, NT, 8], F32, pw)
        esm = sb("esm", [128, 2, NE], F32, pw)
        wout_v = wout_d.rearrange("(kc p) n -> p kc n", p=128)
        sc.dma('pool', lambda e: e.dma_start(out=woutb[:, 0:4, :], in_=wout_v[:, 0:4, :]), 'woutb', writes=['woutb'])
        sc.dma('pool', lambda e: e.dma_start(out=woutb[:, 4:8, :], in_=wout_v[:, 4:8, :]), 'woutb', writes=['woutb'])
        sc.dma('sp', lambda e: e.dma_start(out=wrt[:], in_=wr_d.rearrange("(kc p) n -> p kc n", p=128)), 'wrt', writes=['wrt'])
        modW = sb("modW", [128, 3 * D], F32, pw)
        sc.dma('sp', lambda e: e.dma_start(out=modW[:], in_=modrow_d[0:1, 2 * D:5 * D].partition_broadcast(128)), 'modW', reads=['modrow_d'], writes=['mod'])
        sc.dma('sp', lambda e: e.dma_start(out=g2rep[:], in_=modrow_d[0:1, 5 * D:6 * D].partition_broadcast(128)), 'g2rep', reads=['modrow_d'], writes=['g2rep'])
        GATE1, SHIFT2, GS2 = modW[:, 0:D], modW[:, D:2 * D], modW[:, 2 * D:3 * D]
        def wA(i):
            b = i % 2
            g, k4 = i // 4, i % 4
            yk = ('ytl', g % 2)
            if k4 == 0:
                sc.dma('sp', lambda e, g=g: e.dma_start(out=ytl[g % 2][:], in_=yT_v[:, :, g * 512:(g + 1) * 512]), yk, writes=[yk])
            xk = ('xt2', b)
            sc.dma('sp', lambda e, i=i, b=b: e.dma_start(out=xt2[b][:], in_=x_d[i * 128:(i + 1) * 128, :]), xk, writes=[xk])
            banks = [('ps', 2 * b), ('ps', 2 * b + 1)]

            def mmw(e, i=i, b=b, g=g, k4=k4):
                for dh in range(2):
                    for c in range(8):
                        r = e.matmul(ps[:, 2 * b + dh, :], lhsT=ytl[g % 2][:, c, k4 * 128:(k4 + 1) * 128], rhs=woutb[:, c, dh * 512:(dh + 1) * 512],
                                     start=(c == 0), stop=(c == 7))
                return r
            sc.op('pe', mmw, reads=[yk, 'woutb'], writes=banks)

        def wB(i):
            b = i % 2
            g, k4 = i // 4, i % 4
            yk = ('ytl', g % 2)
            xk = ('xt2', b)
            banks = [('ps', 2 * b), ('ps', 2 * b + 1)]
            T = stw[:, i, :]
            tk = ('stw', i)
            mixv = ps[:, 2 * b:2 * b + 2, :].rearrange("p a n -> p (a n)")
            sc.op('dve', lambda e, b=b, mixv=mixv: e.tensor_tensor(out=tmpw[b][:], in0=mixv, in1=GATE1, op=ALU.mult), reads=banks + ['mod'], writes=[('tmpw', b)])
            sc.op('dve', lambda e, b=b: e.tensor_tensor(out=x1t[b][:], in0=tmpw[b][:], in1=xt2[b][:], op=ALU.add),
                  reads=[('tmpw', b), xk], writes=[('x1t', b)])
            sc.dma('sp', lambda e, i=i, b=b: e.dma_start(out=out_d[i * 128:(i + 1) * 128, :], in_=x1t[b][:]), ('x1s', b), reads=[('x1t', b)], writes=[('outd', i)])
            T = stw[:, i, :]
            tk = ('stw', i)
            sc.op('act', lambda e, b=b, T=T: e.activation(out=junkw[:], in_=x1t[b][:], func=AF.Square, accum_out=T[:, 0:1]), reads=[('x1t', b)], writes=['junkw', tk])
            sc.op('act', lambda e, T=T: e.activation(out=T[:, 1:2], in_=T[:, 0:1], func=AF.Sqrt, scale=1.0 / D, bias=EPS), reads=[tk], writes=[tk])
            sc.op('dve', lambda e, T=T: e.reciprocal(out=T[:, 2:3], in_=T[:, 1:2]), reads=[tk], writes=[tk])
            sc.op('dve', lambda e, b=b, T=T: e.scalar_tensor_tensor(out=tmpw[b][:], in0=x1t[b][:], scalar=T[:, 2:3], in1=GS2, op0=ALU.mult, op1=ALU.mult),
                  reads=[('x1t', b), tk, 'mod'], writes=[('tmpw', b)])
            sc.op('dve', lambda e, b=b: e.tensor_tensor(out=h2f[b][:], in0=tmpw[b][:], in1=SHIFT2, op=ALU.add), reads=[('tmpw', b), 'mod'], writes=[('h2f', b)])
            sc.op('act', lambda e, b=b: e.copy(out=h2b[b][:], in_=h2f[b][:]), reads=[('h2f', b)], writes=[('h2b', b)])
            sc.dma('sp', lambda e, i=i, b=b: e.dma_start(out=h2_d[i * 128:(i + 1) * 128, :], in_=h2b[b][:]), ('h2s', b), reads=[('h2b', b)], writes=[('h2d', i)])

        def wC(i):
            b = i % 2
            g, k4 = i // 4, i % 4
            yk = ('ytl', g % 2)
            xk = ('xt2', b)
            banks = [('ps', 2 * b), ('ps', 2 * b + 1)]
            T = stw[:, i, :]
            tk = ('stw', i)
            tb = [('ps', 4), ('ps', 5)]

            def trh(e, b=b):
                for kc in range(8):
                    r = e.transpose(out=ps[:, 4 + kc // 4, (kc % 4) * 128:(kc % 4 + 1) * 128], in_=h2f[b][:, kc * 128:(kc + 1) * 128], identity=ident)
                return r
            sc.op('pe', trh, reads=[('h2f', b), 'consts'], writes=tb)
            sc.op('act', lambda e, b=b: e.copy(out=h2T[b][:].rearrange("p k t -> p (k t)"), in_=ps[:, 4:6, :].rearrange("p a n -> p (a n)")),
                  reads=tb, writes=[('h2T', b)])
            lb = ('ps', 6 + b)

            def mml(e, b=b):
                for kc in range(8):
                    r = e.matmul(ps[:, 6 + b, 0:NE], lhsT=h2T[b][:, kc, :], rhs=wrt[:, kc, :], start=(kc == 0), stop=(kc == 7))
                return r
            sc.op('pe', mml, reads=[('h2T', b), 'wrt'], writes=[lb])
            sc.op('dve', lambda e, b=b, T=T: e.tensor_reduce(out=T[:, 3:4], in_=ps[:, 6 + b, 0:NE], axis=AX.X, op=ALU.max), reads=[lb], writes=[tk])
            sc.op('dve', lambda e, T=T: e.tensor_scalar(out=T[:, 4:5], in0=T[:, 3:4], scalar1=-1.0, scalar2=None, op0=ALU.mult), reads=[tk], writes=[tk])
            sc.op('act', lambda e, b=b, T=T: e.activation(out=esm[:, b, :], in_=ps[:, 6 + b, 0:NE], func=AF.Exp, bias=T[:, 4:5], accum_out=T[:, 5:6]),
                  reads=[lb, tk], writes=[('esm', b), tk])
            sc.op('dve', lambda e, T=T: e.reciprocal(out=T[:, 6:7], in_=T[:, 5:6]), reads=[tk], writes=[tk])
            sc.op('dve', lambda e, b=b, i=i, T=T: e.tensor_scalar(out=aff[:, i, :], in0=esm[:, b, :], scalar1=T[:, 6:7], scalar2=None, op0=ALU.mult),
                  reads=[('esm', b), tk], writes=['aff'])

        wA(0)
        for i in range(NT):
            if i + 1 < NT:
                wA(i + 1)
            wB(i)
            wC(i)
        if debug:
            o = dbg_out("dbg_aff", [128, NT, NE])
            sc.dma('sp', lambda e: e.dma_start(out=o[:, :, :], in_=aff[:]), 'dbgaff', reads=['aff'])
        sc.barrier()

    if stage <= 4:
        sc.finish()
        return nc, dbg

    with ExitStack() as pr:
        affT = sb("affT", [NE, S], F32, pr)
        junkR = sb("junkR", [NE, S], F32, pr)
        mkT = sb("mkT", [NE, S], F32, pr)
        csT = sb("csT", [NE, S], F32, pr)
        bs = sb("bs", [NE, 4], F32, pr)
        r1 = sb("r1", [128, NT, NE], F32, pr)
        for rnd in range(2):
            banks = [('ps', k) for k in range(4)]

            def tra(e, rnd=rnd):
                for k in range(16):
                    i = rnd * 16 + k
                    r = e.transpose(out=ps[0:NE, k // 4, (k % 4) * 128:(k % 4 + 1) * 128], in_=aff[:, i, :], identity=ident)
                return r
            sc.op('pe', tra, reads=['aff', 'consts'], writes=banks)
            sc.op('act', lambda e, rnd=rnd: e.copy(out=affT[:, rnd * 2048:(rnd + 1) * 2048], in_=ps[0:NE, 0:4, :].rearrange("p a n -> p (a n)")),
                  reads=banks, writes=['affT'])
        sc.op('dve', lambda e: e.memset(bs[:, 0:1], 0.0), writes=['bs'])
        for n in range(NBIS):
            w = 2.0 ** (-(n + 1))
            sc.op('dve', lambda e, w=w: e.tensor_scalar(out=bs[:, 1:2], in0=bs[:, 0:1], scalar1=w, scalar2=None, op0=ALU.add), reads=['bs'], writes=['bs'])
            sc.op('dve', lambda e: e.tensor_scalar(out=junkR[:], in0=affT[:], scalar1=bs[:, 1:2], scalar2=None, op0=ALU.is_gt, op1=ALU.add,
                                                   accum_out=bs[:, 2:3]), reads=['affT', 'bs'], writes=['junkR', 'bs'])
            sc.op('dve', lambda e: e.tensor_scalar(out=bs[:, 3:4], in0=bs[:, 2:3], scalar1=CAP - 0.5, scalar2=None, op0=ALU.is_gt), reads=['bs'], writes=['bs'])
            sc.op('dve', lambda e, w=w: e.scalar_tensor_tensor(out=bs[:, 0:1], in0=bs[:, 3:4], scalar=w, in1=bs[:, 0:1], op0=ALU.mult, op1=ALU.add),
                  reads=['bs'], writes=['bs'])
        sc.op('dve', lambda e: e.tensor_scalar(out=mkT[:], in0=affT[:], scalar1=bs[:, 0:1], scalar2=None, op0=ALU.is_gt), reads=['affT', 'bs'], writes=['mkT'])
        sc.op('pool', lambda e: e.memset(junkR[:], 1.0), reads=[], writes=['junkR'])
        sc.op('dve', lambda e: e.tensor_tensor_scan(out=csT[:], data0=junkR[:], data1=mkT[:], initial=0.0, op0=ALU.mult, op1=ALU.add),
              reads=['junkR', 'mkT'], writes=['csT'])
        sc.op('dve', lambda e: e.tensor_tensor(out=csT[:], in0=csT[:], in1=mkT[:], op=ALU.mult), reads=['csT', 'mkT'], writes=['csT'])
        sc.op('dve', lambda e: e.tensor_scalar(out=csT[:], in0=csT[:], scalar1=-1.0, scalar2=None, op0=ALU.add), reads=['csT'], writes=['csT'])
        rb = ('ps', 4)

        def trr(e):
            for i in range(NT):
                r = e.transpose(out=ps[:, 4, i * NE:(i + 1) * NE], in_=csT[:, i * 128:(i + 1) * 128], identity=consts[0:NE, C_IDENT:C_IDENT + NE])
            return r
        sc.op('pe', trr, reads=['csT', 'consts'], writes=[rb])
        sc.op('act', lambda e: e.copy(out=rank_tok[:].rearrange("p t e -> p (t e)"), in_=ps[:, 4, :]), reads=[rb], writes=['rank_tok'])
        sc.op('pool', lambda e: e.tensor_copy(out=R5[:, :, :, 0], in_=consts[:, C_TLO:C_TLO + 1].unsqueeze(2).broadcast_to([128, NT, NE])),
              reads=['consts'], writes=['R5'])
        sc.op('pool', lambda e: e.tensor_copy(out=R5[:, :, :, 1], in_=consts[:, C_THI:C_THI + NT].unsqueeze(2).broadcast_to([128, NT, NE])),
              reads=['consts'], writes=['R5'])
        sc.op('dve', lambda e: e.tensor_copy(out=R5[:, :, :, 2], in_=aff[:]), reads=['aff'], writes=['R5'])
        sc.op('dve', lambda e: e.tensor_tensor(out=r1[:], in0=aff[:], in1=R5[:, :, :, 2], op=ALU.subtract), reads=['aff', 'R5'], writes=['r1'])
        sc.op('dve', lambda e: e.tensor_copy(out=R5[:, :, :, 3], in_=r1[:]), reads=['r1'], writes=['R5'])
        sc.op('dve', lambda e: e.tensor_tensor(out=r1[:], in0=r1[:], in1=R5[:, :, :, 3], op=ALU.subtract), reads=['r1', 'R5'], writes=['r1'])
        sc.op('dve', lambda e: e.tensor_copy(out=R5[:, :, :, 4], in_=r1[:]), reads=['r1'], writes=['R5'])
        if debug:
            o = dbg_out("dbg_rank", [128, NT, NE])
            sc.dma('sp', lambda e: e.dma_start(out=o[:, :, :], in_=rank_tok[:]), 'dbgrank', reads=['rank_tok'])
            o = dbg_out("dbg_thr", [NE, 4])
            sc.dma('sp', lambda e: e.dma_start(out=o[:, :], in_=bs[:]), 'dbgthr', reads=['bs'])
        sc.barrier()

    if stage <= 4.5:
        sc.finish()
        return nc, dbg

    n_exp = NE if stage >= 6 else int(round((stage - 5) * 10)) + 1
    with ExitStack() as pe_:
        Pm = sb("Pm", [128, NT, 512], BF16, pe_)
        xe = sb("xe", [128, 4, D], BF16, pe_)
        xeT2 = [sb("xeT%d" % k, [128, 8, 512], BF16, pe_) for k in range(2)]
        aT = sb("aT", [128, 16, 512], BF16, pe_)
        sgt = [sb("sgt%d" % k, [128, 512], BF16, pe_) for k in range(2)]
        yv = sb("yv", [128, 4, D], F32, pe_)
        idf = sb("idf", [128, 2, 8], F32, pe_)
        idp = sb("idp", [128, 2, 32], F32, pe_)
        idxi = [sb("idxi%d" % k, [128, 4], I32, pe_) for k in range(2)]
        def prep_a(e_):
            for i in range(NT):
                sc.op('dve', lambda e, i=i: e.tensor_scalar(out=Pm[:, i, :], in0=consts[:, C_IOTA:C_IOTA + 512], scalar1=rank_tok[:, i, e_:e_ + 1],
                                                            scalar2=None, op0=ALU.is_equal), reads=['consts', 'rank_tok'], writes=['Pm'])

        def prep_b(e_):
            b = e_ % 2
            ibank = ('ps', 7)

            def imm(e):
                for cc in range(4):
                    for i in range(NT):
                        r = e.matmul(ps[:, 7, cc * 8:cc * 8 + 5], lhsT=Pm[:, i, cc * 128:(cc + 1) * 128], rhs=R5[:, i, e_, :],
                                     start=(i == 0), stop=(i == NT - 1))
                return r
            sc.op('pe', imm, reads=['Pm', 'R5'], writes=[ibank])
            sc.op('act', lambda e, b=b: e.copy(out=idp[:, b, :].rearrange("p (c k) -> p c k", k=8)[:, :, 0:5], in_=ps[:, 7, 0:32].rearrange("p (c k) -> p c k", k=8)[:, :, 0:5]), reads=[ibank], writes=[('idp', b)])
            ibank = ('idp', b)
            I3 = idp[:, b, :].rearrange("p (c k) -> p c k", k=8)
            F_ = idf[:, b, :]
            fk = ('idf', b)
            sc.op('dve', lambda e, F_=F_, I3=I3: e.scalar_tensor_tensor(out=F_[:, 0:4].unsqueeze(2), in0=I3[:, :, 1:2], scalar=128.0, in1=I3[:, :, 0:1],
                                                                      op0=ALU.mult, op1=ALU.add), reads=[ibank], writes=[fk])
            sc.op('dve', lambda e, F_=F_, I3=I3: e.tensor_reduce(out=F_[:, 4:8], in_=I3[:, :, 2:5], axis=AX.X, op=ALU.add), reads=[ibank], writes=[fk])
            ik = ('idxi', b)
            sc.op('dve', lambda e, F_=F_, b=b: e.tensor_copy(out=idxi[b][:], in_=F_[:, 0:4]), reads=[fk], writes=[ik])
            for cc in range(4):
                sc.dma('pool', lambda e, cc=cc, b=b: e.indirect_dma_start(
                    out=xe[:, cc, :], out_offset=None, in_=h2_d[:, :],
                    in_offset=bass.IndirectOffsetOnAxis(ap=idxi[b][:, cc:cc + 1], axis=0)), 'xe', reads=[ik], writes=['xe'])

        def prep_c(e_):
            xeT = xeT2[e_ % 2]
            for kc in range(8):
                tb = 6 + (kc % 2)
                tbank = ('ps', tb)
                pb = ps[:, tb, :].bitcast(BF16)

                def trx(e, kc=kc, pb=pb):
                    for cc in range(4):
                        r = e.transpose(out=pb[:, cc * 128:(cc + 1) * 128], in_=xe[:, cc, kc * 128:(kc + 1) * 128], identity=identb[:])
                    return r
                sc.op('pe', trx, reads=['xe', 'identb'], writes=[tbank])
                sc.op('act', lambda e, kc=kc, pb=pb, xeT=xeT: e.copy(out=xeT[:, kc, :], in_=pb[:, 0:512]), reads=[tbank], writes=[('xeT', e_ % 2)])

        prep_a(0)
        prep_b(0)
        prep_c(0)
        for e_ in range(n_exp):
            b = e_ % 2
            xeT = xeT2[b]
            xk_ = ('xeT', b)
            if e_ + 1 < n_exp:
                prep_a(e_ + 1)
            for fh in range(2):
                gk, uk = ('ring', 2 * fh), ('ring', 2 * fh + 1)
                Wg, Wu = ring[2 * fh], ring[2 * fh + 1]
                for fo in range(8):
                    gb, ubk = fo % 2, 2 + (fo % 2)

                    def mmg(e, Wg=Wg, fo=fo, gb=gb):
                        for kc in range(8):
                            r = e.matmul(ps[:, gb, :], lhsT=Wg[:, kc, fo * 128:(fo + 1) * 128], rhs=xeT[:, kc, :], start=(kc == 0), stop=(kc == 7))
                        return r

                    def mmu(e, Wu=Wu, fo=fo, ubk=ubk):
                        for kc in range(8):
                            r = e.matmul(ps[:, ubk, :], lhsT=Wu[:, kc, fo * 128:(fo + 1) * 128], rhs=xeT[:, kc, :], start=(kc == 0), stop=(kc == 7))
                        return r
                    sc.op('pe', mmg, reads=[gk, xk_], writes=[('ps', gb)])
                    sc.op('pe', mmu, reads=[uk, xk_], writes=[('ps', ubk)])
                    sg = sgt[fo % 2]
                    sk = ('sgt', fo % 2)
                    sc.op('act', lambda e, sg=sg, gb=gb: e.activation(out=sg[:], in_=ps[:, gb, :], func=AF.Silu), reads=[('ps', gb)], writes=[sk])
                    fidx = fh * 8 + fo
                    sc.op('dve', lambda e, sg=sg, ubk=ubk, fidx=fidx: e.tensor_tensor(out=aT[:, fidx, :], in0=ps[:, ubk, :], in1=sg[:], op=ALU.mult),
                          reads=[('ps', ubk), sk], writes=[('aT', fidx)])
                if e_ + 1 < n_exp:
                    load_piece(e_ + 1, 2 * fh)
                    load_piece(e_ + 1, 2 * fh + 1)
                    if fh == 0:
                        prep_b(e_ + 1)
                    else:
                        prep_c(e_ + 1)
            F_ = idf[:, b, :]
            for cc in range(4):
                for dh in range(2):
                    db = 4 + ((cc * 2 + dh) % 2)

                    def mmd(e, cc=cc, dh=dh, db=db):
                        for fc in range(16):
                            r = e.matmul(ps[:, db, :], lhsT=aT[:, fc, cc * 128:(cc + 1) * 128], rhs=ring[4 + fc // 8][:, fc % 8, dh * 512:(dh + 1) * 512],
                                         start=(fc == 0), stop=(fc == 15))
                        return r
                    sc.op('pe', mmd, reads=[('aT', f) for f in range(16)] + [('ring', 4), ('ring', 5)], writes=[('ps', db)])
                    sc.op('dve', lambda e, cc=cc, dh=dh, db=db, F_=F_: e.scalar_tensor_tensor(
                        out=yv[:, cc, dh * 512:(dh + 1) * 512], in0=ps[:, db, :], scalar=F_[:, 4 + cc:5 + cc], in1=g2rep[:, dh * 512:(dh + 1) * 512],
                        op0=ALU.mult, op1=ALU.mult), reads=[('ps', db), ('idf', b), 'g2rep'], writes=[('yv', cc)])
            if e_ + 1 < n_exp:
                load_piece(e_ + 1, 4)
                load_piece(e_ + 1, 5)
            for cc in range(4):
                sc.dma('pool', lambda e, cc=cc, b=b: e.indirect_dma_start(
                    out=out_d[:, :], out_offset=bass.IndirectOffsetOnAxis(ap=idxi[b][:, cc:cc + 1], axis=0),
                    in_=yv[:, cc, :], in_offset=None, compute_op=ALU.add), 'scat', reads=[('yv', cc), ('idxi', b)], writes=['outd'])
        sc.barrier()
    sW.close()
    sc.finish()
    return nc, dbg


def prep_inputs(inputs, b):
    f = lambda a: np.ascontiguousarray(a, dtype=np.float32)
    m = {
        "x": f(inputs["x"][b]),
        "cT": f(inputs["c"][b].reshape(8, 128).T),
        "pos": np.ascontiguousarray(inputs["positions"][b].reshape(NT, 128).T.astype(np.int32)),
        "norm1_g": f(inputs["norm1_g"][0:1]),
        "norm2_g": f(inputs["norm2_g"][0:1]),
        "w_ada": f(inputs["w_ada"][0]),
        "b_ada": f(inputs["b_ada"][0:1]),
        "w_in": f(inputs["w_in"][0]),
        "convT": f(inputs["mlstm_conv_w"][0].T.reshape(8, 128, 5).transpose(1, 0, 2).reshape(128, 40)),
        "gate_b": f(inputs["mlstm_gate_b"][0:1]),
        "mnorm_g": f(inputs["mlstm_norm_g"][0:1]),
        "qk_g": f(inputs["diff_qk_g"][0].reshape(1, 128)),
        "lam": f(inputs["diff_lambda"][0].reshape(1, 256)),
        "subln_g": f(inputs["diff_subln_g"][0:1]),
        "w_out": f(inputs["w_out"][0]),
        "w_router": f(inputs["w_router"][0]),
        "consts": make_consts(),
    }
    return m


def kernel(**inputs):
    nc, _ = build()
    shared = None
    in_maps = []
    for b in range(8):
        m = prep_inputs(inputs, b)
        if shared is None:
            shared = {
                "w_gate_e": np.ascontiguousarray(inputs["w_gate_e"][0].reshape(NE * D, 2 * D), dtype=np.float32),
                "w_up_e": np.ascontiguousarray(inputs["w_up_e"][0].reshape(NE * D, 2 * D), dtype=np.float32),
                "w_down_e": np.ascontiguousarray(inputs["w_down_e"][0].reshape(NE * 2 * D, D), dtype=np.float32),
            }
        m.update(shared)
        in_maps.append(m)
    res = run_bass_kernel_spmd(nc, in_maps, core_ids=list(range(8)))
    return np.stack([np.asarray(r["out"]) for r in res.results], axis=0).astype(np.float32)
```
